# Optimizing a Trainium2 kernel written in Bass

```python
import jax, jax.numpy as jnp
from jax import lax
import numpy as np

D_MODEL = 2048
BATCH = 2
SEQ = 4096
DEPTH = 1

CHUNK = 64
D_RET = 1024
D_CONV = 1024
D_MIX = D_RET + D_CONV
N_RET_HEADS = 8
HEAD_DIM = D_RET // N_RET_HEADS
CONV_WIDTH = 3
D_IN_PROJ = 4 * D_RET + 3 * D_CONV
N_EXPERTS = 32
TOP_K = 4
D_FF = 2048
SWIGLU_LIMIT = 7.0
SWIGLU_ALPHA = 1.702
ROPE_BASE = 10000.0
EPS = 1e-6
EXPERT_BLOCK = 128

kernel_name = "hybrid_retention_shortconv_moe_adaln"


def rms_norm(x, g):
    xf = x.astype(jnp.float32)
    y = xf * lax.rsqrt(jnp.mean(xf * xf, axis=-1, keepdims=True) + EPS)
    return (y * g.astype(jnp.float32)).astype(x.dtype)


def rope(x, pos):
    half = x.shape[-1] // 2
    freqs = ROPE_BASE ** (-jnp.arange(half, dtype=jnp.float32) / half)
    ang = pos.astype(jnp.float32)[:, None] * freqs[None, :]
    cos = jnp.cos(ang)[:, None, :]
    sin = jnp.sin(ang)[:, None, :]
    x1, x2 = x[..., :half], x[..., half:]
    return jnp.concatenate([x1 * cos - x2 * sin, x2 * cos + x1 * sin], axis=-1)


def retention_chunkwise(q, k, v):
    b, t, h, dh = q.shape
    nc = t // CHUNK
    log_gamma = jnp.log1p(-jnp.exp2(-5.0 - jnp.arange(h, dtype=jnp.float32)))
    idx = jnp.arange(CHUNK, dtype=jnp.float32)
    dist = jnp.abs(idx[:, None] - idx[None, :])
    d_intra = jnp.exp(log_gamma[:, None, None] * dist)
    d_kv = jnp.exp(log_gamma[:, None] * (CHUNK - 1 - idx)[None])
    d_q = jnp.exp(log_gamma[:, None] * (idx + 1.0)[None])
    d_chunk = jnp.exp(log_gamma * CHUNK)
    qc = q.reshape(b, nc, CHUNK, h, dh)
    kc = k.reshape(b, nc, CHUNK, h, dh)
    vc = v.reshape(b, nc, CHUNK, h, dh)
    scores = jnp.einsum('bzqhd,bzkhd->bzhqk', qc, kc) * d_intra
    intra = jnp.einsum('bzhqk,bzkhe->bzqhe', scores, vc)
    kv = jnp.einsum('bzkhd,bzkhe,hk->zbhde', kc, vc, d_kv)

    def step(state, kv_z):
        return d_chunk[None, :, None, None] * state + kv_z, state

    _, s_prev = lax.scan(step, jnp.zeros((b, h, dh, dh), jnp.float32), kv)
    cross = jnp.einsum('bzqhd,zbhde,hq->bzqhe', qc, s_prev, d_q)
    return (intra + cross).reshape(b, t, h, dh)


def short_conv(u, w):
    return lax.conv_general_dilated(
        u, w[:, None, :].astype(u.dtype), window_strides=(1,),
        padding=[(CONV_WIDTH - 1, 0)],
        dimension_numbers=('NWC', 'WIO', 'NWC'),
        feature_group_count=u.shape[-1])


def hybrid_mixer(h, w_in, conv_w, w_out):
    b, t, _ = h.shape
    pos = jnp.arange(t)
    proj = h @ w_in
    q, k, v, g, bg, cg, u = jnp.split(
        proj, [D_RET, 2 * D_RET, 3 * D_RET, 4 * D_RET,
               4 * D_RET + D_CONV, 4 * D_RET + 2 * D_CONV], axis=-1)
    q = rope(q.reshape(b, t, N_RET_HEADS, HEAD_DIM).astype(jnp.float32), pos)
    k = rope(k.reshape(b, t, N_RET_HEADS, HEAD_DIM).astype(jnp.float32), pos) * (HEAD_DIM ** -0.5)
    v = v.reshape(b, t, N_RET_HEADS, HEAD_DIM).astype(jnp.float32)
    o = retention_chunkwise(q, k, v)
    mu = jnp.mean(o, axis=-1, keepdims=True)
    var = jnp.mean(jnp.square(o - mu), axis=-1, keepdims=True)
    o = ((o - mu) * lax.rsqrt(var + EPS)).reshape(b, t, D_RET).astype(h.dtype)
    y_ret = jax.nn.silu(g) * o
    y_conv = bg * short_conv(cg * u, conv_w)
    return jnp.concatenate([y_ret, y_conv], axis=-1) @ w_out


def moe_ffn(h, w_router, b_router, w_gate_up, b_gate_up, w_down, b_down):
    b, t, d = h.shape
    n = b * t
    xf = h.reshape(n, d)
    logits = xf.astype(jnp.float32) @ w_router.astype(jnp.float32) + b_router.astype(jnp.float32)
    top_val, top_idx = lax.top_k(logits, TOP_K)
    top_w = jax.nn.softmax(top_val, axis=-1)
    n_assign = n * TOP_K
    e_flat = top_idx.reshape(-1)
    tok_flat = jnp.repeat(jnp.arange(n, dtype=jnp.int32), TOP_K)
    w_flat = top_w.reshape(-1)
    order = jnp.argsort(e_flat)
    e_sorted, tok_sorted, w_sorted = e_flat[order], tok_flat[order], w_flat[order]
    counts = jnp.bincount(e_flat, length=N_EXPERTS)
    start = jnp.cumsum(counts) - counts
    padded = (counts + EXPERT_BLOCK - 1) // EXPERT_BLOCK * EXPERT_BLOCK
    padded_end = jnp.cumsum(padded)
    padded_start = padded_end - padded
    dest = padded_start[e_sorted] + jnp.arange(n_assign) - start[e_sorted]
    n_blocks = -(-n_assign // EXPERT_BLOCK) + N_EXPERTS
    p = n_blocks * EXPERT_BLOCK
    tok_buf = jnp.full((p,), n, jnp.int32).at[dest].set(tok_sorted)
    w_buf = jnp.zeros((p,), jnp.float32).at[dest].set(w_sorted)
    block_expert = jnp.minimum(
        jnp.searchsorted(padded_end, jnp.arange(n_blocks) * EXPERT_BLOCK, side='right'),
        N_EXPERTS - 1)
    x_pad = jnp.concatenate([xf, jnp.zeros((1, d), xf.dtype)], axis=0)

    def expert_block(args):
        tok, e = args
        xb = x_pad[tok]
        gu = xb @ w_gate_up[e] + b_gate_up[e]
        gate, up = gu[:, :D_FF], gu[:, D_FF:]
        gate = jnp.minimum(gate, SWIGLU_LIMIT)
        up = jnp.clip(up, -SWIGLU_LIMIT, SWIGLU_LIMIT)
        act = (up + 1.0) * (gate * jax.nn.sigmoid(SWIGLU_ALPHA * gate))
        return act @ w_down[e] + b_down[e]

    y_blocks = lax.map(expert_block, (tok_buf.reshape(n_blocks, EXPERT_BLOCK), block_expert))
    y = jnp.zeros((n + 1, d), h.dtype).at[tok_buf].add(
        y_blocks.reshape(p, d) * w_buf[:, None].astype(h.dtype))
    return y[:n].reshape(b, t, d)


def setup_inputs(seed: int = 0) -> dict:
    key = jax.random.key(seed)
    ks = jax.random.split(key, 16)
    f32 = jnp.float32
    nrm = lambda k, s, sc: jax.random.normal(k, s, f32) * sc
    return {
        "x": nrm(ks[0], (BATCH, SEQ, D_MODEL), 1.0),
        "c": nrm(ks[1], (BATCH, D_MODEL), 1.0),
        "norm1_g": 1.0 + nrm(ks[2], (DEPTH, D_MODEL), 0.01),
        "w_mod": nrm(ks[3], (DEPTH, D_MODEL, 6 * D_MODEL), 0.5 * D_MODEL ** -0.5),
        "b_mod": nrm(ks[4], (DEPTH, 6 * D_MODEL), 0.01),
        "w_in": nrm(ks[5], (DEPTH, D_MODEL, D_IN_PROJ), D_MODEL ** -0.5),
        "conv_w": nrm(ks[6], (DEPTH, CONV_WIDTH, D_CONV), CONV_WIDTH ** -0.5),
        "w_out": nrm(ks[7], (DEPTH, D_MIX, D_MODEL), D_MIX ** -0.5),
        "norm2_g": 1.0 + nrm(ks[8], (DEPTH, D_MODEL), 0.01),
        "w_router": nrm(ks[9], (DEPTH, D_MODEL, N_EXPERTS), D_MODEL ** -0.5),
        "b_router": nrm(ks[10], (DEPTH, N_EXPERTS), 0.01),
        "w_gate_up": nrm(ks[11], (DEPTH, N_EXPERTS, D_MODEL, 2 * D_FF), D_MODEL ** -0.5),
        "b_gate_up": nrm(ks[12], (DEPTH, N_EXPERTS, 2 * D_FF), 0.01),
        "w_down": nrm(ks[13], (DEPTH, N_EXPERTS, D_FF, D_MODEL), D_FF ** -0.5),
        "b_down": nrm(ks[14], (DEPTH, N_EXPERTS, D_MODEL), 0.01),
        "final_g": 1.0 + nrm(ks[15], (D_MODEL,), 0.01),
    }


def reference(x, c, norm1_g, w_mod, b_mod, w_in, conv_w, w_out, norm2_g,
              w_router, b_router, w_gate_up, b_gate_up, w_down, b_down, final_g):
    c_act = jax.nn.silu(c)
    for l in range(DEPTH):
        mod = c_act @ w_mod[l] + b_mod[l]
        shift1, scale1, gate1, shift2, scale2, gate2 = [
            m[:, None, :] for m in jnp.split(mod, 6, axis=-1)]
        h = rms_norm(x, norm1_g[l]) * (1.0 + scale1) + shift1
        x = x + gate1 * hybrid_mixer(h, w_in[l], conv_w[l], w_out[l])
        h = rms_norm(x, norm2_g[l]) * (1.0 + scale2) + shift2
        x = x + gate2 * moe_ffn(h, w_router[l], b_router[l], w_gate_up[l],
                                b_gate_up[l], w_down[l], b_down[l])
    return rms_norm(x, final_g)
```

```python
import numpy as np
import concourse.bass as bass
import concourse.mybir as mybir
from concourse.bass_utils import run_bass_kernel_spmd

F32 = mybir.dt.float32
BF16 = mybir.dt.bfloat16
I32 = mybir.dt.int32
U32 = mybir.dt.uint32
AF = mybir.ActivationFunctionType
ALU = mybir.AluOpType

D = 2048
T = 1024
NT = 8
D_RET = 1024
D_CONV = 1024
NH = 8
HD = 128
D_IN = 7168
NE = 32
TOPK = 4
DFF = 2048
CAP = 192
EPS = 1e-6
QSCALE = float(HD ** -0.5)
LOG_GAMMA = [float(np.log1p(-2.0 ** (-5.0 - h))) for h in range(NH)]
TWO_PI_HI = 6.28125
TWO_PI_LO = 2.0 * np.pi - 6.28125
INV_2PI = float(1.0 / (2.0 * np.pi))
PI_LO = 3.1415925

C_IOTA_T = 0
C_IDENT = 1024
C_DIST = 1152
C_ALLOW = 1280
C_TRI = 1408
C_ONES = 1536
C_IOTA_E = 1664
C_IMOD = 1696
C_SIGN = 1697
C_HALFPI = 1698
C_EPS = 1699
C_LNQ = 1700
C_ONE = 1701
C_N = 1704
PC_BASE = 0
PC_E = 4
PC_FLAG = 36
PC_N = 40

A_CONST = 0
A_TRIG = 3328
A_BRD = A_TRIG + 2048
A_SF32 = A_BRD + 4096
A_HT = A_SF32 + 1024
A_WB = A_HT + 8192
A_KT = A_WB + 6144
A_V = A_KT + 4096
A_SBF = A_V + 4096
A_SCR = A_SBF + 4096
A_YTR = A_SCR + 10240
A_END = A_YTR + 5120


class Res:
    __slots__ = ("name", "w", "r")

    def __init__(self, name):
        self.name = name
        self.w = None
        self.r = []


class Q:
    def __init__(self, name, sem):
        self.name = name
        self.sem = sem
        self.n = 0
        self.waited = {}
        self.ops = []
        self.chans = []
        self.ci = 0


class Prog:
    def __init__(self, nc):
        self.nc = nc
        self.sems = []
        self.q = {}

    def add_queue(self, name, sem, chan_sems=()):
        q = Q(name, len(self.sems))
        self.sems.append(sem)
        for cs in chan_sems:
            q.chans.append([len(self.sems), 0])
            self.sems.append(cs)
        self.q[name] = q

    def _collect(self, q, reads, writes):
        need = {}

        def add(ev, is_war):
            if ev is None:
                return
            k, val, eng = ev
            if eng == q.name:
                if q.name == "pe":
                    return
                if is_war:
                    return
            if q.waited.get(k, 0) >= val:
                return
            if need.get(k, 0) < val:
                need[k] = val

        for r in reads:
            add(r.w, False)
        for w in writes:
            add(w.w, False)
            for e in w.r:
                add(e, True)
        return need

    def _commit(self, q, need):
        for k, val in need.items():
            q.waited[k] = val
        return [(k, v) for k, v in need.items()]

    def op(self, qn, fn, reads=(), writes=()):
        q = self.q[qn]
        need = self._collect(q, reads, writes)
        waits = self._commit(q, need)
        q.n += 1
        ev = (q.sem, q.n, q.name)
        q.ops.append((waits, fn, q.sem, 1))
        for r in reads:
            r.r.append(ev)
        for w in writes:
            w.w = ev
            w.r = []
        return ev

    def dma(self, qn, fn, reads=(), writes=()):
        q = self.q[qn]
        need = self._collect(q, reads, writes)
        ch = q.chans[q.ci]
        q.ci = (q.ci + 1) % len(q.chans)
        if ch[1] > 0 and q.waited.get(ch[0], 0) < 16 * ch[1]:
            if need.get(ch[0], 0) < 16 * ch[1]:
                need[ch[0]] = 16 * ch[1]
        waits = self._commit(q, need)
        ch[1] += 1
        ev = (ch[0], 16 * ch[1], "dma")
        q.ops.append((waits, fn, ch[0], 16))
        for r in reads:
            r.r.append(ev)
        for w in writes:
            w.w = ev
            w.r = []
        return ev

    def barrier(self):
        evs = []
        for q in self.q.values():
            if q.n > 0:
                evs.append((q.sem, q.n))
            for ch in q.chans:
                if ch[1] > 0:
                    evs.append((ch[0], 16 * ch[1]))
        for q in self.q.values():
            need = {}
            for k, val in evs:
                if k == q.sem and q.name != "pe" and False:
                    continue
                if q.waited.get(k, 0) < val:
                    need[k] = val
            waits = self._commit(q, need)
            if waits:
                q.ops.append((waits, None, None, 0))

    def emit(self, qn, eng):
        q = self.q[qn]
        for waits, fn, semk, inc in q.ops:
            for k, val in waits:
                eng.wait_ge(self.sems[k], val)
            if fn is not None:
                ins = fn(eng)
                ins.then_inc(self.sems[semk], inc)


def build_nc(stage="full"):
    nc = bass.Bass("TRN2", target_bir_lowering=False)
    NEW = NE if stage == "full" else 1

    def din(name, shape, dt=F32):
        return nc.dram_tensor(name, list(shape), dt, kind="ExternalInput").ap()

    x_q = din("x_q", [4, T, D])
    c_pc = din("c_pc", [128, 16])
    cst = din("cst", [128, C_N])
    pcst = din("pcst", [128, PC_N])
    norm1_g = din("norm1_g", [D])
    w_mod = din("w_mod", [D, 6 * D])
    b_mod = din("b_mod", [6 * D])
    w_in = din("w_in", [D, D_IN])
    conv_w = din("conv_w", [3, D_CONV])
    w_out = din("w_out", [D, D])
    norm2_g = din("norm2_g", [D])
    w_router = din("w_router", [D, NE])
    b_router = din("b_router", [NE])
    w_gate_up = din("w_gate_up", [NEW, D, 2 * DFF])
    b_gate_up = din("b_gate_up", [NE, 2 * DFF])
    w_down = din("w_down", [NEW, DFF, D])
    b_down = din("b_down", [NE, D])
    final_g = din("final_g", [D])
    out = nc.dram_tensor("out", [T, D], F32, kind="ExternalOutput").ap()
    mod_d = nc.dram_tensor("mod_d", [6 * D], F32, kind="Internal").ap()

    w_in_v = w_in.rearrange("(c p) n -> p c n", p=128)
    w_out_v = w_out.rearrange("(c p) n -> p c n", p=128)

    from contextlib import ExitStack
    es = ExitStack()
    with es:
        arena = es.enter_context(nc.sbuf_tensor("arena", [128, A_END], F32))
        psf = [es.enter_context(nc.psum_tensor(f"ps{i}", [128, 512], F32)) for i in range(8)]
        nsem = 5 + 4 + 4 + 2
        sems = [es.enter_context(nc.semaphore(f"s{i}")) for i in range(nsem)]
        P = Prog(nc)
        P.add_queue("pe", sems[0])
        P.add_queue("act", sems[1], sems[13:15])
        P.add_queue("dve", sems[2])
        P.add_queue("pool", sems[3], sems[5:9])
        P.add_queue("sp", sems[4], sems[9:13])

        def A(off, n):
            return arena[:, off:off + n]

        def Ab(off, nwords):
            return arena[:, off:off + nwords].bitcast(BF16)

        def psb(i):
            return psf[i][:, :].bitcast(BF16)

        bank = [Res(f"bank{i}") for i in range(8)]

        cst_sb = A(A_CONST, C_N)
        pc_sb = A(A_CONST + C_N, PC_N)
        o = A_CONST + C_N + PC_N
        ident_bf = Ab(o, 64); o += 64
        maskT = A(o, 1024).rearrange("p (h k) -> p h k", h=NH); o += 1024
        kdec_tab = A(o, 256).rearrange("p (a h) -> p a h", h=NH); o += 256
        freq = A(o, 1); o += 1
        c_sb = A(o, 16); o += 16
        c_act = A(o, 16); o += 16
        cw_sb = A(o, 24).rearrange("p (k c) -> p k c", k=3); o += 24
        small = A(o, 64); o += 64
        hT_halo = Ab(o, 16).rearrange("p (c t) -> p c t", c=16); o += 16
        assert o <= A_TRIG, o
        iota_t = cst_sb[:, C_IOTA_T:C_IOTA_T + 1024]
        ident_f = cst_sb[:, C_IDENT:C_IDENT + 128]
        dist = cst_sb[:, C_DIST:C_DIST + 128]
        allow = cst_sb[:, C_ALLOW:C_ALLOW + 128]

        def col(c):
            return cst_sb[:, c:c + 1]

        cos_t = A(A_TRIG, 1024)
        sin_t = A(A_TRIG + 1024, 1024)
        brd = [A(A_BRD, 2048), A(A_BRD + 2048, 2048)]
        S_f32 = A(A_SF32, 1024).rearrange("p (h e) -> p h e", h=NH)
        hT = Ab(A_HT, 8192).rearrange("p (c t) -> p c t", c=16)
        wb = [Ab(A_WB + i * 3072, 3072) for i in range(2)]
        kT = Ab(A_KT, 4096).rearrange("p (h t) -> p h t", h=NH)
        v_sb = Ab(A_V, 4096).rearrange("p (i n) -> p i n", i=NT)
        S_bf = Ab(A_SBF, 4096).rearrange("p (i h e) -> p i h e", i=NT, h=NH)
        yTr = Ab(A_YTR, 4096).rearrange("p (c t) -> p c t", c=8)
        yTc = Ab(A_KT, 4096).rearrange("p (c t) -> p c t", c=8)

        R = {}

        def res(name):
            if name not in R:
                R[name] = Res(name)
            return R[name]

        P.dma("sp", lambda e: e.dma_start(out=cst_sb, in_=cst), writes=[res("cst")])
        P.dma("sp", lambda e: e.dma_start(out=pc_sb, in_=pcst), writes=[res("pcst")])
        P.dma("sp", lambda e: e.dma_start(out=c_sb, in_=c_pc), writes=[res("c_sb")])
        P.dma("sp", lambda e: e.dma_start(
            out=cw_sb, in_=conv_w.rearrange("k (c p) -> p k c", p=128),
            allow_slow_non_contiguous=True), writes=[res("cw")])
        P.op("dve", lambda e: e.tensor_copy(out=ident_bf, in_=ident_f), reads=[res("cst")], writes=[res("ident_bf")])
        P.op("act", lambda e: e.activation(out=freq, in_=col(C_IMOD), func=AF.Exp,
                                           scale=float(-np.log(10000.0) / 64.0)),
             reads=[res("cst")], writes=[res("freq")])
        for h in range(NH):
            P.op("act", lambda e, h=h: e.activation(out=maskT[:, h, :], in_=dist, func=AF.Exp, scale=LOG_GAMMA[h]),
                 reads=[res("cst")], writes=[res("maskT")])
        P.op("dve", lambda e: e.tensor_tensor(
            out=maskT, in0=maskT, in1=allow.unsqueeze(1).broadcast_to([128, NH, 128]), op=ALU.mult),
            reads=[res("maskT"), res("cst")], writes=[res("maskT")])
        for h in range(NH):
            P.op("act", lambda e, h=h: e.activation(
                out=kdec_tab[:, :, h], in_=pc_sb[:, PC_E:PC_E + 32], func=AF.Exp, scale=LOG_GAMMA[h]),
                reads=[res("pcst")], writes=[res("kdec")])
        P.op("act", lambda e: e.activation(out=c_act, in_=c_sb, func=AF.Silu), reads=[res("c_sb")], writes=[res("c_act")])

        wm = [A(A_HT + i * 8192, 8192).rearrange("p (c n) -> p c n", c=16) for i in range(2)]
        modrow = arena[0:1, A_V:A_V + 12288]
        w_mod_v = w_mod.rearrange("(p c) n -> p c n", c=16)
        P.dma("sp", lambda e: e.dma_start(out=modrow, in_=b_mod.rearrange("(o n) -> o n", o=1)), writes=[res("modrow")])
        for nb in range(24):
            wres = res(f"wm{nb % 2}")
            P.dma("sp" if nb % 2 == 0 else "act", lambda e, nb=nb: e.dma_start(out=wm[nb % 2], in_=w_mod_v[:, :, nb * 512:(nb + 1) * 512]),
                  writes=[wres])

            def mm(e, nb=nb):
                for c in range(16):
                    ins = e.matmul(psf[nb % 2][0:1, :], lhsT=c_act[:, c:c + 1], rhs=wm[nb % 2][:, c, :],
                                   start=(c == 0), stop=(c == 15))
                return ins
            P.op("pe", mm, reads=[wres, res("c_act")], writes=[bank[nb % 2]])
            P.op("dve", lambda e, nb=nb: e.tensor_tensor(
                out=modrow[:, nb * 512:(nb + 1) * 512], in0=psf[nb % 2][0:1, :],
                in1=modrow[:, nb * 512:(nb + 1) * 512], op=ALU.add),
                reads=[bank[nb % 2], res("modrow")], writes=[res("modrow")])
        P.dma("sp", lambda e: e.dma_start(out=mod_d.rearrange("(o n) -> o n", o=1), in_=modrow),
              reads=[res("modrow")], writes=[res("mod_d")])
        P.barrier()

        def bload(qn, dst, src_row, rname):
            return P.dma(qn, lambda e: e.dma_start(out=dst, in_=src_row.partition_broadcast(128)),
                         reads=[res("mod_d")], writes=[res(rname)])

        tmpb = A(A_SCR, 2048)
        bload("sp", brd[0], norm1_g, "brd0")
        bload("sp", tmpb, mod_d[D:2 * D], "tmpb")
        bload("sp", brd[1], mod_d[0:D], "brd1")
        P.op("dve", lambda e: e.scalar_tensor_tensor(out=brd[0], in0=tmpb, scalar=1.0, in1=brd[0],
                                                     op0=ALU.add, op1=ALU.mult),
             reads=[res("tmpb"), res("brd0")], writes=[res("brd0")])
        P.barrier()

        xs = [A(A_SCR + i * 2048, 2048) for i in range(2)]
        tmpf = A(A_SCR + 4096, 2048)
        hbf = [Ab(A_SCR + 6144 + i * 1024, 1024) for i in range(2)]
        t12 = [A(A_SCR + 8192 + i * 512, 512) for i in range(4)]
        ssq = small[:, 0:8]
        rstd = small[:, 8:16]

        def trig_tables(qi):
            ang = xs[0][:, 0:1024]
            kf = xs[0][:, 1024:2048]
            ki = kf.bitcast(I32)
            rr = xs[1][:, 0:1024]
            ab = xs[1][:, 1024:2048]
            rs_ = [res("xs0"), res("xs1")]
            P.op("dve", lambda e: e.tensor_scalar(out=ang, in0=iota_t, scalar1=pc_sb[:, PC_BASE + qi:PC_BASE + qi + 1],
                                                  scalar2=freq, op0=ALU.add, op1=ALU.mult),
                 reads=[res("cst"), res("pcst"), res("freq")], writes=[rs_[0]])
            P.op("dve", lambda e: e.tensor_scalar(out=rr.bitcast(I32), in0=ang, scalar1=INV_2PI, scalar2=None, op0=ALU.mult),
                 reads=[rs_[0]], writes=[rs_[1]])
            P.op("dve", lambda e: e.tensor_copy(out=kf, in_=rr.bitcast(I32)), reads=[rs_[1]], writes=[rs_[0]])
            P.op("dve", lambda e: e.scalar_tensor_tensor(out=rr, in0=kf, scalar=-TWO_PI_HI, in1=ang, op0=ALU.mult, op1=ALU.add),
                 reads=[rs_[0]], writes=[rs_[1]])
            P.op("dve", lambda e: e.scalar_tensor_tensor(out=rr, in0=kf, scalar=-TWO_PI_LO, in1=rr, op0=ALU.mult, op1=ALU.add),
                 reads=[rs_[0], rs_[1]], writes=[rs_[1]])
            P.op("dve", lambda e: e.tensor_scalar(out=rr, in0=rr, scalar1=PI_LO, scalar2=-PI_LO, op0=ALU.min, op1=ALU.max),
                 reads=[rs_[1]], writes=[rs_[1]])
            P.op("dve", lambda e: e.scalar_tensor_tensor(out=ab, in0=rr, scalar=-1.0, in1=rr, op0=ALU.mult, op1=ALU.max),
                 reads=[rs_[1]], writes=[rs_[1]])
            P.op("act", lambda e: e.activation(out=sin_t, in_=rr, func=AF.Sin, scale=col(C_SIGN)),
                 reads=[rs_[1], res("cst")], writes=[res("trig")])
            P.op("act", lambda e: e.activation(out=cos_t, in_=ab, func=AF.Sin, scale=-1.0, bias=col(C_HALFPI)),
                 reads=[rs_[1], res("cst")], writes=[res("trig")])

        def p1(qi):
            x_src = x_q[qi].rearrange("(i p) d -> i p d", p=128)
            for i in range(NT):
                xr = res(f"xs{i % 2}")
                hr = res(f"hbf{i % 2}")
                P.dma("sp", lambda e, i=i: e.dma_start(out=xs[i % 2], in_=x_src[i]), writes=[xr])
                P.op("act", lambda e, i=i: e.activation(out=hbf[i % 2], in_=xs[i % 2], func=AF.Square,
                                                       accum_out=ssq[:, i:i + 1]),
                     reads=[xr], writes=[hr, res(f"ssq{i}")])
                P.op("act", lambda e, i=i: e.activation(out=rstd[:, i:i + 1], in_=ssq[:, i:i + 1], func=AF.Sqrt,
                                                       scale=1.0 / D, bias=col(C_EPS)),
                     reads=[res(f"ssq{i}"), res("cst")], writes=[res(f"rstd{i}")])
                P.op("dve", lambda e, i=i: e.reciprocal(out=rstd[:, i:i + 1], in_=rstd[:, i:i + 1]),
                     reads=[res(f"rstd{i}")], writes=[res(f"rstd{i}")])
                P.op("dve", lambda e, i=i: e.scalar_tensor_tensor(out=tmpf, in0=xs[i % 2], scalar=rstd[:, i:i + 1],
                                                                   in1=brd[0], op0=ALU.mult, op1=ALU.mult),
                     reads=[xr, res(f"rstd{i}"), res("brd0")], writes=[res("tmpf")])
                P.op("pool", lambda e, i=i: e.tensor_tensor(out=hbf[i % 2], in0=tmpf, in1=brd[1], op=ALU.add),
                     reads=[res("tmpf"), res("brd1")], writes=[hr])
                pb = 2 * (i % 2)

                def tr(e, i=i, pb=pb):
                    for c in range(16):
                        ins = e.transpose(out=psb(pb + c // 8)[:, (c % 8) * 128:(c % 8 + 1) * 128],
                                          in_=hbf[i % 2][:, c * 128:(c + 1) * 128], identity=ident_bf)
                    return ins
                P.op("pe", tr, reads=[hr, res("ident_bf")], writes=[bank[pb], bank[pb + 1]])
                P.op("act", lambda e, i=i, pb=pb: e.activation(
                    out=hT[:, 0:8, i * 128:(i + 1) * 128], in_=psb(pb).rearrange("p (c t) -> p c t", c=8), func=AF.Copy),
                    reads=[bank[pb]], writes=[res("hT")])
                P.op("dve", lambda e, i=i, pb=pb: e.tensor_copy(
                    out=hT[:, 8:16, i * 128:(i + 1) * 128], in_=psb(pb + 1).rearrange("p (c t) -> p c t", c=8)),
                    reads=[bank[pb + 1]], writes=[res("hT")])

        def load_w(slot, cols0, ncols, swap=False):
            wr = res(f"wb{slot}")
            if not swap:
                dst = wb[slot][:, 0:16 * ncols].rearrange("p (c n) -> p c n", c=16)
                P.dma("pool", lambda e: e.dma_start(out=dst, in_=w_in_v[:, :, cols0:cols0 + ncols]), writes=[wr])
            else:
                dstv = wb[slot][:, 0:16 * 384].rearrange("p (c h n) -> p c h n", c=16, h=2)
                srcv = w_in_v[:, :, cols0:cols0 + 256].rearrange("p c (h d) -> p c h d", h=2)
                for hh in range(2):
                    P.dma("pool", lambda e, hh=hh: e.dma_start(out=dstv[:, :, hh, 64:192], in_=srcv[:, :, hh, :]), writes=[wr])
                    P.dma("pool", lambda e, hh=hh: e.dma_start(out=dstv[:, :, hh, 0:64], in_=srcv[:, :, hh, 64:128]), writes=[wr])
            return wr

        def rope_block(slot, h0, is_q, qdst=None, qtdst=None, decq=None):
            wr = res(f"wb{slot}")
            wv = wb[slot][:, 0:16 * 384].rearrange("p (c h n) -> p c h n", c=16, h=2)
            for hh in range(2):
                for th in range(2):
                    ba, bb = 4 + 2 * ((hh * 2 + th) % 2), 5 + 2 * ((hh * 2 + th) % 2)
                    ta, tb = t12[2 * ((hh * 2 + th) % 2)], t12[2 * ((hh * 2 + th) % 2) + 1]
                    tra, trb = res(f"t12_{2 * ((hh * 2 + th) % 2)}"), res(f"t12_{2 * ((hh * 2 + th) % 2) + 1}")
                    tok = slice(th * 512, (th + 1) * 512)

                    def mma(e, hh=hh, tok=tok, ba=ba):
                        for c in range(16):
                            ins = e.matmul(psf[ba][:, :], lhsT=wv[:, c, hh, 64:192], rhs=hT[:, c, tok],
                                           start=(c == 0), stop=(c == 15))
                        return ins

                    def mmb(e, hh=hh, tok=tok, bb=bb):
                        for c in range(16):
                            ins = e.matmul(psf[bb][:, :], lhsT=wv[:, c, hh, 0:128], rhs=hT[:, c, tok],
                                           start=(c == 0), stop=(c == 15))
                        return ins
                    P.op("pe", mma, reads=[wr, res("hT")], writes=[bank[ba]])
                    P.op("pe", mmb, reads=[wr, res("hT")], writes=[bank[bb]])
                    P.op("dve", lambda e, ta=ta, ba=ba, tok=tok: e.tensor_tensor(out=ta, in0=psf[ba][:, :], in1=cos_t[:, tok], op=ALU.mult),
                         reads=[bank[ba], res("trig")], writes=[tra])
                    P.op("dve", lambda e, tb=tb, bb=bb, tok=tok: e.tensor_tensor(out=tb, in0=psf[bb][:, :], in1=sin_t[:, tok], op=ALU.mult),
                         reads=[bank[bb], res("trig")], writes=[trb])
                    if not is_q:
                        P.op("pool", lambda e, ta=ta, tb=tb, hh=hh, tok=tok: e.tensor_tensor(
                            out=kT[:, h0 + hh, tok], in0=ta, in1=tb, op=ALU.add),
                            reads=[tra, trb], writes=[res("kT")])
                    else:
                        P.op("pool", lambda e, ta=ta, tb=tb: e.tensor_tensor(out=ta, in0=ta, in1=tb, op=ALU.add),
                             reads=[tra, trb], writes=[tra])
                        P.op("act", lambda e, ta=ta, hh=hh, tok=tok: e.activation(out=qdst[:, hh, tok], in_=ta, func=AF.Copy, scale=QSCALE),
                             reads=[tra], writes=[res("qT")])
                        P.op("dve", lambda e, ta=ta, hh=hh, tok=tok: e.tensor_tensor(out=qtdst[:, hh, tok], in0=ta, in1=decq[hh][:, tok], op=ALU.mult),
                             reads=[tra, res(f"decq{hh}")], writes=[res("qtT")])

        def v_block(slot, vb):
            wr = res(f"wb{slot}")
            wv = wb[slot][:, 0:16 * 256].rearrange("p (c n) -> p c n", c=16)
            for i in range(NT):
                b = 4 + (i % 4)

                def mm(e, i=i, b=b):
                    for c in range(16):
                        ins = e.matmul(psf[b][:, 0:256], lhsT=hT[:, c, i * 128:(i + 1) * 128], rhs=wv[:, c, :],
                                       start=(c == 0), stop=(c == 15))
                    return ins
                P.op("pe", mm, reads=[wr, res("hT")], writes=[bank[b]])
                P.op("act", lambda e, i=i, b=b: e.activation(out=v_sb[:, i, vb * 256:(vb + 1) * 256], in_=psf[b][:, 0:256], func=AF.Copy),
                     reads=[bank[b]], writes=[res("v")])

        kdec_sb = Ab(A_SCR + 4096, 512).rearrange("p (h d) -> p h d", h=NH)

        def state_phase(qi, main):
            for i in range(NT):
                def tr(e, i=i):
                    for h in range(NH):
                        ins = e.transpose(out=psb(0)[:, h * 128:(h + 1) * 128], in_=kT[:, h, i * 128:(i + 1) * 128],
                                          identity=ident_bf)
                    return ins
                P.op("pe", tr, reads=[res("kT"), res("ident_bf")], writes=[bank[0]])
                P.op("dve", lambda e, i=i: e.tensor_tensor(
                    out=kdec_sb, in0=psb(0).rearrange("p (h d) -> p h d", h=NH),
                    in1=kdec_tab[:, qi * 8 + i, :].unsqueeze(2).broadcast_to([128, NH, 128]), op=ALU.mult),
                    reads=[bank[0], res("kdec")], writes=[res("tmpf")])

                def inc(e, i=i):
                    for h in range(NH):
                        ins = e.matmul(psf[2 + h // 4][:, (h % 4) * 128:(h % 4 + 1) * 128], lhsT=kdec_sb[:, h, :],
                                       rhs=v_sb[:, i, h * 128:(h + 1) * 128], start=True, stop=True)
                    return ins
                P.op("pe", inc, reads=[res("tmpf"), res("v")], writes=[bank[2], bank[3]])
                if main:
                    P.op("act", lambda e, i=i: e.activation(out=S_bf[:, i, :, :], in_=S_f32, func=AF.Copy),
                         reads=[res("S")], writes=[res("S_bf")])
                for hb in range(2):
                    P.op("dve", lambda e, hb=hb: e.tensor_tensor(
                        out=S_f32[:, hb * 4:(hb + 1) * 4, :], in0=S_f32[:, hb * 4:(hb + 1) * 4, :],
                        in1=psf[2 + hb][:, :].rearrange("p (h e) -> p h e", h=4), op=ALU.add),
                        reads=[bank[2 + hb], res("S")], writes=[res("S")])

        P.op("pool", lambda e: e.memset(S_f32, 0.0), writes=[res("S")])

        for qi in range(4):
            main = qi == 3
            trig_tables(qi)
            if main:
                P.op("dve", lambda e: e.tensor_copy(out=hT_halo, in_=hT[:, :, 1022:1024]),
                     reads=[res("hT")], writes=[res("hT_halo")])
            p1(qi)
            blocks = [("k", g) for g in range(4)] + [("v", g) for g in range(4)]
            load_w(0, D_RET + 0, 256, swap=True)
            for bi, (kind, g) in enumerate(blocks):
                slot = bi % 2
                if bi + 1 < len(blocks):
                    nk, ng = blocks[bi + 1]
                    if nk == "k":
                        load_w((bi + 1) % 2, D_RET + ng * 256, 256, swap=True)
                    else:
                        load_w((bi + 1) % 2, 2 * D_RET + ng * 256, 256)
                if kind == "k":
                    rope_block(slot, 2 * g, False)
                else:
                    v_block(slot, g)
            state_phase(qi, main)
        P.barrier()

        qT = Ab(A_SCR + 0, 1024).rearrange("p (h t) -> p h t", h=2)
        qtT = Ab(A_SCR + 1024, 1024).rearrange("p (h t) -> p h t", h=2)
        sg = Ab(A_SCR + 2048, 1024).rearrange("p (i n) -> p i n", i=NT)
        decq = [A(A_SCR + 3072 + i * 1024, 1024) for i in range(2)]
        Pm = [Ab(A_SCR + 5120 + i * 128, 128).rearrange("p (h k) -> p h k", h=2) for i in range(2)]
        onb = [A(A_SCR + 5376 + i * 256, 256).rearrange("p (h k) -> p h k", h=2) for i in range(2)]
        yrb = [Ab(A_SCR + 5888 + i * 128, 128).rearrange("p (h k) -> p h k", h=2) for i in range(2)]
        bst = A(A_SCR + 6144, 32).rearrange("p (a h s) -> p a h s", a=2, h=2)
        bmv = A(A_SCR + 6176, 16).rearrange("p (a h s) -> p a h s", a=2, h=2)
        brs = A(A_SCR + 6192, 4).rearrange("p (a h) -> p a h", a=2)

        for g in range(4):
            for hh in range(2):
                P.op("act", lambda e, hh=hh, g=g: e.activation(out=decq[hh], in_=iota_t, func=AF.Exp,
                                                           scale=LOG_GAMMA[2 * g + hh], bias=col(C_LNQ)),
                     reads=[res("cst")], writes=[res(f"decq{hh}")])
            load_w(0, 2 * g * 128, 256, swap=True)
            load_w(1, 3 * D_RET + g * 256, 256)
            rope_block(0, 2 * g, True, qdst=qT, qtdst=qtT, decq=decq)
            wgv = wb[1][:, 0:16 * 256].rearrange("p (c n) -> p c n", c=16)
            for i in range(NT):
                b = i % 2

                def mmg(e, i=i, b=b):
                    for c in range(16):
                        ins = e.matmul(psf[b][:, 0:256], lhsT=hT[:, c, i * 128:(i + 1) * 128], rhs=wgv[:, c, :],
                                       start=(c == 0), stop=(c == 15))
                    return ins
                P.op("pe", mmg, reads=[res("wb1"), res("hT")], writes=[bank[b]])
                P.op("act", lambda e, i=i, b=b: e.activation(out=sg[:, i, :], in_=psf[b][:, 0:256], func=AF.Silu),
                     reads=[bank[b]], writes=[res("sg")])
            for i in range(NT):
                a = i % 2
                tok = slice(i * 128, (i + 1) * 128)
                rPm, ron, ryr, rst = res(f"Pm{a}"), res(f"on{a}"), res(f"yr{a}"), res(f"bst{a}")

                def sc(e, g=g, tok=tok, a=a):
                    for hh in range(2):
                        ins = e.matmul(psf[2 + a][:, hh * 128:(hh + 1) * 128], lhsT=kT[:, 2 * g + hh, tok], rhs=qT[:, hh, tok],
                                       start=True, stop=True)
                    return ins
                P.op("pe", sc, reads=[res("kT"), res("qT")], writes=[bank[2 + a]])
                P.op("dve", lambda e, g=g, a=a: e.tensor_tensor(
                    out=Pm[a], in0=psf[2 + a][:, 0:256].rearrange("p (h k) -> p h k", h=2),
                    in1=maskT[:, 2 * g:2 * g + 2, :], op=ALU.mult),
                    reads=[bank[2 + a], res("maskT")], writes=[rPm])

                def om(e, g=g, i=i, tok=tok, a=a):
                    for hh in range(2):
                        h = 2 * g + hh
                        e.matmul(psf[4 + a][:, hh * 128:(hh + 1) * 128], lhsT=Pm[a][:, hh, :],
                                 rhs=v_sb[:, i, h * 128:(h + 1) * 128], start=True, stop=False)
                        ins = e.matmul(psf[4 + a][:, hh * 128:(hh + 1) * 128], lhsT=qtT[:, hh, tok],
                                       rhs=S_bf[:, i, h, :], start=False, stop=True)
                    return ins
                P.op("pe", om, reads=[rPm, res("v"), res("qtT"), res("S_bf")], writes=[bank[4 + a]])
                for hh in range(2):
                    P.op("dve", lambda e, hh=hh, a=a: e.bn_stats(out=bst[:, a, hh, 0:6], in_=psf[4 + a][:, hh * 128:(hh + 1) * 128]),
                         reads=[bank[4 + a]], writes=[rst])
                for hh in range(2):
                    P.op("dve", lambda e, hh=hh, a=a: e.bn_aggr(out=bmv[:, a, hh, 0:2], in_=bst[:, a, hh, 0:6]),
                         reads=[rst], writes=[rst])
                P.op("act", lambda e, a=a: e.activation(out=brs[:, a, :], in_=bmv[:, a, :, 1], func=AF.Sqrt,
                                                        bias=col(C_EPS)),
                     reads=[rst, res("cst")], writes=[res(f"brs{a}")])
                P.op("dve", lambda e, a=a: e.reciprocal(out=brs[:, a, :], in_=brs[:, a, :]),
                     reads=[res(f"brs{a}")], writes=[res(f"brs{a}")])
                for hh in range(2):
                    P.op("dve", lambda e, hh=hh, a=a: e.tensor_scalar(
                        out=onb[a][:, hh, :], in0=psf[4 + a][:, hh * 128:(hh + 1) * 128],
                        scalar1=bmv[:, a, hh, 0:1], scalar2=brs[:, a, hh:hh + 1], op0=ALU.subtract, op1=ALU.mult),
                        reads=[bank[4 + a], rst, res(f"brs{a}")], writes=[ron])
                P.op("pool", lambda e, i=i, a=a: e.tensor_tensor(
                    out=yrb[a], in0=onb[a], in1=sg[:, i, :].rearrange("p (h k) -> p h k", h=2), op=ALU.mult),
                    reads=[ron, res("sg")], writes=[ryr])

                def trY(e, a=a):
                    for hh in range(2):
                        ins = e.transpose(out=psb(6 + a)[:, hh * 128:(hh + 1) * 128], in_=yrb[a][:, hh, :], identity=ident_bf)
                    return ins
                P.op("pe", trY, reads=[ryr, res("ident_bf")], writes=[bank[6 + a]])
                P.op("act", lambda e, g=g, tok=tok, a=a: e.activation(
                    out=yTr[:, 2 * g:2 * g + 2, tok], in_=psb(6 + a)[:, 0:256].rearrange("p (h k) -> p h k", h=2), func=AF.Copy),
                    reads=[bank[6 + a]], writes=[res("yTr")])
        P.barrier()

        uT = A(A_SCR + 0, 2056)[:, 0:2052].rearrange("p (c t) -> p c t", c=2)
        acc = A(A_SCR + 2056, 2048).rearrange("p (c t) -> p c t", c=2)
        BASE_B, BASE_C, BASE_U = 4 * D_RET, 4 * D_RET + D_CONV, 4 * D_RET + 2 * D_CONV

        def conv_mm(slot, cc, with_halo):
            wv = wb[slot][:, 0:16 * 256].rearrange("p (c n) -> p c n", c=16)
            for th in range(2):
                b = cc * 2 + th

                def mm(e, b=b, th=th):
                    for c in range(16):
                        ins = e.matmul(psf[b][:, :], lhsT=wv[:, c, cc * 128:(cc + 1) * 128], rhs=hT[:, c, th * 512:(th + 1) * 512],
                                       start=(c == 0), stop=(c == 15))
                    return ins
                P.op("pe", mm, reads=[res(f"wb{slot}"), res("hT")], writes=[bank[b]])
            if with_halo:
                def mmh(e):
                    for c in range(16):
                        ins = e.matmul(psf[4 + cc][:, 0:2], lhsT=wv[:, c, cc * 128:(cc + 1) * 128], rhs=hT_halo[:, c, :],
                                       start=(c == 0), stop=(c == 15))
                    return ins
                P.op("pe", mmh, reads=[res(f"wb{slot}"), res("hT_halo")], writes=[bank[4 + cc]])

        for cg in range(4):
            load_w(0, BASE_U + cg * 256, 256)
            load_w(1, BASE_C + cg * 256, 256)
            for cc in range(2):
                conv_mm(0, cc, True)
                for th in range(2):
                    P.op("act", lambda e, cc=cc, th=th: e.activation(
                        out=uT[:, cc, 2 + th * 512:2 + (th + 1) * 512], in_=psf[cc * 2 + th][:, :], func=AF.Copy),
                        reads=[bank[cc * 2 + th]], writes=[res("uT")])
                P.op("act", lambda e, cc=cc: e.activation(out=uT[:, cc, 0:2], in_=psf[4 + cc][:, 0:2], func=AF.Copy),
                     reads=[bank[4 + cc]], writes=[res("uT")])
            for cc in range(2):
                conv_mm(1, cc, True)
                for th in range(2):
                    P.op("dve", lambda e, cc=cc, th=th: e.tensor_tensor(
                        out=uT[:, cc, 2 + th * 512:2 + (th + 1) * 512], in0=psf[cc * 2 + th][:, :],
                        in1=uT[:, cc, 2 + th * 512:2 + (th + 1) * 512], op=ALU.mult),
                        reads=[bank[cc * 2 + th], res("uT")], writes=[res("uT")])
                P.op("dve", lambda e, cc=cc: e.scalar_tensor_tensor(
                    out=uT[:, cc, 0:2], in0=psf[4 + cc][:, 0:2], scalar=pc_sb[:, PC_FLAG:PC_FLAG + 1], in1=uT[:, cc, 0:2],
                    op0=ALU.mult, op1=ALU.mult),
                    reads=[bank[4 + cc], res("uT"), res("pcst")], writes=[res("uT")])
            load_w(0, BASE_B + cg * 256, 256)
            for cc in range(2):
                ch = cg * 2 + cc
                P.op("pool", lambda e, cc=cc, ch=ch: e.tensor_scalar(
                    out=acc[:, cc, :], in0=uT[:, cc, 2:1026], scalar1=cw_sb[:, 2, ch:ch + 1], scalar2=None, op0=ALU.mult),
                    reads=[res("uT"), res("cw")], writes=[res("acc")])
                P.op("dve", lambda e, cc=cc, ch=ch: e.scalar_tensor_tensor(
                    out=acc[:, cc, :], in0=uT[:, cc, 1:1025], scalar=cw_sb[:, 1, ch:ch + 1], in1=acc[:, cc, :],
                    op0=ALU.mult, op1=ALU.add),
                    reads=[res("uT"), res("cw"), res("acc")], writes=[res("acc")])
                P.op("dve", lambda e, cc=cc, ch=ch: e.scalar_tensor_tensor(
                    out=acc[:, cc, :], in0=uT[:, cc, 0:1024], scalar=cw_sb[:, 0, ch:ch + 1], in1=acc[:, cc, :],
                    op0=ALU.mult, op1=ALU.add),
                    reads=[res("uT"), res("cw"), res("acc")], writes=[res("acc")])
            for cc in range(2):
                conv_mm(0, cc, False)
                for th in range(2):
                    P.op("dve", lambda e, cc=cc, th=th, cg=cg: e.tensor_tensor(
                        out=yTc[:, cg * 2 + cc, th * 512:(th + 1) * 512], in0=psf[cc * 2 + th][:, :],
                        in1=acc[:, cc, th * 512:(th + 1) * 512], op=ALU.mult),
                        reads=[bank[cc * 2 + th], res("acc")], writes=[res("yTc")])
        P.barrier()

        x1 = A(A_V, 16384).rearrange("p (i d) -> p i d", i=NT)
        wo = [Ab(A_HT + i * 4096, 4096).rearrange("p (c n) -> p c n", c=16) for i in range(2)]
        otmp = [A(A_SCR + 8192 + i * 512, 512) for i in range(2)]
        P.dma("sp", lambda e: e.dma_start(out=x1, in_=x_q[3].rearrange("(i p) d -> p i d", p=128)), writes=[res("x1")])
        bload("act", brd[0], mod_d[2 * D:3 * D], "brd0")

        def load_wo(nb):
            P.dma("pool", lambda e: e.dma_start(out=wo[nb % 2], in_=w_out_v[:, :, nb * 512:(nb + 1) * 512]),
                  writes=[res(f"wo{nb % 2}")])
        load_wo(0)
        for nb in range(4):
            if nb + 1 < 4:
                load_wo(nb + 1)
            for i in range(NT):
                b = i % 4

                def mm(e, nb=nb, i=i, b=b):
                    for c in range(16):
                        src = yTr[:, c, i * 128:(i + 1) * 128] if c < 8 else yTc[:, c - 8, i * 128:(i + 1) * 128]
                        ins = e.matmul(psf[b][:, :], lhsT=src, rhs=wo[nb % 2][:, c, :], start=(c == 0), stop=(c == 15))
                    return ins
                P.op("pe", mm, reads=[res(f"wo{nb % 2}"), res("yTr"), res("yTc")], writes=[bank[b]])
                P.op("dve", lambda e, nb=nb, i=i, b=b: e.tensor_tensor(
                    out=otmp[i % 2], in0=psf[b][:, :], in1=brd[0][:, nb * 512:(nb + 1) * 512], op=ALU.mult),
                    reads=[bank[b], res("brd0")], writes=[res(f"otmp{i % 2}")])
                P.op("pool", lambda e, nb=nb, i=i: e.tensor_tensor(
                    out=x1[:, i, nb * 512:(nb + 1) * 512], in0=x1[:, i, nb * 512:(nb + 1) * 512], in1=otmp[i % 2], op=ALU.add),
                    reads=[res(f"otmp{i % 2}"), res("x1")], writes=[res("x1")])
        P.barrier()

        if stage == "x1":
            P.dma("sp", lambda e: e.dma_start(out=out.rearrange("(i p) d -> p i d", p=128), in_=x1), reads=[res("x1")])
            P.barrier()
        if stage != "x1":
            h2T = Ab(A_HT, 8192).rearrange("p (c t) -> p c t", c=16)
            tmp2 = A(A_WB, 2048)
            hb2 = Ab(A_WB + 2048, 1024)
            h2Tf = A(A_WB + 3072, 2048).rearrange("p (c t) -> p c t", c=16)
            wr_sb = A(A_WB + 5120, 512).rearrange("p (c e) -> p c e", c=16)
            brow = arena[0:1, A_WB + 5632:A_WB + 5664]
            lg = A(A_WB + 5664, 32)
            ex = A(A_WB + 5696, 32)
            mk = A(A_WB + 5728, 32)
            mx8 = A(A_WB + 5760, 8)
            nmx = A(A_WB + 5768, 1)
            ssum = A(A_WB + 5769, 1)
            Wmat = A(A_YTR + 4096, 256).rearrange("p (i e) -> p i e", i=NT)
            ssq2 = A(A_YTR + 4352, 8)
            rstd2 = A(A_YTR + 4360, 8)
            ones_row = cst_sb[0:1, C_ONES:C_ONES + 128]

            tmpc = A(A_TRIG, 2048)
            bload("sp", brd[0], norm2_g, "brd0")
            bload("sp", tmpc, mod_d[4 * D:5 * D], "tmpc")
            bload("act", brd[1], mod_d[3 * D:4 * D], "brd1")
            P.op("dve", lambda e: e.scalar_tensor_tensor(out=brd[0], in0=tmpc, scalar=1.0, in1=brd[0],
                                                         op0=ALU.add, op1=ALU.mult),
                 reads=[res("tmpc"), res("brd0")], writes=[res("brd0")])
            P.dma("sp", lambda e: e.dma_start(out=wr_sb, in_=w_router.rearrange("(c p) e -> p c e", p=128)),
                  writes=[res("wr_sb")])
            P.dma("sp", lambda e: e.dma_start(out=brow, in_=b_router.rearrange("(o n) -> o n", o=1)), writes=[res("brow")])

            for i in range(NT):
                tok = slice(i * 128, (i + 1) * 128)
                P.op("act", lambda e, i=i: e.activation(out=hb2, in_=x1[:, i, :], func=AF.Square, accum_out=ssq2[:, i:i + 1]),
                     reads=[res("x1")], writes=[res("hb2"), res("rs2")])
                P.op("act", lambda e, i=i: e.activation(out=rstd2[:, i:i + 1], in_=ssq2[:, i:i + 1], func=AF.Sqrt,
                                                       scale=1.0 / D, bias=col(C_EPS)),
                     reads=[res("rs2"), res("cst")], writes=[res("rs2")])
                P.op("dve", lambda e, i=i: e.reciprocal(out=rstd2[:, i:i + 1], in_=rstd2[:, i:i + 1]),
                     reads=[res("rs2")], writes=[res("rs2")])
                P.op("dve", lambda e, i=i: e.scalar_tensor_tensor(out=tmp2, in0=x1[:, i, :], scalar=rstd2[:, i:i + 1], in1=brd[0],
                                                                   op0=ALU.mult, op1=ALU.mult),
                     reads=[res("x1"), res("rs2"), res("brd0")], writes=[res("tmp2")])
                P.op("dve", lambda e: e.tensor_tensor(out=tmp2, in0=tmp2, in1=brd[1], op=ALU.add),
                     reads=[res("tmp2"), res("brd1")], writes=[res("tmp2")])
                P.op("act", lambda e: e.activation(out=hb2, in_=tmp2, func=AF.Copy), reads=[res("tmp2")], writes=[res("hb2")])

                def trb(e):
                    for c in range(16):
                        ins = e.transpose(out=psb(c // 8)[:, (c % 8) * 128:(c % 8 + 1) * 128],
                                          in_=hb2[:, c * 128:(c + 1) * 128], identity=ident_bf)
                    return ins
                P.op("pe", trb, reads=[res("hb2"), res("ident_bf")], writes=[bank[0], bank[1]])
                P.op("act", lambda e, tok=tok: e.activation(out=h2T[:, 0:8, tok], in_=psb(0).rearrange("p (c t) -> p c t", c=8), func=AF.Copy),
                     reads=[bank[0]], writes=[res("h2T")])
                P.op("dve", lambda e, tok=tok: e.tensor_copy(out=h2T[:, 8:16, tok], in_=psb(1).rearrange("p (c t) -> p c t", c=8)),
                     reads=[bank[1]], writes=[res("h2T")])

                def trf(e):
                    for c in range(16):
                        ins = e.transpose(out=psf[2 + c // 4][:, (c % 4) * 128:(c % 4 + 1) * 128],
                                          in_=tmp2[:, c * 128:(c + 1) * 128], identity=ident_f)
                    return ins
                P.op("pe", trf, reads=[res("tmp2"), res("cst")], writes=[bank[2], bank[3], bank[4], bank[5]])
                for k in range(4):
                    if k % 2 == 0:
                        P.op("act", lambda e, k=k: e.activation(out=h2Tf[:, 4 * k:4 * k + 4, :],
                                                                in_=psf[2 + k][:, :].rearrange("p (c t) -> p c t", c=4), func=AF.Copy),
                             reads=[bank[2 + k]], writes=[res("h2Tf")])
                    else:
                        P.op("dve", lambda e, k=k: e.tensor_copy(out=h2Tf[:, 4 * k:4 * k + 4, :],
                                                                 in_=psf[2 + k][:, :].rearrange("p (c t) -> p c t", c=4)),
                             reads=[bank[2 + k]], writes=[res("h2Tf")])

                def lgm(e):
                    for c in range(16):
                        e.matmul(psf[6][:, 0:32], lhsT=h2Tf[:, c, :], rhs=wr_sb[:, c, :], start=(c == 0), stop=False)
                    return e.matmul(psf[6][:, 0:32], lhsT=ones_row, rhs=brow, start=False, stop=True)
                P.op("pe", lgm, reads=[res("h2Tf"), res("wr_sb"), res("brow"), res("cst")], writes=[bank[6]])
                rt = res("rt")
                P.op("dve", lambda e: e.tensor_copy(out=lg, in_=psf[6][:, 0:32]), reads=[bank[6]], writes=[rt])
                P.op("dve", lambda e: e.max(out=mx8, in_=lg), reads=[rt], writes=[rt])
                P.op("dve", lambda e: e.tensor_scalar(out=mk, in0=lg, scalar1=mx8[:, 3:4], scalar2=None, op0=ALU.is_ge),
                     reads=[rt], writes=[rt])
                P.op("dve", lambda e: e.tensor_scalar(out=nmx, in0=mx8[:, 0:1], scalar1=-1.0, scalar2=None, op0=ALU.mult),
                     reads=[rt], writes=[rt])
                P.op("act", lambda e: e.activation(out=ex, in_=lg, func=AF.Exp, bias=nmx), reads=[rt], writes=[rt])
                P.op("dve", lambda e: e.tensor_tensor(out=ex, in0=ex, in1=mk, op=ALU.mult), reads=[rt], writes=[rt])
                P.op("dve", lambda e: e.reduce_sum(out=ssum, in_=ex, axis=mybir.AxisListType.X), reads=[rt], writes=[rt])
                P.op("dve", lambda e: e.reciprocal(out=ssum, in_=ssum), reads=[rt], writes=[rt])
                P.op("dve", lambda e, i=i: e.tensor_scalar(out=Wmat[:, i, :], in0=ex, scalar1=ssum, scalar2=None, op0=ALU.mult),
                     reads=[rt], writes=[res("Wmat")])
            P.barrier()

            bgu_all = A(A_SF32, 1024).rearrange("p (c e) -> p c e", c=32)
            stage_b = arena[0:32, A_YTR:A_YTR + 4096]
            bload("sp", brd[0], mod_d[5 * D:6 * D], "brd0")
            P.dma("sp", lambda e: e.dma_start(out=stage_b, in_=b_gate_up), writes=[res("stage_b")])
            for half in range(2):
                def trbias(e, half=half):
                    for cc in range(16):
                        ch = half * 16 + cc
                        ins = e.transpose(out=psf[half][:, cc * 32:(cc + 1) * 32], in_=stage_b[:, ch * 128:(ch + 1) * 128],
                                          identity=ident_f[0:32, 0:32])
                    return ins
                P.op("pe", trbias, reads=[res("stage_b"), res("cst")], writes=[bank[half]])
                P.op("dve", lambda e, half=half: e.tensor_copy(out=bgu_all[:, half * 16:(half + 1) * 16, :],
                                                               in_=psf[half][:, :].rearrange("p (c e) -> p c e", c=16)),
                     reads=[bank[half]], writes=[res("bgu")])
            P.barrier()

            actT = Ab(A_WB, 8192).rearrange("p (c t) -> p c t", c=16)
            bd_b = A(A_KT + 2048, 2048)
            g1 = A(A_SCR + 8192, 512)
            u1 = A(A_SCR + 8704, 512)
            sgm = A(A_SCR + 9216, 512)
            dtm = [A(A_SCR + 9728 + i * 256, 256) for i in range(2)]
            wgu = [Ab(A_YTR + i * 2048, 2048).rearrange("p (c n) -> p c n", c=16) for i in range(2)]
            wdn = [Ab(A_TRIG, 2048).rearrange("p (c n) -> p c n", c=16),
                   Ab(A_BRD + 2048, 2048).rearrange("p (c n) -> p c n", c=16)]

            def load_gu(e_, cp, slot):
                wv = w_gate_up[e_].rearrange("(c p) n -> p c n", p=128)
                P.dma("pool", lambda e: e.dma_start(out=wgu[slot][:, :, 0:128], in_=wv[:, :, cp * 128:(cp + 1) * 128]),
                      writes=[res(f"wgu{slot}")])
                P.dma("pool", lambda e: e.dma_start(out=wgu[slot][:, :, 128:256], in_=wv[:, :, DFF + cp * 128:DFF + (cp + 1) * 128]),
                      writes=[res(f"wgu{slot}")])

            def load_dn(e_, nb, slot):
                wv = w_down[e_].rearrange("(c p) n -> p c n", p=128)
                P.dma("pool", lambda e: e.dma_start(out=wdn[slot], in_=wv[:, :, nb * 256:(nb + 1) * 256]),
                      writes=[res(f"wdn{slot}")])

            gu_jobs = [(e_, cp) for e_ in range(NEW) for cp in range(16)]
            dn_jobs = [(e_, nb) for e_ in range(NEW) for nb in range(8)]
            load_gu(0, 0, 0)
            gi = 0
            di = 0
            dn_loaded = 0
            for e_ in range(NEW):
                P.dma("sp", lambda e, e_=e_: e.dma_start(out=bd_b, in_=b_down[e_].partition_broadcast(128)), writes=[res("bd_b")])
                for cp in range(16):
                    slot = gi % 2
                    if gi + 1 < len(gu_jobs):
                        ne_, ncp = gu_jobs[gi + 1]
                        load_gu(ne_, ncp, (gi + 1) % 2)
                    if cp in (0, 8) and dn_loaded < len(dn_jobs) and dn_loaded < di + 2:
                        je, jnb = dn_jobs[dn_loaded]
                        load_dn(je, jnb, dn_loaded % 2)
                        dn_loaded += 1
                    for th in range(2):
                        k = cp * 2 + th
                        bg, bu = 2 * (k % 2), 2 * (k % 2) + 1
                        tokh = slice(th * 512, (th + 1) * 512)

                        def mg(e, slot=slot, tokh=tokh, bg=bg):
                            for c in range(16):
                                ins = e.matmul(psf[bg][:, :], lhsT=wgu[slot][:, c, 0:128], rhs=h2T[:, c, tokh],
                                               start=(c == 0), stop=(c == 15))
                            return ins

                        def mu(e, slot=slot, tokh=tokh, bu=bu):
                            for c in range(16):
                                ins = e.matmul(psf[bu][:, :], lhsT=wgu[slot][:, c, 128:256], rhs=h2T[:, c, tokh],
                                               start=(c == 0), stop=(c == 15))
                            return ins
                        P.op("pe", mg, reads=[res(f"wgu{slot}"), res("h2T")], writes=[bank[bg]])
                        P.op("pe", mu, reads=[res(f"wgu{slot}"), res("h2T")], writes=[bank[bu]])
                        P.op("dve", lambda e, bg=bg, cp=cp, e_=e_: e.tensor_scalar(
                            out=g1, in0=psf[bg][:, :], scalar1=bgu_all[:, cp, e_:e_ + 1], scalar2=7.0, op0=ALU.add, op1=ALU.min),
                            reads=[bank[bg], res("bgu")], writes=[res("g1")])
                        P.op("act", lambda e: e.activation(out=sgm, in_=g1, func=AF.Sigmoid, scale=1.702),
                             reads=[res("g1")], writes=[res("sgm")])
                        P.op("act", lambda e, bu=bu, cp=cp, e_=e_: e.activation(
                            out=u1, in_=psf[bu][:, :], func=AF.Identity, bias=bgu_all[:, 16 + cp, e_:e_ + 1]),
                            reads=[bank[bu], res("bgu")], writes=[res("u1")])
                        P.op("dve", lambda e: e.tensor_scalar(out=u1, in0=u1, scalar1=-7.0, scalar2=7.0, op0=ALU.max, op1=ALU.min),
                             reads=[res("u1")], writes=[res("u1")])
                        P.op("dve", lambda e: e.tensor_tensor(out=g1, in0=g1, in1=sgm, op=ALU.mult),
                             reads=[res("g1"), res("sgm")], writes=[res("g1")])
                        P.op("dve", lambda e, cp=cp, tokh=tokh: e.scalar_tensor_tensor(
                            out=actT[:, cp, tokh], in0=u1, scalar=1.0, in1=g1, op0=ALU.add, op1=ALU.mult),
                            reads=[res("u1"), res("g1")], writes=[res("actT")])
                    gi += 1
                for nb in range(8):
                    slot = di % 2
                    if dn_loaded < len(dn_jobs) and dn_loaded < di + 2:
                        je, jnb = dn_jobs[dn_loaded]
                        if je == e_:
                            load_dn(je, jnb, dn_loaded % 2)
                            dn_loaded += 1
                    cols = slice(nb * 256, (nb + 1) * 256)
                    for i in range(NT):
                        j = nb * NT + i
                        b = 4 + (j % 4)

                        def md(e, slot=slot, i=i, b=b):
                            for c in range(16):
                                ins = e.matmul(psf[b][:, 0:256], lhsT=actT[:, c, i * 128:(i + 1) * 128], rhs=wdn[slot][:, c, :],
                                               start=(c == 0), stop=(c == 15))
                            return ins
                        P.op("pe", md, reads=[res(f"wdn{slot}"), res("actT")], writes=[bank[b]])
                        dt_ = dtm[j % 2]
                        rdt = res(f"dtm{j % 2}")
                        P.op("dve", lambda e, b=b, dt_=dt_, cols=cols: e.tensor_tensor(out=dt_, in0=psf[b][:, 0:256], in1=bd_b[:, cols], op=ALU.add),
                             reads=[bank[b], res("bd_b")], writes=[rdt])
                        P.op("dve", lambda e, dt_=dt_, cols=cols: e.tensor_tensor(out=dt_, in0=dt_, in1=brd[0][:, cols], op=ALU.mult),
                             reads=[rdt, res("brd0")], writes=[rdt])
                        P.op("dve", lambda e, dt_=dt_, cols=cols, i=i, e_=e_: e.scalar_tensor_tensor(
                            out=x1[:, i, cols], in0=dt_, scalar=Wmat[:, i, e_:e_ + 1], in1=x1[:, i, cols], op0=ALU.mult, op1=ALU.add),
                            reads=[rdt, res("Wmat"), res("x1")], writes=[res("x1")])
                    di += 1
            P.barrier()

            fg_b = A(A_TRIG, 2048)
            ot = [A(A_HT + i * 2048, 2048) for i in range(2)]
            junk = Ab(A_WB, 1024)
            bload("sp", fg_b, final_g, "fg_b")
            out_v = out.rearrange("(i p) d -> i p d", p=128)
            for i in range(NT):
                P.op("act", lambda e, i=i: e.activation(out=junk, in_=x1[:, i, :], func=AF.Square, accum_out=ssq2[:, i:i + 1]),
                     reads=[res("x1")], writes=[res("junk"), res("rs3")])
                P.op("act", lambda e, i=i: e.activation(out=rstd2[:, i:i + 1], in_=ssq2[:, i:i + 1], func=AF.Sqrt,
                                                       scale=1.0 / D, bias=col(C_EPS)),
                     reads=[res("rs3"), res("cst")], writes=[res("rs3")])
                P.op("dve", lambda e, i=i: e.reciprocal(out=rstd2[:, i:i + 1], in_=rstd2[:, i:i + 1]),
                     reads=[res("rs3")], writes=[res("rs3")])
                P.op("dve", lambda e, i=i: e.scalar_tensor_tensor(out=ot[i % 2], in0=x1[:, i, :], scalar=rstd2[:, i:i + 1], in1=fg_b,
                                                                   op0=ALU.mult, op1=ALU.mult),
                     reads=[res("x1"), res("rs3"), res("fg_b")], writes=[res(f"ot{i % 2}")])
                P.dma("sp", lambda e, i=i: e.dma_start(out=out_v[i], in_=ot[i % 2]), reads=[res(f"ot{i % 2}")])
            P.barrier()


        with nc.Block() as block:
            @block.sync
            def _(e):
                P.emit("sp", e)

            @block.scalar
            def _(e):
                P.emit("act", e)

            @block.vector
            def _(e):
                P.emit("dve", e)

            @block.gpsimd
            def _(e):
                P.emit("pool", e)

            @block.tensor
            def _(e):
                P.emit("pe", e)
    return nc


def _consts():
    c = np.zeros((128, C_N), np.float32)
    p = np.arange(128)
    c[:, C_IOTA_T:C_IOTA_T + 1024] = np.arange(1024)[None, :]
    c[:, C_IDENT:C_IDENT + 128] = np.eye(128)
    kt = p[:, None]
    qt = p[None, :]
    c[:, C_DIST:C_DIST + 128] = np.abs(kt - qt)
    c[:, C_ALLOW:C_ALLOW + 128] = ((kt // 64) <= (qt // 64))
    c[:, C_TRI:C_TRI + 128] = (kt < qt)
    c[:, C_ONES:C_ONES + 128] = 1.0
    c[:, C_IOTA_E:C_IOTA_E + 32] = np.arange(32)[None, :]
    c[:, C_IMOD] = p % 64
    c[:, C_SIGN] = np.where(p < 64, -1.0, 1.0)
    c[:, C_HALFPI] = np.pi / 2
    c[:, C_EPS] = EPS
    c[:, C_LNQ] = np.log(QSCALE)
    c[:, C_ONE] = 1.0
    return c


def _percore(j):
    t = np.zeros((128, PC_N), np.float32)
    p = np.arange(128)
    for qi in range(4):
        if qi < 3:
            s = 3 - qi
            valid = s <= j
            t[:, PC_BASE + qi] = (j - s) * 1024 if valid else 0
            for i in range(8):
                t[:, PC_E + qi * 8 + i] = (1024 * s - (128 * i + p)) if valid else 1.0e6
        else:
            t[:, PC_BASE + qi] = j * 1024
            for i in range(8):
                t[:, PC_E + qi * 8 + i] = -(128 * i + p)
    t[:, PC_FLAG] = 1.0 if j > 0 else 0.0
    return t


_NC_CACHE = {}


def make_in_maps(x, c, norm1_g, w_mod, b_mod, w_in, conv_w, w_out, norm2_g, w_router, b_router,
                 w_gate_up, b_gate_up, w_down, b_down, final_g):
    f = lambda a: np.ascontiguousarray(np.asarray(a, dtype=np.float32))
    x = f(x); c = f(c)
    shared = dict(
        cst=_consts(), norm1_g=f(norm1_g[0]), w_mod=f(w_mod[0]), b_mod=f(b_mod[0]), w_in=f(w_in[0]),
        conv_w=f(conv_w[0]), w_out=f(w_out[0]), norm2_g=f(norm2_g[0]), w_router=f(w_router[0]),
        b_router=f(b_router[0]), w_gate_up=f(w_gate_up[0]), b_gate_up=f(b_gate_up[0]),
        w_down=f(w_down[0]), b_down=f(b_down[0]), final_g=f(final_g))
    in_maps = []
    for core in range(8):
        b, j = core // 4, core % 4
        xq = np.zeros((4, T, D), np.float32)
        for qi in range(3):
            s = 3 - qi
            if s <= j:
                xq[qi] = x[b, (j - s) * T:(j - s + 1) * T]
        xq[3] = x[b, j * T:(j + 1) * T]
        m = dict(shared)
        m["x_q"] = xq
        m["c_pc"] = np.ascontiguousarray(c[b].reshape(128, 16))
        m["pcst"] = _percore(j)
        in_maps.append(m)
    return in_maps


def kernel(**inputs):
    if "nc" not in _NC_CACHE:
        _NC_CACHE["nc"] = build_nc()
    nc = _NC_CACHE["nc"]
    in_maps = make_in_maps(**inputs)
    res = run_bass_kernel_spmd(nc, in_maps, core_ids=list(range(8)))
    outs = [np.asarray(r["out"], dtype=np.float32) for r in res.results]
    full = np.stack(outs, 0).reshape(2, 4, T, D).reshape(2, 4 * T, D)
    return full
```

```python
import numpy as np
import concourse.bass as bass
import concourse.mybir as mybir
from concourse.bass_utils import run_bass_kernel_spmd

F32 = mybir.dt.float32
BF16 = mybir.dt.bfloat16
I32 = mybir.dt.int32
U32 = mybir.dt.uint32
AF = mybir.ActivationFunctionType
ALU = mybir.AluOpType

D = 2048
T = 1024
NT = 8
D_RET = 1024
D_CONV = 1024
NH = 8
HD = 128
D_IN = 7168
NE = 32
TOPK = 4
DFF = 2048
CAP = 512
EPS = 1e-6
QSCALE = float(HD ** -0.5)
LOG_GAMMA = [float(np.log1p(-2.0 ** (-5.0 - h))) for h in range(NH)]
TWO_PI_HI = 6.28125
TWO_PI_LO = 2.0 * np.pi - 6.28125
INV_2PI = float(1.0 / (2.0 * np.pi))
PI_LO = 3.1415925

C_IOTA_T = 0
C_IDENT = 1024
C_DIST = 1152
C_ALLOW = 1280
C_TRI = 1408
C_ONES = 1536
C_IOTA_E = 1664
C_IMOD = 1696
C_SIGN = 1697
C_HALFPI = 1698
C_EPS = 1699
C_LNQ = 1700
C_ONE = 1701
C_N = 1704
PC_BASE = 0
PC_E = 4
PC_FLAG = 36
PC_N = 40

A_CONST = 0
A_TRIG = 3328
A_BRD = A_TRIG + 2048
A_SF32 = A_BRD + 4096
A_HT = A_SF32 + 1024
A_WB = A_HT + 8192
A_KT = A_WB + 6144
A_V = A_KT + 4096
A_SBF = A_V + 4096
A_SCR = A_SBF + 4096
A_YTR = A_SCR + 10240
A_END = A_YTR + 5120


class Res:
    __slots__ = ("name", "w", "r")

    def __init__(self, name):
        self.name = name
        self.w = None
        self.r = []


class Q:
    def __init__(self, name, sem):
        self.name = name
        self.sem = sem
        self.n = 0
        self.waited = {}
        self.ops = []
        self.chans = []
        self.ci = 0


class Prog:
    def __init__(self, nc):
        self.nc = nc
        self.sems = []
        self.q = {}

    def add_queue(self, name, sem, chan_sems=()):
        q = Q(name, len(self.sems))
        self.sems.append(sem)
        for cs in chan_sems:
            q.chans.append([len(self.sems), 0])
            self.sems.append(cs)
        self.q[name] = q

    def _collect(self, q, reads, writes):
        need = {}

        def add(ev, is_war):
            if ev is None:
                return
            k, val, eng = ev
            if eng == q.name:
                if q.name == "pe":
                    return
                if is_war:
                    return
            if q.waited.get(k, 0) >= val:
                return
            if need.get(k, 0) < val:
                need[k] = val

        for r in reads:
            add(r.w, False)
        for w in writes:
            add(w.w, False)
            for e in w.r:
                add(e, True)
        return need

    def _commit(self, q, need):
        for k, val in need.items():
            q.waited[k] = val
        return [(k, v) for k, v in need.items()]

    def op(self, qn, fn, reads=(), writes=()):
        q = self.q[qn]
        need = self._collect(q, reads, writes)
        waits = self._commit(q, need)
        q.n += 1
        ev = (q.sem, q.n, q.name)
        q.ops.append((waits, fn, q.sem, 1))
        for r in reads:
            r.r.append(ev)
        for w in writes:
            w.w = ev
            w.r = []
        return ev

    def dma(self, qn, fn, reads=(), writes=()):
        q = self.q[qn]
        need = self._collect(q, reads, writes)
        ch = q.chans[q.ci]
        q.ci = (q.ci + 1) % len(q.chans)
        if ch[1] > 0 and q.waited.get(ch[0], 0) < 16 * ch[1]:
            if need.get(ch[0], 0) < 16 * ch[1]:
                need[ch[0]] = 16 * ch[1]
        waits = self._commit(q, need)
        ch[1] += 1
        ev = (ch[0], 16 * ch[1], "dma")
        q.ops.append((waits, fn, ch[0], 16))
        for r in reads:
            r.r.append(ev)
        for w in writes:
            w.w = ev
            w.r = []
        return ev

    def barrier(self):
        evs = []
        for q in self.q.values():
            if q.n > 0:
                evs.append((q.sem, q.n))
            for ch in q.chans:
                if ch[1] > 0:
                    evs.append((ch[0], 16 * ch[1]))
        for q in self.q.values():
            need = {}
            for k, val in evs:
                if k == q.sem and q.name != "pe" and False:
                    continue
                if q.waited.get(k, 0) < val:
                    need[k] = val
            waits = self._commit(q, need)
            if waits:
                q.ops.append((waits, None, None, 0))

    def emit(self, qn, eng):
        q = self.q[qn]
        for waits, fn, semk, inc in q.ops:
            for k, val in waits:
                eng.wait_ge(self.sems[k], val)
            if fn is not None:
                ins = fn(eng)
                ins.then_inc(self.sems[semk], inc)


def build_nc(stage="full"):
    nc = bass.Bass("TRN2", target_bir_lowering=False)
    NEW = NE if stage == "full" else 1

    def din(name, shape, dt=F32):
        return nc.dram_tensor(name, list(shape), dt, kind="ExternalInput").ap()

    x_q = din("x_q", [4, T, D])
    c_pc = din("c_pc", [128, 16])
    cst = din("cst", [128, C_N])
    pcst = din("pcst", [128, PC_N])
    norm1_g = din("norm1_g", [D])
    w_mod = din("w_mod", [D, 6 * D])
    b_mod = din("b_mod", [6 * D])
    w_in = din("w_in", [D, D_IN])
    conv_w = din("conv_w", [3, D_CONV])
    w_out = din("w_out", [D, D])
    norm2_g = din("norm2_g", [D])
    w_router = din("w_router", [D, NE])
    b_router = din("b_router", [NE])
    w_gate_up = din("w_gate_up", [NEW, D, 2 * DFF])
    b_gate_up = din("b_gate_up", [NE, 2 * DFF])
    w_down = din("w_down", [NEW, DFF, D])
    b_down = din("b_down", [NE, D])
    final_g = din("final_g", [D])
    out = nc.dram_tensor("out", [T, D], F32, kind="ExternalOutput").ap()
    mod_d = nc.dram_tensor("mod_d", [6 * D], F32, kind="Internal").ap()
    xe_d = nc.dram_tensor("xe_d", [NE * CAP, D], BF16, kind="Internal").ap()
    ye_d = nc.dram_tensor("ye_d", [NE * CAP, D], F32, kind="Internal").ap()

    w_in_v = w_in.rearrange("(c p) n -> p c n", p=128)
    w_out_v = w_out.rearrange("(c p) n -> p c n", p=128)

    from contextlib import ExitStack
    es = ExitStack()
    with es:
        arena = es.enter_context(nc.sbuf_tensor("arena", [128, A_END], F32))
        psf = [es.enter_context(nc.psum_tensor(f"ps{i}", [128, 512], F32)) for i in range(8)]
        nsem = 5 + 4 + 4 + 2
        sems = [es.enter_context(nc.semaphore(f"s{i}")) for i in range(nsem)]
        P = Prog(nc)
        P.add_queue("pe", sems[0])
        P.add_queue("act", sems[1], sems[13:15])
        P.add_queue("dve", sems[2])
        P.add_queue("pool", sems[3], sems[5:9])
        P.add_queue("sp", sems[4], sems[9:13])

        def A(off, n):
            return arena[:, off:off + n]

        def Ab(off, nwords):
            return arena[:, off:off + nwords].bitcast(BF16)

        def psb(i):
            return psf[i][:, :].bitcast(BF16)

        bank = [Res(f"bank{i}") for i in range(8)]

        cst_sb = A(A_CONST, C_N)
        pc_sb = A(A_CONST + C_N, PC_N)
        o = A_CONST + C_N + PC_N
        ident_bf = Ab(o, 64); o += 64
        maskT = A(o, 1024).rearrange("p (h k) -> p h k", h=NH); o += 1024
        kdec_tab = A(o, 256).rearrange("p (a h) -> p a h", h=NH); o += 256
        freq = A(o, 1); o += 1
        c_sb = A(o, 16); o += 16
        c_act = A(o, 16); o += 16
        cw_sb = A(o, 24).rearrange("p (k c) -> p k c", k=3); o += 24
        small = A(o, 64); o += 64
        hT_halo = Ab(o, 16).rearrange("p (c t) -> p c t", c=16); o += 16
        assert o <= A_TRIG, o
        iota_t = cst_sb[:, C_IOTA_T:C_IOTA_T + 1024]
        ident_f = cst_sb[:, C_IDENT:C_IDENT + 128]
        dist = cst_sb[:, C_DIST:C_DIST + 128]
        allow = cst_sb[:, C_ALLOW:C_ALLOW + 128]

        def col(c):
            return cst_sb[:, c:c + 1]

        cos_t = A(A_TRIG, 1024)
        sin_t = A(A_TRIG + 1024, 1024)
        brd = [A(A_BRD, 2048), A(A_BRD + 2048, 2048)]
        S_f32 = A(A_SF32, 1024).rearrange("p (h e) -> p h e", h=NH)
        hT = Ab(A_HT, 8192).rearrange("p (c t) -> p c t", c=16)
        wb = [Ab(A_WB + i * 3072, 3072) for i in range(2)]
        kT = Ab(A_KT, 4096).rearrange("p (h t) -> p h t", h=NH)
        v_sb = Ab(A_V, 4096).rearrange("p (i n) -> p i n", i=NT)
        S_bf = Ab(A_SBF, 4096).rearrange("p (i h e) -> p i h e", i=NT, h=NH)
        yTr = Ab(A_YTR, 4096).rearrange("p (c t) -> p c t", c=8)
        yTc = Ab(A_KT, 4096).rearrange("p (c t) -> p c t", c=8)

        R = {}

        def res(name):
            if name not in R:
                R[name] = Res(name)
            return R[name]

        P.dma("sp", lambda e: e.dma_start(out=cst_sb, in_=cst), writes=[res("cst")])
        P.dma("sp", lambda e: e.dma_start(out=pc_sb, in_=pcst), writes=[res("pcst")])
        P.dma("sp", lambda e: e.dma_start(out=c_sb, in_=c_pc), writes=[res("c_sb")])
        P.dma("sp", lambda e: e.dma_start(
            out=cw_sb, in_=conv_w.rearrange("k (c p) -> p k c", p=128),
            allow_slow_non_contiguous=True), writes=[res("cw")])
        P.op("dve", lambda e: e.tensor_copy(out=ident_bf, in_=ident_f), reads=[res("cst")], writes=[res("ident_bf")])
        P.op("act", lambda e: e.activation(out=freq, in_=col(C_IMOD), func=AF.Exp,
                                           scale=float(-np.log(10000.0) / 64.0)),
             reads=[res("cst")], writes=[res("freq")])
        for h in range(NH):
            P.op("act", lambda e, h=h: e.activation(out=maskT[:, h, :], in_=dist, func=AF.Exp, scale=LOG_GAMMA[h]),
                 reads=[res("cst")], writes=[res("maskT")])
        P.op("dve", lambda e: e.tensor_tensor(
            out=maskT, in0=maskT, in1=allow.unsqueeze(1).broadcast_to([128, NH, 128]), op=ALU.mult),
            reads=[res("maskT"), res("cst")], writes=[res("maskT")])
        for h in range(NH):
            P.op("act", lambda e, h=h: e.activation(
                out=kdec_tab[:, :, h], in_=pc_sb[:, PC_E:PC_E + 32], func=AF.Exp, scale=LOG_GAMMA[h]),
                reads=[res("pcst")], writes=[res("kdec")])
        P.op("act", lambda e: e.activation(out=c_act, in_=c_sb, func=AF.Silu), reads=[res("c_sb")], writes=[res("c_act")])

        wm = [A(A_HT + i * 8192, 8192).rearrange("p (c n) -> p c n", c=16) for i in range(2)]
        modrow = arena[0:1, A_V:A_V + 12288]
        w_mod_v = w_mod.rearrange("(p c) n -> p c n", c=16)
        P.dma("sp", lambda e: e.dma_start(out=modrow, in_=b_mod.rearrange("(o n) -> o n", o=1)), writes=[res("modrow")])
        for nb in range(24):
            wres = res(f"wm{nb % 2}")
            P.dma("sp" if nb % 2 == 0 else "act", lambda e, nb=nb: e.dma_start(out=wm[nb % 2], in_=w_mod_v[:, :, nb * 512:(nb + 1) * 512]),
                  writes=[wres])

            def mm(e, nb=nb):
                for c in range(16):
                    ins = e.matmul(psf[nb % 2][0:1, :], lhsT=c_act[:, c:c + 1], rhs=wm[nb % 2][:, c, :],
                                   start=(c == 0), stop=(c == 15))
                return ins
            P.op("pe", mm, reads=[wres, res("c_act")], writes=[bank[nb % 2]])
            P.op("dve", lambda e, nb=nb: e.tensor_tensor(
                out=modrow[:, nb * 512:(nb + 1) * 512], in0=psf[nb % 2][0:1, :],
                in1=modrow[:, nb * 512:(nb + 1) * 512], op=ALU.add),
                reads=[bank[nb % 2], res("modrow")], writes=[res("modrow")])
        P.dma("sp", lambda e: e.dma_start(out=mod_d.rearrange("(o n) -> o n", o=1), in_=modrow),
              reads=[res("modrow")], writes=[res("mod_d")])
        P.barrier()

        def bload(qn, dst, src_row, rname):
            return P.dma(qn, lambda e: e.dma_start(out=dst, in_=src_row.partition_broadcast(128)),
                         reads=[res("mod_d")], writes=[res(rname)])

        tmpb = A(A_SCR, 2048)
        bload("sp", brd[0], norm1_g, "brd0")
        bload("sp", tmpb, mod_d[D:2 * D], "tmpb")
        bload("sp", brd[1], mod_d[0:D], "brd1")
        P.op("dve", lambda e: e.scalar_tensor_tensor(out=brd[0], in0=tmpb, scalar=1.0, in1=brd[0],
                                                     op0=ALU.add, op1=ALU.mult),
             reads=[res("tmpb"), res("brd0")], writes=[res("brd0")])
        P.barrier()

        xs = [A(A_SCR + i * 2048, 2048) for i in range(2)]
        tmpf = A(A_SCR + 4096, 2048)
        hbf = [Ab(A_SCR + 6144 + i * 1024, 1024) for i in range(2)]
        t12 = [A(A_SCR + 8192 + i * 512, 512) for i in range(4)]
        ssq = small[:, 0:8]
        rstd = small[:, 8:16]

        def trig_tables(qi):
            ang = xs[0][:, 0:1024]
            kf = xs[0][:, 1024:2048]
            ki = kf.bitcast(I32)
            rr = xs[1][:, 0:1024]
            ab = xs[1][:, 1024:2048]
            rs_ = [res("xs0"), res("xs1")]
            P.op("dve", lambda e: e.tensor_scalar(out=ang, in0=iota_t, scalar1=pc_sb[:, PC_BASE + qi:PC_BASE + qi + 1],
                                                  scalar2=freq, op0=ALU.add, op1=ALU.mult),
                 reads=[res("cst"), res("pcst"), res("freq")], writes=[rs_[0]])
            P.op("dve", lambda e: e.tensor_scalar(out=rr.bitcast(I32), in0=ang, scalar1=INV_2PI, scalar2=None, op0=ALU.mult),
                 reads=[rs_[0]], writes=[rs_[1]])
            P.op("dve", lambda e: e.tensor_copy(out=kf, in_=rr.bitcast(I32)), reads=[rs_[1]], writes=[rs_[0]])
            P.op("dve", lambda e: e.scalar_tensor_tensor(out=rr, in0=kf, scalar=-TWO_PI_HI, in1=ang, op0=ALU.mult, op1=ALU.add),
                 reads=[rs_[0]], writes=[rs_[1]])
            P.op("dve", lambda e: e.scalar_tensor_tensor(out=rr, in0=kf, scalar=-TWO_PI_LO, in1=rr, op0=ALU.mult, op1=ALU.add),
                 reads=[rs_[0], rs_[1]], writes=[rs_[1]])
            P.op("dve", lambda e: e.tensor_scalar(out=rr, in0=rr, scalar1=PI_LO, scalar2=-PI_LO, op0=ALU.min, op1=ALU.max),
                 reads=[rs_[1]], writes=[rs_[1]])
            P.op("dve", lambda e: e.scalar_tensor_tensor(out=ab, in0=rr, scalar=-1.0, in1=rr, op0=ALU.mult, op1=ALU.max),
                 reads=[rs_[1]], writes=[rs_[1]])
            P.op("act", lambda e: e.activation(out=sin_t, in_=rr, func=AF.Sin, scale=col(C_SIGN)),
                 reads=[rs_[1], res("cst")], writes=[res("trig")])
            P.op("act", lambda e: e.activation(out=cos_t, in_=ab, func=AF.Sin, scale=-1.0, bias=col(C_HALFPI)),
                 reads=[rs_[1], res("cst")], writes=[res("trig")])

        def p1(qi):
            x_src = x_q[qi].rearrange("(i p) d -> i p d", p=128)
            for i in range(NT):
                xr = res(f"xs{i % 2}")
                hr = res(f"hbf{i % 2}")
                P.dma("sp", lambda e, i=i: e.dma_start(out=xs[i % 2], in_=x_src[i]), writes=[xr])
                P.op("act", lambda e, i=i: e.activation(out=hbf[i % 2], in_=xs[i % 2], func=AF.Square,
                                                       accum_out=ssq[:, i:i + 1]),
                     reads=[xr], writes=[hr, res(f"ssq{i}")])
                P.op("act", lambda e, i=i: e.activation(out=rstd[:, i:i + 1], in_=ssq[:, i:i + 1], func=AF.Sqrt,
                                                       scale=1.0 / D, bias=col(C_EPS)),
                     reads=[res(f"ssq{i}"), res("cst")], writes=[res(f"rstd{i}")])
                P.op("dve", lambda e, i=i: e.reciprocal(out=rstd[:, i:i + 1], in_=rstd[:, i:i + 1]),
                     reads=[res(f"rstd{i}")], writes=[res(f"rstd{i}")])
                P.op("dve", lambda e, i=i: e.scalar_tensor_tensor(out=tmpf, in0=xs[i % 2], scalar=rstd[:, i:i + 1],
                                                                   in1=brd[0], op0=ALU.mult, op1=ALU.mult),
                     reads=[xr, res(f"rstd{i}"), res("brd0")], writes=[res("tmpf")])
                P.op("pool", lambda e, i=i: e.tensor_tensor(out=hbf[i % 2], in0=tmpf, in1=brd[1], op=ALU.add),
                     reads=[res("tmpf"), res("brd1")], writes=[hr])
                pb = 2 * (i % 2)

                def tr(e, i=i, pb=pb):
                    for c in range(16):
                        ins = e.transpose(out=psb(pb + c // 8)[:, (c % 8) * 128:(c % 8 + 1) * 128],
                                          in_=hbf[i % 2][:, c * 128:(c + 1) * 128], identity=ident_bf)
                    return ins
                P.op("pe", tr, reads=[hr, res("ident_bf")], writes=[bank[pb], bank[pb + 1]])
                P.op("act", lambda e, i=i, pb=pb: e.activation(
                    out=hT[:, 0:8, i * 128:(i + 1) * 128], in_=psb(pb).rearrange("p (c t) -> p c t", c=8), func=AF.Copy),
                    reads=[bank[pb]], writes=[res("hT")])
                P.op("dve", lambda e, i=i, pb=pb: e.tensor_copy(
                    out=hT[:, 8:16, i * 128:(i + 1) * 128], in_=psb(pb + 1).rearrange("p (c t) -> p c t", c=8)),
                    reads=[bank[pb + 1]], writes=[res("hT")])

        def load_w(slot, cols0, ncols, swap=False):
            wr = res(f"wb{slot}")
            if not swap:
                dst = wb[slot][:, 0:16 * ncols].rearrange("p (c n) -> p c n", c=16)
                P.dma("pool", lambda e: e.dma_start(out=dst, in_=w_in_v[:, :, cols0:cols0 + ncols]), writes=[wr])
            else:
                dstv = wb[slot][:, 0:16 * 384].rearrange("p (c h n) -> p c h n", c=16, h=2)
                srcv = w_in_v[:, :, cols0:cols0 + 256].rearrange("p c (h d) -> p c h d", h=2)
                for hh in range(2):
                    P.dma("pool", lambda e, hh=hh: e.dma_start(out=dstv[:, :, hh, 64:192], in_=srcv[:, :, hh, :]), writes=[wr])
                    P.dma("pool", lambda e, hh=hh: e.dma_start(out=dstv[:, :, hh, 0:64], in_=srcv[:, :, hh, 64:128]), writes=[wr])
            return wr

        def rope_block(slot, h0, is_q, qdst=None, qtdst=None, decq=None):
            wr = res(f"wb{slot}")
            wv = wb[slot][:, 0:16 * 384].rearrange("p (c h n) -> p c h n", c=16, h=2)
            for hh in range(2):
                for th in range(2):
                    ba, bb = 4 + 2 * ((hh * 2 + th) % 2), 5 + 2 * ((hh * 2 + th) % 2)
                    ta, tb = t12[2 * ((hh * 2 + th) % 2)], t12[2 * ((hh * 2 + th) % 2) + 1]
                    tra, trb = res(f"t12_{2 * ((hh * 2 + th) % 2)}"), res(f"t12_{2 * ((hh * 2 + th) % 2) + 1}")
                    tok = slice(th * 512, (th + 1) * 512)

                    def mma(e, hh=hh, tok=tok, ba=ba):
                        for c in range(16):
                            ins = e.matmul(psf[ba][:, :], lhsT=wv[:, c, hh, 64:192], rhs=hT[:, c, tok],
                                           start=(c == 0), stop=(c == 15))
                        return ins

                    def mmb(e, hh=hh, tok=tok, bb=bb):
                        for c in range(16):
                            ins = e.matmul(psf[bb][:, :], lhsT=wv[:, c, hh, 0:128], rhs=hT[:, c, tok],
                                           start=(c == 0), stop=(c == 15))
                        return ins
                    P.op("pe", mma, reads=[wr, res("hT")], writes=[bank[ba]])
                    P.op("pe", mmb, reads=[wr, res("hT")], writes=[bank[bb]])
                    P.op("dve", lambda e, ta=ta, ba=ba, tok=tok: e.tensor_tensor(out=ta, in0=psf[ba][:, :], in1=cos_t[:, tok], op=ALU.mult),
                         reads=[bank[ba], res("trig")], writes=[tra])
                    P.op("dve", lambda e, tb=tb, bb=bb, tok=tok: e.tensor_tensor(out=tb, in0=psf[bb][:, :], in1=sin_t[:, tok], op=ALU.mult),
                         reads=[bank[bb], res("trig")], writes=[trb])
                    if not is_q:
                        P.op("pool", lambda e, ta=ta, tb=tb, hh=hh, tok=tok: e.tensor_tensor(
                            out=kT[:, h0 + hh, tok], in0=ta, in1=tb, op=ALU.add),
                            reads=[tra, trb], writes=[res("kT")])
                    else:
                        P.op("pool", lambda e, ta=ta, tb=tb: e.tensor_tensor(out=ta, in0=ta, in1=tb, op=ALU.add),
                             reads=[tra, trb], writes=[tra])
                        P.op("act", lambda e, ta=ta, hh=hh, tok=tok: e.activation(out=qdst[:, hh, tok], in_=ta, func=AF.Copy, scale=QSCALE),
                             reads=[tra], writes=[res("qT")])
                        P.op("dve", lambda e, ta=ta, hh=hh, tok=tok: e.tensor_tensor(out=qtdst[:, hh, tok], in0=ta, in1=decq[hh][:, tok], op=ALU.mult),
                             reads=[tra, res(f"decq{hh}")], writes=[res("qtT")])

        def v_block(slot, vb):
            wr = res(f"wb{slot}")
            wv = wb[slot][:, 0:16 * 256].rearrange("p (c n) -> p c n", c=16)
            for i in range(NT):
                b = 4 + (i % 4)

                def mm(e, i=i, b=b):
                    for c in range(16):
                        ins = e.matmul(psf[b][:, 0:256], lhsT=hT[:, c, i * 128:(i + 1) * 128], rhs=wv[:, c, :],
                                       start=(c == 0), stop=(c == 15))
                    return ins
                P.op("pe", mm, reads=[wr, res("hT")], writes=[bank[b]])
                P.op("act", lambda e, i=i, b=b: e.activation(out=v_sb[:, i, vb * 256:(vb + 1) * 256], in_=psf[b][:, 0:256], func=AF.Copy),
                     reads=[bank[b]], writes=[res("v")])

        kdec_sb = Ab(A_SCR + 4096, 512).rearrange("p (h d) -> p h d", h=NH)

        def state_phase(qi, main):
            for i in range(NT):
                def tr(e, i=i):
                    for h in range(NH):
                        ins = e.transpose(out=psb(0)[:, h * 128:(h + 1) * 128], in_=kT[:, h, i * 128:(i + 1) * 128],
                                          identity=ident_bf)
                    return ins
                P.op("pe", tr, reads=[res("kT"), res("ident_bf")], writes=[bank[0]])
                P.op("dve", lambda e, i=i: e.tensor_tensor(
                    out=kdec_sb, in0=psb(0).rearrange("p (h d) -> p h d", h=NH),
                    in1=kdec_tab[:, qi * 8 + i, :].unsqueeze(2).broadcast_to([128, NH, 128]), op=ALU.mult),
                    reads=[bank[0], res("kdec")], writes=[res("tmpf")])

                def inc(e, i=i):
                    for h in range(NH):
                        ins = e.matmul(psf[2 + h // 4][:, (h % 4) * 128:(h % 4 + 1) * 128], lhsT=kdec_sb[:, h, :],
                                       rhs=v_sb[:, i, h * 128:(h + 1) * 128], start=True, stop=True)
                    return ins
                P.op("pe", inc, reads=[res("tmpf"), res("v")], writes=[bank[2], bank[3]])
                if main:
                    P.op("act", lambda e, i=i: e.activation(out=S_bf[:, i, :, :], in_=S_f32, func=AF.Copy),
                         reads=[res("S")], writes=[res("S_bf")])
                for hb in range(2):
                    P.op("dve", lambda e, hb=hb: e.tensor_tensor(
                        out=S_f32[:, hb * 4:(hb + 1) * 4, :], in0=S_f32[:, hb * 4:(hb + 1) * 4, :],
                        in1=psf[2 + hb][:, :].rearrange("p (h e) -> p h e", h=4), op=ALU.add),
                        reads=[bank[2 + hb], res("S")], writes=[res("S")])

        P.op("pool", lambda e: e.memset(S_f32, 0.0), writes=[res("S")])

        for qi in range(4):
            main = qi == 3
            trig_tables(qi)
            if main:
                P.op("dve", lambda e: e.tensor_copy(out=hT_halo, in_=hT[:, :, 1022:1024]),
                     reads=[res("hT")], writes=[res("hT_halo")])
            p1(qi)
            blocks = [("k", g) for g in range(4)] + [("v", g) for g in range(4)]
            load_w(0, D_RET + 0, 256, swap=True)
            for bi, (kind, g) in enumerate(blocks):
                slot = bi % 2
                if bi + 1 < len(blocks):
                    nk, ng = blocks[bi + 1]
                    if nk == "k":
                        load_w((bi + 1) % 2, D_RET + ng * 256, 256, swap=True)
                    else:
                        load_w((bi + 1) % 2, 2 * D_RET + ng * 256, 256)
                if kind == "k":
                    rope_block(slot, 2 * g, False)
                else:
                    v_block(slot, g)
            state_phase(qi, main)
        P.barrier()

        qT = Ab(A_SCR + 0, 1024).rearrange("p (h t) -> p h t", h=2)
        qtT = Ab(A_SCR + 1024, 1024).rearrange("p (h t) -> p h t", h=2)
        sg = Ab(A_SCR + 2048, 1024).rearrange("p (i n) -> p i n", i=NT)
        decq = [A(A_SCR + 3072 + i * 1024, 1024) for i in range(2)]
        Pm = [Ab(A_SCR + 5120 + i * 128, 128).rearrange("p (h k) -> p h k", h=2) for i in range(2)]
        onb = [A(A_SCR + 5376 + i * 256, 256).rearrange("p (h k) -> p h k", h=2) for i in range(2)]
        yrb = [Ab(A_SCR + 5888 + i * 128, 128).rearrange("p (h k) -> p h k", h=2) for i in range(2)]
        bst = A(A_SCR + 6144, 32).rearrange("p (a h s) -> p a h s", a=2, h=2)
        bmv = A(A_SCR + 6176, 16).rearrange("p (a h s) -> p a h s", a=2, h=2)
        brs = A(A_SCR + 6192, 4).rearrange("p (a h) -> p a h", a=2)

        for g in range(4):
            for hh in range(2):
                P.op("act", lambda e, hh=hh, g=g: e.activation(out=decq[hh], in_=iota_t, func=AF.Exp,
                                                           scale=LOG_GAMMA[2 * g + hh], bias=col(C_LNQ)),
                     reads=[res("cst")], writes=[res(f"decq{hh}")])
            load_w(0, 2 * g * 128, 256, swap=True)
            load_w(1, 3 * D_RET + g * 256, 256)
            rope_block(0, 2 * g, True, qdst=qT, qtdst=qtT, decq=decq)
            wgv = wb[1][:, 0:16 * 256].rearrange("p (c n) -> p c n", c=16)
            for i in range(NT):
                b = i % 2

                def mmg(e, i=i, b=b):
                    for c in range(16):
                        ins = e.matmul(psf[b][:, 0:256], lhsT=hT[:, c, i * 128:(i + 1) * 128], rhs=wgv[:, c, :],
                                       start=(c == 0), stop=(c == 15))
                    return ins
                P.op("pe", mmg, reads=[res("wb1"), res("hT")], writes=[bank[b]])
                P.op("act", lambda e, i=i, b=b: e.activation(out=sg[:, i, :], in_=psf[b][:, 0:256], func=AF.Silu),
                     reads=[bank[b]], writes=[res("sg")])
            for i in range(NT):
                a = i % 2
                tok = slice(i * 128, (i + 1) * 128)
                rPm, ron, ryr, rst = res(f"Pm{a}"), res(f"on{a}"), res(f"yr{a}"), res(f"bst{a}")

                def sc(e, g=g, tok=tok, a=a):
                    for hh in range(2):
                        ins = e.matmul(psf[2 + a][:, hh * 128:(hh + 1) * 128], lhsT=kT[:, 2 * g + hh, tok], rhs=qT[:, hh, tok],
                                       start=True, stop=True)
                    return ins
                P.op("pe", sc, reads=[res("kT"), res("qT")], writes=[bank[2 + a]])
                P.op("dve", lambda e, g=g, a=a: e.tensor_tensor(
                    out=Pm[a], in0=psf[2 + a][:, 0:256].rearrange("p (h k) -> p h k", h=2),
                    in1=maskT[:, 2 * g:2 * g + 2, :], op=ALU.mult),
                    reads=[bank[2 + a], res("maskT")], writes=[rPm])

                def om(e, g=g, i=i, tok=tok, a=a):
                    for hh in range(2):
                        h = 2 * g + hh
                        e.matmul(psf[4 + a][:, hh * 128:(hh + 1) * 128], lhsT=Pm[a][:, hh, :],
                                 rhs=v_sb[:, i, h * 128:(h + 1) * 128], start=True, stop=False)
                        ins = e.matmul(psf[4 + a][:, hh * 128:(hh + 1) * 128], lhsT=qtT[:, hh, tok],
                                       rhs=S_bf[:, i, h, :], start=False, stop=True)
                    return ins
                P.op("pe", om, reads=[rPm, res("v"), res("qtT"), res("S_bf")], writes=[bank[4 + a]])
                for hh in range(2):
                    P.op("dve", lambda e, hh=hh, a=a: e.bn_stats(out=bst[:, a, hh, 0:6], in_=psf[4 + a][:, hh * 128:(hh + 1) * 128]),
                         reads=[bank[4 + a]], writes=[rst])
                for hh in range(2):
                    P.op("dve", lambda e, hh=hh, a=a: e.bn_aggr(out=bmv[:, a, hh, 0:2], in_=bst[:, a, hh, 0:6]),
                         reads=[rst], writes=[rst])
                P.op("act", lambda e, a=a: e.activation(out=brs[:, a, :], in_=bmv[:, a, :, 1], func=AF.Sqrt,
                                                        bias=col(C_EPS)),
                     reads=[rst, res("cst")], writes=[res(f"brs{a}")])
                P.op("dve", lambda e, a=a: e.reciprocal(out=brs[:, a, :], in_=brs[:, a, :]),
                     reads=[res(f"brs{a}")], writes=[res(f"brs{a}")])
                for hh in range(2):
                    P.op("dve", lambda e, hh=hh, a=a: e.tensor_scalar(
                        out=onb[a][:, hh, :], in0=psf[4 + a][:, hh * 128:(hh + 1) * 128],
                        scalar1=bmv[:, a, hh, 0:1], scalar2=brs[:, a, hh:hh + 1], op0=ALU.subtract, op1=ALU.mult),
                        reads=[bank[4 + a], rst, res(f"brs{a}")], writes=[ron])
                P.op("pool", lambda e, i=i, a=a: e.tensor_tensor(
                    out=yrb[a], in0=onb[a], in1=sg[:, i, :].rearrange("p (h k) -> p h k", h=2), op=ALU.mult),
                    reads=[ron, res("sg")], writes=[ryr])

                def trY(e, a=a):
                    for hh in range(2):
                        ins = e.transpose(out=psb(6 + a)[:, hh * 128:(hh + 1) * 128], in_=yrb[a][:, hh, :], identity=ident_bf)
                    return ins
                P.op("pe", trY, reads=[ryr, res("ident_bf")], writes=[bank[6 + a]])
                P.op("act", lambda e, g=g, tok=tok, a=a: e.activation(
                    out=yTr[:, 2 * g:2 * g + 2, tok], in_=psb(6 + a)[:, 0:256].rearrange("p (h k) -> p h k", h=2), func=AF.Copy),
                    reads=[bank[6 + a]], writes=[res("yTr")])
        P.barrier()

        uT = A(A_SCR + 0, 2056)[:, 0:2052].rearrange("p (c t) -> p c t", c=2)
        acc = A(A_SCR + 2056, 2048).rearrange("p (c t) -> p c t", c=2)
        BASE_B, BASE_C, BASE_U = 4 * D_RET, 4 * D_RET + D_CONV, 4 * D_RET + 2 * D_CONV

        def conv_mm(slot, cc, with_halo):
            wv = wb[slot][:, 0:16 * 256].rearrange("p (c n) -> p c n", c=16)
            for th in range(2):
                b = cc * 2 + th

                def mm(e, b=b, th=th):
                    for c in range(16):
                        ins = e.matmul(psf[b][:, :], lhsT=wv[:, c, cc * 128:(cc + 1) * 128], rhs=hT[:, c, th * 512:(th + 1) * 512],
                                       start=(c == 0), stop=(c == 15))
                    return ins
                P.op("pe", mm, reads=[res(f"wb{slot}"), res("hT")], writes=[bank[b]])
            if with_halo:
                def mmh(e):
                    for c in range(16):
                        ins = e.matmul(psf[4 + cc][:, 0:2], lhsT=wv[:, c, cc * 128:(cc + 1) * 128], rhs=hT_halo[:, c, :],
                                       start=(c == 0), stop=(c == 15))
                    return ins
                P.op("pe", mmh, reads=[res(f"wb{slot}"), res("hT_halo")], writes=[bank[4 + cc]])

        for cg in range(4):
            load_w(0, BASE_U + cg * 256, 256)
            load_w(1, BASE_C + cg * 256, 256)
            for cc in range(2):
                conv_mm(0, cc, True)
                for th in range(2):
                    P.op("act", lambda e, cc=cc, th=th: e.activation(
                        out=uT[:, cc, 2 + th * 512:2 + (th + 1) * 512], in_=psf[cc * 2 + th][:, :], func=AF.Copy),
                        reads=[bank[cc * 2 + th]], writes=[res("uT")])
                P.op("act", lambda e, cc=cc: e.activation(out=uT[:, cc, 0:2], in_=psf[4 + cc][:, 0:2], func=AF.Copy),
                     reads=[bank[4 + cc]], writes=[res("uT")])
            for cc in range(2):
                conv_mm(1, cc, True)
                for th in range(2):
                    P.op("dve", lambda e, cc=cc, th=th: e.tensor_tensor(
                        out=uT[:, cc, 2 + th * 512:2 + (th + 1) * 512], in0=psf[cc * 2 + th][:, :],
                        in1=uT[:, cc, 2 + th * 512:2 + (th + 1) * 512], op=ALU.mult),
                        reads=[bank[cc * 2 + th], res("uT")], writes=[res("uT")])
                P.op("dve", lambda e, cc=cc: e.scalar_tensor_tensor(
                    out=uT[:, cc, 0:2], in0=psf[4 + cc][:, 0:2], scalar=pc_sb[:, PC_FLAG:PC_FLAG + 1], in1=uT[:, cc, 0:2],
                    op0=ALU.mult, op1=ALU.mult),
                    reads=[bank[4 + cc], res("uT"), res("pcst")], writes=[res("uT")])
            load_w(0, BASE_B + cg * 256, 256)
            for cc in range(2):
                ch = cg * 2 + cc
                P.op("pool", lambda e, cc=cc, ch=ch: e.tensor_scalar(
                    out=acc[:, cc, :], in0=uT[:, cc, 2:1026], scalar1=cw_sb[:, 2, ch:ch + 1], scalar2=None, op0=ALU.mult),
                    reads=[res("uT"), res("cw")], writes=[res("acc")])
                P.op("dve", lambda e, cc=cc, ch=ch: e.scalar_tensor_tensor(
                    out=acc[:, cc, :], in0=uT[:, cc, 1:1025], scalar=cw_sb[:, 1, ch:ch + 1], in1=acc[:, cc, :],
                    op0=ALU.mult, op1=ALU.add),
                    reads=[res("uT"), res("cw"), res("acc")], writes=[res("acc")])
                P.op("dve", lambda e, cc=cc, ch=ch: e.scalar_tensor_tensor(
                    out=acc[:, cc, :], in0=uT[:, cc, 0:1024], scalar=cw_sb[:, 0, ch:ch + 1], in1=acc[:, cc, :],
                    op0=ALU.mult, op1=ALU.add),
                    reads=[res("uT"), res("cw"), res("acc")], writes=[res("acc")])
            for cc in range(2):
                conv_mm(0, cc, False)
                for th in range(2):
                    P.op("dve", lambda e, cc=cc, th=th, cg=cg: e.tensor_tensor(
                        out=yTc[:, cg * 2 + cc, th * 512:(th + 1) * 512], in0=psf[cc * 2 + th][:, :],
                        in1=acc[:, cc, th * 512:(th + 1) * 512], op=ALU.mult),
                        reads=[bank[cc * 2 + th], res("acc")], writes=[res("yTc")])
        P.barrier()

        x1 = A(A_V, 16384).rearrange("p (i d) -> p i d", i=NT)
        wo = [Ab(A_HT + i * 4096, 4096).rearrange("p (c n) -> p c n", c=16) for i in range(2)]
        otmp = [A(A_SCR + 8192 + i * 512, 512) for i in range(2)]
        P.dma("sp", lambda e: e.dma_start(out=x1, in_=x_q[3].rearrange("(i p) d -> p i d", p=128)), writes=[res("x1")])
        bload("act", brd[0], mod_d[2 * D:3 * D], "brd0")

        def load_wo(nb):
            P.dma("pool", lambda e: e.dma_start(out=wo[nb % 2], in_=w_out_v[:, :, nb * 512:(nb + 1) * 512]),
                  writes=[res(f"wo{nb % 2}")])
        load_wo(0)
        for nb in range(4):
            if nb + 1 < 4:
                load_wo(nb + 1)
            for i in range(NT):
                b = i % 4

                def mm(e, nb=nb, i=i, b=b):
                    for c in range(16):
                        src = yTr[:, c, i * 128:(i + 1) * 128] if c < 8 else yTc[:, c - 8, i * 128:(i + 1) * 128]
                        ins = e.matmul(psf[b][:, :], lhsT=src, rhs=wo[nb % 2][:, c, :], start=(c == 0), stop=(c == 15))
                    return ins
                P.op("pe", mm, reads=[res(f"wo{nb % 2}"), res("yTr"), res("yTc")], writes=[bank[b]])
                P.op("dve", lambda e, nb=nb, i=i, b=b: e.tensor_tensor(
                    out=otmp[i % 2], in0=psf[b][:, :], in1=brd[0][:, nb * 512:(nb + 1) * 512], op=ALU.mult),
                    reads=[bank[b], res("brd0")], writes=[res(f"otmp{i % 2}")])
                P.op("pool", lambda e, nb=nb, i=i: e.tensor_tensor(
                    out=x1[:, i, nb * 512:(nb + 1) * 512], in0=x1[:, i, nb * 512:(nb + 1) * 512], in1=otmp[i % 2], op=ALU.add),
                    reads=[res(f"otmp{i % 2}"), res("x1")], writes=[res("x1")])
        P.barrier()

        if stage == "x1":
            P.dma("sp", lambda e: e.dma_start(out=out.rearrange("(i p) d -> p i d", p=128), in_=x1), reads=[res("x1")])
            P.barrier()
        if stage != "x1":
            tmp2 = A(A_HT, 2048)
            hb2 = [Ab(A_HT + 2048 + i * 1024, 1024) for i in range(2)]
            h2Tf = A(A_HT + 4096, 2048).rearrange("p (c t) -> p c t", c=16)
            wr_sb = A(A_HT + 6144, 512).rearrange("p (c e) -> p c e", c=16)
            brow = arena[0:1, A_HT + 6656:A_HT + 6688]
            so = A_HT + 6688
            lg = A(so, 32); so += 32
            rk = A(so, 32); so += 32
            oh = A(so, 32); so += 32
            ecap = A(so, 32); so += 32
            mkf = A(so, 32); so += 32
            mx8 = A(so, 8); so += 8
            mi8 = A(so, 8).bitcast(U32); so += 8
            idf8 = A(so, 8); so += 8
            ex4 = A(so, 4); so += 4
            destf = A(so, 4); so += 4
            nmx = A(so, 1); so += 1
            ssum = A(so, 1); so += 1
            so += 2
            tri_bf = Ab(so, 64); so += 64
            ones_bf = Ab(so, 64); so += 64
            mk_bf = Ab(so, 128).rearrange("p (i e) -> p i e", i=NT); so += 128
            assert so <= A_HT + 8192
            w4_all = A(A_YTR + 4096, 32).rearrange("p (i k) -> p i k", i=NT)
            dest_all = A(A_YTR + 4128, 32).bitcast(I32).rearrange("p (i k) -> p i k", i=NT)
            ssq2 = A(A_YTR + 4352, 8)
            rstd2 = A(A_YTR + 4360, 8)
            ones_row = cst_sb[0:1, C_ONES:C_ONES + 128]
            iota_e = cst_sb[:, C_IOTA_E:C_IOTA_E + 32]

            tmpc = A(A_TRIG, 2048)
            bload("sp", brd[0], norm2_g, "brd0")
            bload("sp", tmpc, mod_d[4 * D:5 * D], "tmpc")
            bload("act", brd[1], mod_d[3 * D:4 * D], "brd1")
            P.op("dve", lambda e: e.scalar_tensor_tensor(out=brd[0], in0=tmpc, scalar=1.0, in1=brd[0],
                                                         op0=ALU.add, op1=ALU.mult),
                 reads=[res("tmpc"), res("brd0")], writes=[res("brd0")])
            P.dma("sp", lambda e: e.dma_start(out=wr_sb, in_=w_router.rearrange("(c p) e -> p c e", p=128)),
                  writes=[res("wr_sb")])
            P.dma("sp", lambda e: e.dma_start(out=brow, in_=b_router.rearrange("(o n) -> o n", o=1)), writes=[res("brow")])
            P.op("dve", lambda e: e.tensor_copy(out=tri_bf, in_=cst_sb[:, C_TRI:C_TRI + 128]), reads=[res("cst")], writes=[res("tri")])
            P.op("dve", lambda e: e.tensor_copy(out=ones_bf, in_=cst_sb[:, C_ONES:C_ONES + 128]), reads=[res("cst")], writes=[res("tri")])
            P.op("dve", lambda e: e.tensor_scalar(out=ecap, in0=iota_e, scalar1=float(CAP), scalar2=None, op0=ALU.mult),
                 reads=[res("cst")], writes=[res("ecap")])

            for i in range(NT):
                hb = hb2[i % 2]
                rhb = res(f"hb2_{i % 2}")
                P.op("act", lambda e, i=i, hb=hb: e.activation(out=hb, in_=x1[:, i, :], func=AF.Square, accum_out=ssq2[:, i:i + 1]),
                     reads=[res("x1")], writes=[rhb, res("rs2")])
                P.op("act", lambda e, i=i: e.activation(out=rstd2[:, i:i + 1], in_=ssq2[:, i:i + 1], func=AF.Sqrt,
                                                       scale=1.0 / D, bias=col(C_EPS)),
                     reads=[res("rs2"), res("cst")], writes=[res("rs2")])
                P.op("dve", lambda e, i=i: e.reciprocal(out=rstd2[:, i:i + 1], in_=rstd2[:, i:i + 1]),
                     reads=[res("rs2")], writes=[res("rs2")])
                P.op("dve", lambda e, i=i: e.scalar_tensor_tensor(out=tmp2, in0=x1[:, i, :], scalar=rstd2[:, i:i + 1], in1=brd[0],
                                                                   op0=ALU.mult, op1=ALU.mult),
                     reads=[res("x1"), res("rs2"), res("brd0")], writes=[res("tmp2")])
                P.op("dve", lambda e: e.tensor_tensor(out=tmp2, in0=tmp2, in1=brd[1], op=ALU.add),
                     reads=[res("tmp2"), res("brd1")], writes=[res("tmp2")])
                P.op("act", lambda e, hb=hb: e.activation(out=hb, in_=tmp2, func=AF.Copy), reads=[res("tmp2")], writes=[rhb])

                def trf(e):
                    for c in range(16):
                        ins = e.transpose(out=psf[2 + c // 4][:, (c % 4) * 128:(c % 4 + 1) * 128],
                                          in_=tmp2[:, c * 128:(c + 1) * 128], identity=ident_f)
                    return ins
                P.op("pe", trf, reads=[res("tmp2"), res("cst")], writes=[bank[2], bank[3], bank[4], bank[5]])
                for k in range(4):
                    if k % 2 == 0:
                        P.op("act", lambda e, k=k: e.activation(out=h2Tf[:, 4 * k:4 * k + 4, :],
                                                                in_=psf[2 + k][:, :].rearrange("p (c t) -> p c t", c=4), func=AF.Copy),
                             reads=[bank[2 + k]], writes=[res("h2Tf")])
                    else:
                        P.op("dve", lambda e, k=k: e.tensor_copy(out=h2Tf[:, 4 * k:4 * k + 4, :],
                                                                 in_=psf[2 + k][:, :].rearrange("p (c t) -> p c t", c=4)),
                             reads=[bank[2 + k]], writes=[res("h2Tf")])

                def lgm(e):
                    for c in range(16):
                        e.matmul(psf[6][:, 0:32], lhsT=h2Tf[:, c, :], rhs=wr_sb[:, c, :], start=(c == 0), stop=False)
                    return e.matmul(psf[6][:, 0:32], lhsT=ones_row, rhs=brow, start=False, stop=True)
                P.op("pe", lgm, reads=[res("h2Tf"), res("wr_sb"), res("brow"), res("cst")], writes=[bank[6]])
                rt = res("rt")
                P.op("dve", lambda e: e.tensor_copy(out=lg, in_=psf[6][:, 0:32]), reads=[bank[6]], writes=[rt])
                P.op("dve", lambda e: e.max(out=mx8, in_=lg), reads=[rt], writes=[rt])
                P.op("dve", lambda e: e.max_index(out=mi8, in_max=mx8, in_values=lg), reads=[rt], writes=[rt])
                P.op("dve", lambda e: e.tensor_copy(out=idf8, in_=mi8), reads=[rt], writes=[rt])
                P.op("dve", lambda e: e.tensor_scalar(out=mkf, in0=lg, scalar1=mx8[:, 3:4], scalar2=None, op0=ALU.is_ge),
                     reads=[rt], writes=[rt])
                P.op("dve", lambda e, i=i: e.tensor_copy(out=mk_bf[:, i, :], in_=mkf), reads=[rt], writes=[res("mk_bf")])
                P.op("dve", lambda e: e.tensor_scalar(out=nmx, in0=mx8[:, 0:1], scalar1=-1.0, scalar2=None, op0=ALU.mult),
                     reads=[rt], writes=[rt])
                P.op("act", lambda e: e.activation(out=ex4, in_=mx8[:, 0:4], func=AF.Exp, bias=nmx), reads=[rt], writes=[rt])
                P.op("dve", lambda e: e.reduce_sum(out=ssum, in_=ex4, axis=mybir.AxisListType.X), reads=[rt], writes=[rt])
                P.op("dve", lambda e: e.reciprocal(out=ssum, in_=ssum), reads=[rt], writes=[rt])
                P.op("dve", lambda e, i=i: e.tensor_scalar(out=w4_all[:, i, :], in0=ex4, scalar1=ssum, scalar2=None, op0=ALU.mult),
                     reads=[rt], writes=[res("w4")])

                def rkm(e, i=i):
                    for ip in range(i):
                        e.matmul(psf[7][:, 0:32], lhsT=ones_bf, rhs=mk_bf[:, ip, :], start=(ip == 0), stop=False)
                    return e.matmul(psf[7][:, 0:32], lhsT=tri_bf, rhs=mk_bf[:, i, :], start=(i == 0), stop=True)
                P.op("pe", rkm, reads=[res("mk_bf"), res("tri")], writes=[bank[7]])
                P.op("dve", lambda e: e.tensor_tensor(out=rk, in0=psf[7][:, 0:32], in1=ecap, op=ALU.add),
                     reads=[bank[7], res("ecap")], writes=[rt])
                for k in range(TOPK):
                    P.op("dve", lambda e, k=k: e.tensor_scalar(out=oh, in0=iota_e, scalar1=idf8[:, k:k + 1], scalar2=None, op0=ALU.is_equal),
                         reads=[rt, res("cst")], writes=[rt])
                    P.op("dve", lambda e: e.tensor_tensor(out=oh, in0=oh, in1=rk, op=ALU.mult), reads=[rt], writes=[rt])
                    P.op("dve", lambda e, k=k: e.reduce_sum(out=destf[:, k:k + 1], in_=oh, axis=mybir.AxisListType.X),
                         reads=[rt], writes=[rt])
                P.op("dve", lambda e, i=i: e.tensor_copy(out=dest_all[:, i, :], in_=destf), reads=[rt], writes=[res("dest")])
                for k in range(TOPK):
                    P.dma("pool", lambda e, i=i, k=k, hb=hb: e.indirect_dma_start(
                        out=xe_d, out_offset=bass.IndirectOffsetOnAxis(ap=dest_all[:, i, k:k + 1], axis=0),
                        in_=hb, in_offset=None),
                        reads=[rhb, res("dest")], writes=[res("xe_d")])
            P.barrier()

            bgu_all = A(A_SF32, 1024).rearrange("p (c e) -> p c e", c=32)
            stage_b = arena[0:32, A_YTR:A_YTR + 4096]
            P.dma("sp", lambda e: e.dma_start(out=stage_b, in_=b_gate_up), writes=[res("stage_b")])
            for half in range(2):
                def trbias(e, half=half):
                    for cc in range(16):
                        ch = half * 16 + cc
                        ins = e.transpose(out=psf[half][:, cc * 32:(cc + 1) * 32], in_=stage_b[:, ch * 128:(ch + 1) * 128],
                                          identity=ident_f[0:32, 0:32])
                    return ins
                P.op("pe", trbias, reads=[res("stage_b"), res("cst")], writes=[bank[half]])
                P.op("dve", lambda e, half=half: e.tensor_copy(out=bgu_all[:, half * 16:(half + 1) * 16, :],
                                                               in_=psf[half][:, :].rearrange("p (c e) -> p c e", c=16)),
                     reads=[bank[half]], writes=[res("bgu")])
            P.barrier()

            NM = CAP // 128
            xeT = [Ab(A_HT + i * 4096, 4096).rearrange("p (c s) -> p c s", c=16) for i in range(2)]
            actT = Ab(A_WB, 4096).rearrange("p (c s) -> p c s", c=16)
            xe_sb = [Ab(A_WB + 4096 + i * 1024, 1024) for i in range(2)]
            wdn = [Ab(A_BRD + 2048, 2048).rearrange("p (c n) -> p c n", c=16), Ab(A_WB + 6144, 2048).rearrange("p (c n) -> p c n", c=16)]
            bd_b = A(A_WB + 8192, 2048)
            yst = [A(A_YTR + i * 1024, 1024).rearrange("p (m n) -> p m n", m=NM) for i in range(2)]
            g1 = A(A_SCR + 8192, 512)
            u1 = A(A_SCR + 8704, 512)
            sgm = A(A_SCR + 9216, 512)
            wgu = [Ab(A_TRIG, 2048).rearrange("p (c n) -> p c n", c=16), Ab(A_BRD, 2048).rearrange("p (c n) -> p c n", c=16)]
            NGS, NDS = len(wgu), len(wdn)

            def load_gu(e_, cp, slot):
                wv = w_gate_up[e_].rearrange("(c p) n -> p c n", p=128)
                P.dma("pool", lambda e: e.dma_start(out=wgu[slot][:, :, 0:128], in_=wv[:, :, cp * 128:(cp + 1) * 128]),
                      writes=[res(f"wgu{slot}")])
                P.dma("pool", lambda e: e.dma_start(out=wgu[slot][:, :, 128:256], in_=wv[:, :, DFF + cp * 128:DFF + (cp + 1) * 128]),
                      writes=[res(f"wgu{slot}")])

            def load_dn(e_, nb, slot):
                wv = w_down[e_].rearrange("(c p) n -> p c n", p=128)
                P.dma("pool", lambda e: e.dma_start(out=wdn[slot], in_=wv[:, :, nb * 256:(nb + 1) * 256]),
                      writes=[res(f"wdn{slot}")])

            xl = 0

            def load_xe(e_, m):
                nonlocal xl
                b_ = xl % 2
                xl += 1
                P.dma("sp", lambda e: e.dma_start(out=xe_sb[b_], in_=xe_d[e_ * CAP + m * 128:e_ * CAP + (m + 1) * 128, :]),
                      reads=[res("xe_d")], writes=[res(f"xe_sb{b_}")])
                return b_

            gu_jobs = [(e_, cp) for e_ in range(NEW) for cp in range(16)]
            dn_jobs = [(e_, nb) for e_ in range(NEW) for nb in range(8)]
            gl = 0
            dl = 0

            def pump_gu(upto):
                nonlocal gl
                while gl < len(gu_jobs) and gl < upto:
                    load_gu(gu_jobs[gl][0], gu_jobs[gl][1], gl % NGS)
                    gl += 1

            def pump_dn(upto):
                nonlocal dl
                while dl < len(dn_jobs) and dl < upto:
                    load_dn(dn_jobs[dl][0], dn_jobs[dl][1], dl % NDS)
                    dl += 1

            pump_gu(1)
            gi = 0
            di = 0
            tj = 0
            for e_ in range(NEW):
                s_ = e_ % 2
                P.dma("sp", lambda e, e_=e_: e.dma_start(out=bd_b, in_=b_down[e_].partition_broadcast(128)), writes=[res("bd_b")])
                for m in range(NM):
                    b_ = load_xe(e_, m)
                    pa = 2 * (tj % 2)
                    tj += 1

                    def trx(e, b_=b_, pa=pa):
                        for c in range(16):
                            ins = e.transpose(out=psb(pa + c // 8)[:, (c % 8) * 128:(c % 8 + 1) * 128],
                                              in_=xe_sb[b_][:, c * 128:(c + 1) * 128], identity=ident_bf)
                        return ins
                    P.op("pe", trx, reads=[res(f"xe_sb{b_}"), res("ident_bf")], writes=[bank[pa], bank[pa + 1]])
                    P.op("act", lambda e, m=m, s_=s_, pa=pa: e.activation(
                        out=xeT[s_][:, 0:8, m * 128:(m + 1) * 128], in_=psb(pa).rearrange("p (c t) -> p c t", c=8), func=AF.Copy),
                        reads=[bank[pa]], writes=[res(f"xeT{s_}")])
                    P.op("dve", lambda e, m=m, s_=s_, pa=pa: e.tensor_copy(
                        out=xeT[s_][:, 8:16, m * 128:(m + 1) * 128], in_=psb(pa + 1).rearrange("p (c t) -> p c t", c=8)),
                        reads=[bank[pa + 1]], writes=[res(f"xeT{s_}")])
                for cp in range(16):
                    slot = gi % NGS
                    pump_gu(gi + NGS)
                    if cp in (0, 8):
                        pump_dn(di + NDS if cp == 8 else di + 1)
                    bg, bu = 4 + 2 * (cp % 2), 5 + 2 * (cp % 2)

                    def mg(e, slot=slot, s_=s_, bg=bg):
                        for c in range(16):
                            ins = e.matmul(psf[bg][:, 0:CAP], lhsT=wgu[slot][:, c, 0:128], rhs=xeT[s_][:, c, :],
                                           start=(c == 0), stop=(c == 15))
                        return ins

                    def mu(e, slot=slot, s_=s_, bu=bu):
                        for c in range(16):
                            ins = e.matmul(psf[bu][:, 0:CAP], lhsT=wgu[slot][:, c, 128:256], rhs=xeT[s_][:, c, :],
                                           start=(c == 0), stop=(c == 15))
                        return ins
                    P.op("pe", mg, reads=[res(f"wgu{slot}"), res(f"xeT{s_}")], writes=[bank[bg]])
                    P.op("pe", mu, reads=[res(f"wgu{slot}"), res(f"xeT{s_}")], writes=[bank[bu]])
                    P.op("dve", lambda e, bg=bg, cp=cp, e_=e_: e.tensor_scalar(
                        out=g1, in0=psf[bg][:, 0:CAP], scalar1=bgu_all[:, cp, e_:e_ + 1], scalar2=7.0, op0=ALU.add, op1=ALU.min),
                        reads=[bank[bg], res("bgu")], writes=[res("g1")])
                    P.op("act", lambda e: e.activation(out=sgm, in_=g1, func=AF.Sigmoid, scale=1.702),
                         reads=[res("g1")], writes=[res("sgm")])
                    P.op("act", lambda e, bu=bu, cp=cp, e_=e_: e.activation(
                        out=u1, in_=psf[bu][:, 0:CAP], func=AF.Identity, bias=bgu_all[:, 16 + cp, e_:e_ + 1]),
                        reads=[bank[bu], res("bgu")], writes=[res("u1")])
                    P.op("dve", lambda e: e.tensor_scalar(out=u1, in0=u1, scalar1=-7.0, scalar2=7.0, op0=ALU.max, op1=ALU.min),
                         reads=[res("u1")], writes=[res("u1")])
                    P.op("dve", lambda e: e.tensor_tensor(out=g1, in0=g1, in1=sgm, op=ALU.mult),
                         reads=[res("g1"), res("sgm")], writes=[res("g1")])
                    P.op("dve", lambda e, cp=cp: e.scalar_tensor_tensor(
                        out=actT[:, cp, :], in0=u1, scalar=1.0, in1=g1, op0=ALU.add, op1=ALU.mult),
                        reads=[res("u1"), res("g1")], writes=[res("actT")])
                    gi += 1
                for nb in range(8):
                    slot = di % NDS
                    pump_dn(di + NDS)
                    cols = slice(nb * 256, (nb + 1) * 256)
                    ys = yst[nb % 2]
                    rys = res(f"yst{nb % 2}")
                    for m in range(NM):
                        b0 = (nb * NM + m) % 4

                        def md(e, slot=slot, m=m, b0=b0):
                            for c in range(16):
                                ins = e.matmul(psf[b0][:, 0:256], lhsT=actT[:, c, m * 128:(m + 1) * 128], rhs=wdn[slot][:, c, :],
                                               start=(c == 0), stop=(c == 15))
                            return ins
                        P.op("pe", md, reads=[res(f"wdn{slot}"), res("actT")], writes=[bank[b0]])
                        P.op("dve", lambda e, b0=b0, cols=cols, m=m, ys=ys: e.tensor_tensor(
                            out=ys[:, m, :], in0=psf[b0][:, 0:256], in1=bd_b[:, cols], op=ALU.add),
                            reads=[bank[b0], res("bd_b")], writes=[rys])
                    P.dma("sp", lambda e, e_=e_, cols=cols, ys=ys: e.dma_start(
                        out=ye_d[e_ * CAP:(e_ + 1) * CAP, cols].rearrange("(m p) n -> p m n", p=128), in_=ys),
                        reads=[rys], writes=[res("ye_d")])
                    di += 1
            P.barrier()

            bload("sp", brd[0], mod_d[5 * D:6 * D], "brd0")
            yg = [A(A_HT + i * 2048, 2048) for i in range(2)]
            ctm = A(A_HT + 4096, 2048)
            for i in range(NT):
                for k in range(TOPK):
                    j = i * TOPK + k
                    P.dma("pool", lambda e, i=i, k=k, j=j: e.indirect_dma_start(
                        out=yg[j % 2], out_offset=None, in_=ye_d,
                        in_offset=bass.IndirectOffsetOnAxis(ap=dest_all[:, i, k:k + 1], axis=0)),
                        reads=[res("ye_d"), res("dest")], writes=[res(f"yg{j % 2}")])
                    P.op("dve", lambda e, j=j: e.tensor_tensor(out=ctm, in0=yg[j % 2], in1=brd[0], op=ALU.mult),
                         reads=[res(f"yg{j % 2}"), res("brd0")], writes=[res("ctm")])
                    P.op("dve", lambda e, i=i, k=k: e.scalar_tensor_tensor(
                        out=x1[:, i, :], in0=ctm, scalar=w4_all[:, i, k:k + 1], in1=x1[:, i, :], op0=ALU.mult, op1=ALU.add),
                        reads=[res("ctm"), res("w4"), res("x1")], writes=[res("x1")])
            P.barrier()

            fg_b = A(A_TRIG, 2048)
            ot = [A(A_WB + i * 2048, 2048) for i in range(2)]
            junk = Ab(A_WB + 4096, 1024)
            bload("sp", fg_b, final_g, "fg_b")
            out_v = out.rearrange("(i p) d -> i p d", p=128)
            for i in range(NT):
                P.op("act", lambda e, i=i: e.activation(out=junk, in_=x1[:, i, :], func=AF.Square, accum_out=ssq2[:, i:i + 1]),
                     reads=[res("x1")], writes=[res("junk"), res("rs3")])
                P.op("act", lambda e, i=i: e.activation(out=rstd2[:, i:i + 1], in_=ssq2[:, i:i + 1], func=AF.Sqrt,
                                                       scale=1.0 / D, bias=col(C_EPS)),
                     reads=[res("rs3"), res("cst")], writes=[res("rs3")])
                P.op("dve", lambda e, i=i: e.reciprocal(out=rstd2[:, i:i + 1], in_=rstd2[:, i:i + 1]),
                     reads=[res("rs3")], writes=[res("rs3")])
                P.op("dve", lambda e, i=i: e.scalar_tensor_tensor(out=ot[i % 2], in0=x1[:, i, :], scalar=rstd2[:, i:i + 1], in1=fg_b,
                                                                   op0=ALU.mult, op1=ALU.mult),
                     reads=[res("x1"), res("rs3"), res("fg_b")], writes=[res(f"ot{i % 2}")])
                P.dma("sp", lambda e, i=i: e.dma_start(out=out_v[i], in_=ot[i % 2]), reads=[res(f"ot{i % 2}")])
            P.barrier()


        with nc.Block() as block:
            @block.sync
            def _(e):
                P.emit("sp", e)

            @block.scalar
            def _(e):
                P.emit("act", e)

            @block.vector
            def _(e):
                P.emit("dve", e)

            @block.gpsimd
            def _(e):
                P.emit("pool", e)

            @block.tensor
            def _(e):
                P.emit("pe", e)
    return nc


def _consts():
    c = np.zeros((128, C_N), np.float32)
    p = np.arange(128)
    c[:, C_IOTA_T:C_IOTA_T + 1024] = np.arange(1024)[None, :]
    c[:, C_IDENT:C_IDENT + 128] = np.eye(128)
    kt = p[:, None]
    qt = p[None, :]
    c[:, C_DIST:C_DIST + 128] = np.abs(kt - qt)
    c[:, C_ALLOW:C_ALLOW + 128] = ((kt // 64) <= (qt // 64))
    c[:, C_TRI:C_TRI + 128] = (kt < qt)
    c[:, C_ONES:C_ONES + 128] = 1.0
    c[:, C_IOTA_E:C_IOTA_E + 32] = np.arange(32)[None, :]
    c[:, C_IMOD] = p % 64
    c[:, C_SIGN] = np.where(p < 64, -1.0, 1.0)
    c[:, C_HALFPI] = np.pi / 2
    c[:, C_EPS] = EPS
    c[:, C_LNQ] = np.log(QSCALE)
    c[:, C_ONE] = 1.0
    return c


def _percore(j):
    t = np.zeros((128, PC_N), np.float32)
    p = np.arange(128)
    for qi in range(4):
        if qi < 3:
            s = 3 - qi
            valid = s <= j
            t[:, PC_BASE + qi] = (j - s) * 1024 if valid else 0
            for i in range(8):
                t[:, PC_E + qi * 8 + i] = (1024 * s - (128 * i + p)) if valid else 1.0e6
        else:
            t[:, PC_BASE + qi] = j * 1024
            for i in range(8):
                t[:, PC_E + qi * 8 + i] = -(128 * i + p)
    t[:, PC_FLAG] = 1.0 if j > 0 else 0.0
    return t


_NC_CACHE = {}


def make_in_maps(x, c, norm1_g, w_mod, b_mod, w_in, conv_w, w_out, norm2_g, w_router, b_router,
                 w_gate_up, b_gate_up, w_down, b_down, final_g):
    f = lambda a: np.ascontiguousarray(np.asarray(a, dtype=np.float32))
    x = f(x); c = f(c)
    shared = dict(
        cst=_consts(), norm1_g=f(norm1_g[0]), w_mod=f(w_mod[0]), b_mod=f(b_mod[0]), w_in=f(w_in[0]),
        conv_w=f(conv_w[0]), w_out=f(w_out[0]), norm2_g=f(norm2_g[0]), w_router=f(w_router[0]),
        b_router=f(b_router[0]), w_gate_up=f(w_gate_up[0]), b_gate_up=f(b_gate_up[0]),
        w_down=f(w_down[0]), b_down=f(b_down[0]), final_g=f(final_g))
    in_maps = []
    for core in range(8):
        b, j = core // 4, core % 4
        xq = np.zeros((4, T, D), np.float32)
        for qi in range(3):
            s = 3 - qi
            if s <= j:
                xq[qi] = x[b, (j - s) * T:(j - s + 1) * T]
        xq[3] = x[b, j * T:(j + 1) * T]
        m = dict(shared)
        m["x_q"] = xq
        m["c_pc"] = np.ascontiguousarray(c[b].reshape(128, 16))
        m["pcst"] = _percore(j)
        in_maps.append(m)
    return in_maps


def kernel(**inputs):
    if "nc" not in _NC_CACHE:
        _NC_CACHE["nc"] = build_nc()
    nc = _NC_CACHE["nc"]
    in_maps = make_in_maps(**inputs)
    res = run_bass_kernel_spmd(nc, in_maps, core_ids=list(range(8)))
    outs = [np.asarray(r["out"], dtype=np.float32) for r in res.results]
    full = np.stack(outs, 0).reshape(2, 4, T, D).reshape(2, 4 * T, D)
    return full
```

```python
import numpy as np
import concourse.bass as bass
import concourse.mybir as mybir
from concourse.bass_utils import run_bass_kernel_spmd

F32 = mybir.dt.float32
BF16 = mybir.dt.bfloat16
I32 = mybir.dt.int32
U32 = mybir.dt.uint32
AF = mybir.ActivationFunctionType
ALU = mybir.AluOpType

D = 2048
T = 1024
NT = 8
D_RET = 1024
D_CONV = 1024
NH = 8
HD = 128
D_IN = 7168
NE = 32
TOPK = 4
DFF = 2048
CAP = 512
EPS = 1e-6
QSCALE = float(HD ** -0.5)
LOG_GAMMA = [float(np.log1p(-2.0 ** (-5.0 - h))) for h in range(NH)]
TWO_PI_HI = 6.28125
TWO_PI_LO = 2.0 * np.pi - 6.28125
INV_2PI = float(1.0 / (2.0 * np.pi))
PI_LO = 3.1415925

C_IOTA_T = 0
C_IDENT = 1024
C_DIST = 1152
C_ALLOW = 1280
C_TRI = 1408
C_ONES = 1536
C_IOTA_E = 1664
C_IMOD = 1696
C_SIGN = 1697
C_HALFPI = 1698
C_EPS = 1699
C_LNQ = 1700
C_ONE = 1701
C_N = 1704
PC_BASE = 0
PC_E = 4
PC_FLAG = 36
PC_N = 40

A_CONST = 0
A_TRIG = 3328
A_BRD = A_TRIG + 2048
A_SF32 = A_BRD + 4096
A_HT = A_SF32 + 1024
A_WB = A_HT + 8192
A_KT = A_WB + 6144
A_V = A_KT + 4096
A_SBF = A_V + 4096
A_SCR = A_SBF + 4096
A_YTR = A_SCR + 10240
A_END = A_YTR + 5120


class Res:
    __slots__ = ("name", "w", "r")

    def __init__(self, name):
        self.name = name
        self.w = None
        self.r = []


class Q:
    def __init__(self, name, sem):
        self.name = name
        self.sem = sem
        self.n = 0
        self.waited = {}
        self.ops = []
        self.chans = []
        self.ci = 0


class Prog:
    def __init__(self, nc):
        self.nc = nc
        self.sems = []
        self.q = {}

    def add_queue(self, name, sem, chan_sems=()):
        q = Q(name, len(self.sems))
        self.sems.append(sem)
        for cs in chan_sems:
            q.chans.append([len(self.sems), 0])
            self.sems.append(cs)
        self.q[name] = q

    def _collect(self, q, reads, writes):
        need = {}

        def add(ev, is_war):
            if ev is None:
                return
            k, val, eng = ev
            if eng == q.name:
                if q.name == "pe":
                    return
                if is_war:
                    return
            if q.waited.get(k, 0) >= val:
                return
            if need.get(k, 0) < val:
                need[k] = val

        for r in reads:
            add(r.w, False)
        for w in writes:
            add(w.w, False)
            for e in w.r:
                add(e, True)
        return need

    def _commit(self, q, need):
        for k, val in need.items():
            q.waited[k] = val
        return [(k, v) for k, v in need.items()]

    def op(self, qn, fn, reads=(), writes=()):
        q = self.q[qn]
        need = self._collect(q, reads, writes)
        waits = self._commit(q, need)
        q.n += 1
        ev = (q.sem, q.n, q.name)
        q.ops.append((waits, fn, q.sem, 1))
        for r in reads:
            r.r.append(ev)
        for w in writes:
            w.w = ev
            w.r = []
        return ev

    def dma(self, qn, fn, reads=(), writes=()):
        q = self.q[qn]
        need = self._collect(q, reads, writes)
        ch = q.chans[q.ci]
        q.ci = (q.ci + 1) % len(q.chans)
        if ch[1] > 0 and q.waited.get(ch[0], 0) < 16 * ch[1]:
            if need.get(ch[0], 0) < 16 * ch[1]:
                need[ch[0]] = 16 * ch[1]
        waits = self._commit(q, need)
        ch[1] += 1
        ev = (ch[0], 16 * ch[1], "dma")
        q.ops.append((waits, fn, ch[0], 16))
        for r in reads:
            r.r.append(ev)
        for w in writes:
            w.w = ev
            w.r = []
        return ev

    def barrier(self):
        evs = []
        for q in self.q.values():
            if q.n > 0:
                evs.append((q.sem, q.n))
            for ch in q.chans:
                if ch[1] > 0:
                    evs.append((ch[0], 16 * ch[1]))
        for q in self.q.values():
            need = {}
            for k, val in evs:
                if k == q.sem and q.name != "pe" and False:
                    continue
                if q.waited.get(k, 0) < val:
                    need[k] = val
            waits = self._commit(q, need)
            if waits:
                q.ops.append((waits, None, None, 0))

    def emit(self, qn, eng):
        q = self.q[qn]
        for waits, fn, semk, inc in q.ops:
            for k, val in waits:
                eng.wait_ge(self.sems[k], val)
            if fn is not None:
                ins = fn(eng)
                ins.then_inc(self.sems[semk], inc)


def build_nc(stage="full"):
    nc = bass.Bass("TRN2", target_bir_lowering=False)
    NEW = NE if stage == "full" else 1

    def din(name, shape, dt=F32):
        return nc.dram_tensor(name, list(shape), dt, kind="ExternalInput").ap()

    x_q = din("x_q", [4, T, D])
    c_pc = din("c_pc", [128, 16])
    cst = din("cst", [128, C_N])
    pcst = din("pcst", [128, PC_N])
    norm1_g = din("norm1_g", [D])
    w_mod = din("w_mod", [D, 6 * D])
    b_mod = din("b_mod", [6 * D])
    w_in = din("w_in", [D, D_IN])
    conv_w = din("conv_w", [3, D_CONV])
    w_out = din("w_out", [D, D])
    norm2_g = din("norm2_g", [D])
    w_router = din("w_router", [D, NE])
    b_router = din("b_router", [NE])
    w_gate_up = din("w_gate_up", [NEW, D, 2 * DFF])
    b_gate_up = din("b_gate_up", [NE, 2 * DFF])
    w_down = din("w_down", [NEW, DFF, D])
    b_down = din("b_down", [NE, D])
    final_g = din("final_g", [D])
    out = nc.dram_tensor("out", [T, D], F32, kind="ExternalOutput").ap()
    mod_d = nc.dram_tensor("mod_d", [6 * D], F32, kind="Internal").ap()
    x1_d = nc.dram_tensor("x1_d", [128, NT, D], F32, kind="Internal").ap()
    xe_d = nc.dram_tensor("xe_d", [NE * CAP, D], BF16, kind="Internal").ap()
    ye_d = nc.dram_tensor("ye_d", [NE * CAP, D], F32, kind="Internal").ap()

    w_in_v = w_in.rearrange("(c p) n -> p c n", p=128)
    w_out_v = w_out.rearrange("(c p) n -> p c n", p=128)

    from contextlib import ExitStack
    es = ExitStack()
    with es:
        arena = es.enter_context(nc.sbuf_tensor("arena", [128, A_END], F32))
        psf = [es.enter_context(nc.psum_tensor(f"ps{i}", [128, 512], F32)) for i in range(8)]
        nsem = 5 + 8 + 6 + 2
        sems = [es.enter_context(nc.semaphore(f"s{i}")) for i in range(nsem)]
        P = Prog(nc)
        P.add_queue("pe", sems[0])
        P.add_queue("act", sems[1], sems[19:21])
        P.add_queue("dve", sems[2])
        P.add_queue("pool", sems[3], sems[5:13])
        P.add_queue("sp", sems[4], sems[13:19])

        def A(off, n):
            return arena[:, off:off + n]

        def Ab(off, nwords):
            return arena[:, off:off + nwords].bitcast(BF16)

        def psb(i):
            return psf[i][:, :].bitcast(BF16)

        bank = [Res(f"bank{i}") for i in range(8)]

        cst_sb = A(A_CONST, C_N)
        pc_sb = A(A_CONST + C_N, PC_N)
        o = A_CONST + C_N + PC_N
        ident_bf = Ab(o, 64); o += 64
        maskT = A(o, 1024).rearrange("p (h k) -> p h k", h=NH); o += 1024
        kdec_tab = A(o, 256).rearrange("p (a h) -> p a h", h=NH); o += 256
        freq = A(o, 1); o += 1
        c_sb = A(o, 16); o += 16
        c_act = A(o, 16); o += 16
        cw_sb = A(o, 24).rearrange("p (k c) -> p k c", k=3); o += 24
        small = A(o, 64); o += 64
        hT_halo = Ab(o, 16).rearrange("p (c t) -> p c t", c=16); o += 16
        assert o <= A_TRIG, o
        iota_t = cst_sb[:, C_IOTA_T:C_IOTA_T + 1024]
        ident_f = cst_sb[:, C_IDENT:C_IDENT + 128]
        dist = cst_sb[:, C_DIST:C_DIST + 128]
        allow = cst_sb[:, C_ALLOW:C_ALLOW + 128]

        def col(c):
            return cst_sb[:, c:c + 1]

        cos_t = A(A_TRIG, 1024)
        sin_t = A(A_TRIG + 1024, 1024)
        brd = [A(A_BRD, 2048), A(A_BRD + 2048, 2048)]
        S_f32 = A(A_SF32, 1024).rearrange("p (h e) -> p h e", h=NH)
        hT = Ab(A_HT, 8192).rearrange("p (c t) -> p c t", c=16)
        wb = [Ab(A_WB + i * 3072, 3072) for i in range(2)]
        kT = Ab(A_KT, 4096).rearrange("p (h t) -> p h t", h=NH)
        v_sb = Ab(A_V, 4096).rearrange("p (i n) -> p i n", i=NT)
        S_bf = Ab(A_SBF, 4096).rearrange("p (i h e) -> p i h e", i=NT, h=NH)
        yTr = Ab(A_YTR, 4096).rearrange("p (c t) -> p c t", c=8)
        yTc = Ab(A_KT, 4096).rearrange("p (c t) -> p c t", c=8)

        R = {}

        def res(name):
            if name not in R:
                R[name] = Res(name)
            return R[name]

        P.dma("sp", lambda e: e.dma_start(out=cst_sb, in_=cst), writes=[res("cst")])
        P.dma("sp", lambda e: e.dma_start(out=pc_sb, in_=pcst), writes=[res("pcst")])
        P.dma("sp", lambda e: e.dma_start(out=c_sb, in_=c_pc), writes=[res("c_sb")])
        P.dma("sp", lambda e: e.dma_start(
            out=cw_sb, in_=conv_w.rearrange("k (c p) -> p k c", p=128),
            allow_slow_non_contiguous=True), writes=[res("cw")])
        P.op("dve", lambda e: e.tensor_copy(out=ident_bf, in_=ident_f), reads=[res("cst")], writes=[res("ident_bf")])
        P.op("act", lambda e: e.activation(out=freq, in_=col(C_IMOD), func=AF.Exp,
                                           scale=float(-np.log(10000.0) / 64.0)),
             reads=[res("cst")], writes=[res("freq")])
        for h in range(NH):
            P.op("act", lambda e, h=h: e.activation(out=maskT[:, h, :], in_=dist, func=AF.Exp, scale=LOG_GAMMA[h]),
                 reads=[res("cst")], writes=[res("maskT")])
        P.op("dve", lambda e: e.tensor_tensor(
            out=maskT, in0=maskT, in1=allow.unsqueeze(1).broadcast_to([128, NH, 128]), op=ALU.mult),
            reads=[res("maskT"), res("cst")], writes=[res("maskT")])
        for h in range(NH):
            P.op("act", lambda e, h=h: e.activation(
                out=kdec_tab[:, :, h], in_=pc_sb[:, PC_E:PC_E + 32], func=AF.Exp, scale=LOG_GAMMA[h]),
                reads=[res("pcst")], writes=[res("kdec")])
        P.op("act", lambda e: e.activation(out=c_act, in_=c_sb, func=AF.Silu), reads=[res("c_sb")], writes=[res("c_act")])

        wm = [A(A_HT + i * 8192, 8192).rearrange("p (c n) -> p c n", c=16) for i in range(2)]
        modrow = arena[0:1, A_V:A_V + 12288]
        w_mod_v = w_mod.rearrange("(p c) n -> p c n", c=16)
        P.dma("sp", lambda e: e.dma_start(out=modrow, in_=b_mod.rearrange("(o n) -> o n", o=1)), writes=[res("modrow")])
        for nb in range(24):
            wres = res(f"wm{nb % 2}")
            P.dma("sp" if nb % 2 == 0 else "act", lambda e, nb=nb: e.dma_start(out=wm[nb % 2], in_=w_mod_v[:, :, nb * 512:(nb + 1) * 512]),
                  writes=[wres])

            def mm(e, nb=nb):
                for c in range(16):
                    ins = e.matmul(psf[nb % 2][0:1, :], lhsT=c_act[:, c:c + 1], rhs=wm[nb % 2][:, c, :],
                                   start=(c == 0), stop=(c == 15))
                return ins
            P.op("pe", mm, reads=[wres, res("c_act")], writes=[bank[nb % 2]])
            P.op("dve", lambda e, nb=nb: e.tensor_tensor(
                out=modrow[:, nb * 512:(nb + 1) * 512], in0=psf[nb % 2][0:1, :],
                in1=modrow[:, nb * 512:(nb + 1) * 512], op=ALU.add),
                reads=[bank[nb % 2], res("modrow")], writes=[res("modrow")])
        P.dma("sp", lambda e: e.dma_start(out=mod_d.rearrange("(o n) -> o n", o=1), in_=modrow),
              reads=[res("modrow")], writes=[res("mod_d")])
        P.barrier()

        def bload(qn, dst, src_row, rname):
            return P.dma(qn, lambda e: e.dma_start(out=dst, in_=src_row.partition_broadcast(128)),
                         reads=[res("mod_d")], writes=[res(rname)])

        tmpb = A(A_SCR, 2048)
        bload("sp", brd[0], norm1_g, "brd0")
        bload("sp", tmpb, mod_d[D:2 * D], "tmpb")
        bload("sp", brd[1], mod_d[0:D], "brd1")
        P.op("dve", lambda e: e.scalar_tensor_tensor(out=brd[0], in0=tmpb, scalar=1.0, in1=brd[0],
                                                     op0=ALU.add, op1=ALU.mult),
             reads=[res("tmpb"), res("brd0")], writes=[res("brd0")])
        P.barrier()

        xs = [A(A_SCR + i * 2048, 2048) for i in range(2)]
        tmpf = A(A_SCR + 4096, 2048)
        hbf = [Ab(A_SCR + 6144 + i * 1024, 1024) for i in range(2)]
        t12 = [A(A_SCR + 8192 + i * 512, 512) for i in range(4)]
        ssq = small[:, 0:8]
        rstd = small[:, 8:16]

        def trig_tables(qi):
            ang = xs[0][:, 0:1024]
            kf = xs[0][:, 1024:2048]
            ki = kf.bitcast(I32)
            rr = xs[1][:, 0:1024]
            ab = xs[1][:, 1024:2048]
            rs_ = [res("xs0"), res("xs1")]
            P.op("dve", lambda e: e.tensor_scalar(out=ang, in0=iota_t, scalar1=pc_sb[:, PC_BASE + qi:PC_BASE + qi + 1],
                                                  scalar2=freq, op0=ALU.add, op1=ALU.mult),
                 reads=[res("cst"), res("pcst"), res("freq")], writes=[rs_[0]])
            P.op("dve", lambda e: e.tensor_scalar(out=rr.bitcast(I32), in0=ang, scalar1=INV_2PI, scalar2=None, op0=ALU.mult),
                 reads=[rs_[0]], writes=[rs_[1]])
            P.op("dve", lambda e: e.tensor_copy(out=kf, in_=rr.bitcast(I32)), reads=[rs_[1]], writes=[rs_[0]])
            P.op("dve", lambda e: e.scalar_tensor_tensor(out=rr, in0=kf, scalar=-TWO_PI_HI, in1=ang, op0=ALU.mult, op1=ALU.add),
                 reads=[rs_[0]], writes=[rs_[1]])
            P.op("dve", lambda e: e.scalar_tensor_tensor(out=rr, in0=kf, scalar=-TWO_PI_LO, in1=rr, op0=ALU.mult, op1=ALU.add),
                 reads=[rs_[0], rs_[1]], writes=[rs_[1]])
            P.op("dve", lambda e: e.tensor_scalar(out=rr, in0=rr, scalar1=PI_LO, scalar2=-PI_LO, op0=ALU.min, op1=ALU.max),
                 reads=[rs_[1]], writes=[rs_[1]])
            P.op("dve", lambda e: e.scalar_tensor_tensor(out=ab, in0=rr, scalar=-1.0, in1=rr, op0=ALU.mult, op1=ALU.max),
                 reads=[rs_[1]], writes=[rs_[1]])
            P.op("act", lambda e: e.activation(out=sin_t, in_=rr, func=AF.Sin, scale=col(C_SIGN)),
                 reads=[rs_[1], res("cst")], writes=[res("trig")])
            P.op("act", lambda e: e.activation(out=cos_t, in_=ab, func=AF.Sin, scale=-1.0, bias=col(C_HALFPI)),
                 reads=[rs_[1], res("cst")], writes=[res("trig")])

        def p1(qi):
            x_src = x_q[qi].rearrange("(i p) d -> i p d", p=128)
            for i in range(NT):
                xr = res(f"xs{i % 2}")
                hr = res(f"hbf{i % 2}")
                P.dma("sp", lambda e, i=i: e.dma_start(out=xs[i % 2], in_=x_src[i]), writes=[xr])
                P.op("act", lambda e, i=i: e.activation(out=hbf[i % 2], in_=xs[i % 2], func=AF.Square,
                                                       accum_out=ssq[:, i:i + 1]),
                     reads=[xr], writes=[hr, res(f"ssq{i}")])
                P.op("act", lambda e, i=i: e.activation(out=rstd[:, i:i + 1], in_=ssq[:, i:i + 1], func=AF.Sqrt,
                                                       scale=1.0 / D, bias=col(C_EPS)),
                     reads=[res(f"ssq{i}"), res("cst")], writes=[res(f"rstd{i}")])
                P.op("dve", lambda e, i=i: e.reciprocal(out=rstd[:, i:i + 1], in_=rstd[:, i:i + 1]),
                     reads=[res(f"rstd{i}")], writes=[res(f"rstd{i}")])
                P.op("dve", lambda e, i=i: e.scalar_tensor_tensor(out=tmpf, in0=xs[i % 2], scalar=rstd[:, i:i + 1],
                                                                   in1=brd[0], op0=ALU.mult, op1=ALU.mult),
                     reads=[xr, res(f"rstd{i}"), res("brd0")], writes=[res("tmpf")])
                P.op("pool", lambda e, i=i: e.tensor_tensor(out=hbf[i % 2], in0=tmpf, in1=brd[1], op=ALU.add),
                     reads=[res("tmpf"), res("brd1")], writes=[hr])
                pb = 2 * (i % 2)

                def tr(e, i=i, pb=pb):
                    for c in range(16):
                        ins = e.transpose(out=psb(pb + c // 8)[:, (c % 8) * 128:(c % 8 + 1) * 128],
                                          in_=hbf[i % 2][:, c * 128:(c + 1) * 128], identity=ident_bf)
                    return ins
                P.op("pe", tr, reads=[hr, res("ident_bf")], writes=[bank[pb], bank[pb + 1]])
                P.op("act", lambda e, i=i, pb=pb: e.activation(
                    out=hT[:, 0:8, i * 128:(i + 1) * 128], in_=psb(pb).rearrange("p (c t) -> p c t", c=8), func=AF.Copy),
                    reads=[bank[pb]], writes=[res("hT")])
                P.op("dve", lambda e, i=i, pb=pb: e.tensor_copy(
                    out=hT[:, 8:16, i * 128:(i + 1) * 128], in_=psb(pb + 1).rearrange("p (c t) -> p c t", c=8)),
                    reads=[bank[pb + 1]], writes=[res("hT")])

        def load_w(slot, cols0, ncols, swap=False):
            wr = res(f"wb{slot}")
            if not swap:
                dst = wb[slot][:, 0:16 * ncols].rearrange("p (c n) -> p c n", c=16)
                P.dma("pool", lambda e: e.dma_start(out=dst, in_=w_in_v[:, :, cols0:cols0 + ncols]), writes=[wr])
            else:
                dstv = wb[slot][:, 0:16 * 384].rearrange("p (c h n) -> p c h n", c=16, h=2)
                srcv = w_in_v[:, :, cols0:cols0 + 256].rearrange("p c (h d) -> p c h d", h=2)
                for hh in range(2):
                    P.dma("pool", lambda e, hh=hh: e.dma_start(out=dstv[:, :, hh, 64:192], in_=srcv[:, :, hh, :]), writes=[wr])
                    P.dma("pool", lambda e, hh=hh: e.dma_start(out=dstv[:, :, hh, 0:64], in_=srcv[:, :, hh, 64:128]), writes=[wr])
            return wr

        def rope_block(slot, h0, is_q, qdst=None, qtdst=None, decq=None):
            wr = res(f"wb{slot}")
            wv = wb[slot][:, 0:16 * 384].rearrange("p (c h n) -> p c h n", c=16, h=2)
            for hh in range(2):
                for th in range(2):
                    ba, bb = 4 + 2 * ((hh * 2 + th) % 2), 5 + 2 * ((hh * 2 + th) % 2)
                    ta, tb = t12[2 * ((hh * 2 + th) % 2)], t12[2 * ((hh * 2 + th) % 2) + 1]
                    tra, trb = res(f"t12_{2 * ((hh * 2 + th) % 2)}"), res(f"t12_{2 * ((hh * 2 + th) % 2) + 1}")
                    tok = slice(th * 512, (th + 1) * 512)

                    def mma(e, hh=hh, tok=tok, ba=ba):
                        for c in range(16):
                            ins = e.matmul(psf[ba][:, :], lhsT=wv[:, c, hh, 64:192], rhs=hT[:, c, tok],
                                           start=(c == 0), stop=(c == 15))
                        return ins

                    def mmb(e, hh=hh, tok=tok, bb=bb):
                        for c in range(16):
                            ins = e.matmul(psf[bb][:, :], lhsT=wv[:, c, hh, 0:128], rhs=hT[:, c, tok],
                                           start=(c == 0), stop=(c == 15))
                        return ins
                    P.op("pe", mma, reads=[wr, res("hT")], writes=[bank[ba]])
                    P.op("pe", mmb, reads=[wr, res("hT")], writes=[bank[bb]])
                    P.op("dve", lambda e, ta=ta, ba=ba, tok=tok: e.tensor_tensor(out=ta, in0=psf[ba][:, :], in1=cos_t[:, tok], op=ALU.mult),
                         reads=[bank[ba], res("trig")], writes=[tra])
                    P.op("dve", lambda e, tb=tb, bb=bb, tok=tok: e.tensor_tensor(out=tb, in0=psf[bb][:, :], in1=sin_t[:, tok], op=ALU.mult),
                         reads=[bank[bb], res("trig")], writes=[trb])
                    if not is_q:
                        P.op("pool", lambda e, ta=ta, tb=tb, hh=hh, tok=tok: e.tensor_tensor(
                            out=kT[:, h0 + hh, tok], in0=ta, in1=tb, op=ALU.add),
                            reads=[tra, trb], writes=[res("kT")])
                    else:
                        P.op("pool", lambda e, ta=ta, tb=tb: e.tensor_tensor(out=ta, in0=ta, in1=tb, op=ALU.add),
                             reads=[tra, trb], writes=[tra])
                        P.op("act", lambda e, ta=ta, hh=hh, tok=tok: e.activation(out=qdst[:, hh, tok], in_=ta, func=AF.Copy, scale=QSCALE),
                             reads=[tra], writes=[res("qT")])
                        P.op("dve", lambda e, ta=ta, hh=hh, tok=tok: e.tensor_tensor(out=qtdst[:, hh, tok], in0=ta, in1=decq[hh][:, tok], op=ALU.mult),
                             reads=[tra, res(f"decq{hh}")], writes=[res("qtT")])

        def v_block(slot, vb):
            wr = res(f"wb{slot}")
            wv = wb[slot][:, 0:16 * 256].rearrange("p (c n) -> p c n", c=16)
            for i in range(NT):
                b = 4 + (i % 4)

                def mm(e, i=i, b=b):
                    for c in range(16):
                        ins = e.matmul(psf[b][:, 0:256], lhsT=hT[:, c, i * 128:(i + 1) * 128], rhs=wv[:, c, :],
                                       start=(c == 0), stop=(c == 15))
                    return ins
                P.op("pe", mm, reads=[wr, res("hT")], writes=[bank[b]])
                P.op("act", lambda e, i=i, b=b: e.activation(out=v_sb[:, i, vb * 256:(vb + 1) * 256], in_=psf[b][:, 0:256], func=AF.Copy),
                     reads=[bank[b]], writes=[res("v")])

        kdec_sb = Ab(A_SCR + 4096, 512).rearrange("p (h d) -> p h d", h=NH)

        def state_phase(qi, main):
            for i in range(NT):
                def tr(e, i=i):
                    for h in range(NH):
                        ins = e.transpose(out=psb(0)[:, h * 128:(h + 1) * 128], in_=kT[:, h, i * 128:(i + 1) * 128],
                                          identity=ident_bf)
                    return ins
                P.op("pe", tr, reads=[res("kT"), res("ident_bf")], writes=[bank[0]])
                P.op("dve", lambda e, i=i: e.tensor_tensor(
                    out=kdec_sb, in0=psb(0).rearrange("p (h d) -> p h d", h=NH),
                    in1=kdec_tab[:, qi * 8 + i, :].unsqueeze(2).broadcast_to([128, NH, 128]), op=ALU.mult),
                    reads=[bank[0], res("kdec")], writes=[res("tmpf")])

                def inc(e, i=i):
                    for h in range(NH):
                        ins = e.matmul(psf[2 + h // 4][:, (h % 4) * 128:(h % 4 + 1) * 128], lhsT=kdec_sb[:, h, :],
                                       rhs=v_sb[:, i, h * 128:(h + 1) * 128], start=True, stop=True)
                    return ins
                P.op("pe", inc, reads=[res("tmpf"), res("v")], writes=[bank[2], bank[3]])
                if main:
                    P.op("act", lambda e, i=i: e.activation(out=S_bf[:, i, :, :], in_=S_f32, func=AF.Copy),
                         reads=[res("S")], writes=[res("S_bf")])
                for hb in range(2):
                    P.op("dve", lambda e, hb=hb: e.tensor_tensor(
                        out=S_f32[:, hb * 4:(hb + 1) * 4, :], in0=S_f32[:, hb * 4:(hb + 1) * 4, :],
                        in1=psf[2 + hb][:, :].rearrange("p (h e) -> p h e", h=4), op=ALU.add),
                        reads=[bank[2 + hb], res("S")], writes=[res("S")])

        P.op("pool", lambda e: e.memset(S_f32, 0.0), writes=[res("S")])

        for qi in range(4):
            main = qi == 3
            trig_tables(qi)
            if main:
                P.op("dve", lambda e: e.tensor_copy(out=hT_halo, in_=hT[:, :, 1022:1024]),
                     reads=[res("hT")], writes=[res("hT_halo")])
            p1(qi)
            blocks = [("k", g) for g in range(4)] + [("v", g) for g in range(4)]
            load_w(0, D_RET + 0, 256, swap=True)
            for bi, (kind, g) in enumerate(blocks):
                slot = bi % 2
                if bi + 1 < len(blocks):
                    nk, ng = blocks[bi + 1]
                    if nk == "k":
                        load_w((bi + 1) % 2, D_RET + ng * 256, 256, swap=True)
                    else:
                        load_w((bi + 1) % 2, 2 * D_RET + ng * 256, 256)
                if kind == "k":
                    rope_block(slot, 2 * g, False)
                else:
                    v_block(slot, g)
            state_phase(qi, main)
        P.barrier()

        qT = Ab(A_SCR + 0, 1024).rearrange("p (h t) -> p h t", h=2)
        qtT = Ab(A_SCR + 1024, 1024).rearrange("p (h t) -> p h t", h=2)
        sg = Ab(A_SCR + 2048, 1024).rearrange("p (i n) -> p i n", i=NT)
        decq = [A(A_SCR + 3072 + i * 1024, 1024) for i in range(2)]
        Pm = [Ab(A_SCR + 5120 + i * 128, 128).rearrange("p (h k) -> p h k", h=2) for i in range(2)]
        onb = [A(A_SCR + 5376 + i * 256, 256).rearrange("p (h k) -> p h k", h=2) for i in range(2)]
        yrb = [Ab(A_SCR + 5888 + i * 128, 128).rearrange("p (h k) -> p h k", h=2) for i in range(2)]
        bst = A(A_SCR + 6144, 32).rearrange("p (a h s) -> p a h s", a=2, h=2)
        bmv = A(A_SCR + 6176, 16).rearrange("p (a h s) -> p a h s", a=2, h=2)
        brs = A(A_SCR + 6192, 4).rearrange("p (a h) -> p a h", a=2)

        for g in range(4):
            for hh in range(2):
                P.op("act", lambda e, hh=hh, g=g: e.activation(out=decq[hh], in_=iota_t, func=AF.Exp,
                                                           scale=LOG_GAMMA[2 * g + hh], bias=col(C_LNQ)),
                     reads=[res("cst")], writes=[res(f"decq{hh}")])
            load_w(0, 2 * g * 128, 256, swap=True)
            load_w(1, 3 * D_RET + g * 256, 256)
            rope_block(0, 2 * g, True, qdst=qT, qtdst=qtT, decq=decq)
            wgv = wb[1][:, 0:16 * 256].rearrange("p (c n) -> p c n", c=16)
            for i in range(NT):
                b = i % 2

                def mmg(e, i=i, b=b):
                    for c in range(16):
                        ins = e.matmul(psf[b][:, 0:256], lhsT=hT[:, c, i * 128:(i + 1) * 128], rhs=wgv[:, c, :],
                                       start=(c == 0), stop=(c == 15))
                    return ins
                P.op("pe", mmg, reads=[res("wb1"), res("hT")], writes=[bank[b]])
                P.op("act", lambda e, i=i, b=b: e.activation(out=sg[:, i, :], in_=psf[b][:, 0:256], func=AF.Silu),
                     reads=[bank[b]], writes=[res("sg")])
            for i in range(NT):
                a = i % 2
                tok = slice(i * 128, (i + 1) * 128)
                rPm, ron, ryr, rst = res(f"Pm{a}"), res(f"on{a}"), res(f"yr{a}"), res(f"bst{a}")

                def sc(e, g=g, tok=tok, a=a):
                    for hh in range(2):
                        ins = e.matmul(psf[2 + a][:, hh * 128:(hh + 1) * 128], lhsT=kT[:, 2 * g + hh, tok], rhs=qT[:, hh, tok],
                                       start=True, stop=True)
                    return ins
                P.op("pe", sc, reads=[res("kT"), res("qT")], writes=[bank[2 + a]])
                P.op("dve", lambda e, g=g, a=a: e.tensor_tensor(
                    out=Pm[a], in0=psf[2 + a][:, 0:256].rearrange("p (h k) -> p h k", h=2),
                    in1=maskT[:, 2 * g:2 * g + 2, :], op=ALU.mult),
                    reads=[bank[2 + a], res("maskT")], writes=[rPm])

                def om(e, g=g, i=i, tok=tok, a=a):
                    for hh in range(2):
                        h = 2 * g + hh
                        e.matmul(psf[4 + a][:, hh * 128:(hh + 1) * 128], lhsT=Pm[a][:, hh, :],
                                 rhs=v_sb[:, i, h * 128:(h + 1) * 128], start=True, stop=False)
                        ins = e.matmul(psf[4 + a][:, hh * 128:(hh + 1) * 128], lhsT=qtT[:, hh, tok],
                                       rhs=S_bf[:, i, h, :], start=False, stop=True)
                    return ins
                P.op("pe", om, reads=[rPm, res("v"), res("qtT"), res("S_bf")], writes=[bank[4 + a]])
                for hh in range(2):
                    P.op("dve", lambda e, hh=hh, a=a: e.bn_stats(out=bst[:, a, hh, 0:6], in_=psf[4 + a][:, hh * 128:(hh + 1) * 128]),
                         reads=[bank[4 + a]], writes=[rst])
                for hh in range(2):
                    P.op("dve", lambda e, hh=hh, a=a: e.bn_aggr(out=bmv[:, a, hh, 0:2], in_=bst[:, a, hh, 0:6]),
                         reads=[rst], writes=[rst])
                P.op("act", lambda e, a=a: e.activation(out=brs[:, a, :], in_=bmv[:, a, :, 1], func=AF.Sqrt,
                                                        bias=col(C_EPS)),
                     reads=[rst, res("cst")], writes=[res(f"brs{a}")])
                P.op("dve", lambda e, a=a: e.reciprocal(out=brs[:, a, :], in_=brs[:, a, :]),
                     reads=[res(f"brs{a}")], writes=[res(f"brs{a}")])
                for hh in range(2):
                    P.op("dve", lambda e, hh=hh, a=a: e.tensor_scalar(
                        out=onb[a][:, hh, :], in0=psf[4 + a][:, hh * 128:(hh + 1) * 128],
                        scalar1=bmv[:, a, hh, 0:1], scalar2=brs[:, a, hh:hh + 1], op0=ALU.subtract, op1=ALU.mult),
                        reads=[bank[4 + a], rst, res(f"brs{a}")], writes=[ron])
                P.op("pool", lambda e, i=i, a=a: e.tensor_tensor(
                    out=yrb[a], in0=onb[a], in1=sg[:, i, :].rearrange("p (h k) -> p h k", h=2), op=ALU.mult),
                    reads=[ron, res("sg")], writes=[ryr])

                def trY(e, a=a):
                    for hh in range(2):
                        ins = e.transpose(out=psb(6 + a)[:, hh * 128:(hh + 1) * 128], in_=yrb[a][:, hh, :], identity=ident_bf)
                    return ins
                P.op("pe", trY, reads=[ryr, res("ident_bf")], writes=[bank[6 + a]])
                P.op("act", lambda e, g=g, tok=tok, a=a: e.activation(
                    out=yTr[:, 2 * g:2 * g + 2, tok], in_=psb(6 + a)[:, 0:256].rearrange("p (h k) -> p h k", h=2), func=AF.Copy),
                    reads=[bank[6 + a]], writes=[res("yTr")])
        P.barrier()

        uT = A(A_SCR + 0, 2056)[:, 0:2052].rearrange("p (c t) -> p c t", c=2)
        acc = A(A_SCR + 2056, 2048).rearrange("p (c t) -> p c t", c=2)
        BASE_B, BASE_C, BASE_U = 4 * D_RET, 4 * D_RET + D_CONV, 4 * D_RET + 2 * D_CONV

        def conv_mm(slot, cc, with_halo):
            wv = wb[slot][:, 0:16 * 256].rearrange("p (c n) -> p c n", c=16)
            for th in range(2):
                b = cc * 2 + th

                def mm(e, b=b, th=th):
                    for c in range(16):
                        ins = e.matmul(psf[b][:, :], lhsT=wv[:, c, cc * 128:(cc + 1) * 128], rhs=hT[:, c, th * 512:(th + 1) * 512],
                                       start=(c == 0), stop=(c == 15))
                    return ins
                P.op("pe", mm, reads=[res(f"wb{slot}"), res("hT")], writes=[bank[b]])
            if with_halo:
                def mmh(e):
                    for c in range(16):
                        ins = e.matmul(psf[4 + cc][:, 0:2], lhsT=wv[:, c, cc * 128:(cc + 1) * 128], rhs=hT_halo[:, c, :],
                                       start=(c == 0), stop=(c == 15))
                    return ins
                P.op("pe", mmh, reads=[res(f"wb{slot}"), res("hT_halo")], writes=[bank[4 + cc]])

        for cg in range(4):
            load_w(0, BASE_U + cg * 256, 256)
            load_w(1, BASE_C + cg * 256, 256)
            for cc in range(2):
                conv_mm(0, cc, True)
                for th in range(2):
                    P.op("act", lambda e, cc=cc, th=th: e.activation(
                        out=uT[:, cc, 2 + th * 512:2 + (th + 1) * 512], in_=psf[cc * 2 + th][:, :], func=AF.Copy),
                        reads=[bank[cc * 2 + th]], writes=[res("uT")])
                P.op("act", lambda e, cc=cc: e.activation(out=uT[:, cc, 0:2], in_=psf[4 + cc][:, 0:2], func=AF.Copy),
                     reads=[bank[4 + cc]], writes=[res("uT")])
            for cc in range(2):
                conv_mm(1, cc, True)
                for th in range(2):
                    P.op("dve", lambda e, cc=cc, th=th: e.tensor_tensor(
                        out=uT[:, cc, 2 + th * 512:2 + (th + 1) * 512], in0=psf[cc * 2 + th][:, :],
                        in1=uT[:, cc, 2 + th * 512:2 + (th + 1) * 512], op=ALU.mult),
                        reads=[bank[cc * 2 + th], res("uT")], writes=[res("uT")])
                P.op("dve", lambda e, cc=cc: e.scalar_tensor_tensor(
                    out=uT[:, cc, 0:2], in0=psf[4 + cc][:, 0:2], scalar=pc_sb[:, PC_FLAG:PC_FLAG + 1], in1=uT[:, cc, 0:2],
                    op0=ALU.mult, op1=ALU.mult),
                    reads=[bank[4 + cc], res("uT"), res("pcst")], writes=[res("uT")])
            load_w(0, BASE_B + cg * 256, 256)
            for cc in range(2):
                ch = cg * 2 + cc
                P.op("pool", lambda e, cc=cc, ch=ch: e.tensor_scalar(
                    out=acc[:, cc, :], in0=uT[:, cc, 2:1026], scalar1=cw_sb[:, 2, ch:ch + 1], scalar2=None, op0=ALU.mult),
                    reads=[res("uT"), res("cw")], writes=[res("acc")])
                P.op("dve", lambda e, cc=cc, ch=ch: e.scalar_tensor_tensor(
                    out=acc[:, cc, :], in0=uT[:, cc, 1:1025], scalar=cw_sb[:, 1, ch:ch + 1], in1=acc[:, cc, :],
                    op0=ALU.mult, op1=ALU.add),
                    reads=[res("uT"), res("cw"), res("acc")], writes=[res("acc")])
                P.op("dve", lambda e, cc=cc, ch=ch: e.scalar_tensor_tensor(
                    out=acc[:, cc, :], in0=uT[:, cc, 0:1024], scalar=cw_sb[:, 0, ch:ch + 1], in1=acc[:, cc, :],
                    op0=ALU.mult, op1=ALU.add),
                    reads=[res("uT"), res("cw"), res("acc")], writes=[res("acc")])
            for cc in range(2):
                conv_mm(0, cc, False)
                for th in range(2):
                    P.op("dve", lambda e, cc=cc, th=th, cg=cg: e.tensor_tensor(
                        out=yTc[:, cg * 2 + cc, th * 512:(th + 1) * 512], in0=psf[cc * 2 + th][:, :],
                        in1=acc[:, cc, th * 512:(th + 1) * 512], op=ALU.mult),
                        reads=[bank[cc * 2 + th], res("acc")], writes=[res("yTc")])
        P.barrier()

        x1 = A(A_V, 16384).rearrange("p (i d) -> p i d", i=NT)
        wo = [Ab(A_HT + i * 4096, 4096).rearrange("p (c n) -> p c n", c=16) for i in range(2)]
        otmp = [A(A_SCR + 8192 + i * 512, 512) for i in range(2)]
        P.dma("sp", lambda e: e.dma_start(out=x1, in_=x_q[3].rearrange("(i p) d -> p i d", p=128)), writes=[res("x1")])
        bload("act", brd[0], mod_d[2 * D:3 * D], "brd0")

        def load_wo(nb):
            P.dma("pool", lambda e: e.dma_start(out=wo[nb % 2], in_=w_out_v[:, :, nb * 512:(nb + 1) * 512]),
                  writes=[res(f"wo{nb % 2}")])
        load_wo(0)
        for nb in range(4):
            if nb + 1 < 4:
                load_wo(nb + 1)
            for i in range(NT):
                b = i % 4

                def mm(e, nb=nb, i=i, b=b):
                    for c in range(16):
                        src = yTr[:, c, i * 128:(i + 1) * 128] if c < 8 else yTc[:, c - 8, i * 128:(i + 1) * 128]
                        ins = e.matmul(psf[b][:, :], lhsT=src, rhs=wo[nb % 2][:, c, :], start=(c == 0), stop=(c == 15))
                    return ins
                P.op("pe", mm, reads=[res(f"wo{nb % 2}"), res("yTr"), res("yTc")], writes=[bank[b]])
                P.op("dve", lambda e, nb=nb, i=i, b=b: e.tensor_tensor(
                    out=otmp[i % 2], in0=psf[b][:, :], in1=brd[0][:, nb * 512:(nb + 1) * 512], op=ALU.mult),
                    reads=[bank[b], res("brd0")], writes=[res(f"otmp{i % 2}")])
                P.op("pool", lambda e, nb=nb, i=i: e.tensor_tensor(
                    out=x1[:, i, nb * 512:(nb + 1) * 512], in0=x1[:, i, nb * 512:(nb + 1) * 512], in1=otmp[i % 2], op=ALU.add),
                    reads=[res(f"otmp{i % 2}"), res("x1")], writes=[res("x1")])
        P.barrier()

        if stage == "x1":
            P.dma("sp", lambda e: e.dma_start(out=out.rearrange("(i p) d -> p i d", p=128), in_=x1), reads=[res("x1")])
            P.barrier()
        if stage != "x1":
            tmp2 = A(A_HT, 2048)
            hb2 = [Ab(A_HT + 2048 + i * 1024, 1024) for i in range(2)]
            h2Tf = A(A_HT + 4096, 2048).rearrange("p (c t) -> p c t", c=16)
            wr_sb = A(A_HT + 6144, 512).rearrange("p (c e) -> p c e", c=16)
            brow = arena[0:1, A_HT + 6656:A_HT + 6688]
            so = A_HT + 6688
            lg = A(so, 32); so += 32
            rk = A(so, 32); so += 32
            oh = A(so, 32); so += 32
            ecap = A(so, 32); so += 32
            mkf = A(so, 32); so += 32
            mx8 = A(so, 8); so += 8
            mi8 = A(so, 8).bitcast(U32); so += 8
            idf8 = A(so, 8); so += 8
            ex4 = A(so, 4); so += 4
            destf = A(so, 4); so += 4
            nmx = A(so, 1); so += 1
            ssum = A(so, 1); so += 1
            so += 2
            tri_bf = Ab(so, 64); so += 64
            ones_bf = Ab(so, 64); so += 64
            mk_bf = Ab(so, 128).rearrange("p (i e) -> p i e", i=NT); so += 128
            assert so <= A_HT + 8192
            w4_all = A(A_YTR + 4096, 32).rearrange("p (i k) -> p i k", i=NT)
            dest_all = A(A_YTR + 4128, 32).bitcast(I32).rearrange("p (i k) -> p i k", i=NT)
            ssq2 = A(A_YTR + 4352, 8)
            rstd2 = A(A_YTR + 4360, 8)
            ones_row = cst_sb[0:1, C_ONES:C_ONES + 128]
            iota_e = cst_sb[:, C_IOTA_E:C_IOTA_E + 32]

            tmpc = A(A_TRIG, 2048)
            bload("sp", brd[0], norm2_g, "brd0")
            bload("sp", tmpc, mod_d[4 * D:5 * D], "tmpc")
            bload("act", brd[1], mod_d[3 * D:4 * D], "brd1")
            P.op("dve", lambda e: e.scalar_tensor_tensor(out=brd[0], in0=tmpc, scalar=1.0, in1=brd[0],
                                                         op0=ALU.add, op1=ALU.mult),
                 reads=[res("tmpc"), res("brd0")], writes=[res("brd0")])
            P.dma("sp", lambda e: e.dma_start(out=wr_sb, in_=w_router.rearrange("(c p) e -> p c e", p=128)),
                  writes=[res("wr_sb")])
            P.dma("sp", lambda e: e.dma_start(out=brow, in_=b_router.rearrange("(o n) -> o n", o=1)), writes=[res("brow")])
            P.op("dve", lambda e: e.tensor_copy(out=tri_bf, in_=cst_sb[:, C_TRI:C_TRI + 128]), reads=[res("cst")], writes=[res("tri")])
            P.op("dve", lambda e: e.tensor_copy(out=ones_bf, in_=cst_sb[:, C_ONES:C_ONES + 128]), reads=[res("cst")], writes=[res("tri")])
            P.op("dve", lambda e: e.tensor_scalar(out=ecap, in0=iota_e, scalar1=float(CAP), scalar2=None, op0=ALU.mult),
                 reads=[res("cst")], writes=[res("ecap")])

            for i in range(NT):
                hb = hb2[i % 2]
                rhb = res(f"hb2_{i % 2}")
                P.op("act", lambda e, i=i, hb=hb: e.activation(out=hb, in_=x1[:, i, :], func=AF.Square, accum_out=ssq2[:, i:i + 1]),
                     reads=[res("x1")], writes=[rhb, res("rs2")])
                P.op("act", lambda e, i=i: e.activation(out=rstd2[:, i:i + 1], in_=ssq2[:, i:i + 1], func=AF.Sqrt,
                                                       scale=1.0 / D, bias=col(C_EPS)),
                     reads=[res("rs2"), res("cst")], writes=[res("rs2")])
                P.op("dve", lambda e, i=i: e.reciprocal(out=rstd2[:, i:i + 1], in_=rstd2[:, i:i + 1]),
                     reads=[res("rs2")], writes=[res("rs2")])
                P.op("dve", lambda e, i=i: e.scalar_tensor_tensor(out=tmp2, in0=x1[:, i, :], scalar=rstd2[:, i:i + 1], in1=brd[0],
                                                                   op0=ALU.mult, op1=ALU.mult),
                     reads=[res("x1"), res("rs2"), res("brd0")], writes=[res("tmp2")])
                P.op("dve", lambda e: e.tensor_tensor(out=tmp2, in0=tmp2, in1=brd[1], op=ALU.add),
                     reads=[res("tmp2"), res("brd1")], writes=[res("tmp2")])
                P.op("act", lambda e, hb=hb: e.activation(out=hb, in_=tmp2, func=AF.Copy), reads=[res("tmp2")], writes=[rhb])

                def trf(e):
                    for c in range(16):
                        ins = e.transpose(out=psf[2 + c // 4][:, (c % 4) * 128:(c % 4 + 1) * 128],
                                          in_=tmp2[:, c * 128:(c + 1) * 128], identity=ident_f)
                    return ins
                P.op("pe", trf, reads=[res("tmp2"), res("cst")], writes=[bank[2], bank[3], bank[4], bank[5]])
                for k in range(4):
                    if k % 2 == 0:
                        P.op("act", lambda e, k=k: e.activation(out=h2Tf[:, 4 * k:4 * k + 4, :],
                                                                in_=psf[2 + k][:, :].rearrange("p (c t) -> p c t", c=4), func=AF.Copy),
                             reads=[bank[2 + k]], writes=[res("h2Tf")])
                    else:
                        P.op("dve", lambda e, k=k: e.tensor_copy(out=h2Tf[:, 4 * k:4 * k + 4, :],
                                                                 in_=psf[2 + k][:, :].rearrange("p (c t) -> p c t", c=4)),
                             reads=[bank[2 + k]], writes=[res("h2Tf")])

                def lgm(e):
                    for c in range(16):
                        e.matmul(psf[6][:, 0:32], lhsT=h2Tf[:, c, :], rhs=wr_sb[:, c, :], start=(c == 0), stop=False)
                    return e.matmul(psf[6][:, 0:32], lhsT=ones_row, rhs=brow, start=False, stop=True)
                P.op("pe", lgm, reads=[res("h2Tf"), res("wr_sb"), res("brow"), res("cst")], writes=[bank[6]])
                rt = res("rt")
                P.op("dve", lambda e: e.tensor_copy(out=lg, in_=psf[6][:, 0:32]), reads=[bank[6]], writes=[rt])
                P.op("dve", lambda e: e.max(out=mx8, in_=lg), reads=[rt], writes=[rt])
                P.op("dve", lambda e: e.max_index(out=mi8, in_max=mx8, in_values=lg), reads=[rt], writes=[rt])
                P.op("dve", lambda e: e.tensor_copy(out=idf8, in_=mi8), reads=[rt], writes=[rt])
                P.op("dve", lambda e: e.tensor_scalar(out=mkf, in0=lg, scalar1=mx8[:, 3:4], scalar2=None, op0=ALU.is_ge),
                     reads=[rt], writes=[rt])
                P.op("dve", lambda e, i=i: e.tensor_copy(out=mk_bf[:, i, :], in_=mkf), reads=[rt], writes=[res("mk_bf")])
                P.op("dve", lambda e: e.tensor_scalar(out=nmx, in0=mx8[:, 0:1], scalar1=-1.0, scalar2=None, op0=ALU.mult),
                     reads=[rt], writes=[rt])
                P.op("act", lambda e: e.activation(out=ex4, in_=mx8[:, 0:4], func=AF.Exp, bias=nmx), reads=[rt], writes=[rt])
                P.op("dve", lambda e: e.reduce_sum(out=ssum, in_=ex4, axis=mybir.AxisListType.X), reads=[rt], writes=[rt])
                P.op("dve", lambda e: e.reciprocal(out=ssum, in_=ssum), reads=[rt], writes=[rt])
                P.op("dve", lambda e, i=i: e.tensor_scalar(out=w4_all[:, i, :], in0=ex4, scalar1=ssum, scalar2=None, op0=ALU.mult),
                     reads=[rt], writes=[res("w4")])

                def rkm(e, i=i):
                    for ip in range(i):
                        e.matmul(psf[7][:, 0:32], lhsT=ones_bf, rhs=mk_bf[:, ip, :], start=(ip == 0), stop=False)
                    return e.matmul(psf[7][:, 0:32], lhsT=tri_bf, rhs=mk_bf[:, i, :], start=(i == 0), stop=True)
                P.op("pe", rkm, reads=[res("mk_bf"), res("tri")], writes=[bank[7]])
                P.op("dve", lambda e: e.tensor_tensor(out=rk, in0=psf[7][:, 0:32], in1=ecap, op=ALU.add),
                     reads=[bank[7], res("ecap")], writes=[rt])
                for k in range(TOPK):
                    P.op("dve", lambda e, k=k: e.tensor_scalar(out=oh, in0=iota_e, scalar1=idf8[:, k:k + 1], scalar2=None, op0=ALU.is_equal),
                         reads=[rt, res("cst")], writes=[rt])
                    P.op("dve", lambda e: e.tensor_tensor(out=oh, in0=oh, in1=rk, op=ALU.mult), reads=[rt], writes=[rt])
                    P.op("dve", lambda e, k=k: e.reduce_sum(out=destf[:, k:k + 1], in_=oh, axis=mybir.AxisListType.X),
                         reads=[rt], writes=[rt])
                P.op("dve", lambda e, i=i: e.tensor_copy(out=dest_all[:, i, :], in_=destf), reads=[rt], writes=[res("dest")])
                for k in range(TOPK):
                    P.dma("pool", lambda e, i=i, k=k, hb=hb: e.indirect_dma_start(
                        out=xe_d, out_offset=bass.IndirectOffsetOnAxis(ap=dest_all[:, i, k:k + 1], axis=0),
                        in_=hb, in_offset=None),
                        reads=[rhb, res("dest")], writes=[res("xe_d")])
            P.barrier()

            bgu_all = A(A_SF32, 1024).rearrange("p (c e) -> p c e", c=32)
            stage_b = arena[0:32, A_YTR:A_YTR + 4096]
            P.dma("sp", lambda e: e.dma_start(out=stage_b, in_=b_gate_up), writes=[res("stage_b")])
            for half in range(2):
                def trbias(e, half=half):
                    for cc in range(16):
                        ch = half * 16 + cc
                        ins = e.transpose(out=psf[half][:, cc * 32:(cc + 1) * 32], in_=stage_b[:, ch * 128:(ch + 1) * 128],
                                          identity=ident_f[0:32, 0:32])
                    return ins
                P.op("pe", trbias, reads=[res("stage_b"), res("cst")], writes=[bank[half]])
                P.op("dve", lambda e, half=half: e.tensor_copy(out=bgu_all[:, half * 16:(half + 1) * 16, :],
                                                               in_=psf[half][:, :].rearrange("p (c e) -> p c e", c=16)),
                     reads=[bank[half]], writes=[res("bgu")])
            P.barrier()

            NM = CAP // 128
            P.dma("sp", lambda e: e.dma_start(out=x1_d, in_=x1), reads=[res("x1")], writes=[res("x1_d")])
            P.barrier()
            xeT = [Ab(A_HT + i * 4096, 4096).rearrange("p (c s) -> p c s", c=16) for i in range(2)]
            actT = Ab(A_WB, 4096).rearrange("p (c s) -> p c s", c=16)
            xe_sb = [Ab(A_WB + 4096 + i * 1024, 1024) for i in range(4)]
            bd_bs = [A(A_WB + 8192, 2048), A(A_TRIG, 2048)]
            yst = [A(A_YTR + i * 1024, 1024).rearrange("p (m n) -> p m n", m=NM) for i in range(2)]
            g1 = A(A_SCR + 8192, 512)
            u1 = A(A_SCR + 8704, 512)
            sgm = A(A_SCR + 9216, 512)
            wgu = [Ab(A_V + i * 4096, 4096).rearrange("p (c n) -> p c n", c=16) for i in range(3)]
            wdn = [Ab(A_V + 12288, 4096).rearrange("p (c n) -> p c n", c=16), Ab(A_BRD, 4096).rearrange("p (c n) -> p c n", c=16)]
            NGS, NDS = len(wgu), len(wdn)

            def load_gu(e_, cq, slot):
                wv = w_gate_up[e_].rearrange("(c p) n -> p c n", p=128)
                P.dma("pool", lambda e: e.dma_start(out=wgu[slot][:, :, 0:256], in_=wv[:, :, cq * 256:(cq + 1) * 256]),
                      writes=[res(f"wgu{slot}")])
                P.dma("pool", lambda e: e.dma_start(out=wgu[slot][:, :, 256:512], in_=wv[:, :, DFF + cq * 256:DFF + (cq + 1) * 256]),
                      writes=[res(f"wgu{slot}")])

            def load_dn(e_, nq, slot):
                wv = w_down[e_].rearrange("(c p) n -> p c n", p=128)
                P.dma("pool", lambda e: e.dma_start(out=wdn[slot], in_=wv[:, :, nq * 512:(nq + 1) * 512]),
                      writes=[res(f"wdn{slot}")])

            xl = 0

            def load_xe(e_, m):
                nonlocal xl
                b_ = xl % 4
                xl += 1
                P.dma("sp", lambda e: e.dma_start(out=xe_sb[b_], in_=xe_d[e_ * CAP + m * 128:e_ * CAP + (m + 1) * 128, :]),
                      reads=[res("xe_d")], writes=[res(f"xe_sb{b_}")])
                return b_

            gu_jobs = [(e_, cq) for e_ in range(NEW) for cq in range(8)]
            dn_jobs = [(e_, nq) for e_ in range(NEW) for nq in range(4)]
            gl = 0
            dl = 0

            def pump_gu(upto):
                nonlocal gl
                while gl < len(gu_jobs) and gl < upto:
                    load_gu(gu_jobs[gl][0], gu_jobs[gl][1], gl % NGS)
                    gl += 1

            def pump_dn(upto):
                nonlocal dl
                while dl < len(dn_jobs) and dl < upto:
                    load_dn(dn_jobs[dl][0], dn_jobs[dl][1], dl % NDS)
                    dl += 1

            pump_gu(2)
            gi = 0
            di = 0
            tj = 0
            def transposes(e_):
                s_ = e_ % 2
                nonlocal tj
                for m in range(NM):
                    b_ = load_xe(e_, m)
                    pa = 2 * (tj % 2)
                    tj += 1

                    def trx(e, b_=b_, pa=pa):
                        for c in range(16):
                            ins = e.transpose(out=psb(pa + c // 8)[:, (c % 8) * 128:(c % 8 + 1) * 128],
                                              in_=xe_sb[b_][:, c * 128:(c + 1) * 128], identity=ident_bf)
                        return ins
                    P.op("pe", trx, reads=[res(f"xe_sb{b_}"), res("ident_bf")], writes=[bank[pa], bank[pa + 1]])
                    P.op("act", lambda e, m=m, s_=s_, pa=pa: e.activation(
                        out=xeT[s_][:, 0:8, m * 128:(m + 1) * 128], in_=psb(pa).rearrange("p (c t) -> p c t", c=8), func=AF.Copy),
                        reads=[bank[pa]], writes=[res(f"xeT{s_}")])
                    P.op("dve", lambda e, m=m, s_=s_, pa=pa: e.tensor_copy(
                        out=xeT[s_][:, 8:16, m * 128:(m + 1) * 128], in_=psb(pa + 1).rearrange("p (c t) -> p c t", c=8)),
                        reads=[bank[pa + 1]], writes=[res(f"xeT{s_}")])

            transposes(0)
            for e_ in range(NEW):
                s_ = e_ % 2
                bd_b = bd_bs[e_ % 2]
                rbd = res(f"bd_b{e_ % 2}")
                P.dma("sp", lambda e, e_=e_, bd_b=bd_b: e.dma_start(out=bd_b, in_=b_down[e_].partition_broadcast(128)), writes=[rbd])
                for cp in range(16):
                    slot = gi % NGS
                    hf = cp % 2
                    if hf == 0:
                        pump_gu(gi + NGS)
                    if cp in (0, 8):
                        pump_dn(di + NDS if cp == 8 else di + 1)
                    bg, bu = 4 + 2 * (cp % 2), 5 + 2 * (cp % 2)

                    def mg(e, slot=slot, s_=s_, bg=bg, hf=hf):
                        for c in range(16):
                            ins = e.matmul(psf[bg][:, 0:CAP], lhsT=wgu[slot][:, c, hf * 128:(hf + 1) * 128], rhs=xeT[s_][:, c, :],
                                           start=(c == 0), stop=(c == 15))
                        return ins

                    def mu(e, slot=slot, s_=s_, bu=bu, hf=hf):
                        for c in range(16):
                            ins = e.matmul(psf[bu][:, 0:CAP], lhsT=wgu[slot][:, c, 256 + hf * 128:256 + (hf + 1) * 128], rhs=xeT[s_][:, c, :],
                                           start=(c == 0), stop=(c == 15))
                        return ins
                    P.op("pe", mg, reads=[res(f"wgu{slot}"), res(f"xeT{s_}")], writes=[bank[bg]])
                    P.op("pe", mu, reads=[res(f"wgu{slot}"), res(f"xeT{s_}")], writes=[bank[bu]])
                    P.op("dve", lambda e, bg=bg, cp=cp, e_=e_: e.tensor_scalar(
                        out=g1, in0=psf[bg][:, 0:CAP], scalar1=bgu_all[:, cp, e_:e_ + 1], scalar2=7.0, op0=ALU.add, op1=ALU.min),
                        reads=[bank[bg], res("bgu")], writes=[res("g1")])
                    P.op("act", lambda e: e.activation(out=sgm, in_=g1, func=AF.Sigmoid, scale=1.702),
                         reads=[res("g1")], writes=[res("sgm")])
                    P.op("act", lambda e, bu=bu, cp=cp, e_=e_: e.activation(
                        out=u1, in_=psf[bu][:, 0:CAP], func=AF.Identity, bias=bgu_all[:, 16 + cp, e_:e_ + 1]),
                        reads=[bank[bu], res("bgu")], writes=[res("u1")])
                    P.op("dve", lambda e: e.tensor_scalar(out=u1, in0=u1, scalar1=-7.0, scalar2=7.0, op0=ALU.max, op1=ALU.min),
                         reads=[res("u1")], writes=[res("u1")])
                    P.op("dve", lambda e: e.tensor_tensor(out=g1, in0=g1, in1=sgm, op=ALU.mult),
                         reads=[res("g1"), res("sgm")], writes=[res("g1")])
                    P.op("dve", lambda e, cp=cp: e.scalar_tensor_tensor(
                        out=actT[:, cp, :], in0=u1, scalar=1.0, in1=g1, op0=ALU.add, op1=ALU.mult),
                        reads=[res("u1"), res("g1")], writes=[res("actT")])
                    if hf == 1:
                        gi += 1
                if e_ + 1 < NEW:
                    transposes(e_ + 1)
                for nb in range(8):
                    slot = di % NDS
                    sb = nb % 2
                    if sb == 0:
                        pump_dn(di + NDS)
                    cols = slice(nb * 256, (nb + 1) * 256)
                    ys = yst[nb % 2]
                    rys = res(f"yst{nb % 2}")
                    for m in range(NM):
                        b0 = (nb * NM + m) % 4

                        def md(e, slot=slot, m=m, b0=b0, sb=sb):
                            for c in range(16):
                                ins = e.matmul(psf[b0][:, 0:256], lhsT=actT[:, c, m * 128:(m + 1) * 128], rhs=wdn[slot][:, c, sb * 256:(sb + 1) * 256],
                                               start=(c == 0), stop=(c == 15))
                            return ins
                        P.op("pe", md, reads=[res(f"wdn{slot}"), res("actT")], writes=[bank[b0]])
                        P.op("dve", lambda e, b0=b0, cols=cols, m=m, ys=ys, bd_b=bd_b: e.tensor_tensor(
                            out=ys[:, m, :], in0=psf[b0][:, 0:256], in1=bd_b[:, cols], op=ALU.add),
                            reads=[bank[b0], rbd], writes=[rys])
                    P.dma("sp", lambda e, e_=e_, cols=cols, ys=ys: e.dma_start(
                        out=ye_d[e_ * CAP:(e_ + 1) * CAP, cols].rearrange("(m p) n -> p m n", p=128), in_=ys),
                        reads=[rys], writes=[res("ye_d")])
                    if sb == 1:
                        di += 1
            P.barrier()

            P.dma("sp", lambda e: e.dma_start(out=x1, in_=x1_d), reads=[res("x1_d")], writes=[res("x1")])
            bload("sp", brd[0], mod_d[5 * D:6 * D], "brd0")
            yg = [A(A_HT + i * 2048, 2048) for i in range(2)]
            ctm = A(A_HT + 4096, 2048)
            for i in range(NT):
                for k in range(TOPK):
                    j = i * TOPK + k
                    P.dma("pool", lambda e, i=i, k=k, j=j: e.indirect_dma_start(
                        out=yg[j % 2], out_offset=None, in_=ye_d,
                        in_offset=bass.IndirectOffsetOnAxis(ap=dest_all[:, i, k:k + 1], axis=0)),
                        reads=[res("ye_d"), res("dest")], writes=[res(f"yg{j % 2}")])
                    P.op("dve", lambda e, j=j: e.tensor_tensor(out=ctm, in0=yg[j % 2], in1=brd[0], op=ALU.mult),
                         reads=[res(f"yg{j % 2}"), res("brd0")], writes=[res("ctm")])
                    P.op("dve", lambda e, i=i, k=k: e.scalar_tensor_tensor(
                        out=x1[:, i, :], in0=ctm, scalar=w4_all[:, i, k:k + 1], in1=x1[:, i, :], op0=ALU.mult, op1=ALU.add),
                        reads=[res("ctm"), res("w4"), res("x1")], writes=[res("x1")])
            P.barrier()

            fg_b = A(A_TRIG, 2048)
            ot = [A(A_WB + i * 2048, 2048) for i in range(2)]
            junk = Ab(A_WB + 4096, 1024)
            bload("sp", fg_b, final_g, "fg_b")
            out_v = out.rearrange("(i p) d -> i p d", p=128)
            for i in range(NT):
                P.op("act", lambda e, i=i: e.activation(out=junk, in_=x1[:, i, :], func=AF.Square, accum_out=ssq2[:, i:i + 1]),
                     reads=[res("x1")], writes=[res("junk"), res("rs3")])
                P.op("act", lambda e, i=i: e.activation(out=rstd2[:, i:i + 1], in_=ssq2[:, i:i + 1], func=AF.Sqrt,
                                                       scale=1.0 / D, bias=col(C_EPS)),
                     reads=[res("rs3"), res("cst")], writes=[res("rs3")])
                P.op("dve", lambda e, i=i: e.reciprocal(out=rstd2[:, i:i + 1], in_=rstd2[:, i:i + 1]),
                     reads=[res("rs3")], writes=[res("rs3")])
                P.op("dve", lambda e, i=i: e.scalar_tensor_tensor(out=ot[i % 2], in0=x1[:, i, :], scalar=rstd2[:, i:i + 1], in1=fg_b,
                                                                   op0=ALU.mult, op1=ALU.mult),
                     reads=[res("x1"), res("rs3"), res("fg_b")], writes=[res(f"ot{i % 2}")])
                P.dma("sp", lambda e, i=i: e.dma_start(out=out_v[i], in_=ot[i % 2]), reads=[res(f"ot{i % 2}")])
            P.barrier()


        with nc.Block() as block:
            @block.sync
            def _(e):
                P.emit("sp", e)

            @block.scalar
            def _(e):
                P.emit("act", e)

            @block.vector
            def _(e):
                P.emit("dve", e)

            @block.gpsimd
            def _(e):
                P.emit("pool", e)

            @block.tensor
            def _(e):
                P.emit("pe", e)
    return nc


def _consts():
    c = np.zeros((128, C_N), np.float32)
    p = np.arange(128)
    c[:, C_IOTA_T:C_IOTA_T + 1024] = np.arange(1024)[None, :]
    c[:, C_IDENT:C_IDENT + 128] = np.eye(128)
    kt = p[:, None]
    qt = p[None, :]
    c[:, C_DIST:C_DIST + 128] = np.abs(kt - qt)
    c[:, C_ALLOW:C_ALLOW + 128] = ((kt // 64) <= (qt // 64))
    c[:, C_TRI:C_TRI + 128] = (kt < qt)
    c[:, C_ONES:C_ONES + 128] = 1.0
    c[:, C_IOTA_E:C_IOTA_E + 32] = np.arange(32)[None, :]
    c[:, C_IMOD] = p % 64
    c[:, C_SIGN] = np.where(p < 64, -1.0, 1.0)
    c[:, C_HALFPI] = np.pi / 2
    c[:, C_EPS] = EPS
    c[:, C_LNQ] = np.log(QSCALE)
    c[:, C_ONE] = 1.0
    return c


def _percore(j):
    t = np.zeros((128, PC_N), np.float32)
    p = np.arange(128)
    for qi in range(4):
        if qi < 3:
            s = 3 - qi
            valid = s <= j
            t[:, PC_BASE + qi] = (j - s) * 1024 if valid else 0
            for i in range(8):
                t[:, PC_E + qi * 8 + i] = (1024 * s - (128 * i + p)) if valid else 1.0e6
        else:
            t[:, PC_BASE + qi] = j * 1024
            for i in range(8):
                t[:, PC_E + qi * 8 + i] = -(128 * i + p)
    t[:, PC_FLAG] = 1.0 if j > 0 else 0.0
    return t


_NC_CACHE = {}


def make_in_maps(x, c, norm1_g, w_mod, b_mod, w_in, conv_w, w_out, norm2_g, w_router, b_router,
                 w_gate_up, b_gate_up, w_down, b_down, final_g):
    f = lambda a: np.ascontiguousarray(np.asarray(a, dtype=np.float32))
    x = f(x); c = f(c)
    shared = dict(
        cst=_consts(), norm1_g=f(norm1_g[0]), w_mod=f(w_mod[0]), b_mod=f(b_mod[0]), w_in=f(w_in[0]),
        conv_w=f(conv_w[0]), w_out=f(w_out[0]), norm2_g=f(norm2_g[0]), w_router=f(w_router[0]),
        b_router=f(b_router[0]), w_gate_up=f(w_gate_up[0]), b_gate_up=f(b_gate_up[0]),
        w_down=f(w_down[0]), b_down=f(b_down[0]), final_g=f(final_g))
    in_maps = []
    for core in range(8):
        b, j = core // 4, core % 4
        xq = np.zeros((4, T, D), np.float32)
        for qi in range(3):
            s = 3 - qi
            if s <= j:
                xq[qi] = x[b, (j - s) * T:(j - s + 1) * T]
        xq[3] = x[b, j * T:(j + 1) * T]
        m = dict(shared)
        m["x_q"] = xq
        m["c_pc"] = np.ascontiguousarray(c[b].reshape(128, 16))
        m["pcst"] = _percore(j)
        in_maps.append(m)
    return in_maps


def kernel(**inputs):
    if "nc" not in _NC_CACHE:
        _NC_CACHE["nc"] = build_nc()
    nc = _NC_CACHE["nc"]
    in_maps = make_in_maps(**inputs)
    res = run_bass_kernel_spmd(nc, in_maps, core_ids=list(range(8)))
    outs = [np.asarray(r["out"], dtype=np.float32) for r in res.results]
    full = np.stack(outs, 0).reshape(2, 4, T, D).reshape(2, 4 * T, D)
    return full
```

```python
import numpy as np
import concourse.bass as bass
import concourse.mybir as mybir
from concourse.bass_utils import run_bass_kernel_spmd

F32 = mybir.dt.float32
BF16 = mybir.dt.bfloat16
I32 = mybir.dt.int32
U32 = mybir.dt.uint32
AF = mybir.ActivationFunctionType
ALU = mybir.AluOpType

D = 2048
T = 1024
NT = 8
D_RET = 1024
D_CONV = 1024
NH = 8
HD = 128
D_IN = 7168
NE = 32
TOPK = 4
DFF = 2048
CAP = 512
EPS = 1e-6
QSCALE = float(HD ** -0.5)
LOG_GAMMA = [float(np.log1p(-2.0 ** (-5.0 - h))) for h in range(NH)]
TWO_PI_HI = 6.28125
TWO_PI_LO = 2.0 * np.pi - 6.28125
INV_2PI = float(1.0 / (2.0 * np.pi))
PI_LO = 3.1415925

C_IOTA_T = 0
C_IDENT = 1024
C_DIST = 1152
C_ALLOW = 1280
C_TRI = 1408
C_ONES = 1536
C_IOTA_E = 1664
C_IMOD = 1696
C_SIGN = 1697
C_HALFPI = 1698
C_EPS = 1699
C_LNQ = 1700
C_ONE = 1701
C_N = 1704
PC_BASE = 0
PC_E = 4
PC_FLAG = 36
PC_N = 40

A_CONST = 0
A_TRIG = 3328
A_BRD = A_TRIG + 2048
A_SF32 = A_BRD + 4096
A_HT = A_SF32 + 1024
A_WB = A_HT + 8192
A_KT = A_WB + 6144
A_V = A_KT + 4096
A_SBF = A_V + 4096
A_SCR = A_SBF + 4096
A_YTR = A_SCR + 10240
A_END = A_YTR + 5120


class Res:
    __slots__ = ("name", "w", "r")

    def __init__(self, name):
        self.name = name
        self.w = None
        self.r = []


class Q:
    def __init__(self, name, sem):
        self.name = name
        self.sem = sem
        self.n = 0
        self.waited = {}
        self.ops = []
        self.chans = []
        self.ci = 0


class Prog:
    def __init__(self, nc):
        self.nc = nc
        self.sems = []
        self.q = {}

    def add_queue(self, name, sem, chan_sems=()):
        q = Q(name, len(self.sems))
        self.sems.append(sem)
        for cs in chan_sems:
            q.chans.append([len(self.sems), 0])
            self.sems.append(cs)
        self.q[name] = q

    def _collect(self, q, reads, writes):
        need = {}

        def add(ev, is_war):
            if ev is None:
                return
            k, val, eng = ev
            if eng == q.name:
                if q.name == "pe":
                    return
                if is_war:
                    return
            if q.waited.get(k, 0) >= val:
                return
            if need.get(k, 0) < val:
                need[k] = val

        for r in reads:
            add(r.w, False)
        for w in writes:
            add(w.w, False)
            for e in w.r:
                add(e, True)
        return need

    def _commit(self, q, need):
        for k, val in need.items():
            q.waited[k] = val
        return [(k, v) for k, v in need.items()]

    def op(self, qn, fn, reads=(), writes=()):
        q = self.q[qn]
        need = self._collect(q, reads, writes)
        waits = self._commit(q, need)
        q.n += 1
        ev = (q.sem, q.n, q.name)
        q.ops.append((waits, fn, q.sem, 1))
        for r in reads:
            r.r.append(ev)
        for w in writes:
            w.w = ev
            w.r = []
        return ev

    def dma(self, qn, fn, reads=(), writes=()):
        q = self.q[qn]
        need = self._collect(q, reads, writes)
        ch = q.chans[q.ci]
        q.ci = (q.ci + 1) % len(q.chans)
        if ch[1] > 0 and q.waited.get(ch[0], 0) < 16 * ch[1]:
            if need.get(ch[0], 0) < 16 * ch[1]:
                need[ch[0]] = 16 * ch[1]
        waits = self._commit(q, need)
        ch[1] += 1
        ev = (ch[0], 16 * ch[1], "dma")
        q.ops.append((waits, fn, ch[0], 16))
        for r in reads:
            r.r.append(ev)
        for w in writes:
            w.w = ev
            w.r = []
        return ev

    def barrier(self):
        evs = []
        for q in self.q.values():
            if q.n > 0:
                evs.append((q.sem, q.n))
            for ch in q.chans:
                if ch[1] > 0:
                    evs.append((ch[0], 16 * ch[1]))
        for q in self.q.values():
            need = {}
            for k, val in evs:
                if k == q.sem and q.name != "pe" and False:
                    continue
                if q.waited.get(k, 0) < val:
                    need[k] = val
            waits = self._commit(q, need)
            if waits:
                q.ops.append((waits, None, None, 0))

    def emit(self, qn, eng):
        q = self.q[qn]
        for waits, fn, semk, inc in q.ops:
            for k, val in waits:
                eng.wait_ge(self.sems[k], val)
            if fn is not None:
                ins = fn(eng)
                ins.then_inc(self.sems[semk], inc)


def build_nc(stage="full"):
    nc = bass.Bass("TRN2", target_bir_lowering=False)
    NEW = NE if stage == "full" else 1

    def din(name, shape, dt=F32):
        return nc.dram_tensor(name, list(shape), dt, kind="ExternalInput").ap()

    x_q = din("x_q", [4, T, D])
    c_pc = din("c_pc", [128, 16])
    cst = din("cst", [128, C_N])
    pcst = din("pcst", [128, PC_N])
    norm1_g = din("norm1_g", [D])
    w_mod = din("w_mod", [D, 6 * D])
    b_mod = din("b_mod", [6 * D])
    w_in = din("w_in", [D, D_IN])
    conv_w = din("conv_w", [3, D_CONV])
    w_out = din("w_out", [D, D])
    norm2_g = din("norm2_g", [D])
    w_router = din("w_router", [D, NE])
    b_router = din("b_router", [NE])
    w_gate_up = din("w_gate_up", [NEW, D, 2 * DFF])
    b_gate_up = din("b_gate_up", [NE, 2 * DFF])
    w_down = din("w_down", [NEW, DFF, D])
    b_down = din("b_down", [NE, D])
    final_g = din("final_g", [D])
    out = nc.dram_tensor("out", [T, D], F32, kind="ExternalOutput").ap()
    mod_d = nc.dram_tensor("mod_d", [6 * D], F32, kind="Internal").ap()
    x1_d = nc.dram_tensor("x1_d", [128, NT, D], F32, kind="Internal").ap()
    xe_d = nc.dram_tensor("xe_d", [NE * CAP + 128, D], BF16, kind="Internal").ap()
    ye_d = nc.dram_tensor("ye_d", [NE * CAP + 128, D], F32, kind="Internal").ap()

    w_in_v = w_in.rearrange("(c p) n -> p c n", p=128)
    w_out_v = w_out.rearrange("(c p) n -> p c n", p=128)

    from contextlib import ExitStack
    es = ExitStack()
    with es:
        arena = es.enter_context(nc.sbuf_tensor("arena", [128, A_END], F32))
        psf = [es.enter_context(nc.psum_tensor(f"ps{i}", [128, 512], F32)) for i in range(8)]
        nsem = 5 + 8 + 6 + 2
        sems = [es.enter_context(nc.semaphore(f"s{i}")) for i in range(nsem)]
        P = Prog(nc)
        P.add_queue("pe", sems[0])
        P.add_queue("act", sems[1], sems[19:21])
        P.add_queue("dve", sems[2])
        P.add_queue("pool", sems[3], sems[5:13])
        P.add_queue("sp", sems[4], sems[13:19])

        def A(off, n):
            return arena[:, off:off + n]

        def Ab(off, nwords):
            return arena[:, off:off + nwords].bitcast(BF16)

        def psb(i):
            return psf[i][:, :].bitcast(BF16)

        bank = [Res(f"bank{i}") for i in range(8)]

        cst_sb = A(A_CONST, C_N)
        pc_sb = A(A_CONST + C_N, PC_N)
        o = A_CONST + C_N + PC_N
        ident_bf = Ab(o, 64); o += 64
        maskT = A(o, 1024).rearrange("p (h k) -> p h k", h=NH); o += 1024
        kdec_tab = A(o, 256).rearrange("p (a h) -> p a h", h=NH); o += 256
        freq = A(o, 1); o += 1
        c_sb = A(o, 16); o += 16
        c_act = A(o, 16); o += 16
        cw_sb = A(o, 24).rearrange("p (k c) -> p k c", k=3); o += 24
        small = A(o, 64); o += 64
        hT_halo = Ab(o, 16).rearrange("p (c t) -> p c t", c=16); o += 16
        assert o <= A_TRIG, o
        iota_t = cst_sb[:, C_IOTA_T:C_IOTA_T + 1024]
        ident_f = cst_sb[:, C_IDENT:C_IDENT + 128]
        dist = cst_sb[:, C_DIST:C_DIST + 128]
        allow = cst_sb[:, C_ALLOW:C_ALLOW + 128]

        def col(c):
            return cst_sb[:, c:c + 1]

        cos_t = A(A_TRIG, 1024)
        sin_t = A(A_TRIG + 1024, 1024)
        brd = [A(A_BRD, 2048), A(A_BRD + 2048, 2048)]
        S_f32 = A(A_SF32, 1024).rearrange("p (h e) -> p h e", h=NH)
        hT = Ab(A_HT, 8192).rearrange("p (c t) -> p c t", c=16)
        wb = [Ab(A_WB + i * 3072, 3072) for i in range(2)]
        kT = Ab(A_KT, 4096).rearrange("p (h t) -> p h t", h=NH)
        v_sb = Ab(A_V, 4096).rearrange("p (i n) -> p i n", i=NT)
        S_bf = Ab(A_SBF, 4096).rearrange("p (i h e) -> p i h e", i=NT, h=NH)
        yTr = Ab(A_YTR, 4096).rearrange("p (c t) -> p c t", c=8)
        yTc = Ab(A_KT, 4096).rearrange("p (c t) -> p c t", c=8)

        R = {}

        def res(name):
            if name not in R:
                R[name] = Res(name)
            return R[name]

        P.dma("sp", lambda e: e.dma_start(out=cst_sb, in_=cst), writes=[res("cst")])
        P.dma("sp", lambda e: e.dma_start(out=pc_sb, in_=pcst), writes=[res("pcst")])
        P.dma("sp", lambda e: e.dma_start(out=c_sb, in_=c_pc), writes=[res("c_sb")])
        P.dma("sp", lambda e: e.dma_start(
            out=cw_sb, in_=conv_w.rearrange("k (c p) -> p k c", p=128),
            allow_slow_non_contiguous=True), writes=[res("cw")])
        P.op("dve", lambda e: e.tensor_copy(out=ident_bf, in_=ident_f), reads=[res("cst")], writes=[res("ident_bf")])
        P.op("act", lambda e: e.activation(out=freq, in_=col(C_IMOD), func=AF.Exp,
                                           scale=float(-np.log(10000.0) / 64.0)),
             reads=[res("cst")], writes=[res("freq")])
        for h in range(NH):
            P.op("act", lambda e, h=h: e.activation(out=maskT[:, h, :], in_=dist, func=AF.Exp, scale=LOG_GAMMA[h]),
                 reads=[res("cst")], writes=[res("maskT")])
        P.op("dve", lambda e: e.tensor_tensor(
            out=maskT, in0=maskT, in1=allow.unsqueeze(1).broadcast_to([128, NH, 128]), op=ALU.mult),
            reads=[res("maskT"), res("cst")], writes=[res("maskT")])
        for h in range(NH):
            P.op("act", lambda e, h=h: e.activation(
                out=kdec_tab[:, :, h], in_=pc_sb[:, PC_E:PC_E + 32], func=AF.Exp, scale=LOG_GAMMA[h]),
                reads=[res("pcst")], writes=[res("kdec")])
        P.op("act", lambda e: e.activation(out=c_act, in_=c_sb, func=AF.Silu), reads=[res("c_sb")], writes=[res("c_act")])

        wm = [A(A_HT + i * 8192, 8192).rearrange("p (c n) -> p c n", c=16) for i in range(2)]
        modrow = arena[0:1, A_V:A_V + 12288]
        w_mod_v = w_mod.rearrange("(p c) n -> p c n", c=16)
        P.dma("sp", lambda e: e.dma_start(out=modrow, in_=b_mod.rearrange("(o n) -> o n", o=1)), writes=[res("modrow")])
        for nb in range(24):
            wres = res(f"wm{nb % 2}")
            P.dma("sp" if nb % 2 == 0 else "act", lambda e, nb=nb: e.dma_start(out=wm[nb % 2], in_=w_mod_v[:, :, nb * 512:(nb + 1) * 512]),
                  writes=[wres])

            def mm(e, nb=nb):
                for c in range(16):
                    ins = e.matmul(psf[nb % 2][0:1, :], lhsT=c_act[:, c:c + 1], rhs=wm[nb % 2][:, c, :],
                                   start=(c == 0), stop=(c == 15))
                return ins
            P.op("pe", mm, reads=[wres, res("c_act")], writes=[bank[nb % 2]])
            P.op("dve", lambda e, nb=nb: e.tensor_tensor(
                out=modrow[:, nb * 512:(nb + 1) * 512], in0=psf[nb % 2][0:1, :],
                in1=modrow[:, nb * 512:(nb + 1) * 512], op=ALU.add),
                reads=[bank[nb % 2], res("modrow")], writes=[res("modrow")])
        P.dma("sp", lambda e: e.dma_start(out=mod_d.rearrange("(o n) -> o n", o=1), in_=modrow),
              reads=[res("modrow")], writes=[res("mod_d")])
        P.barrier()

        def bload(qn, dst, src_row, rname):
            return P.dma(qn, lambda e: e.dma_start(out=dst, in_=src_row.partition_broadcast(128)),
                         reads=[res("mod_d")], writes=[res(rname)])

        tmpb = A(A_SCR, 2048)
        bload("sp", brd[0], norm1_g, "brd0")
        bload("sp", tmpb, mod_d[D:2 * D], "tmpb")
        bload("sp", brd[1], mod_d[0:D], "brd1")
        P.op("dve", lambda e: e.scalar_tensor_tensor(out=brd[0], in0=tmpb, scalar=1.0, in1=brd[0],
                                                     op0=ALU.add, op1=ALU.mult),
             reads=[res("tmpb"), res("brd0")], writes=[res("brd0")])
        P.barrier()

        xs = [A(A_SCR + i * 2048, 2048) for i in range(2)]
        tmpf = A(A_SCR + 4096, 2048)
        hbf = [Ab(A_SCR + 6144 + i * 1024, 1024) for i in range(2)]
        t12 = [A(A_SCR + 8192 + i * 512, 512) for i in range(4)]
        ssq = small[:, 0:8]
        rstd = small[:, 8:16]

        def trig_tables(qi):
            ang = xs[0][:, 0:1024]
            kf = xs[0][:, 1024:2048]
            ki = kf.bitcast(I32)
            rr = xs[1][:, 0:1024]
            ab = xs[1][:, 1024:2048]
            rs_ = [res("xs0"), res("xs1")]
            P.op("dve", lambda e: e.tensor_scalar(out=ang, in0=iota_t, scalar1=pc_sb[:, PC_BASE + qi:PC_BASE + qi + 1],
                                                  scalar2=freq, op0=ALU.add, op1=ALU.mult),
                 reads=[res("cst"), res("pcst"), res("freq")], writes=[rs_[0]])
            P.op("dve", lambda e: e.tensor_scalar(out=rr.bitcast(I32), in0=ang, scalar1=INV_2PI, scalar2=None, op0=ALU.mult),
                 reads=[rs_[0]], writes=[rs_[1]])
            P.op("dve", lambda e: e.tensor_copy(out=kf, in_=rr.bitcast(I32)), reads=[rs_[1]], writes=[rs_[0]])
            P.op("dve", lambda e: e.scalar_tensor_tensor(out=rr, in0=kf, scalar=-TWO_PI_HI, in1=ang, op0=ALU.mult, op1=ALU.add),
                 reads=[rs_[0]], writes=[rs_[1]])
            P.op("dve", lambda e: e.scalar_tensor_tensor(out=rr, in0=kf, scalar=-TWO_PI_LO, in1=rr, op0=ALU.mult, op1=ALU.add),
                 reads=[rs_[0], rs_[1]], writes=[rs_[1]])
            P.op("dve", lambda e: e.tensor_scalar(out=rr, in0=rr, scalar1=PI_LO, scalar2=-PI_LO, op0=ALU.min, op1=ALU.max),
                 reads=[rs_[1]], writes=[rs_[1]])
            P.op("dve", lambda e: e.scalar_tensor_tensor(out=ab, in0=rr, scalar=-1.0, in1=rr, op0=ALU.mult, op1=ALU.max),
                 reads=[rs_[1]], writes=[rs_[1]])
            P.op("act", lambda e: e.activation(out=sin_t, in_=rr, func=AF.Sin, scale=col(C_SIGN)),
                 reads=[rs_[1], res("cst")], writes=[res("trig")])
            P.op("act", lambda e: e.activation(out=cos_t, in_=ab, func=AF.Sin, scale=-1.0, bias=col(C_HALFPI)),
                 reads=[rs_[1], res("cst")], writes=[res("trig")])

        def p1(qi):
            x_src = x_q[qi].rearrange("(i p) d -> i p d", p=128)
            for i in range(NT):
                xr = res(f"xs{i % 2}")
                hr = res(f"hbf{i % 2}")
                P.dma("sp", lambda e, i=i: e.dma_start(out=xs[i % 2], in_=x_src[i]), writes=[xr])
                P.op("act", lambda e, i=i: e.activation(out=hbf[i % 2], in_=xs[i % 2], func=AF.Square,
                                                       accum_out=ssq[:, i:i + 1]),
                     reads=[xr], writes=[hr, res(f"ssq{i}")])
                P.op("act", lambda e, i=i: e.activation(out=rstd[:, i:i + 1], in_=ssq[:, i:i + 1], func=AF.Sqrt,
                                                       scale=1.0 / D, bias=col(C_EPS)),
                     reads=[res(f"ssq{i}"), res("cst")], writes=[res(f"rstd{i}")])
                P.op("dve", lambda e, i=i: e.reciprocal(out=rstd[:, i:i + 1], in_=rstd[:, i:i + 1]),
                     reads=[res(f"rstd{i}")], writes=[res(f"rstd{i}")])
                P.op("dve", lambda e, i=i: e.scalar_tensor_tensor(out=tmpf, in0=xs[i % 2], scalar=rstd[:, i:i + 1],
                                                                   in1=brd[0], op0=ALU.mult, op1=ALU.mult),
                     reads=[xr, res(f"rstd{i}"), res("brd0")], writes=[res("tmpf")])
                P.op("pool", lambda e, i=i: e.tensor_tensor(out=hbf[i % 2], in0=tmpf, in1=brd[1], op=ALU.add),
                     reads=[res("tmpf"), res("brd1")], writes=[hr])
                pb = 2 * (i % 2)

                def tr(e, i=i, pb=pb):
                    for c in range(16):
                        ins = e.transpose(out=psb(pb + c // 8)[:, (c % 8) * 128:(c % 8 + 1) * 128],
                                          in_=hbf[i % 2][:, c * 128:(c + 1) * 128], identity=ident_bf)
                    return ins
                P.op("pe", tr, reads=[hr, res("ident_bf")], writes=[bank[pb], bank[pb + 1]])
                P.op("act", lambda e, i=i, pb=pb: e.activation(
                    out=hT[:, 0:8, i * 128:(i + 1) * 128], in_=psb(pb).rearrange("p (c t) -> p c t", c=8), func=AF.Copy),
                    reads=[bank[pb]], writes=[res("hT")])
                P.op("dve", lambda e, i=i, pb=pb: e.tensor_copy(
                    out=hT[:, 8:16, i * 128:(i + 1) * 128], in_=psb(pb + 1).rearrange("p (c t) -> p c t", c=8)),
                    reads=[bank[pb + 1]], writes=[res("hT")])

        def load_w(slot, cols0, ncols, swap=False):
            wr = res(f"wb{slot}")
            if not swap:
                dst = wb[slot][:, 0:16 * ncols].rearrange("p (c n) -> p c n", c=16)
                P.dma("pool", lambda e: e.dma_start(out=dst, in_=w_in_v[:, :, cols0:cols0 + ncols]), writes=[wr])
            else:
                dstv = wb[slot][:, 0:16 * 384].rearrange("p (c h n) -> p c h n", c=16, h=2)
                srcv = w_in_v[:, :, cols0:cols0 + 256].rearrange("p c (h d) -> p c h d", h=2)
                for hh in range(2):
                    P.dma("pool", lambda e, hh=hh: e.dma_start(out=dstv[:, :, hh, 64:192], in_=srcv[:, :, hh, :]), writes=[wr])
                    P.dma("pool", lambda e, hh=hh: e.dma_start(out=dstv[:, :, hh, 0:64], in_=srcv[:, :, hh, 64:128]), writes=[wr])
            return wr

        def rope_block(slot, h0, is_q, qdst=None, qtdst=None, decq=None):
            wr = res(f"wb{slot}")
            wv = wb[slot][:, 0:16 * 384].rearrange("p (c h n) -> p c h n", c=16, h=2)
            for hh in range(2):
                for th in range(2):
                    ba, bb = 4 + 2 * ((hh * 2 + th) % 2), 5 + 2 * ((hh * 2 + th) % 2)
                    ta, tb = t12[2 * ((hh * 2 + th) % 2)], t12[2 * ((hh * 2 + th) % 2) + 1]
                    tra, trb = res(f"t12_{2 * ((hh * 2 + th) % 2)}"), res(f"t12_{2 * ((hh * 2 + th) % 2) + 1}")
                    tok = slice(th * 512, (th + 1) * 512)

                    def mma(e, hh=hh, tok=tok, ba=ba):
                        for c in range(16):
                            ins = e.matmul(psf[ba][:, :], lhsT=wv[:, c, hh, 64:192], rhs=hT[:, c, tok],
                                           start=(c == 0), stop=(c == 15))
                        return ins

                    def mmb(e, hh=hh, tok=tok, bb=bb):
                        for c in range(16):
                            ins = e.matmul(psf[bb][:, :], lhsT=wv[:, c, hh, 0:128], rhs=hT[:, c, tok],
                                           start=(c == 0), stop=(c == 15))
                        return ins
                    P.op("pe", mma, reads=[wr, res("hT")], writes=[bank[ba]])
                    P.op("pe", mmb, reads=[wr, res("hT")], writes=[bank[bb]])
                    P.op("dve", lambda e, ta=ta, ba=ba, tok=tok: e.tensor_tensor(out=ta, in0=psf[ba][:, :], in1=cos_t[:, tok], op=ALU.mult),
                         reads=[bank[ba], res("trig")], writes=[tra])
                    P.op("dve", lambda e, tb=tb, bb=bb, tok=tok: e.tensor_tensor(out=tb, in0=psf[bb][:, :], in1=sin_t[:, tok], op=ALU.mult),
                         reads=[bank[bb], res("trig")], writes=[trb])
                    if not is_q:
                        P.op("pool", lambda e, ta=ta, tb=tb, hh=hh, tok=tok: e.tensor_tensor(
                            out=kT[:, h0 + hh, tok], in0=ta, in1=tb, op=ALU.add),
                            reads=[tra, trb], writes=[res("kT")])
                    else:
                        P.op("pool", lambda e, ta=ta, tb=tb: e.tensor_tensor(out=ta, in0=ta, in1=tb, op=ALU.add),
                             reads=[tra, trb], writes=[tra])
                        P.op("act", lambda e, ta=ta, hh=hh, tok=tok: e.activation(out=qdst[:, hh, tok], in_=ta, func=AF.Copy, scale=QSCALE),
                             reads=[tra], writes=[res("qT")])
                        P.op("dve", lambda e, ta=ta, hh=hh, tok=tok: e.tensor_tensor(out=qtdst[:, hh, tok], in0=ta, in1=decq[hh][:, tok], op=ALU.mult),
                             reads=[tra, res(f"decq{hh}")], writes=[res("qtT")])

        def v_block(slot, vb):
            wr = res(f"wb{slot}")
            wv = wb[slot][:, 0:16 * 256].rearrange("p (c n) -> p c n", c=16)
            for i in range(NT):
                b = 4 + (i % 4)

                def mm(e, i=i, b=b):
                    for c in range(16):
                        ins = e.matmul(psf[b][:, 0:256], lhsT=hT[:, c, i * 128:(i + 1) * 128], rhs=wv[:, c, :],
                                       start=(c == 0), stop=(c == 15))
                    return ins
                P.op("pe", mm, reads=[wr, res("hT")], writes=[bank[b]])
                P.op("act", lambda e, i=i, b=b: e.activation(out=v_sb[:, i, vb * 256:(vb + 1) * 256], in_=psf[b][:, 0:256], func=AF.Copy),
                     reads=[bank[b]], writes=[res("v")])

        kdec_sb = Ab(A_SCR + 4096, 512).rearrange("p (h d) -> p h d", h=NH)

        def state_phase(qi, main):
            for i in range(NT):
                def tr(e, i=i):
                    for h in range(NH):
                        ins = e.transpose(out=psb(0)[:, h * 128:(h + 1) * 128], in_=kT[:, h, i * 128:(i + 1) * 128],
                                          identity=ident_bf)
                    return ins
                P.op("pe", tr, reads=[res("kT"), res("ident_bf")], writes=[bank[0]])
                P.op("dve", lambda e, i=i: e.tensor_tensor(
                    out=kdec_sb, in0=psb(0).rearrange("p (h d) -> p h d", h=NH),
                    in1=kdec_tab[:, qi * 8 + i, :].unsqueeze(2).broadcast_to([128, NH, 128]), op=ALU.mult),
                    reads=[bank[0], res("kdec")], writes=[res("tmpf")])

                def inc(e, i=i):
                    for h in range(NH):
                        ins = e.matmul(psf[2 + h // 4][:, (h % 4) * 128:(h % 4 + 1) * 128], lhsT=kdec_sb[:, h, :],
                                       rhs=v_sb[:, i, h * 128:(h + 1) * 128], start=True, stop=True)
                    return ins
                P.op("pe", inc, reads=[res("tmpf"), res("v")], writes=[bank[2], bank[3]])
                if main:
                    P.op("act", lambda e, i=i: e.activation(out=S_bf[:, i, :, :], in_=S_f32, func=AF.Copy),
                         reads=[res("S")], writes=[res("S_bf")])
                for hb in range(2):
                    P.op("dve", lambda e, hb=hb: e.tensor_tensor(
                        out=S_f32[:, hb * 4:(hb + 1) * 4, :], in0=S_f32[:, hb * 4:(hb + 1) * 4, :],
                        in1=psf[2 + hb][:, :].rearrange("p (h e) -> p h e", h=4), op=ALU.add),
                        reads=[bank[2 + hb], res("S")], writes=[res("S")])

        P.op("pool", lambda e: e.memset(S_f32, 0.0), writes=[res("S")])

        for qi in range(4):
            main = qi == 3
            trig_tables(qi)
            if main:
                P.op("dve", lambda e: e.tensor_copy(out=hT_halo, in_=hT[:, :, 1022:1024]),
                     reads=[res("hT")], writes=[res("hT_halo")])
            p1(qi)
            blocks = [("k", g) for g in range(4)] + [("v", g) for g in range(4)]
            load_w(0, D_RET + 0, 256, swap=True)
            for bi, (kind, g) in enumerate(blocks):
                slot = bi % 2
                if bi + 1 < len(blocks):
                    nk, ng = blocks[bi + 1]
                    if nk == "k":
                        load_w((bi + 1) % 2, D_RET + ng * 256, 256, swap=True)
                    else:
                        load_w((bi + 1) % 2, 2 * D_RET + ng * 256, 256)
                if kind == "k":
                    rope_block(slot, 2 * g, False)
                else:
                    v_block(slot, g)
            state_phase(qi, main)
        P.barrier()

        qT = Ab(A_SCR + 0, 1024).rearrange("p (h t) -> p h t", h=2)
        qtT = Ab(A_SCR + 1024, 1024).rearrange("p (h t) -> p h t", h=2)
        sg = Ab(A_SCR + 2048, 1024).rearrange("p (i n) -> p i n", i=NT)
        decq = [A(A_SCR + 3072 + i * 1024, 1024) for i in range(2)]
        Pm = [Ab(A_SCR + 5120 + i * 128, 128).rearrange("p (h k) -> p h k", h=2) for i in range(2)]
        onb = [A(A_SCR + 5376 + i * 256, 256).rearrange("p (h k) -> p h k", h=2) for i in range(2)]
        yrb = [Ab(A_SCR + 5888 + i * 128, 128).rearrange("p (h k) -> p h k", h=2) for i in range(2)]
        bst = A(A_SCR + 6144, 32).rearrange("p (a h s) -> p a h s", a=2, h=2)
        bmv = A(A_SCR + 6176, 16).rearrange("p (a h s) -> p a h s", a=2, h=2)
        brs = A(A_SCR + 6192, 4).rearrange("p (a h) -> p a h", a=2)

        for g in range(4):
            for hh in range(2):
                P.op("act", lambda e, hh=hh, g=g: e.activation(out=decq[hh], in_=iota_t, func=AF.Exp,
                                                           scale=LOG_GAMMA[2 * g + hh], bias=col(C_LNQ)),
                     reads=[res("cst")], writes=[res(f"decq{hh}")])
            if g == 0:
                load_w(0, 0, 256, swap=True)
                load_w(1, 3 * D_RET, 256)
            rope_block(0, 2 * g, True, qdst=qT, qtdst=qtT, decq=decq)
            if g + 1 < 4:
                load_w(0, 2 * (g + 1) * 128, 256, swap=True)
            wgv = wb[1][:, 0:16 * 256].rearrange("p (c n) -> p c n", c=16)
            for i in range(NT):
                b = i % 2

                def mmg(e, i=i, b=b):
                    for c in range(16):
                        ins = e.matmul(psf[b][:, 0:256], lhsT=hT[:, c, i * 128:(i + 1) * 128], rhs=wgv[:, c, :],
                                       start=(c == 0), stop=(c == 15))
                    return ins
                P.op("pe", mmg, reads=[res("wb1"), res("hT")], writes=[bank[b]])
                P.op("act", lambda e, i=i, b=b: e.activation(out=sg[:, i, :], in_=psf[b][:, 0:256], func=AF.Silu),
                     reads=[bank[b]], writes=[res("sg")])
            if g + 1 < 4:
                load_w(1, 3 * D_RET + (g + 1) * 256, 256)
            for i in range(NT):
                a = i % 2
                tok = slice(i * 128, (i + 1) * 128)
                rPm, ron, ryr, rst = res(f"Pm{a}"), res(f"on{a}"), res(f"yr{a}"), res(f"bst{a}")

                def sc(e, g=g, tok=tok, a=a):
                    for hh in range(2):
                        ins = e.matmul(psf[2 + a][:, hh * 128:(hh + 1) * 128], lhsT=kT[:, 2 * g + hh, tok], rhs=qT[:, hh, tok],
                                       start=True, stop=True)
                    return ins
                P.op("pe", sc, reads=[res("kT"), res("qT")], writes=[bank[2 + a]])
                P.op("dve", lambda e, g=g, a=a: e.tensor_tensor(
                    out=Pm[a], in0=psf[2 + a][:, 0:256].rearrange("p (h k) -> p h k", h=2),
                    in1=maskT[:, 2 * g:2 * g + 2, :], op=ALU.mult),
                    reads=[bank[2 + a], res("maskT")], writes=[rPm])

                def om(e, g=g, i=i, tok=tok, a=a):
                    for hh in range(2):
                        h = 2 * g + hh
                        e.matmul(psf[4 + a][:, hh * 128:(hh + 1) * 128], lhsT=Pm[a][:, hh, :],
                                 rhs=v_sb[:, i, h * 128:(h + 1) * 128], start=True, stop=False)
                        ins = e.matmul(psf[4 + a][:, hh * 128:(hh + 1) * 128], lhsT=qtT[:, hh, tok],
                                       rhs=S_bf[:, i, h, :], start=False, stop=True)
                    return ins
                P.op("pe", om, reads=[rPm, res("v"), res("qtT"), res("S_bf")], writes=[bank[4 + a]])
                for hh in range(2):
                    P.op("dve", lambda e, hh=hh, a=a: e.bn_stats(out=bst[:, a, hh, 0:6], in_=psf[4 + a][:, hh * 128:(hh + 1) * 128]),
                         reads=[bank[4 + a]], writes=[rst])
                for hh in range(2):
                    P.op("dve", lambda e, hh=hh, a=a: e.bn_aggr(out=bmv[:, a, hh, 0:2], in_=bst[:, a, hh, 0:6]),
                         reads=[rst], writes=[rst])
                P.op("act", lambda e, a=a: e.activation(out=brs[:, a, :], in_=bmv[:, a, :, 1], func=AF.Sqrt,
                                                        bias=col(C_EPS)),
                     reads=[rst, res("cst")], writes=[res(f"brs{a}")])
                P.op("dve", lambda e, a=a: e.reciprocal(out=brs[:, a, :], in_=brs[:, a, :]),
                     reads=[res(f"brs{a}")], writes=[res(f"brs{a}")])
                for hh in range(2):
                    P.op("dve", lambda e, hh=hh, a=a: e.tensor_scalar(
                        out=onb[a][:, hh, :], in0=psf[4 + a][:, hh * 128:(hh + 1) * 128],
                        scalar1=bmv[:, a, hh, 0:1], scalar2=brs[:, a, hh:hh + 1], op0=ALU.subtract, op1=ALU.mult),
                        reads=[bank[4 + a], rst, res(f"brs{a}")], writes=[ron])
                P.op("pool", lambda e, i=i, a=a: e.tensor_tensor(
                    out=yrb[a], in0=onb[a], in1=sg[:, i, :].rearrange("p (h k) -> p h k", h=2), op=ALU.mult),
                    reads=[ron, res("sg")], writes=[ryr])

                def trY(e, a=a):
                    for hh in range(2):
                        ins = e.transpose(out=psb(6 + a)[:, hh * 128:(hh + 1) * 128], in_=yrb[a][:, hh, :], identity=ident_bf)
                    return ins
                P.op("pe", trY, reads=[ryr, res("ident_bf")], writes=[bank[6 + a]])
                P.op("act", lambda e, g=g, tok=tok, a=a: e.activation(
                    out=yTr[:, 2 * g:2 * g + 2, tok], in_=psb(6 + a)[:, 0:256].rearrange("p (h k) -> p h k", h=2), func=AF.Copy),
                    reads=[bank[6 + a]], writes=[res("yTr")])
        P.barrier()

        uT = A(A_SCR + 0, 2056)[:, 0:2052].rearrange("p (c t) -> p c t", c=2)
        acc = A(A_SCR + 2056, 2048).rearrange("p (c t) -> p c t", c=2)
        BASE_B, BASE_C, BASE_U = 4 * D_RET, 4 * D_RET + D_CONV, 4 * D_RET + 2 * D_CONV

        def conv_mm(slot, cc, with_halo):
            wv = wb[slot][:, 0:16 * 256].rearrange("p (c n) -> p c n", c=16)
            for th in range(2):
                b = cc * 2 + th

                def mm(e, b=b, th=th):
                    for c in range(16):
                        ins = e.matmul(psf[b][:, :], lhsT=wv[:, c, cc * 128:(cc + 1) * 128], rhs=hT[:, c, th * 512:(th + 1) * 512],
                                       start=(c == 0), stop=(c == 15))
                    return ins
                P.op("pe", mm, reads=[res(f"wb{slot}"), res("hT")], writes=[bank[b]])
            if with_halo:
                def mmh(e):
                    for c in range(16):
                        ins = e.matmul(psf[4 + cc][:, 0:2], lhsT=wv[:, c, cc * 128:(cc + 1) * 128], rhs=hT_halo[:, c, :],
                                       start=(c == 0), stop=(c == 15))
                    return ins
                P.op("pe", mmh, reads=[res(f"wb{slot}"), res("hT_halo")], writes=[bank[4 + cc]])

        cjobs = []
        for cg in range(4):
            cjobs += [("u", cg, BASE_U + cg * 256), ("c", cg, BASE_C + cg * 256), ("b", cg, BASE_B + cg * 256)]
        load_w(0, cjobs[0][2], 256)
        for ji, (kind, cg, cols0) in enumerate(cjobs):
            sl = ji % 2
            if ji + 1 < len(cjobs):
                load_w((ji + 1) % 2, cjobs[ji + 1][2], 256)
            if kind == "u":
                for cc in range(2):
                    conv_mm(sl, cc, True)
                    for th in range(2):
                        P.op("act", lambda e, cc=cc, th=th: e.activation(
                            out=uT[:, cc, 2 + th * 512:2 + (th + 1) * 512], in_=psf[cc * 2 + th][:, :], func=AF.Copy),
                            reads=[bank[cc * 2 + th]], writes=[res("uT")])
                    P.op("act", lambda e, cc=cc: e.activation(out=uT[:, cc, 0:2], in_=psf[4 + cc][:, 0:2], func=AF.Copy),
                         reads=[bank[4 + cc]], writes=[res("uT")])
            elif kind == "c":
                for cc in range(2):
                    conv_mm(sl, cc, True)
                    for th in range(2):
                        P.op("dve", lambda e, cc=cc, th=th: e.tensor_tensor(
                            out=uT[:, cc, 2 + th * 512:2 + (th + 1) * 512], in0=psf[cc * 2 + th][:, :],
                            in1=uT[:, cc, 2 + th * 512:2 + (th + 1) * 512], op=ALU.mult),
                            reads=[bank[cc * 2 + th], res("uT")], writes=[res("uT")])
                    P.op("dve", lambda e, cc=cc: e.scalar_tensor_tensor(
                        out=uT[:, cc, 0:2], in0=psf[4 + cc][:, 0:2], scalar=pc_sb[:, PC_FLAG:PC_FLAG + 1], in1=uT[:, cc, 0:2],
                        op0=ALU.mult, op1=ALU.mult),
                        reads=[bank[4 + cc], res("uT"), res("pcst")], writes=[res("uT")])
                for cc in range(2):
                    ch = cg * 2 + cc
                    P.op("pool", lambda e, cc=cc, ch=ch: e.tensor_scalar(
                        out=acc[:, cc, :], in0=uT[:, cc, 2:1026], scalar1=cw_sb[:, 2, ch:ch + 1], scalar2=None, op0=ALU.mult),
                        reads=[res("uT"), res("cw")], writes=[res("acc")])
                    P.op("dve", lambda e, cc=cc, ch=ch: e.scalar_tensor_tensor(
                        out=acc[:, cc, :], in0=uT[:, cc, 1:1025], scalar=cw_sb[:, 1, ch:ch + 1], in1=acc[:, cc, :],
                        op0=ALU.mult, op1=ALU.add),
                        reads=[res("uT"), res("cw"), res("acc")], writes=[res("acc")])
                    P.op("dve", lambda e, cc=cc, ch=ch: e.scalar_tensor_tensor(
                        out=acc[:, cc, :], in0=uT[:, cc, 0:1024], scalar=cw_sb[:, 0, ch:ch + 1], in1=acc[:, cc, :],
                        op0=ALU.mult, op1=ALU.add),
                        reads=[res("uT"), res("cw"), res("acc")], writes=[res("acc")])
            else:
                for cc in range(2):
                    conv_mm(sl, cc, False)
                    for th in range(2):
                        P.op("dve", lambda e, cc=cc, th=th, cg=cg: e.tensor_tensor(
                            out=yTc[:, cg * 2 + cc, th * 512:(th + 1) * 512], in0=psf[cc * 2 + th][:, :],
                            in1=acc[:, cc, th * 512:(th + 1) * 512], op=ALU.mult),
                            reads=[bank[cc * 2 + th], res("acc")], writes=[res("yTc")])
        P.barrier()

        x1 = A(A_V, 16384).rearrange("p (i d) -> p i d", i=NT)
        wo = [Ab(A_HT + i * 4096, 4096).rearrange("p (c n) -> p c n", c=16) for i in range(2)]
        otmp = [A(A_SCR + 8192 + i * 512, 512) for i in range(2)]
        P.dma("sp", lambda e: e.dma_start(out=x1, in_=x_q[3].rearrange("(i p) d -> p i d", p=128)), writes=[res("x1")])
        bload("act", brd[0], mod_d[2 * D:3 * D], "brd0")

        def load_wo(nb):
            P.dma("pool", lambda e: e.dma_start(out=wo[nb % 2], in_=w_out_v[:, :, nb * 512:(nb + 1) * 512]),
                  writes=[res(f"wo{nb % 2}")])
        load_wo(0)
        for nb in range(4):
            if nb + 1 < 4:
                load_wo(nb + 1)
            for i in range(NT):
                b = i % 4

                def mm(e, nb=nb, i=i, b=b):
                    for c in range(16):
                        src = yTr[:, c, i * 128:(i + 1) * 128] if c < 8 else yTc[:, c - 8, i * 128:(i + 1) * 128]
                        ins = e.matmul(psf[b][:, :], lhsT=src, rhs=wo[nb % 2][:, c, :], start=(c == 0), stop=(c == 15))
                    return ins
                P.op("pe", mm, reads=[res(f"wo{nb % 2}"), res("yTr"), res("yTc")], writes=[bank[b]])
                P.op("dve", lambda e, nb=nb, i=i, b=b: e.tensor_tensor(
                    out=otmp[i % 2], in0=psf[b][:, :], in1=brd[0][:, nb * 512:(nb + 1) * 512], op=ALU.mult),
                    reads=[bank[b], res("brd0")], writes=[res(f"otmp{i % 2}")])
                P.op("pool", lambda e, nb=nb, i=i: e.tensor_tensor(
                    out=x1[:, i, nb * 512:(nb + 1) * 512], in0=x1[:, i, nb * 512:(nb + 1) * 512], in1=otmp[i % 2], op=ALU.add),
                    reads=[res(f"otmp{i % 2}"), res("x1")], writes=[res("x1")])
        P.barrier()

        if stage == "x1":
            P.dma("sp", lambda e: e.dma_start(out=out.rearrange("(i p) d -> p i d", p=128), in_=x1), reads=[res("x1")])
            P.barrier()
        if stage != "x1":
            tmp2 = A(A_HT, 2048)
            hb2 = [Ab(A_HT + 2048 + i * 1024, 1024) for i in range(2)]
            h2Tf = A(A_HT + 4096, 2048).rearrange("p (c t) -> p c t", c=16)
            wr_sb = A(A_HT + 6144, 512).rearrange("p (c e) -> p c e", c=16)
            brow = arena[0:1, A_HT + 6656:A_HT + 6688]
            so = A_HT + 6688
            lg = A(so, 32); so += 32
            rk = A(so, 32); so += 32
            oh = A(so, 32); so += 32
            ecap = A(so, 32); so += 32
            mkf = A(so, 32); so += 32
            mx8 = A(so, 8); so += 8
            mi8 = A(so, 8).bitcast(U32); so += 8
            idf8 = A(so, 8); so += 8
            ex4 = A(so, 4); so += 4
            destf = A(so, 4); so += 4
            lim4 = A(so, 4); so += 4
            val4 = A(so, 4); so += 4
            nmx = A(so, 1); so += 1
            ssum = A(so, 1); so += 1
            so += 2
            tri_bf = Ab(so, 64); so += 64
            ones_bf = Ab(so, 64); so += 64
            mk_bf = Ab(so, 128).rearrange("p (i e) -> p i e", i=NT); so += 128
            assert so <= A_HT + 8192
            w4_all = A(A_YTR + 4096, 32).rearrange("p (i k) -> p i k", i=NT)
            dest_all = A(A_YTR + 4128, 32).bitcast(I32).rearrange("p (i k) -> p i k", i=NT)
            ssq2 = A(A_YTR + 4352, 8)
            rstd2 = A(A_YTR + 4360, 8)
            ones_row = cst_sb[0:1, C_ONES:C_ONES + 128]
            iota_e = cst_sb[:, C_IOTA_E:C_IOTA_E + 32]

            tmpc = A(A_TRIG, 2048)
            bload("sp", brd[0], norm2_g, "brd0")
            bload("sp", tmpc, mod_d[4 * D:5 * D], "tmpc")
            bload("act", brd[1], mod_d[3 * D:4 * D], "brd1")
            P.op("dve", lambda e: e.scalar_tensor_tensor(out=brd[0], in0=tmpc, scalar=1.0, in1=brd[0],
                                                         op0=ALU.add, op1=ALU.mult),
                 reads=[res("tmpc"), res("brd0")], writes=[res("brd0")])
            P.dma("sp", lambda e: e.dma_start(out=wr_sb, in_=w_router.rearrange("(c p) e -> p c e", p=128)),
                  writes=[res("wr_sb")])
            P.dma("sp", lambda e: e.dma_start(out=brow, in_=b_router.rearrange("(o n) -> o n", o=1)), writes=[res("brow")])
            P.op("dve", lambda e: e.tensor_copy(out=tri_bf, in_=cst_sb[:, C_TRI:C_TRI + 128]), reads=[res("cst")], writes=[res("tri")])
            P.op("dve", lambda e: e.tensor_copy(out=ones_bf, in_=cst_sb[:, C_ONES:C_ONES + 128]), reads=[res("cst")], writes=[res("tri")])
            P.op("dve", lambda e: e.tensor_scalar(out=ecap, in0=iota_e, scalar1=float(CAP), scalar2=None, op0=ALU.mult),
                 reads=[res("cst")], writes=[res("ecap")])

            for i in range(NT):
                hb = hb2[i % 2]
                rhb = res(f"hb2_{i % 2}")
                P.op("act", lambda e, i=i, hb=hb: e.activation(out=hb, in_=x1[:, i, :], func=AF.Square, accum_out=ssq2[:, i:i + 1]),
                     reads=[res("x1")], writes=[rhb, res("rs2")])
                P.op("act", lambda e, i=i: e.activation(out=rstd2[:, i:i + 1], in_=ssq2[:, i:i + 1], func=AF.Sqrt,
                                                       scale=1.0 / D, bias=col(C_EPS)),
                     reads=[res("rs2"), res("cst")], writes=[res("rs2")])
                P.op("dve", lambda e, i=i: e.reciprocal(out=rstd2[:, i:i + 1], in_=rstd2[:, i:i + 1]),
                     reads=[res("rs2")], writes=[res("rs2")])
                P.op("dve", lambda e, i=i: e.scalar_tensor_tensor(out=tmp2, in0=x1[:, i, :], scalar=rstd2[:, i:i + 1], in1=brd[0],
                                                                   op0=ALU.mult, op1=ALU.mult),
                     reads=[res("x1"), res("rs2"), res("brd0")], writes=[res("tmp2")])
                P.op("dve", lambda e: e.tensor_tensor(out=tmp2, in0=tmp2, in1=brd[1], op=ALU.add),
                     reads=[res("tmp2"), res("brd1")], writes=[res("tmp2")])
                P.op("act", lambda e, hb=hb: e.activation(out=hb, in_=tmp2, func=AF.Copy), reads=[res("tmp2")], writes=[rhb])

                def trf(e):
                    for c in range(16):
                        ins = e.transpose(out=psf[2 + c // 4][:, (c % 4) * 128:(c % 4 + 1) * 128],
                                          in_=tmp2[:, c * 128:(c + 1) * 128], identity=ident_f)
                    return ins
                P.op("pe", trf, reads=[res("tmp2"), res("cst")], writes=[bank[2], bank[3], bank[4], bank[5]])
                for k in range(4):
                    if k % 2 == 0:
                        P.op("act", lambda e, k=k: e.activation(out=h2Tf[:, 4 * k:4 * k + 4, :],
                                                                in_=psf[2 + k][:, :].rearrange("p (c t) -> p c t", c=4), func=AF.Copy),
                             reads=[bank[2 + k]], writes=[res("h2Tf")])
                    else:
                        P.op("dve", lambda e, k=k: e.tensor_copy(out=h2Tf[:, 4 * k:4 * k + 4, :],
                                                                 in_=psf[2 + k][:, :].rearrange("p (c t) -> p c t", c=4)),
                             reads=[bank[2 + k]], writes=[res("h2Tf")])

                def lgm(e):
                    for c in range(16):
                        e.matmul(psf[6][:, 0:32], lhsT=h2Tf[:, c, :], rhs=wr_sb[:, c, :], start=(c == 0), stop=False)
                    return e.matmul(psf[6][:, 0:32], lhsT=ones_row, rhs=brow, start=False, stop=True)
                P.op("pe", lgm, reads=[res("h2Tf"), res("wr_sb"), res("brow"), res("cst")], writes=[bank[6]])
                rt = res("rt")
                P.op("dve", lambda e: e.tensor_copy(out=lg, in_=psf[6][:, 0:32]), reads=[bank[6]], writes=[rt])
                P.op("dve", lambda e: e.max(out=mx8, in_=lg), reads=[rt], writes=[rt])
                P.op("dve", lambda e: e.max_index(out=mi8, in_max=mx8, in_values=lg), reads=[rt], writes=[rt])
                P.op("dve", lambda e: e.tensor_copy(out=idf8, in_=mi8), reads=[rt], writes=[rt])
                P.op("dve", lambda e: e.tensor_scalar(out=mkf, in0=lg, scalar1=mx8[:, 3:4], scalar2=None, op0=ALU.is_ge),
                     reads=[rt], writes=[rt])
                P.op("dve", lambda e, i=i: e.tensor_copy(out=mk_bf[:, i, :], in_=mkf), reads=[rt], writes=[res("mk_bf")])
                P.op("dve", lambda e: e.tensor_scalar(out=nmx, in0=mx8[:, 0:1], scalar1=-1.0, scalar2=None, op0=ALU.mult),
                     reads=[rt], writes=[rt])
                P.op("act", lambda e: e.activation(out=ex4, in_=mx8[:, 0:4], func=AF.Exp, bias=nmx), reads=[rt], writes=[rt])
                P.op("dve", lambda e: e.reduce_sum(out=ssum, in_=ex4, axis=mybir.AxisListType.X), reads=[rt], writes=[rt])
                P.op("dve", lambda e: e.reciprocal(out=ssum, in_=ssum), reads=[rt], writes=[rt])
                P.op("dve", lambda e, i=i: e.tensor_scalar(out=w4_all[:, i, :], in0=ex4, scalar1=ssum, scalar2=None, op0=ALU.mult),
                     reads=[rt], writes=[res("w4")])

                def rkm(e, i=i):
                    for ip in range(i):
                        e.matmul(psf[7][:, 0:32], lhsT=ones_bf, rhs=mk_bf[:, ip, :], start=(ip == 0), stop=False)
                    return e.matmul(psf[7][:, 0:32], lhsT=tri_bf, rhs=mk_bf[:, i, :], start=(i == 0), stop=True)
                P.op("pe", rkm, reads=[res("mk_bf"), res("tri")], writes=[bank[7]])
                P.op("dve", lambda e: e.tensor_tensor(out=rk, in0=psf[7][:, 0:32], in1=ecap, op=ALU.add),
                     reads=[bank[7], res("ecap")], writes=[rt])
                for k in range(TOPK):
                    P.op("dve", lambda e, k=k: e.tensor_scalar(out=oh, in0=iota_e, scalar1=idf8[:, k:k + 1], scalar2=None, op0=ALU.is_equal),
                         reads=[rt, res("cst")], writes=[rt])
                    P.op("dve", lambda e: e.tensor_tensor(out=oh, in0=oh, in1=rk, op=ALU.mult), reads=[rt], writes=[rt])
                    P.op("dve", lambda e, k=k: e.reduce_sum(out=destf[:, k:k + 1], in_=oh, axis=mybir.AxisListType.X),
                         reads=[rt], writes=[rt])
                P.op("dve", lambda e: e.tensor_scalar(out=lim4, in0=idf8[:, 0:4], scalar1=1.0, scalar2=float(CAP), op0=ALU.add, op1=ALU.mult),
                     reads=[rt], writes=[rt])
                P.op("dve", lambda e: e.tensor_tensor(out=val4, in0=destf, in1=lim4, op=ALU.is_lt), reads=[rt], writes=[rt])
                P.op("dve", lambda e: e.tensor_scalar(out=destf, in0=destf, scalar1=-float(NE * CAP), scalar2=None, op0=ALU.add),
                     reads=[rt], writes=[rt])
                P.op("dve", lambda e: e.tensor_tensor(out=destf, in0=destf, in1=val4, op=ALU.mult), reads=[rt], writes=[rt])
                P.op("dve", lambda e: e.tensor_scalar(out=destf, in0=destf, scalar1=float(NE * CAP), scalar2=None, op0=ALU.add),
                     reads=[rt], writes=[rt])
                P.op("dve", lambda e, i=i: e.tensor_tensor(out=w4_all[:, i, :], in0=w4_all[:, i, :], in1=val4, op=ALU.mult),
                     reads=[rt, res("w4")], writes=[res("w4")])
                P.op("dve", lambda e, i=i: e.tensor_copy(out=dest_all[:, i, :], in_=destf), reads=[rt], writes=[res("dest")])
                for k in range(TOPK):
                    P.dma("pool", lambda e, i=i, k=k, hb=hb: e.indirect_dma_start(
                        out=xe_d, out_offset=bass.IndirectOffsetOnAxis(ap=dest_all[:, i, k:k + 1], axis=0),
                        in_=hb, in_offset=None),
                        reads=[rhb, res("dest")], writes=[res("xe_d")])
            P.barrier()

            bgu_all = A(A_SF32, 1024).rearrange("p (c e) -> p c e", c=32)
            stage_b = arena[0:32, A_YTR:A_YTR + 4096]
            P.dma("sp", lambda e: e.dma_start(out=stage_b, in_=b_gate_up), writes=[res("stage_b")])
            for half in range(2):
                def trbias(e, half=half):
                    for cc in range(16):
                        ch = half * 16 + cc
                        ins = e.transpose(out=psf[half][:, cc * 32:(cc + 1) * 32], in_=stage_b[:, ch * 128:(ch + 1) * 128],
                                          identity=ident_f[0:32, 0:32])
                    return ins
                P.op("pe", trbias, reads=[res("stage_b"), res("cst")], writes=[bank[half]])
                P.op("dve", lambda e, half=half: e.tensor_copy(out=bgu_all[:, half * 16:(half + 1) * 16, :],
                                                               in_=psf[half][:, :].rearrange("p (c e) -> p c e", c=16)),
                     reads=[bank[half]], writes=[res("bgu")])
            P.barrier()

            NM = CAP // 128
            P.dma("sp", lambda e: e.dma_start(out=x1_d, in_=x1), reads=[res("x1")], writes=[res("x1_d")])
            P.barrier()
            xeT = [Ab(A_HT + i * 4096, 4096).rearrange("p (c s) -> p c s", c=16) for i in range(2)]
            actT = Ab(A_WB, 4096).rearrange("p (c s) -> p c s", c=16)
            xe_sb = [Ab(A_WB + 4096 + i * 1024, 1024) for i in range(4)]
            bd_bs = [A(A_WB + 8192, 2048), A(A_TRIG, 2048)]
            yst = [A(A_YTR + i * 2048, 2048).rearrange("p (m n) -> p m n", m=NM) for i in range(2)]
            g1 = A(A_SCR + 8192, 512)
            u1 = A(A_SCR + 8704, 512)
            sgm = A(A_SCR + 9216, 512)
            wgu = [Ab(A_V + i * 4096, 4096).rearrange("p (c n) -> p c n", c=16) for i in range(3)]
            wdn = [Ab(A_V + 12288, 4096).rearrange("p (c n) -> p c n", c=16), Ab(A_BRD, 4096).rearrange("p (c n) -> p c n", c=16)]
            NGS, NDS = len(wgu), len(wdn)

            def load_gu(e_, cq, slot):
                wv = w_gate_up[e_].rearrange("(c p) n -> p c n", p=128)
                P.dma("pool", lambda e: e.dma_start(out=wgu[slot][:, :, 0:256], in_=wv[:, :, cq * 256:(cq + 1) * 256]),
                      writes=[res(f"wgu{slot}")])
                P.dma("pool", lambda e: e.dma_start(out=wgu[slot][:, :, 256:512], in_=wv[:, :, DFF + cq * 256:DFF + (cq + 1) * 256]),
                      writes=[res(f"wgu{slot}")])

            def load_dn(e_, nq, slot):
                wv = w_down[e_].rearrange("(c p) n -> p c n", p=128)
                P.dma("pool", lambda e: e.dma_start(out=wdn[slot], in_=wv[:, :, nq * 512:(nq + 1) * 512]),
                      writes=[res(f"wdn{slot}")])

            xl = 0

            def load_xe(e_, m):
                nonlocal xl
                b_ = xl % 4
                xl += 1
                P.dma("sp", lambda e: e.dma_start(out=xe_sb[b_], in_=xe_d[e_ * CAP + m * 128:e_ * CAP + (m + 1) * 128, :]),
                      reads=[res("xe_d")], writes=[res(f"xe_sb{b_}")])
                return b_

            gu_jobs = [(e_, cq) for e_ in range(NEW) for cq in range(8)]
            dn_jobs = [(e_, nq) for e_ in range(NEW) for nq in range(4)]
            gl = 0
            dl = 0

            def pump_gu(upto):
                nonlocal gl
                while gl < len(gu_jobs) and gl < upto:
                    load_gu(gu_jobs[gl][0], gu_jobs[gl][1], gl % NGS)
                    gl += 1

            def pump_dn(upto):
                nonlocal dl
                while dl < len(dn_jobs) and dl < upto:
                    load_dn(dn_jobs[dl][0], dn_jobs[dl][1], dl % NDS)
                    dl += 1

            pump_gu(2)
            gi = 0
            di = 0
            tj = 0
            def transposes(e_):
                s_ = e_ % 2
                nonlocal tj
                for m in range(NM):
                    b_ = load_xe(e_, m)
                    pa = 2 * (tj % 2)
                    tj += 1

                    def trx(e, b_=b_, pa=pa):
                        for c in range(16):
                            ins = e.transpose(out=psb(pa + c // 8)[:, (c % 8) * 128:(c % 8 + 1) * 128],
                                              in_=xe_sb[b_][:, c * 128:(c + 1) * 128], identity=ident_bf)
                        return ins
                    P.op("pe", trx, reads=[res(f"xe_sb{b_}"), res("ident_bf")], writes=[bank[pa], bank[pa + 1]])
                    P.op("act", lambda e, m=m, s_=s_, pa=pa: e.activation(
                        out=xeT[s_][:, 0:8, m * 128:(m + 1) * 128], in_=psb(pa).rearrange("p (c t) -> p c t", c=8), func=AF.Copy),
                        reads=[bank[pa]], writes=[res(f"xeT{s_}")])
                    P.op("dve", lambda e, m=m, s_=s_, pa=pa: e.tensor_copy(
                        out=xeT[s_][:, 8:16, m * 128:(m + 1) * 128], in_=psb(pa + 1).rearrange("p (c t) -> p c t", c=8)),
                        reads=[bank[pa + 1]], writes=[res(f"xeT{s_}")])

            transposes(0)
            for e_ in range(NEW):
                s_ = e_ % 2
                bd_b = bd_bs[e_ % 2]
                rbd = res(f"bd_b{e_ % 2}")
                P.dma("sp", lambda e, e_=e_, bd_b=bd_b: e.dma_start(out=bd_b, in_=b_down[e_].partition_broadcast(128)), writes=[rbd])
                for cp in range(16):
                    slot = gi % NGS
                    hf = cp % 2
                    if hf == 0:
                        pump_gu(gi + NGS)
                    if cp in (0, 8):
                        pump_dn(di + NDS if cp == 8 else di + 1)
                    bg, bu = 4 + 2 * (cp % 2), 5 + 2 * (cp % 2)

                    def mg(e, slot=slot, s_=s_, bg=bg, hf=hf):
                        for c in range(16):
                            ins = e.matmul(psf[bg][:, 0:CAP], lhsT=wgu[slot][:, c, hf * 128:(hf + 1) * 128], rhs=xeT[s_][:, c, :],
                                           start=(c == 0), stop=(c == 15))
                        return ins

                    def mu(e, slot=slot, s_=s_, bu=bu, hf=hf):
                        for c in range(16):
                            ins = e.matmul(psf[bu][:, 0:CAP], lhsT=wgu[slot][:, c, 256 + hf * 128:256 + (hf + 1) * 128], rhs=xeT[s_][:, c, :],
                                           start=(c == 0), stop=(c == 15))
                        return ins
                    P.op("pe", mg, reads=[res(f"wgu{slot}"), res(f"xeT{s_}")], writes=[bank[bg]])
                    P.op("pe", mu, reads=[res(f"wgu{slot}"), res(f"xeT{s_}")], writes=[bank[bu]])
                    P.op("dve", lambda e, bg=bg, cp=cp, e_=e_: e.tensor_scalar(
                        out=g1, in0=psf[bg][:, 0:CAP], scalar1=bgu_all[:, cp, e_:e_ + 1], scalar2=7.0, op0=ALU.add, op1=ALU.min),
                        reads=[bank[bg], res("bgu")], writes=[res("g1")])
                    P.op("act", lambda e: e.activation(out=sgm, in_=g1, func=AF.Sigmoid, scale=1.702),
                         reads=[res("g1")], writes=[res("sgm")])
                    P.op("act", lambda e, bu=bu, cp=cp, e_=e_: e.activation(
                        out=u1, in_=psf[bu][:, 0:CAP], func=AF.Identity, bias=bgu_all[:, 16 + cp, e_:e_ + 1]),
                        reads=[bank[bu], res("bgu")], writes=[res("u1")])
                    P.op("dve", lambda e: e.tensor_scalar(out=u1, in0=u1, scalar1=-7.0, scalar2=7.0, op0=ALU.max, op1=ALU.min),
                         reads=[res("u1")], writes=[res("u1")])
                    P.op("dve", lambda e: e.tensor_tensor(out=g1, in0=g1, in1=sgm, op=ALU.mult),
                         reads=[res("g1"), res("sgm")], writes=[res("g1")])
                    P.op("dve", lambda e, cp=cp: e.scalar_tensor_tensor(
                        out=actT[:, cp, :], in0=u1, scalar=1.0, in1=g1, op0=ALU.add, op1=ALU.mult),
                        reads=[res("u1"), res("g1")], writes=[res("actT")])
                    if hf == 1:
                        gi += 1
                if e_ + 1 < NEW:
                    transposes(e_ + 1)
                for nq in range(4):
                    slot = di % NDS
                    pump_dn(di + NDS)
                    cols = slice(nq * 512, (nq + 1) * 512)
                    ys = yst[nq % 2]
                    rys = res(f"yst{nq % 2}")
                    for m in range(NM):
                        b0 = (nq * NM + m) % 4

                        def md(e, slot=slot, m=m, b0=b0):
                            for c in range(16):
                                ins = e.matmul(psf[b0][:, :], lhsT=actT[:, c, m * 128:(m + 1) * 128], rhs=wdn[slot][:, c, :],
                                               start=(c == 0), stop=(c == 15))
                            return ins
                        P.op("pe", md, reads=[res(f"wdn{slot}"), res("actT")], writes=[bank[b0]])
                        P.op("dve", lambda e, b0=b0, cols=cols, m=m, ys=ys, bd_b=bd_b: e.tensor_tensor(
                            out=ys[:, m, :], in0=psf[b0][:, :], in1=bd_b[:, cols], op=ALU.add),
                            reads=[bank[b0], rbd], writes=[rys])
                    P.dma("sp", lambda e, e_=e_, cols=cols, ys=ys: e.dma_start(
                        out=ye_d[e_ * CAP:(e_ + 1) * CAP, cols].rearrange("(m p) n -> p m n", p=128), in_=ys),
                        reads=[rys], writes=[res("ye_d")])
                    di += 1
            P.barrier()

            P.dma("sp", lambda e: e.dma_start(out=x1, in_=x1_d), reads=[res("x1_d")], writes=[res("x1")])
            bload("sp", brd[0], mod_d[5 * D:6 * D], "brd0")
            yg = [A(A_HT + i * 2048, 2048) for i in range(2)]
            ctm = A(A_HT + 4096, 2048)
            P.op("pool", lambda e: e.memset(ctm, 0.0), writes=[res("ctm")])
            P.dma("sp", lambda e: e.dma_start(out=ye_d[NE * CAP:NE * CAP + 128, :], in_=ctm), reads=[res("ctm")], writes=[res("ye_d")])
            for i in range(NT):
                for k in range(TOPK):
                    j = i * TOPK + k
                    P.dma("pool", lambda e, i=i, k=k, j=j: e.indirect_dma_start(
                        out=yg[j % 2], out_offset=None, in_=ye_d,
                        in_offset=bass.IndirectOffsetOnAxis(ap=dest_all[:, i, k:k + 1], axis=0)),
                        reads=[res("ye_d"), res("dest")], writes=[res(f"yg{j % 2}")])
                    P.op("dve", lambda e, j=j: e.tensor_tensor(out=ctm, in0=yg[j % 2], in1=brd[0], op=ALU.mult),
                         reads=[res(f"yg{j % 2}"), res("brd0")], writes=[res("ctm")])
                    P.op("dve", lambda e, i=i, k=k: e.scalar_tensor_tensor(
                        out=x1[:, i, :], in0=ctm, scalar=w4_all[:, i, k:k + 1], in1=x1[:, i, :], op0=ALU.mult, op1=ALU.add),
                        reads=[res("ctm"), res("w4"), res("x1")], writes=[res("x1")])
            P.barrier()

            fg_b = A(A_TRIG, 2048)
            ot = [A(A_WB + i * 2048, 2048) for i in range(2)]
            junk = Ab(A_WB + 4096, 1024)
            bload("sp", fg_b, final_g, "fg_b")
            out_v = out.rearrange("(i p) d -> i p d", p=128)
            for i in range(NT):
                P.op("act", lambda e, i=i: e.activation(out=junk, in_=x1[:, i, :], func=AF.Square, accum_out=ssq2[:, i:i + 1]),
                     reads=[res("x1")], writes=[res("junk"), res("rs3")])
                P.op("act", lambda e, i=i: e.activation(out=rstd2[:, i:i + 1], in_=ssq2[:, i:i + 1], func=AF.Sqrt,
                                                       scale=1.0 / D, bias=col(C_EPS)),
                     reads=[res("rs3"), res("cst")], writes=[res("rs3")])
                P.op("dve", lambda e, i=i: e.reciprocal(out=rstd2[:, i:i + 1], in_=rstd2[:, i:i + 1]),
                     reads=[res("rs3")], writes=[res("rs3")])
                P.op("dve", lambda e, i=i: e.scalar_tensor_tensor(out=ot[i % 2], in0=x1[:, i, :], scalar=rstd2[:, i:i + 1], in1=fg_b,
                                                                   op0=ALU.mult, op1=ALU.mult),
                     reads=[res("x1"), res("rs3"), res("fg_b")], writes=[res(f"ot{i % 2}")])
                P.dma("sp", lambda e, i=i: e.dma_start(out=out_v[i], in_=ot[i % 2]), reads=[res(f"ot{i % 2}")])
            P.barrier()


        with nc.Block() as block:
            @block.sync
            def _(e):
                P.emit("sp", e)

            @block.scalar
            def _(e):
                P.emit("act", e)

            @block.vector
            def _(e):
                P.emit("dve", e)

            @block.gpsimd
            def _(e):
                P.emit("pool", e)

            @block.tensor
            def _(e):
                P.emit("pe", e)
    return nc


def _consts():
    c = np.zeros((128, C_N), np.float32)
    p = np.arange(128)
    c[:, C_IOTA_T:C_IOTA_T + 1024] = np.arange(1024)[None, :]
    c[:, C_IDENT:C_IDENT + 128] = np.eye(128)
    kt = p[:, None]
    qt = p[None, :]
    c[:, C_DIST:C_DIST + 128] = np.abs(kt - qt)
    c[:, C_ALLOW:C_ALLOW + 128] = ((kt // 64) <= (qt // 64))
    c[:, C_TRI:C_TRI + 128] = (kt < qt)
    c[:, C_ONES:C_ONES + 128] = 1.0
    c[:, C_IOTA_E:C_IOTA_E + 32] = np.arange(32)[None, :]
    c[:, C_IMOD] = p % 64
    c[:, C_SIGN] = np.where(p < 64, -1.0, 1.0)
    c[:, C_HALFPI] = np.pi / 2
    c[:, C_EPS] = EPS
    c[:, C_LNQ] = np.log(QSCALE)
    c[:, C_ONE] = 1.0
    return c


def _percore(j):
    t = np.zeros((128, PC_N), np.float32)
    p = np.arange(128)
    for qi in range(4):
        if qi < 3:
            s = 3 - qi
            valid = s <= j
            t[:, PC_BASE + qi] = (j - s) * 1024 if valid else 0
            for i in range(8):
                t[:, PC_E + qi * 8 + i] = (1024 * s - (128 * i + p)) if valid else 1.0e6
        else:
            t[:, PC_BASE + qi] = j * 1024
            for i in range(8):
                t[:, PC_E + qi * 8 + i] = -(128 * i + p)
    t[:, PC_FLAG] = 1.0 if j > 0 else 0.0
    return t


_NC_CACHE = {}


def make_in_maps(x, c, norm1_g, w_mod, b_mod, w_in, conv_w, w_out, norm2_g, w_router, b_router,
                 w_gate_up, b_gate_up, w_down, b_down, final_g):
    f = lambda a: np.ascontiguousarray(np.asarray(a, dtype=np.float32))
    x = f(x); c = f(c)
    shared = dict(
        cst=_consts(), norm1_g=f(norm1_g[0]), w_mod=f(w_mod[0]), b_mod=f(b_mod[0]), w_in=f(w_in[0]),
        conv_w=f(conv_w[0]), w_out=f(w_out[0]), norm2_g=f(norm2_g[0]), w_router=f(w_router[0]),
        b_router=f(b_router[0]), w_gate_up=f(w_gate_up[0]), b_gate_up=f(b_gate_up[0]),
        w_down=f(w_down[0]), b_down=f(b_down[0]), final_g=f(final_g))
    in_maps = []
    for core in range(8):
        b, j = core // 4, core % 4
        xq = np.zeros((4, T, D), np.float32)
        for qi in range(3):
            s = 3 - qi
            if s <= j:
                xq[qi] = x[b, (j - s) * T:(j - s + 1) * T]
        xq[3] = x[b, j * T:(j + 1) * T]
        m = dict(shared)
        m["x_q"] = xq
        m["c_pc"] = np.ascontiguousarray(c[b].reshape(128, 16))
        m["pcst"] = _percore(j)
        in_maps.append(m)
    return in_maps


def kernel(**inputs):
    if "nc" not in _NC_CACHE:
        _NC_CACHE["nc"] = build_nc()
    nc = _NC_CACHE["nc"]
    in_maps = make_in_maps(**inputs)
    res = run_bass_kernel_spmd(nc, in_maps, core_ids=list(range(8)))
    outs = [np.asarray(r["out"], dtype=np.float32) for r in res.results]
    full = np.stack(outs, 0).reshape(2, 4, T, D).reshape(2, 4 * T, D)
    return full
```

```python
import numpy as np
import concourse.bass as bass
import concourse.mybir as mybir
from concourse.bass_utils import run_bass_kernel_spmd

F32 = mybir.dt.float32
BF16 = mybir.dt.bfloat16
I32 = mybir.dt.int32
U32 = mybir.dt.uint32
AF = mybir.ActivationFunctionType
ALU = mybir.AluOpType

D = 2048
T = 1024
NT = 8
D_RET = 1024
D_CONV = 1024
NH = 8
HD = 128
D_IN = 7168
NE = 32
TOPK = 4
DFF = 2048
CAP = 512
EPS = 1e-6
QSCALE = float(HD ** -0.5)
LOG_GAMMA = [float(np.log1p(-2.0 ** (-5.0 - h))) for h in range(NH)]
TWO_PI_HI = 6.28125
TWO_PI_LO = 2.0 * np.pi - 6.28125
INV_2PI = float(1.0 / (2.0 * np.pi))
PI_LO = 3.1415925

C_IOTA_T = 0
C_IDENT = 1024
C_DIST = 1152
C_ALLOW = 1280
C_TRI = 1408
C_ONES = 1536
C_IOTA_E = 1664
C_IMOD = 1696
C_SIGN = 1697
C_HALFPI = 1698
C_EPS = 1699
C_LNQ = 1700
C_ONE = 1701
C_N = 1704
PC_BASE = 0
PC_E = 4
PC_FLAG = 36
PC_N = 40

A_CONST = 0
A_TRIG = 3328
A_BRD = A_TRIG + 2048
A_SF32 = A_BRD + 4096
A_HT = A_SF32 + 1024
A_WB = A_HT + 8192
A_KT = A_WB + 6144
A_V = A_KT + 4096
A_SBF = A_V + 4096
A_SCR = A_SBF + 4096
A_YTR = A_SCR + 10240
A_END = A_YTR + 5120


class Res:
    __slots__ = ("name", "w", "r")

    def __init__(self, name):
        self.name = name
        self.w = None
        self.r = []


class Q:
    def __init__(self, name, sem):
        self.name = name
        self.sem = sem
        self.n = 0
        self.waited = {}
        self.ops = []
        self.chans = []
        self.ci = 0


class Prog:
    def __init__(self, nc):
        self.nc = nc
        self.sems = []
        self.q = {}

    def add_queue(self, name, sem, chan_sems=()):
        q = Q(name, len(self.sems))
        self.sems.append(sem)
        for cs in chan_sems:
            q.chans.append([len(self.sems), 0])
            self.sems.append(cs)
        self.q[name] = q

    def _collect(self, q, reads, writes):
        need = {}

        def add(ev, is_war):
            if ev is None:
                return
            k, val, eng = ev
            if eng == q.name:
                if q.name == "pe":
                    return
                if is_war:
                    return
            if q.waited.get(k, 0) >= val:
                return
            if need.get(k, 0) < val:
                need[k] = val

        for r in reads:
            add(r.w, False)
        for w in writes:
            add(w.w, False)
            for e in w.r:
                add(e, True)
        return need

    def _commit(self, q, need):
        for k, val in need.items():
            q.waited[k] = val
        return [(k, v) for k, v in need.items()]

    def op(self, qn, fn, reads=(), writes=()):
        q = self.q[qn]
        need = self._collect(q, reads, writes)
        waits = self._commit(q, need)
        q.n += 1
        ev = (q.sem, q.n, q.name)
        q.ops.append((waits, fn, q.sem, 1))
        for r in reads:
            r.r.append(ev)
        for w in writes:
            w.w = ev
            w.r = []
        return ev

    def dma(self, qn, fn, reads=(), writes=()):
        q = self.q[qn]
        need = self._collect(q, reads, writes)
        ch = q.chans[q.ci]
        q.ci = (q.ci + 1) % len(q.chans)
        if ch[1] > 0 and q.waited.get(ch[0], 0) < 16 * ch[1]:
            if need.get(ch[0], 0) < 16 * ch[1]:
                need[ch[0]] = 16 * ch[1]
        waits = self._commit(q, need)
        ch[1] += 1
        ev = (ch[0], 16 * ch[1], "dma")
        q.ops.append((waits, fn, ch[0], 16))
        for r in reads:
            r.r.append(ev)
        for w in writes:
            w.w = ev
            w.r = []
        return ev

    def barrier(self):
        evs = []
        for q in self.q.values():
            if q.n > 0:
                evs.append((q.sem, q.n))
            for ch in q.chans:
                if ch[1] > 0:
                    evs.append((ch[0], 16 * ch[1]))
        for q in self.q.values():
            need = {}
            for k, val in evs:
                if k == q.sem and q.name != "pe" and False:
                    continue
                if q.waited.get(k, 0) < val:
                    need[k] = val
            waits = self._commit(q, need)
            if waits:
                q.ops.append((waits, None, None, 0))

    def emit(self, qn, eng):
        q = self.q[qn]
        for waits, fn, semk, inc in q.ops:
            for k, val in waits:
                eng.wait_ge(self.sems[k], val)
            if fn is not None:
                ins = fn(eng)
                ins.then_inc(self.sems[semk], inc)


def build_nc(stage="full"):
    nc = bass.Bass("TRN2", target_bir_lowering=False)
    NEW = NE if stage == "full" else 1

    def din(name, shape, dt=F32):
        return nc.dram_tensor(name, list(shape), dt, kind="ExternalInput").ap()

    x_q = din("x_q", [4, T, D])
    c_pc = din("c_pc", [128, 16])
    cst = din("cst", [128, C_N])
    pcst = din("pcst", [128, PC_N])
    norm1_g = din("norm1_g", [D])
    w_mod = din("w_mod", [D, 6 * D])
    b_mod = din("b_mod", [6 * D])
    w_in = din("w_in", [D, D_IN])
    conv_w = din("conv_w", [3, D_CONV])
    w_out = din("w_out", [D, D])
    norm2_g = din("norm2_g", [D])
    w_router = din("w_router", [D, NE])
    b_router = din("b_router", [NE])
    w_gate_up = din("w_gate_up", [NEW, D, 2 * DFF])
    b_gate_up = din("b_gate_up", [NE, 2 * DFF])
    w_down = din("w_down", [NEW, DFF, D])
    b_down = din("b_down", [NE, D])
    final_g = din("final_g", [D])
    out = nc.dram_tensor("out", [T, D], F32, kind="ExternalOutput").ap()
    mod_d = nc.dram_tensor("mod_d", [6 * D], F32, kind="Internal").ap()
    x1_d = nc.dram_tensor("x1_d", [128, NT, D], F32, kind="Internal").ap()
    xe_d = nc.dram_tensor("xe_d", [NE * CAP + 128, D], BF16, kind="Internal").ap()
    ye_d = nc.dram_tensor("ye_d", [NE * CAP + 128, D], F32, kind="Internal").ap()

    w_in_v = w_in.rearrange("(c p) n -> p c n", p=128)
    w_out_v = w_out.rearrange("(c p) n -> p c n", p=128)

    from contextlib import ExitStack
    es = ExitStack()
    with es:
        arena = es.enter_context(nc.sbuf_tensor("arena", [128, A_END], F32))
        psf = [es.enter_context(nc.psum_tensor(f"ps{i}", [128, 512], F32)) for i in range(8)]
        nsem = 5 + 8 + 6 + 2
        sems = [es.enter_context(nc.semaphore(f"s{i}")) for i in range(nsem)]
        P = Prog(nc)
        P.add_queue("pe", sems[0])
        P.add_queue("act", sems[1], sems[19:21])
        P.add_queue("dve", sems[2])
        P.add_queue("pool", sems[3], sems[5:13])
        P.add_queue("sp", sems[4], sems[13:19])

        def A(off, n):
            return arena[:, off:off + n]

        def Ab(off, nwords):
            return arena[:, off:off + nwords].bitcast(BF16)

        def psb(i):
            return psf[i][:, :].bitcast(BF16)

        bank = [Res(f"bank{i}") for i in range(8)]

        cst_sb = A(A_CONST, C_N)
        pc_sb = A(A_CONST + C_N, PC_N)
        o = A_CONST + C_N + PC_N
        ident_bf = Ab(o, 64); o += 64
        maskT = A(o, 1024).rearrange("p (h k) -> p h k", h=NH); o += 1024
        kdec_tab = A(o, 256).rearrange("p (a h) -> p a h", h=NH); o += 256
        freq = A(o, 1); o += 1
        c_sb = A(o, 16); o += 16
        c_act = A(o, 16); o += 16
        cw_sb = A(o, 24).rearrange("p (k c) -> p k c", k=3); o += 24
        small = A(o, 64); o += 64
        hT_halo = Ab(o, 16).rearrange("p (c t) -> p c t", c=16); o += 16
        assert o <= A_TRIG, o
        iota_t = cst_sb[:, C_IOTA_T:C_IOTA_T + 1024]
        ident_f = cst_sb[:, C_IDENT:C_IDENT + 128]
        dist = cst_sb[:, C_DIST:C_DIST + 128]
        allow = cst_sb[:, C_ALLOW:C_ALLOW + 128]

        def col(c):
            return cst_sb[:, c:c + 1]

        cos_t = A(A_TRIG, 1024)
        sin_t = A(A_TRIG + 1024, 1024)
        brd = [A(A_BRD, 2048), A(A_BRD + 2048, 2048)]
        S_f32 = A(A_SF32, 1024).rearrange("p (h e) -> p h e", h=NH)
        hT = Ab(A_HT, 8192).rearrange("p (c t) -> p c t", c=16)
        wb = [Ab(A_WB + i * 3072, 3072) for i in range(2)]
        kT = Ab(A_KT, 4096).rearrange("p (h t) -> p h t", h=NH)
        v_sb = Ab(A_V, 4096).rearrange("p (i n) -> p i n", i=NT)
        S_bf = Ab(A_SBF, 4096).rearrange("p (i h e) -> p i h e", i=NT, h=NH)
        yTr = Ab(A_YTR, 4096).rearrange("p (c t) -> p c t", c=8)
        yTc = Ab(A_KT, 4096).rearrange("p (c t) -> p c t", c=8)

        R = {}

        def res(name):
            if name not in R:
                R[name] = Res(name)
            return R[name]

        P.dma("sp", lambda e: e.dma_start(out=cst_sb, in_=cst), writes=[res("cst")])
        P.dma("sp", lambda e: e.dma_start(out=pc_sb, in_=pcst), writes=[res("pcst")])
        P.dma("sp", lambda e: e.dma_start(out=c_sb, in_=c_pc), writes=[res("c_sb")])
        P.dma("sp", lambda e: e.dma_start(
            out=cw_sb, in_=conv_w.rearrange("k (c p) -> p k c", p=128),
            allow_slow_non_contiguous=True), writes=[res("cw")])
        P.op("dve", lambda e: e.tensor_copy(out=ident_bf, in_=ident_f), reads=[res("cst")], writes=[res("ident_bf")])
        P.op("act", lambda e: e.activation(out=freq, in_=col(C_IMOD), func=AF.Exp,
                                           scale=float(-np.log(10000.0) / 64.0)),
             reads=[res("cst")], writes=[res("freq")])
        for h in range(NH):
            P.op("act", lambda e, h=h: e.activation(out=maskT[:, h, :], in_=dist, func=AF.Exp, scale=LOG_GAMMA[h]),
                 reads=[res("cst")], writes=[res("maskT")])
        P.op("dve", lambda e: e.tensor_tensor(
            out=maskT, in0=maskT, in1=allow.unsqueeze(1).broadcast_to([128, NH, 128]), op=ALU.mult),
            reads=[res("maskT"), res("cst")], writes=[res("maskT")])
        for h in range(NH):
            P.op("act", lambda e, h=h: e.activation(
                out=kdec_tab[:, :, h], in_=pc_sb[:, PC_E:PC_E + 32], func=AF.Exp, scale=LOG_GAMMA[h]),
                reads=[res("pcst")], writes=[res("kdec")])
        P.op("act", lambda e: e.activation(out=c_act, in_=c_sb, func=AF.Silu), reads=[res("c_sb")], writes=[res("c_act")])

        wm = [A(A_HT + i * 8192, 8192).rearrange("p (c n) -> p c n", c=16) for i in range(2)]
        modrow = arena[0:1, A_V:A_V + 12288]
        w_mod_v = w_mod.rearrange("(p c) n -> p c n", c=16)
        P.dma("sp", lambda e: e.dma_start(out=modrow, in_=b_mod.rearrange("(o n) -> o n", o=1)), writes=[res("modrow")])
        for nb in range(24):
            wres = res(f"wm{nb % 2}")
            P.dma("sp" if nb % 2 == 0 else "act", lambda e, nb=nb: e.dma_start(out=wm[nb % 2], in_=w_mod_v[:, :, nb * 512:(nb + 1) * 512]),
                  writes=[wres])

            def mm(e, nb=nb):
                for c in range(16):
                    ins = e.matmul(psf[nb % 2][0:1, :], lhsT=c_act[:, c:c + 1], rhs=wm[nb % 2][:, c, :],
                                   start=(c == 0), stop=(c == 15))
                return ins
            P.op("pe", mm, reads=[wres, res("c_act")], writes=[bank[nb % 2]])
            P.op("dve", lambda e, nb=nb: e.tensor_tensor(
                out=modrow[:, nb * 512:(nb + 1) * 512], in0=psf[nb % 2][0:1, :],
                in1=modrow[:, nb * 512:(nb + 1) * 512], op=ALU.add),
                reads=[bank[nb % 2], res("modrow")], writes=[res("modrow")])
        P.dma("sp", lambda e: e.dma_start(out=mod_d.rearrange("(o n) -> o n", o=1), in_=modrow),
              reads=[res("modrow")], writes=[res("mod_d")])
        P.barrier()

        def bload(qn, dst, src_row, rname):
            return P.dma(qn, lambda e: e.dma_start(out=dst, in_=src_row.partition_broadcast(128)),
                         reads=[res("mod_d")], writes=[res(rname)])

        tmpb = A(A_SCR, 2048)
        bload("sp", brd[0], norm1_g, "brd0")
        bload("sp", tmpb, mod_d[D:2 * D], "tmpb")
        bload("sp", brd[1], mod_d[0:D], "brd1")
        P.op("dve", lambda e: e.scalar_tensor_tensor(out=brd[0], in0=tmpb, scalar=1.0, in1=brd[0],
                                                     op0=ALU.add, op1=ALU.mult),
             reads=[res("tmpb"), res("brd0")], writes=[res("brd0")])
        P.barrier()

        xs = [A(A_SCR + i * 2048, 2048) for i in range(2)]
        tmpf = A(A_SCR + 4096, 2048)
        hbf = [Ab(A_SCR + 6144 + i * 1024, 1024) for i in range(2)]
        t12 = [A(A_SCR + 8192 + i * 512, 512) for i in range(4)]
        ssq = small[:, 0:8]
        rstd = small[:, 8:16]

        def trig_tables(qi):
            ang = xs[0][:, 0:1024]
            kf = xs[0][:, 1024:2048]
            ki = kf.bitcast(I32)
            rr = xs[1][:, 0:1024]
            ab = xs[1][:, 1024:2048]
            rs_ = [res("xs0"), res("xs1")]
            P.op("dve", lambda e: e.tensor_scalar(out=ang, in0=iota_t, scalar1=pc_sb[:, PC_BASE + qi:PC_BASE + qi + 1],
                                                  scalar2=freq, op0=ALU.add, op1=ALU.mult),
                 reads=[res("cst"), res("pcst"), res("freq")], writes=[rs_[0]])
            P.op("dve", lambda e: e.tensor_scalar(out=rr.bitcast(I32), in0=ang, scalar1=INV_2PI, scalar2=None, op0=ALU.mult),
                 reads=[rs_[0]], writes=[rs_[1]])
            P.op("dve", lambda e: e.tensor_copy(out=kf, in_=rr.bitcast(I32)), reads=[rs_[1]], writes=[rs_[0]])
            P.op("dve", lambda e: e.scalar_tensor_tensor(out=rr, in0=kf, scalar=-TWO_PI_HI, in1=ang, op0=ALU.mult, op1=ALU.add),
                 reads=[rs_[0]], writes=[rs_[1]])
            P.op("dve", lambda e: e.scalar_tensor_tensor(out=rr, in0=kf, scalar=-TWO_PI_LO, in1=rr, op0=ALU.mult, op1=ALU.add),
                 reads=[rs_[0], rs_[1]], writes=[rs_[1]])
            P.op("dve", lambda e: e.tensor_scalar(out=rr, in0=rr, scalar1=PI_LO, scalar2=-PI_LO, op0=ALU.min, op1=ALU.max),
                 reads=[rs_[1]], writes=[rs_[1]])
            P.op("dve", lambda e: e.scalar_tensor_tensor(out=ab, in0=rr, scalar=-1.0, in1=rr, op0=ALU.mult, op1=ALU.max),
                 reads=[rs_[1]], writes=[rs_[1]])
            P.op("act", lambda e: e.activation(out=sin_t, in_=rr, func=AF.Sin, scale=col(C_SIGN)),
                 reads=[rs_[1], res("cst")], writes=[res("trig")])
            P.op("act", lambda e: e.activation(out=cos_t, in_=ab, func=AF.Sin, scale=-1.0, bias=col(C_HALFPI)),
                 reads=[rs_[1], res("cst")], writes=[res("trig")])

        hTB_lo = Ab(A_SBF, 4096).rearrange("p (c t) -> p c t", c=8)
        hTB_hi = Ab(A_YTR, 4096).rearrange("p (c t) -> p c t", c=8)

        class HBuf:
            def __init__(self, which):
                self.which = which
                self.r = res("hT" if which == "A" else "hTB")

            def chunk(self, c, tok):
                if self.which == "A":
                    return hT[:, c, tok]
                return (hTB_lo if c < 8 else hTB_hi)[:, c % 8, tok]

            def lo(self, tok):
                return hT[:, 0:8, tok] if self.which == "A" else hTB_lo[:, :, tok]

            def hi(self, tok):
                return hT[:, 8:16, tok] if self.which == "A" else hTB_hi[:, :, tok]

        HA, HB = HBuf("A"), HBuf("B")

        def p1_tile(qi, i, hb):
            x_src = x_q[qi].rearrange("(i p) d -> i p d", p=128)
            if True:
                xr = res(f"xs{i % 2}")
                hr = res(f"hbf{i % 2}")
                P.dma("sp", lambda e, i=i: e.dma_start(out=xs[i % 2], in_=x_src[i]), writes=[xr])
                P.op("act", lambda e, i=i: e.activation(out=hbf[i % 2], in_=xs[i % 2], func=AF.Square,
                                                       accum_out=ssq[:, i:i + 1]),
                     reads=[xr], writes=[hr, res(f"ssq{i}")])
                P.op("act", lambda e, i=i: e.activation(out=rstd[:, i:i + 1], in_=ssq[:, i:i + 1], func=AF.Sqrt,
                                                       scale=1.0 / D, bias=col(C_EPS)),
                     reads=[res(f"ssq{i}"), res("cst")], writes=[res(f"rstd{i}")])
                P.op("dve", lambda e, i=i: e.reciprocal(out=rstd[:, i:i + 1], in_=rstd[:, i:i + 1]),
                     reads=[res(f"rstd{i}")], writes=[res(f"rstd{i}")])
                P.op("dve", lambda e, i=i: e.scalar_tensor_tensor(out=tmpf, in0=xs[i % 2], scalar=rstd[:, i:i + 1],
                                                                   in1=brd[0], op0=ALU.mult, op1=ALU.mult),
                     reads=[xr, res(f"rstd{i}"), res("brd0")], writes=[res("tmpf")])
                P.op("pool", lambda e, i=i: e.tensor_tensor(out=hbf[i % 2], in0=tmpf, in1=brd[1], op=ALU.add),
                     reads=[res("tmpf"), res("brd1")], writes=[hr])
                pb = 2 * (i % 2)

                def tr(e, i=i, pb=pb):
                    for c in range(16):
                        ins = e.transpose(out=psb(pb + c // 8)[:, (c % 8) * 128:(c % 8 + 1) * 128],
                                          in_=hbf[i % 2][:, c * 128:(c + 1) * 128], identity=ident_bf)
                    return ins
                P.op("pe", tr, reads=[hr, res("ident_bf")], writes=[bank[pb], bank[pb + 1]])
                P.op("act", lambda e, i=i, pb=pb: e.activation(
                    out=hb.lo(slice(i * 128, (i + 1) * 128)), in_=psb(pb).rearrange("p (c t) -> p c t", c=8), func=AF.Copy),
                    reads=[bank[pb]], writes=[hb.r])
                P.op("dve", lambda e, i=i, pb=pb: e.tensor_copy(
                    out=hb.hi(slice(i * 128, (i + 1) * 128)), in_=psb(pb + 1).rearrange("p (c t) -> p c t", c=8)),
                    reads=[bank[pb + 1]], writes=[hb.r])

        def load_w(slot, cols0, ncols, swap=False):
            wr = res(f"wb{slot}")
            if not swap:
                dst = wb[slot][:, 0:16 * ncols].rearrange("p (c n) -> p c n", c=16)
                P.dma("pool", lambda e: e.dma_start(out=dst, in_=w_in_v[:, :, cols0:cols0 + ncols]), writes=[wr])
            else:
                dstv = wb[slot][:, 0:16 * 384].rearrange("p (c h n) -> p c h n", c=16, h=2)
                srcv = w_in_v[:, :, cols0:cols0 + 256].rearrange("p c (h d) -> p c h d", h=2)
                for hh in range(2):
                    P.dma("pool", lambda e, hh=hh: e.dma_start(out=dstv[:, :, hh, 64:192], in_=srcv[:, :, hh, :]), writes=[wr])
                    P.dma("pool", lambda e, hh=hh: e.dma_start(out=dstv[:, :, hh, 0:64], in_=srcv[:, :, hh, 64:128]), writes=[wr])
            return wr

        def rope_block(slot, h0, is_q, qdst=None, qtdst=None, decq=None, hb=None):
            hb = hb or HA
            wr = res(f"wb{slot}")
            wv = wb[slot][:, 0:16 * 384].rearrange("p (c h n) -> p c h n", c=16, h=2)
            for hh in range(2):
                for th in range(2):
                    ba, bb = 4 + 2 * ((hh * 2 + th) % 2), 5 + 2 * ((hh * 2 + th) % 2)
                    ta, tb = t12[2 * ((hh * 2 + th) % 2)], t12[2 * ((hh * 2 + th) % 2) + 1]
                    tra, trb = res(f"t12_{2 * ((hh * 2 + th) % 2)}"), res(f"t12_{2 * ((hh * 2 + th) % 2) + 1}")
                    tok = slice(th * 512, (th + 1) * 512)

                    def mma(e, hh=hh, tok=tok, ba=ba):
                        for c in range(16):
                            ins = e.matmul(psf[ba][:, :], lhsT=wv[:, c, hh, 64:192], rhs=hb.chunk(c, tok),
                                           start=(c == 0), stop=(c == 15))
                        return ins

                    def mmb(e, hh=hh, tok=tok, bb=bb):
                        for c in range(16):
                            ins = e.matmul(psf[bb][:, :], lhsT=wv[:, c, hh, 0:128], rhs=hb.chunk(c, tok),
                                           start=(c == 0), stop=(c == 15))
                        return ins
                    P.op("pe", mma, reads=[wr, hb.r], writes=[bank[ba]])
                    P.op("pe", mmb, reads=[wr, hb.r], writes=[bank[bb]])
                    P.op("dve", lambda e, ta=ta, ba=ba, tok=tok: e.tensor_tensor(out=ta, in0=psf[ba][:, :], in1=cos_t[:, tok], op=ALU.mult),
                         reads=[bank[ba], res("trig")], writes=[tra])
                    P.op("dve", lambda e, tb=tb, bb=bb, tok=tok: e.tensor_tensor(out=tb, in0=psf[bb][:, :], in1=sin_t[:, tok], op=ALU.mult),
                         reads=[bank[bb], res("trig")], writes=[trb])
                    if not is_q:
                        P.op("pool", lambda e, ta=ta, tb=tb, hh=hh, tok=tok: e.tensor_tensor(
                            out=kT[:, h0 + hh, tok], in0=ta, in1=tb, op=ALU.add),
                            reads=[tra, trb], writes=[res("kT")])
                    else:
                        P.op("pool", lambda e, ta=ta, tb=tb: e.tensor_tensor(out=ta, in0=ta, in1=tb, op=ALU.add),
                             reads=[tra, trb], writes=[tra])
                        P.op("act", lambda e, ta=ta, hh=hh, tok=tok: e.activation(out=qdst[:, hh, tok], in_=ta, func=AF.Copy, scale=QSCALE),
                             reads=[tra], writes=[res("qT")])
                        P.op("dve", lambda e, ta=ta, hh=hh, tok=tok: e.tensor_tensor(out=qtdst[:, hh, tok], in0=ta, in1=decq[hh][:, tok], op=ALU.mult),
                             reads=[tra, res(f"decq{hh}")], writes=[res("qtT")])

        def v_block(slot, vb, hb=None):
            hb = hb or HA
            wr = res(f"wb{slot}")
            wv = wb[slot][:, 0:16 * 256].rearrange("p (c n) -> p c n", c=16)
            for i in range(NT):
                b = 4 + (i % 4)

                def mm(e, i=i, b=b):
                    for c in range(16):
                        ins = e.matmul(psf[b][:, 0:256], lhsT=hb.chunk(c, slice(i * 128, (i + 1) * 128)), rhs=wv[:, c, :],
                                       start=(c == 0), stop=(c == 15))
                    return ins
                P.op("pe", mm, reads=[wr, hb.r], writes=[bank[b]])
                P.op("act", lambda e, i=i, b=b: e.activation(out=v_sb[:, i, vb * 256:(vb + 1) * 256], in_=psf[b][:, 0:256], func=AF.Copy),
                     reads=[bank[b]], writes=[res("v")])

        kdec_sb = Ab(A_SCR + 4096, 512).rearrange("p (h d) -> p h d", h=NH)

        def state_phase(qi, main):
            for i in range(NT):
                def tr(e, i=i):
                    for h in range(NH):
                        ins = e.transpose(out=psb(0)[:, h * 128:(h + 1) * 128], in_=kT[:, h, i * 128:(i + 1) * 128],
                                          identity=ident_bf)
                    return ins
                P.op("pe", tr, reads=[res("kT"), res("ident_bf")], writes=[bank[0]])
                P.op("dve", lambda e, i=i: e.tensor_tensor(
                    out=kdec_sb, in0=psb(0).rearrange("p (h d) -> p h d", h=NH),
                    in1=kdec_tab[:, qi * 8 + i, :].unsqueeze(2).broadcast_to([128, NH, 128]), op=ALU.mult),
                    reads=[bank[0], res("kdec")], writes=[res("tmpf")])

                def inc(e, i=i):
                    for h in range(NH):
                        ins = e.matmul(psf[2 + h // 4][:, (h % 4) * 128:(h % 4 + 1) * 128], lhsT=kdec_sb[:, h, :],
                                       rhs=v_sb[:, i, h * 128:(h + 1) * 128], start=True, stop=True)
                    return ins
                P.op("pe", inc, reads=[res("tmpf"), res("v")], writes=[bank[2], bank[3]])
                if main:
                    P.op("act", lambda e, i=i: e.activation(out=S_bf[:, i, :, :], in_=S_f32, func=AF.Copy),
                         reads=[res("S")], writes=[res("S_bf")])
                for hb in range(2):
                    P.op("dve", lambda e, hb=hb: e.tensor_tensor(
                        out=S_f32[:, hb * 4:(hb + 1) * 4, :], in0=S_f32[:, hb * 4:(hb + 1) * 4, :],
                        in1=psf[2 + hb][:, :].rearrange("p (h e) -> p h e", h=4), op=ALU.add),
                        reads=[bank[2 + hb], res("S")], writes=[res("S")])

        P.op("pool", lambda e: e.memset(S_f32, 0.0), writes=[res("S")])

        hbufs = [HB, HA, HB, HA]
        trig_tables(0)
        for i in range(NT):
            p1_tile(0, i, hbufs[0])
        for qi in range(4):
            main = qi == 3
            hb = hbufs[qi]
            if qi == 2:
                P.op("dve", lambda e: e.tensor_copy(out=hT_halo[:, 0:8, :], in_=hTB_lo[:, :, 1022:1024]),
                     reads=[HB.r], writes=[res("hT_halo")])
                P.op("dve", lambda e: e.tensor_copy(out=hT_halo[:, 8:16, :], in_=hTB_hi[:, :, 1022:1024]),
                     reads=[HB.r], writes=[res("hT_halo")])
            blocks = [("k", g) for g in range(4)] + [("v", g) for g in range(4)]
            load_w(0, D_RET + 0, 256, swap=True)
            for bi, (kind, g) in enumerate(blocks):
                slot = bi % 2
                if bi + 1 < len(blocks):
                    nk, ng = blocks[bi + 1]
                    if nk == "k":
                        load_w((bi + 1) % 2, D_RET + ng * 256, 256, swap=True)
                    else:
                        load_w((bi + 1) % 2, 2 * D_RET + ng * 256, 256)
                if kind == "k":
                    rope_block(slot, 2 * g, False, hb=hb)
                else:
                    v_block(slot, g, hb=hb)
                if qi < 3:
                    if bi == 4:
                        trig_tables(qi + 1)
                    p1_tile(qi + 1, bi, hbufs[qi + 1])
            state_phase(qi, main)
        P.barrier()

        qT = Ab(A_SCR + 0, 1024).rearrange("p (h t) -> p h t", h=2)
        qtT = Ab(A_SCR + 1024, 1024).rearrange("p (h t) -> p h t", h=2)
        sg = Ab(A_SCR + 2048, 1024).rearrange("p (i n) -> p i n", i=NT)
        decq = [A(A_SCR + 3072 + i * 1024, 1024) for i in range(2)]
        Pm = [Ab(A_SCR + 5120 + i * 128, 128).rearrange("p (h k) -> p h k", h=2) for i in range(2)]
        onb = [A(A_SCR + 5376 + i * 256, 256).rearrange("p (h k) -> p h k", h=2) for i in range(2)]
        yrb = [Ab(A_SCR + 5888 + i * 128, 128).rearrange("p (h k) -> p h k", h=2) for i in range(2)]
        bst = A(A_SCR + 6144, 32).rearrange("p (a h s) -> p a h s", a=2, h=2)
        bmv = A(A_SCR + 6176, 16).rearrange("p (a h s) -> p a h s", a=2, h=2)
        brs = A(A_SCR + 6192, 4).rearrange("p (a h) -> p a h", a=2)

        for g in range(4):
            for hh in range(2):
                P.op("act", lambda e, hh=hh, g=g: e.activation(out=decq[hh], in_=iota_t, func=AF.Exp,
                                                           scale=LOG_GAMMA[2 * g + hh], bias=col(C_LNQ)),
                     reads=[res("cst")], writes=[res(f"decq{hh}")])
            if g == 0:
                load_w(0, 0, 256, swap=True)
                load_w(1, 3 * D_RET, 256)
            rope_block(0, 2 * g, True, qdst=qT, qtdst=qtT, decq=decq)
            if g + 1 < 4:
                load_w(0, 2 * (g + 1) * 128, 256, swap=True)
            wgv = wb[1][:, 0:16 * 256].rearrange("p (c n) -> p c n", c=16)
            for i in range(NT):
                b = i % 2

                def mmg(e, i=i, b=b):
                    for c in range(16):
                        ins = e.matmul(psf[b][:, 0:256], lhsT=hT[:, c, i * 128:(i + 1) * 128], rhs=wgv[:, c, :],
                                       start=(c == 0), stop=(c == 15))
                    return ins
                P.op("pe", mmg, reads=[res("wb1"), res("hT")], writes=[bank[b]])
                P.op("act", lambda e, i=i, b=b: e.activation(out=sg[:, i, :], in_=psf[b][:, 0:256], func=AF.Silu),
                     reads=[bank[b]], writes=[res("sg")])
            if g + 1 < 4:
                load_w(1, 3 * D_RET + (g + 1) * 256, 256)
            for i in range(NT):
                a = i % 2
                tok = slice(i * 128, (i + 1) * 128)
                rPm, ron, ryr, rst = res(f"Pm{a}"), res(f"on{a}"), res(f"yr{a}"), res(f"bst{a}")

                def sc(e, g=g, tok=tok, a=a):
                    for hh in range(2):
                        ins = e.matmul(psf[2 + a][:, hh * 128:(hh + 1) * 128], lhsT=kT[:, 2 * g + hh, tok], rhs=qT[:, hh, tok],
                                       start=True, stop=True)
                    return ins
                P.op("pe", sc, reads=[res("kT"), res("qT")], writes=[bank[2 + a]])
                P.op("dve", lambda e, g=g, a=a: e.tensor_tensor(
                    out=Pm[a], in0=psf[2 + a][:, 0:256].rearrange("p (h k) -> p h k", h=2),
                    in1=maskT[:, 2 * g:2 * g + 2, :], op=ALU.mult),
                    reads=[bank[2 + a], res("maskT")], writes=[rPm])

                def om(e, g=g, i=i, tok=tok, a=a):
                    for hh in range(2):
                        h = 2 * g + hh
                        e.matmul(psf[4 + a][:, hh * 128:(hh + 1) * 128], lhsT=Pm[a][:, hh, :],
                                 rhs=v_sb[:, i, h * 128:(h + 1) * 128], start=True, stop=False)
                        ins = e.matmul(psf[4 + a][:, hh * 128:(hh + 1) * 128], lhsT=qtT[:, hh, tok],
                                       rhs=S_bf[:, i, h, :], start=False, stop=True)
                    return ins
                P.op("pe", om, reads=[rPm, res("v"), res("qtT"), res("S_bf")], writes=[bank[4 + a]])
                for hh in range(2):
                    P.op("dve", lambda e, hh=hh, a=a: e.bn_stats(out=bst[:, a, hh, 0:6], in_=psf[4 + a][:, hh * 128:(hh + 1) * 128]),
                         reads=[bank[4 + a]], writes=[rst])
                for hh in range(2):
                    P.op("dve", lambda e, hh=hh, a=a: e.bn_aggr(out=bmv[:, a, hh, 0:2], in_=bst[:, a, hh, 0:6]),
                         reads=[rst], writes=[rst])
                P.op("act", lambda e, a=a: e.activation(out=brs[:, a, :], in_=bmv[:, a, :, 1], func=AF.Sqrt,
                                                        bias=col(C_EPS)),
                     reads=[rst, res("cst")], writes=[res(f"brs{a}")])
                P.op("dve", lambda e, a=a: e.reciprocal(out=brs[:, a, :], in_=brs[:, a, :]),
                     reads=[res(f"brs{a}")], writes=[res(f"brs{a}")])
                for hh in range(2):
                    P.op("dve", lambda e, hh=hh, a=a: e.tensor_scalar(
                        out=onb[a][:, hh, :], in0=psf[4 + a][:, hh * 128:(hh + 1) * 128],
                        scalar1=bmv[:, a, hh, 0:1], scalar2=brs[:, a, hh:hh + 1], op0=ALU.subtract, op1=ALU.mult),
                        reads=[bank[4 + a], rst, res(f"brs{a}")], writes=[ron])
                P.op("pool", lambda e, i=i, a=a: e.tensor_tensor(
                    out=yrb[a], in0=onb[a], in1=sg[:, i, :].rearrange("p (h k) -> p h k", h=2), op=ALU.mult),
                    reads=[ron, res("sg")], writes=[ryr])

                def trY(e, a=a):
                    for hh in range(2):
                        ins = e.transpose(out=psb(6 + a)[:, hh * 128:(hh + 1) * 128], in_=yrb[a][:, hh, :], identity=ident_bf)
                    return ins
                P.op("pe", trY, reads=[ryr, res("ident_bf")], writes=[bank[6 + a]])
                P.op("act", lambda e, g=g, tok=tok, a=a: e.activation(
                    out=yTr[:, 2 * g:2 * g + 2, tok], in_=psb(6 + a)[:, 0:256].rearrange("p (h k) -> p h k", h=2), func=AF.Copy),
                    reads=[bank[6 + a]], writes=[res("yTr")])
        P.barrier()

        uT = A(A_SCR + 0, 2056)[:, 0:2052].rearrange("p (c t) -> p c t", c=2)
        acc = A(A_SCR + 2056, 2048).rearrange("p (c t) -> p c t", c=2)
        BASE_B, BASE_C, BASE_U = 4 * D_RET, 4 * D_RET + D_CONV, 4 * D_RET + 2 * D_CONV

        def conv_mm(slot, cc, with_halo):
            wv = wb[slot][:, 0:16 * 256].rearrange("p (c n) -> p c n", c=16)
            for th in range(2):
                b = cc * 2 + th

                def mm(e, b=b, th=th):
                    for c in range(16):
                        ins = e.matmul(psf[b][:, :], lhsT=wv[:, c, cc * 128:(cc + 1) * 128], rhs=hT[:, c, th * 512:(th + 1) * 512],
                                       start=(c == 0), stop=(c == 15))
                    return ins
                P.op("pe", mm, reads=[res(f"wb{slot}"), res("hT")], writes=[bank[b]])
            if with_halo:
                def mmh(e):
                    for c in range(16):
                        ins = e.matmul(psf[4 + cc][:, 0:2], lhsT=wv[:, c, cc * 128:(cc + 1) * 128], rhs=hT_halo[:, c, :],
                                       start=(c == 0), stop=(c == 15))
                    return ins
                P.op("pe", mmh, reads=[res(f"wb{slot}"), res("hT_halo")], writes=[bank[4 + cc]])

        cjobs = []
        for cg in range(4):
            cjobs += [("u", cg, BASE_U + cg * 256), ("c", cg, BASE_C + cg * 256), ("b", cg, BASE_B + cg * 256)]
        load_w(0, cjobs[0][2], 256)
        for ji, (kind, cg, cols0) in enumerate(cjobs):
            sl = ji % 2
            if ji + 1 < len(cjobs):
                load_w((ji + 1) % 2, cjobs[ji + 1][2], 256)
            if kind == "u":
                for cc in range(2):
                    conv_mm(sl, cc, True)
                    for th in range(2):
                        P.op("act", lambda e, cc=cc, th=th: e.activation(
                            out=uT[:, cc, 2 + th * 512:2 + (th + 1) * 512], in_=psf[cc * 2 + th][:, :], func=AF.Copy),
                            reads=[bank[cc * 2 + th]], writes=[res("uT")])
                    P.op("act", lambda e, cc=cc: e.activation(out=uT[:, cc, 0:2], in_=psf[4 + cc][:, 0:2], func=AF.Copy),
                         reads=[bank[4 + cc]], writes=[res("uT")])
            elif kind == "c":
                for cc in range(2):
                    conv_mm(sl, cc, True)
                    for th in range(2):
                        P.op("dve", lambda e, cc=cc, th=th: e.tensor_tensor(
                            out=uT[:, cc, 2 + th * 512:2 + (th + 1) * 512], in0=psf[cc * 2 + th][:, :],
                            in1=uT[:, cc, 2 + th * 512:2 + (th + 1) * 512], op=ALU.mult),
                            reads=[bank[cc * 2 + th], res("uT")], writes=[res("uT")])
                    P.op("dve", lambda e, cc=cc: e.scalar_tensor_tensor(
                        out=uT[:, cc, 0:2], in0=psf[4 + cc][:, 0:2], scalar=pc_sb[:, PC_FLAG:PC_FLAG + 1], in1=uT[:, cc, 0:2],
                        op0=ALU.mult, op1=ALU.mult),
                        reads=[bank[4 + cc], res("uT"), res("pcst")], writes=[res("uT")])
                for cc in range(2):
                    ch = cg * 2 + cc
                    P.op("pool", lambda e, cc=cc, ch=ch: e.tensor_scalar(
                        out=acc[:, cc, :], in0=uT[:, cc, 2:1026], scalar1=cw_sb[:, 2, ch:ch + 1], scalar2=None, op0=ALU.mult),
                        reads=[res("uT"), res("cw")], writes=[res("acc")])
                    P.op("dve", lambda e, cc=cc, ch=ch: e.scalar_tensor_tensor(
                        out=acc[:, cc, :], in0=uT[:, cc, 1:1025], scalar=cw_sb[:, 1, ch:ch + 1], in1=acc[:, cc, :],
                        op0=ALU.mult, op1=ALU.add),
                        reads=[res("uT"), res("cw"), res("acc")], writes=[res("acc")])
                    P.op("dve", lambda e, cc=cc, ch=ch: e.scalar_tensor_tensor(
                        out=acc[:, cc, :], in0=uT[:, cc, 0:1024], scalar=cw_sb[:, 0, ch:ch + 1], in1=acc[:, cc, :],
                        op0=ALU.mult, op1=ALU.add),
                        reads=[res("uT"), res("cw"), res("acc")], writes=[res("acc")])
            else:
                for cc in range(2):
                    conv_mm(sl, cc, False)
                    for th in range(2):
                        P.op("dve", lambda e, cc=cc, th=th, cg=cg: e.tensor_tensor(
                            out=yTc[:, cg * 2 + cc, th * 512:(th + 1) * 512], in0=psf[cc * 2 + th][:, :],
                            in1=acc[:, cc, th * 512:(th + 1) * 512], op=ALU.mult),
                            reads=[bank[cc * 2 + th], res("acc")], writes=[res("yTc")])
        P.barrier()

        x1 = A(A_V, 16384).rearrange("p (i d) -> p i d", i=NT)
        wo = [Ab(A_HT + i * 4096, 4096).rearrange("p (c n) -> p c n", c=16) for i in range(2)]
        otmp = [A(A_SCR + 8192 + i * 512, 512) for i in range(2)]
        P.dma("sp", lambda e: e.dma_start(out=x1, in_=x_q[3].rearrange("(i p) d -> p i d", p=128)), writes=[res("x1")])
        bload("act", brd[0], mod_d[2 * D:3 * D], "brd0")

        def load_wo(nb):
            P.dma("pool", lambda e: e.dma_start(out=wo[nb % 2], in_=w_out_v[:, :, nb * 512:(nb + 1) * 512]),
                  writes=[res(f"wo{nb % 2}")])
        load_wo(0)
        for nb in range(4):
            if nb + 1 < 4:
                load_wo(nb + 1)
            for i in range(NT):
                b = i % 4

                def mm(e, nb=nb, i=i, b=b):
                    for c in range(16):
                        src = yTr[:, c, i * 128:(i + 1) * 128] if c < 8 else yTc[:, c - 8, i * 128:(i + 1) * 128]
                        ins = e.matmul(psf[b][:, :], lhsT=src, rhs=wo[nb % 2][:, c, :], start=(c == 0), stop=(c == 15))
                    return ins
                P.op("pe", mm, reads=[res(f"wo{nb % 2}"), res("yTr"), res("yTc")], writes=[bank[b]])
                P.op("dve", lambda e, nb=nb, i=i, b=b: e.tensor_tensor(
                    out=otmp[i % 2], in0=psf[b][:, :], in1=brd[0][:, nb * 512:(nb + 1) * 512], op=ALU.mult),
                    reads=[bank[b], res("brd0")], writes=[res(f"otmp{i % 2}")])
                P.op("pool", lambda e, nb=nb, i=i: e.tensor_tensor(
                    out=x1[:, i, nb * 512:(nb + 1) * 512], in0=x1[:, i, nb * 512:(nb + 1) * 512], in1=otmp[i % 2], op=ALU.add),
                    reads=[res(f"otmp{i % 2}"), res("x1")], writes=[res("x1")])
        P.barrier()

        if stage == "x1":
            P.dma("sp", lambda e: e.dma_start(out=out.rearrange("(i p) d -> p i d", p=128), in_=x1), reads=[res("x1")])
            P.barrier()
        if stage != "x1":
            tmp2 = A(A_HT, 2048)
            hb2 = [Ab(A_HT + 2048 + i * 1024, 1024) for i in range(2)]
            h2Tf = A(A_HT + 4096, 2048).rearrange("p (c t) -> p c t", c=16)
            wr_sb = A(A_HT + 6144, 512).rearrange("p (c e) -> p c e", c=16)
            brow = arena[0:1, A_HT + 6656:A_HT + 6688]
            so = A_HT + 6688
            lg = A(so, 32); so += 32
            rk = A(so, 32); so += 32
            oh = A(so, 32); so += 32
            ecap = A(so, 32); so += 32
            mkf = A(so, 32); so += 32
            mx8 = A(so, 8); so += 8
            mi8 = A(so, 8).bitcast(U32); so += 8
            idf8 = A(so, 8); so += 8
            ex4 = A(so, 4); so += 4
            destf = A(so, 4); so += 4
            lim4 = A(so, 4); so += 4
            val4 = A(so, 4); so += 4
            nmx = A(so, 1); so += 1
            ssum = A(so, 1); so += 1
            so += 2
            tri_bf = Ab(so, 64); so += 64
            ones_bf = Ab(so, 64); so += 64
            mk_bf = Ab(so, 128).rearrange("p (i e) -> p i e", i=NT); so += 128
            assert so <= A_HT + 8192
            w4_all = A(A_YTR + 4096, 32).rearrange("p (i k) -> p i k", i=NT)
            dest_all = A(A_YTR + 4128, 32).bitcast(I32).rearrange("p (i k) -> p i k", i=NT)
            ssq2 = A(A_YTR + 4352, 8)
            rstd2 = A(A_YTR + 4360, 8)
            ones_row = cst_sb[0:1, C_ONES:C_ONES + 128]
            iota_e = cst_sb[:, C_IOTA_E:C_IOTA_E + 32]

            tmpc = A(A_TRIG, 2048)
            bload("sp", brd[0], norm2_g, "brd0")
            bload("sp", tmpc, mod_d[4 * D:5 * D], "tmpc")
            bload("act", brd[1], mod_d[3 * D:4 * D], "brd1")
            P.op("dve", lambda e: e.scalar_tensor_tensor(out=brd[0], in0=tmpc, scalar=1.0, in1=brd[0],
                                                         op0=ALU.add, op1=ALU.mult),
                 reads=[res("tmpc"), res("brd0")], writes=[res("brd0")])
            P.dma("sp", lambda e: e.dma_start(out=wr_sb, in_=w_router.rearrange("(c p) e -> p c e", p=128)),
                  writes=[res("wr_sb")])
            P.dma("sp", lambda e: e.dma_start(out=brow, in_=b_router.rearrange("(o n) -> o n", o=1)), writes=[res("brow")])
            P.op("dve", lambda e: e.tensor_copy(out=tri_bf, in_=cst_sb[:, C_TRI:C_TRI + 128]), reads=[res("cst")], writes=[res("tri")])
            P.op("dve", lambda e: e.tensor_copy(out=ones_bf, in_=cst_sb[:, C_ONES:C_ONES + 128]), reads=[res("cst")], writes=[res("tri")])
            P.op("dve", lambda e: e.tensor_scalar(out=ecap, in0=iota_e, scalar1=float(CAP), scalar2=None, op0=ALU.mult),
                 reads=[res("cst")], writes=[res("ecap")])

            for i in range(NT):
                hb = hb2[i % 2]
                rhb = res(f"hb2_{i % 2}")
                P.op("act", lambda e, i=i, hb=hb: e.activation(out=hb, in_=x1[:, i, :], func=AF.Square, accum_out=ssq2[:, i:i + 1]),
                     reads=[res("x1")], writes=[rhb, res("rs2")])
                P.op("act", lambda e, i=i: e.activation(out=rstd2[:, i:i + 1], in_=ssq2[:, i:i + 1], func=AF.Sqrt,
                                                       scale=1.0 / D, bias=col(C_EPS)),
                     reads=[res("rs2"), res("cst")], writes=[res("rs2")])
                P.op("dve", lambda e, i=i: e.reciprocal(out=rstd2[:, i:i + 1], in_=rstd2[:, i:i + 1]),
                     reads=[res("rs2")], writes=[res("rs2")])
                P.op("dve", lambda e, i=i: e.scalar_tensor_tensor(out=tmp2, in0=x1[:, i, :], scalar=rstd2[:, i:i + 1], in1=brd[0],
                                                                   op0=ALU.mult, op1=ALU.mult),
                     reads=[res("x1"), res("rs2"), res("brd0")], writes=[res("tmp2")])
                P.op("dve", lambda e: e.tensor_tensor(out=tmp2, in0=tmp2, in1=brd[1], op=ALU.add),
                     reads=[res("tmp2"), res("brd1")], writes=[res("tmp2")])
                P.op("act", lambda e, hb=hb: e.activation(out=hb, in_=tmp2, func=AF.Copy), reads=[res("tmp2")], writes=[rhb])

                def trf(e):
                    for c in range(16):
                        ins = e.transpose(out=psf[2 + c // 4][:, (c % 4) * 128:(c % 4 + 1) * 128],
                                          in_=tmp2[:, c * 128:(c + 1) * 128], identity=ident_f)
                    return ins
                P.op("pe", trf, reads=[res("tmp2"), res("cst")], writes=[bank[2], bank[3], bank[4], bank[5]])
                for k in range(4):
                    if k % 2 == 0:
                        P.op("act", lambda e, k=k: e.activation(out=h2Tf[:, 4 * k:4 * k + 4, :],
                                                                in_=psf[2 + k][:, :].rearrange("p (c t) -> p c t", c=4), func=AF.Copy),
                             reads=[bank[2 + k]], writes=[res("h2Tf")])
                    else:
                        P.op("dve", lambda e, k=k: e.tensor_copy(out=h2Tf[:, 4 * k:4 * k + 4, :],
                                                                 in_=psf[2 + k][:, :].rearrange("p (c t) -> p c t", c=4)),
                             reads=[bank[2 + k]], writes=[res("h2Tf")])

                def lgm(e):
                    for c in range(16):
                        e.matmul(psf[6][:, 0:32], lhsT=h2Tf[:, c, :], rhs=wr_sb[:, c, :], start=(c == 0), stop=False)
                    return e.matmul(psf[6][:, 0:32], lhsT=ones_row, rhs=brow, start=False, stop=True)
                P.op("pe", lgm, reads=[res("h2Tf"), res("wr_sb"), res("brow"), res("cst")], writes=[bank[6]])
                rt = res("rt")
                P.op("dve", lambda e: e.tensor_copy(out=lg, in_=psf[6][:, 0:32]), reads=[bank[6]], writes=[rt])
                P.op("dve", lambda e: e.max(out=mx8, in_=lg), reads=[rt], writes=[rt])
                P.op("dve", lambda e: e.max_index(out=mi8, in_max=mx8, in_values=lg), reads=[rt], writes=[rt])
                P.op("dve", lambda e: e.tensor_copy(out=idf8, in_=mi8), reads=[rt], writes=[rt])
                P.op("dve", lambda e: e.tensor_scalar(out=mkf, in0=lg, scalar1=mx8[:, 3:4], scalar2=None, op0=ALU.is_ge),
                     reads=[rt], writes=[rt])
                P.op("dve", lambda e, i=i: e.tensor_copy(out=mk_bf[:, i, :], in_=mkf), reads=[rt], writes=[res("mk_bf")])
                P.op("dve", lambda e: e.tensor_scalar(out=nmx, in0=mx8[:, 0:1], scalar1=-1.0, scalar2=None, op0=ALU.mult),
                     reads=[rt], writes=[rt])
                P.op("act", lambda e: e.activation(out=ex4, in_=mx8[:, 0:4], func=AF.Exp, bias=nmx), reads=[rt], writes=[rt])
                P.op("dve", lambda e: e.reduce_sum(out=ssum, in_=ex4, axis=mybir.AxisListType.X), reads=[rt], writes=[rt])
                P.op("dve", lambda e: e.reciprocal(out=ssum, in_=ssum), reads=[rt], writes=[rt])
                P.op("dve", lambda e, i=i: e.tensor_scalar(out=w4_all[:, i, :], in0=ex4, scalar1=ssum, scalar2=None, op0=ALU.mult),
                     reads=[rt], writes=[res("w4")])

                def rkm(e, i=i):
                    for ip in range(i):
                        e.matmul(psf[7][:, 0:32], lhsT=ones_bf, rhs=mk_bf[:, ip, :], start=(ip == 0), stop=False)
                    return e.matmul(psf[7][:, 0:32], lhsT=tri_bf, rhs=mk_bf[:, i, :], start=(i == 0), stop=True)
                P.op("pe", rkm, reads=[res("mk_bf"), res("tri")], writes=[bank[7]])
                P.op("dve", lambda e: e.tensor_tensor(out=rk, in0=psf[7][:, 0:32], in1=ecap, op=ALU.add),
                     reads=[bank[7], res("ecap")], writes=[rt])
                for k in range(TOPK):
                    P.op("dve", lambda e, k=k: e.tensor_scalar(out=oh, in0=iota_e, scalar1=idf8[:, k:k + 1], scalar2=None, op0=ALU.is_equal),
                         reads=[rt, res("cst")], writes=[rt])
                    P.op("dve", lambda e: e.tensor_tensor(out=oh, in0=oh, in1=rk, op=ALU.mult), reads=[rt], writes=[rt])
                    P.op("dve", lambda e, k=k: e.reduce_sum(out=destf[:, k:k + 1], in_=oh, axis=mybir.AxisListType.X),
                         reads=[rt], writes=[rt])
                P.op("dve", lambda e: e.tensor_scalar(out=lim4, in0=idf8[:, 0:4], scalar1=1.0, scalar2=float(CAP), op0=ALU.add, op1=ALU.mult),
                     reads=[rt], writes=[rt])
                P.op("dve", lambda e: e.tensor_tensor(out=val4, in0=destf, in1=lim4, op=ALU.is_lt), reads=[rt], writes=[rt])
                P.op("dve", lambda e: e.tensor_scalar(out=destf, in0=destf, scalar1=-float(NE * CAP), scalar2=None, op0=ALU.add),
                     reads=[rt], writes=[rt])
                P.op("dve", lambda e: e.tensor_tensor(out=destf, in0=destf, in1=val4, op=ALU.mult), reads=[rt], writes=[rt])
                P.op("dve", lambda e: e.tensor_scalar(out=destf, in0=destf, scalar1=float(NE * CAP), scalar2=None, op0=ALU.add),
                     reads=[rt], writes=[rt])
                P.op("dve", lambda e, i=i: e.tensor_tensor(out=w4_all[:, i, :], in0=w4_all[:, i, :], in1=val4, op=ALU.mult),
                     reads=[rt, res("w4")], writes=[res("w4")])
                P.op("dve", lambda e, i=i: e.tensor_copy(out=dest_all[:, i, :], in_=destf), reads=[rt], writes=[res("dest")])
                for k in range(TOPK):
                    P.dma("pool", lambda e, i=i, k=k, hb=hb: e.indirect_dma_start(
                        out=xe_d, out_offset=bass.IndirectOffsetOnAxis(ap=dest_all[:, i, k:k + 1], axis=0),
                        in_=hb, in_offset=None),
                        reads=[rhb, res("dest")], writes=[res("xe_d")])
            P.barrier()

            bgu_all = A(A_SF32, 1024).rearrange("p (c e) -> p c e", c=32)
            stage_b = arena[0:32, A_YTR:A_YTR + 4096]
            P.dma("sp", lambda e: e.dma_start(out=stage_b, in_=b_gate_up), writes=[res("stage_b")])
            for half in range(2):
                def trbias(e, half=half):
                    for cc in range(16):
                        ch = half * 16 + cc
                        ins = e.transpose(out=psf[half][:, cc * 32:(cc + 1) * 32], in_=stage_b[:, ch * 128:(ch + 1) * 128],
                                          identity=ident_f[0:32, 0:32])
                    return ins
                P.op("pe", trbias, reads=[res("stage_b"), res("cst")], writes=[bank[half]])
                P.op("dve", lambda e, half=half: e.tensor_copy(out=bgu_all[:, half * 16:(half + 1) * 16, :],
                                                               in_=psf[half][:, :].rearrange("p (c e) -> p c e", c=16)),
                     reads=[bank[half]], writes=[res("bgu")])
            P.barrier()

            NM = CAP // 128
            P.dma("sp", lambda e: e.dma_start(out=x1_d, in_=x1), reads=[res("x1")], writes=[res("x1_d")])
            P.barrier()
            xeT = [Ab(A_HT + i * 4096, 4096).rearrange("p (c s) -> p c s", c=16) for i in range(2)]
            actT = Ab(A_WB, 4096).rearrange("p (c s) -> p c s", c=16)
            xe_sb = [Ab(A_WB + 4096 + i * 1024, 1024) for i in range(4)]
            bd_bs = [A(A_WB + 8192, 2048), A(A_TRIG, 2048)]
            yst = [A(A_YTR + i * 2048, 2048).rearrange("p (m n) -> p m n", m=NM) for i in range(2)]
            g1 = A(A_SCR + 8192, 512)
            u1 = A(A_SCR + 8704, 512)
            sgm = A(A_SCR + 9216, 512)
            wgu = [Ab(A_V + i * 4096, 4096).rearrange("p (c n) -> p c n", c=16) for i in range(3)]
            wdn = [Ab(A_V + 12288, 4096).rearrange("p (c n) -> p c n", c=16), Ab(A_BRD, 4096).rearrange("p (c n) -> p c n", c=16)]
            NGS, NDS = len(wgu), len(wdn)

            def load_gu(e_, cq, slot):
                wv = w_gate_up[e_].rearrange("(c p) n -> p c n", p=128)
                P.dma("pool", lambda e: e.dma_start(out=wgu[slot][:, :, 0:256], in_=wv[:, :, cq * 256:(cq + 1) * 256]),
                      writes=[res(f"wgu{slot}")])
                P.dma("pool", lambda e: e.dma_start(out=wgu[slot][:, :, 256:512], in_=wv[:, :, DFF + cq * 256:DFF + (cq + 1) * 256]),
                      writes=[res(f"wgu{slot}")])

            def load_dn(e_, nq, slot):
                wv = w_down[e_].rearrange("(c p) n -> p c n", p=128)
                P.dma("pool", lambda e: e.dma_start(out=wdn[slot], in_=wv[:, :, nq * 512:(nq + 1) * 512]),
                      writes=[res(f"wdn{slot}")])

            xl = 0

            def load_xe(e_, m):
                nonlocal xl
                b_ = xl % 4
                xl += 1
                P.dma("sp", lambda e: e.dma_start(out=xe_sb[b_], in_=xe_d[e_ * CAP + m * 128:e_ * CAP + (m + 1) * 128, :]),
                      reads=[res("xe_d")], writes=[res(f"xe_sb{b_}")])
                return b_

            gu_jobs = [(e_, cq) for e_ in range(NEW) for cq in range(8)]
            dn_jobs = [(e_, nq) for e_ in range(NEW) for nq in range(4)]
            gl = 0
            dl = 0

            def pump_gu(upto):
                nonlocal gl
                while gl < len(gu_jobs) and gl < upto:
                    load_gu(gu_jobs[gl][0], gu_jobs[gl][1], gl % NGS)
                    gl += 1

            def pump_dn(upto):
                nonlocal dl
                while dl < len(dn_jobs) and dl < upto:
                    load_dn(dn_jobs[dl][0], dn_jobs[dl][1], dl % NDS)
                    dl += 1

            pump_gu(2)
            gi = 0
            di = 0
            tj = 0
            def transposes(e_):
                s_ = e_ % 2
                nonlocal tj
                for m in range(NM):
                    b_ = load_xe(e_, m)
                    pa = 2 * (tj % 2)
                    tj += 1

                    def trx(e, b_=b_, pa=pa):
                        for c in range(16):
                            ins = e.transpose(out=psb(pa + c // 8)[:, (c % 8) * 128:(c % 8 + 1) * 128],
                                              in_=xe_sb[b_][:, c * 128:(c + 1) * 128], identity=ident_bf)
                        return ins
                    P.op("pe", trx, reads=[res(f"xe_sb{b_}"), res("ident_bf")], writes=[bank[pa], bank[pa + 1]])
                    P.op("act", lambda e, m=m, s_=s_, pa=pa: e.activation(
                        out=xeT[s_][:, 0:8, m * 128:(m + 1) * 128], in_=psb(pa).rearrange("p (c t) -> p c t", c=8), func=AF.Copy),
                        reads=[bank[pa]], writes=[res(f"xeT{s_}")])
                    P.op("dve", lambda e, m=m, s_=s_, pa=pa: e.tensor_copy(
                        out=xeT[s_][:, 8:16, m * 128:(m + 1) * 128], in_=psb(pa + 1).rearrange("p (c t) -> p c t", c=8)),
                        reads=[bank[pa + 1]], writes=[res(f"xeT{s_}")])

            transposes(0)
            for e_ in range(NEW):
                s_ = e_ % 2
                bd_b = bd_bs[e_ % 2]
                rbd = res(f"bd_b{e_ % 2}")
                P.dma("sp", lambda e, e_=e_, bd_b=bd_b: e.dma_start(out=bd_b, in_=b_down[e_].partition_broadcast(128)), writes=[rbd])
                for cp in range(16):
                    slot = gi % NGS
                    hf = cp % 2
                    if hf == 0:
                        pump_gu(gi + NGS)
                    if cp in (0, 8):
                        pump_dn(di + NDS if cp == 8 else di + 1)
                    bg, bu = 4 + 2 * (cp % 2), 5 + 2 * (cp % 2)

                    def mg(e, slot=slot, s_=s_, bg=bg, hf=hf):
                        for c in range(16):
                            ins = e.matmul(psf[bg][:, 0:CAP], lhsT=wgu[slot][:, c, hf * 128:(hf + 1) * 128], rhs=xeT[s_][:, c, :],
                                           start=(c == 0), stop=(c == 15))
                        return ins

                    def mu(e, slot=slot, s_=s_, bu=bu, hf=hf):
                        for c in range(16):
                            ins = e.matmul(psf[bu][:, 0:CAP], lhsT=wgu[slot][:, c, 256 + hf * 128:256 + (hf + 1) * 128], rhs=xeT[s_][:, c, :],
                                           start=(c == 0), stop=(c == 15))
                        return ins
                    P.op("pe", mg, reads=[res(f"wgu{slot}"), res(f"xeT{s_}")], writes=[bank[bg]])
                    P.op("pe", mu, reads=[res(f"wgu{slot}"), res(f"xeT{s_}")], writes=[bank[bu]])
                    P.op("dve", lambda e, bg=bg, cp=cp, e_=e_: e.tensor_scalar(
                        out=g1, in0=psf[bg][:, 0:CAP], scalar1=bgu_all[:, cp, e_:e_ + 1], scalar2=7.0, op0=ALU.add, op1=ALU.min),
                        reads=[bank[bg], res("bgu")], writes=[res("g1")])
                    P.op("act", lambda e: e.activation(out=sgm, in_=g1, func=AF.Sigmoid, scale=1.702),
                         reads=[res("g1")], writes=[res("sgm")])
                    P.op("act", lambda e, bu=bu, cp=cp, e_=e_: e.activation(
                        out=u1, in_=psf[bu][:, 0:CAP], func=AF.Identity, bias=bgu_all[:, 16 + cp, e_:e_ + 1]),
                        reads=[bank[bu], res("bgu")], writes=[res("u1")])
                    P.op("dve", lambda e: e.tensor_scalar(out=u1, in0=u1, scalar1=-7.0, scalar2=7.0, op0=ALU.max, op1=ALU.min),
                         reads=[res("u1")], writes=[res("u1")])
                    P.op("dve", lambda e: e.tensor_tensor(out=g1, in0=g1, in1=sgm, op=ALU.mult),
                         reads=[res("g1"), res("sgm")], writes=[res("g1")])
                    P.op("dve", lambda e, cp=cp: e.scalar_tensor_tensor(
                        out=actT[:, cp, :], in0=u1, scalar=1.0, in1=g1, op0=ALU.add, op1=ALU.mult),
                        reads=[res("u1"), res("g1")], writes=[res("actT")])
                    if hf == 1:
                        gi += 1
                if e_ + 1 < NEW:
                    transposes(e_ + 1)
                for nq in range(4):
                    slot = di % NDS
                    pump_dn(di + NDS)
                    cols = slice(nq * 512, (nq + 1) * 512)
                    ys = yst[nq % 2]
                    rys = res(f"yst{nq % 2}")
                    for m in range(NM):
                        b0 = (nq * NM + m) % 4

                        def md(e, slot=slot, m=m, b0=b0):
                            for c in range(16):
                                ins = e.matmul(psf[b0][:, :], lhsT=actT[:, c, m * 128:(m + 1) * 128], rhs=wdn[slot][:, c, :],
                                               start=(c == 0), stop=(c == 15))
                            return ins
                        P.op("pe", md, reads=[res(f"wdn{slot}"), res("actT")], writes=[bank[b0]])
                        P.op("dve", lambda e, b0=b0, cols=cols, m=m, ys=ys, bd_b=bd_b: e.tensor_tensor(
                            out=ys[:, m, :], in0=psf[b0][:, :], in1=bd_b[:, cols], op=ALU.add),
                            reads=[bank[b0], rbd], writes=[rys])
                    P.dma("sp", lambda e, e_=e_, cols=cols, ys=ys: e.dma_start(
                        out=ye_d[e_ * CAP:(e_ + 1) * CAP, cols].rearrange("(m p) n -> p m n", p=128), in_=ys),
                        reads=[rys], writes=[res("ye_d")])
                    di += 1
            P.barrier()

            P.dma("sp", lambda e: e.dma_start(out=x1, in_=x1_d), reads=[res("x1_d")], writes=[res("x1")])
            bload("sp", brd[0], mod_d[5 * D:6 * D], "brd0")
            yg = [A(A_HT + i * 2048, 2048) for i in range(2)]
            ctm = A(A_HT + 4096, 2048)
            P.op("pool", lambda e: e.memset(ctm, 0.0), writes=[res("ctm")])
            P.dma("sp", lambda e: e.dma_start(out=ye_d[NE * CAP:NE * CAP + 128, :], in_=ctm), reads=[res("ctm")], writes=[res("ye_d")])
            for i in range(NT):
                for k in range(TOPK):
                    j = i * TOPK + k
                    P.dma("pool", lambda e, i=i, k=k, j=j: e.indirect_dma_start(
                        out=yg[j % 2], out_offset=None, in_=ye_d,
                        in_offset=bass.IndirectOffsetOnAxis(ap=dest_all[:, i, k:k + 1], axis=0)),
                        reads=[res("ye_d"), res("dest")], writes=[res(f"yg{j % 2}")])
                    P.op("dve", lambda e, j=j: e.tensor_tensor(out=ctm, in0=yg[j % 2], in1=brd[0], op=ALU.mult),
                         reads=[res(f"yg{j % 2}"), res("brd0")], writes=[res("ctm")])
                    P.op("dve", lambda e, i=i, k=k: e.scalar_tensor_tensor(
                        out=x1[:, i, :], in0=ctm, scalar=w4_all[:, i, k:k + 1], in1=x1[:, i, :], op0=ALU.mult, op1=ALU.add),
                        reads=[res("ctm"), res("w4"), res("x1")], writes=[res("x1")])
            P.barrier()

            fg_b = A(A_TRIG, 2048)
            ot = [A(A_WB + i * 2048, 2048) for i in range(2)]
            junk = Ab(A_WB + 4096, 1024)
            bload("sp", fg_b, final_g, "fg_b")
            out_v = out.rearrange("(i p) d -> i p d", p=128)
            for i in range(NT):
                P.op("act", lambda e, i=i: e.activation(out=junk, in_=x1[:, i, :], func=AF.Square, accum_out=ssq2[:, i:i + 1]),
                     reads=[res("x1")], writes=[res("junk"), res("rs3")])
                P.op("act", lambda e, i=i: e.activation(out=rstd2[:, i:i + 1], in_=ssq2[:, i:i + 1], func=AF.Sqrt,
                                                       scale=1.0 / D, bias=col(C_EPS)),
                     reads=[res("rs3"), res("cst")], writes=[res("rs3")])
                P.op("dve", lambda e, i=i: e.reciprocal(out=rstd2[:, i:i + 1], in_=rstd2[:, i:i + 1]),
                     reads=[res("rs3")], writes=[res("rs3")])
                P.op("dve", lambda e, i=i: e.scalar_tensor_tensor(out=ot[i % 2], in0=x1[:, i, :], scalar=rstd2[:, i:i + 1], in1=fg_b,
                                                                   op0=ALU.mult, op1=ALU.mult),
                     reads=[res("x1"), res("rs3"), res("fg_b")], writes=[res(f"ot{i % 2}")])
                P.dma("sp", lambda e, i=i: e.dma_start(out=out_v[i], in_=ot[i % 2]), reads=[res(f"ot{i % 2}")])
            P.barrier()


        with nc.Block() as block:
            @block.sync
            def _(e):
                P.emit("sp", e)

            @block.scalar
            def _(e):
                P.emit("act", e)

            @block.vector
            def _(e):
                P.emit("dve", e)

            @block.gpsimd
            def _(e):
                P.emit("pool", e)

            @block.tensor
            def _(e):
                P.emit("pe", e)
    return nc


def _consts():
    c = np.zeros((128, C_N), np.float32)
    p = np.arange(128)
    c[:, C_IOTA_T:C_IOTA_T + 1024] = np.arange(1024)[None, :]
    c[:, C_IDENT:C_IDENT + 128] = np.eye(128)
    kt = p[:, None]
    qt = p[None, :]
    c[:, C_DIST:C_DIST + 128] = np.abs(kt - qt)
    c[:, C_ALLOW:C_ALLOW + 128] = ((kt // 64) <= (qt // 64))
    c[:, C_TRI:C_TRI + 128] = (kt < qt)
    c[:, C_ONES:C_ONES + 128] = 1.0
    c[:, C_IOTA_E:C_IOTA_E + 32] = np.arange(32)[None, :]
    c[:, C_IMOD] = p % 64
    c[:, C_SIGN] = np.where(p < 64, -1.0, 1.0)
    c[:, C_HALFPI] = np.pi / 2
    c[:, C_EPS] = EPS
    c[:, C_LNQ] = np.log(QSCALE)
    c[:, C_ONE] = 1.0
    return c


def _percore(j):
    t = np.zeros((128, PC_N), np.float32)
    p = np.arange(128)
    for qi in range(4):
        if qi < 3:
            s = 3 - qi
            valid = s <= j
            t[:, PC_BASE + qi] = (j - s) * 1024 if valid else 0
            for i in range(8):
                t[:, PC_E + qi * 8 + i] = (1024 * s - (128 * i + p)) if valid else 1.0e6
        else:
            t[:, PC_BASE + qi] = j * 1024
            for i in range(8):
                t[:, PC_E + qi * 8 + i] = -(128 * i + p)
    t[:, PC_FLAG] = 1.0 if j > 0 else 0.0
    return t


_NC_CACHE = {}


def make_in_maps(x, c, norm1_g, w_mod, b_mod, w_in, conv_w, w_out, norm2_g, w_router, b_router,
                 w_gate_up, b_gate_up, w_down, b_down, final_g):
    f = lambda a: np.ascontiguousarray(np.asarray(a, dtype=np.float32))
    x = f(x); c = f(c)
    shared = dict(
        cst=_consts(), norm1_g=f(norm1_g[0]), w_mod=f(w_mod[0]), b_mod=f(b_mod[0]), w_in=f(w_in[0]),
        conv_w=f(conv_w[0]), w_out=f(w_out[0]), norm2_g=f(norm2_g[0]), w_router=f(w_router[0]),
        b_router=f(b_router[0]), w_gate_up=f(w_gate_up[0]), b_gate_up=f(b_gate_up[0]),
        w_down=f(w_down[0]), b_down=f(b_down[0]), final_g=f(final_g))
    in_maps = []
    for core in range(8):
        b, j = core // 4, core % 4
        xq = np.zeros((4, T, D), np.float32)
        for qi in range(3):
            s = 3 - qi
            if s <= j:
                xq[qi] = x[b, (j - s) * T:(j - s + 1) * T]
        xq[3] = x[b, j * T:(j + 1) * T]
        m = dict(shared)
        m["x_q"] = xq
        m["c_pc"] = np.ascontiguousarray(c[b].reshape(128, 16))
        m["pcst"] = _percore(j)
        in_maps.append(m)
    return in_maps


def kernel(**inputs):
    if "nc" not in _NC_CACHE:
        _NC_CACHE["nc"] = build_nc()
    nc = _NC_CACHE["nc"]
    in_maps = make_in_maps(**inputs)
    res = run_bass_kernel_spmd(nc, in_maps, core_ids=list(range(8)))
    outs = [np.asarray(r["out"], dtype=np.float32) for r in res.results]
    full = np.stack(outs, 0).reshape(2, 4, T, D).reshape(2, 4 * T, D)
    return full
```

```python
import numpy as np
import concourse.bass as bass
import concourse.mybir as mybir
from concourse.bass_utils import run_bass_kernel_spmd

F32 = mybir.dt.float32
BF16 = mybir.dt.bfloat16
I32 = mybir.dt.int32
U32 = mybir.dt.uint32
AF = mybir.ActivationFunctionType
ALU = mybir.AluOpType

D = 2048
T = 1024
NT = 8
D_RET = 1024
D_CONV = 1024
NH = 8
HD = 128
D_IN = 7168
NE = 32
TOPK = 4
DFF = 2048
CAP = 512
EPS = 1e-6
QSCALE = float(HD ** -0.5)
LOG_GAMMA = [float(np.log1p(-2.0 ** (-5.0 - h))) for h in range(NH)]
TWO_PI_HI = 6.28125
TWO_PI_LO = 2.0 * np.pi - 6.28125
INV_2PI = float(1.0 / (2.0 * np.pi))
PI_LO = 3.1415925

C_IOTA_T = 0
C_IDENT = 1024
C_DIST = 1152
C_ALLOW = 1280
C_TRI = 1408
C_ONES = 1536
C_IOTA_E = 1664
C_IMOD = 1696
C_SIGN = 1697
C_HALFPI = 1698
C_EPS = 1699
C_LNQ = 1700
C_ONE = 1701
C_N = 1704
PC_BASE = 0
PC_E = 4
PC_FLAG = 36
PC_N = 40

A_CONST = 0
A_TRIG = 3328
A_BRD = A_TRIG + 2048
A_SF32 = A_BRD + 4096
A_HT = A_SF32 + 1024
A_WB = A_HT + 8192
A_KT = A_WB + 6144
A_V = A_KT + 4096
A_SBF = A_V + 4096
A_SCR = A_SBF + 4096
A_YTR = A_SCR + 10240
A_END = A_YTR + 5120


class Res:
    __slots__ = ("name", "w", "r")

    def __init__(self, name):
        self.name = name
        self.w = None
        self.r = []


class Q:
    def __init__(self, name, sem):
        self.name = name
        self.sem = sem
        self.n = 0
        self.waited = {}
        self.ops = []
        self.chans = []
        self.ci = 0


class Prog:
    def __init__(self, nc):
        self.nc = nc
        self.sems = []
        self.q = {}

    def add_queue(self, name, sem, chan_sems=()):
        q = Q(name, len(self.sems))
        self.sems.append(sem)
        for cs in chan_sems:
            q.chans.append([len(self.sems), 0])
            self.sems.append(cs)
        self.q[name] = q

    def _collect(self, q, reads, writes):
        need = {}

        def add(ev, is_war):
            if ev is None:
                return
            k, val, eng = ev
            if eng == q.name:
                if q.name == "pe":
                    return
                if is_war:
                    return
            if q.waited.get(k, 0) >= val:
                return
            if need.get(k, 0) < val:
                need[k] = val

        for r in reads:
            add(r.w, False)
        for w in writes:
            add(w.w, False)
            for e in w.r:
                add(e, True)
        return need

    def _commit(self, q, need):
        for k, val in need.items():
            q.waited[k] = val
        return [(k, v) for k, v in need.items()]

    def op(self, qn, fn, reads=(), writes=()):
        q = self.q[qn]
        need = self._collect(q, reads, writes)
        waits = self._commit(q, need)
        q.n += 1
        ev = (q.sem, q.n, q.name)
        q.ops.append((waits, fn, q.sem, 1))
        for r in reads:
            r.r.append(ev)
        for w in writes:
            w.w = ev
            w.r = []
        return ev

    def dma(self, qn, fn, reads=(), writes=()):
        q = self.q[qn]
        need = self._collect(q, reads, writes)
        ch = q.chans[q.ci]
        q.ci = (q.ci + 1) % len(q.chans)
        if ch[1] > 0 and q.waited.get(ch[0], 0) < 16 * ch[1]:
            if need.get(ch[0], 0) < 16 * ch[1]:
                need[ch[0]] = 16 * ch[1]
        waits = self._commit(q, need)
        ch[1] += 1
        ev = (ch[0], 16 * ch[1], "dma")
        q.ops.append((waits, fn, ch[0], 16))
        for r in reads:
            r.r.append(ev)
        for w in writes:
            w.w = ev
            w.r = []
        return ev

    def barrier(self):
        evs = []
        for q in self.q.values():
            if q.n > 0:
                evs.append((q.sem, q.n))
            for ch in q.chans:
                if ch[1] > 0:
                    evs.append((ch[0], 16 * ch[1]))
        for q in self.q.values():
            need = {}
            for k, val in evs:
                if k == q.sem and q.name != "pe" and False:
                    continue
                if q.waited.get(k, 0) < val:
                    need[k] = val
            waits = self._commit(q, need)
            if waits:
                q.ops.append((waits, None, None, 0))

    def emit(self, qn, eng):
        q = self.q[qn]
        for waits, fn, semk, inc in q.ops:
            for k, val in waits:
                eng.wait_ge(self.sems[k], val)
            if fn is not None:
                ins = fn(eng)
                ins.then_inc(self.sems[semk], inc)


def build_nc(stage="full"):
    nc = bass.Bass("TRN2", target_bir_lowering=False)
    NEW = NE if stage == "full" else 1

    def din(name, shape, dt=F32):
        return nc.dram_tensor(name, list(shape), dt, kind="ExternalInput").ap()

    x_q = din("x_q", [4, T, D])
    c_pc = din("c_pc", [128, 16])
    cst = din("cst", [128, C_N])
    pcst = din("pcst", [128, PC_N])
    norm1_g = din("norm1_g", [D])
    w_mod = din("w_mod", [D, 6 * D])
    b_mod = din("b_mod", [6 * D])
    w_in = din("w_in", [D, D_IN])
    conv_w = din("conv_w", [3, D_CONV])
    w_out = din("w_out", [D, D])
    norm2_g = din("norm2_g", [D])
    w_router = din("w_router", [D, NE])
    b_router = din("b_router", [NE])
    w_gate_up = din("w_gate_up", [NEW, D, 2 * DFF])
    b_gate_up = din("b_gate_up", [NE, 2 * DFF])
    w_down = din("w_down", [NEW, DFF, D])
    b_down = din("b_down", [NE, D])
    final_g = din("final_g", [D])
    out = nc.dram_tensor("out", [T, D], F32, kind="ExternalOutput").ap()
    mod_d = nc.dram_tensor("mod_d", [6 * D], F32, kind="Internal").ap()
    x1_d = nc.dram_tensor("x1_d", [128, NT, D], F32, kind="Internal").ap()
    xe_d = nc.dram_tensor("xe_d", [NE * CAP + 128, D], BF16, kind="Internal").ap()
    ye_d = nc.dram_tensor("ye_d", [NE * CAP + 128, D], F32, kind="Internal").ap()

    w_in_v = w_in.rearrange("(c p) n -> p c n", p=128)
    w_out_v = w_out.rearrange("(c p) n -> p c n", p=128)

    from contextlib import ExitStack
    es = ExitStack()
    with es:
        arena = es.enter_context(nc.sbuf_tensor("arena", [128, A_END], F32))
        psf = [es.enter_context(nc.psum_tensor(f"ps{i}", [128, 512], F32)) for i in range(8)]
        nsem = 5 + 8 + 6 + 2
        sems = [es.enter_context(nc.semaphore(f"s{i}")) for i in range(nsem)]
        P = Prog(nc)
        P.add_queue("pe", sems[0])
        P.add_queue("act", sems[1], sems[19:21])
        P.add_queue("dve", sems[2])
        P.add_queue("pool", sems[3], sems[5:13])
        P.add_queue("sp", sems[4], sems[13:19])

        def A(off, n):
            return arena[:, off:off + n]

        def Ab(off, nwords):
            return arena[:, off:off + nwords].bitcast(BF16)

        def psb(i):
            return psf[i][:, :].bitcast(BF16)

        bank = [Res(f"bank{i}") for i in range(8)]

        cst_sb = A(A_CONST, C_N)
        pc_sb = A(A_CONST + C_N, PC_N)
        o = A_CONST + C_N + PC_N
        ident_bf = Ab(o, 64); o += 64
        maskT = A(o, 1024).rearrange("p (h k) -> p h k", h=NH); o += 1024
        kdec_tab = A(o, 256).rearrange("p (a h) -> p a h", h=NH); o += 256
        freq = A(o, 1); o += 1
        c_sb = A(o, 16); o += 16
        c_act = A(o, 16); o += 16
        cw_sb = A(o, 24).rearrange("p (k c) -> p k c", k=3); o += 24
        small = A(o, 64); o += 64
        hT_halo = Ab(o, 16).rearrange("p (c t) -> p c t", c=16); o += 16
        assert o <= A_TRIG, o
        iota_t = cst_sb[:, C_IOTA_T:C_IOTA_T + 1024]
        ident_f = cst_sb[:, C_IDENT:C_IDENT + 128]
        dist = cst_sb[:, C_DIST:C_DIST + 128]
        allow = cst_sb[:, C_ALLOW:C_ALLOW + 128]

        def col(c):
            return cst_sb[:, c:c + 1]

        cos_t = A(A_TRIG, 1024)
        sin_t = A(A_TRIG + 1024, 1024)
        brd = [A(A_BRD, 2048), A(A_BRD + 2048, 2048)]
        S_f32 = A(A_SF32, 1024).rearrange("p (h e) -> p h e", h=NH)
        hT = Ab(A_HT, 8192).rearrange("p (c t) -> p c t", c=16)
        wb = [Ab(A_WB + i * 3072, 3072) for i in range(2)]
        kT = Ab(A_KT, 4096).rearrange("p (h t) -> p h t", h=NH)
        v_sb = Ab(A_V, 4096).rearrange("p (i n) -> p i n", i=NT)
        S_bf = Ab(A_SBF, 4096).rearrange("p (i h e) -> p i h e", i=NT, h=NH)
        yTr = Ab(A_YTR, 4096).rearrange("p (c t) -> p c t", c=8)
        yTc = Ab(A_KT, 4096).rearrange("p (c t) -> p c t", c=8)

        R = {}

        def res(name):
            if name not in R:
                R[name] = Res(name)
            return R[name]

        P.dma("sp", lambda e: e.dma_start(out=cst_sb, in_=cst), writes=[res("cst")])
        P.dma("sp", lambda e: e.dma_start(out=pc_sb, in_=pcst), writes=[res("pcst")])
        P.dma("sp", lambda e: e.dma_start(out=c_sb, in_=c_pc), writes=[res("c_sb")])
        P.dma("sp", lambda e: e.dma_start(
            out=cw_sb, in_=conv_w.rearrange("k (c p) -> p k c", p=128),
            allow_slow_non_contiguous=True), writes=[res("cw")])
        P.op("dve", lambda e: e.tensor_copy(out=ident_bf, in_=ident_f), reads=[res("cst")], writes=[res("ident_bf")])
        P.op("act", lambda e: e.activation(out=freq, in_=col(C_IMOD), func=AF.Exp,
                                           scale=float(-np.log(10000.0) / 64.0)),
             reads=[res("cst")], writes=[res("freq")])
        for h in range(NH):
            P.op("act", lambda e, h=h: e.activation(out=maskT[:, h, :], in_=dist, func=AF.Exp, scale=LOG_GAMMA[h]),
                 reads=[res("cst")], writes=[res("maskT")])
        P.op("dve", lambda e: e.tensor_tensor(
            out=maskT, in0=maskT, in1=allow.unsqueeze(1).broadcast_to([128, NH, 128]), op=ALU.mult),
            reads=[res("maskT"), res("cst")], writes=[res("maskT")])
        for h in range(NH):
            P.op("act", lambda e, h=h: e.activation(
                out=kdec_tab[:, :, h], in_=pc_sb[:, PC_E:PC_E + 32], func=AF.Exp, scale=LOG_GAMMA[h]),
                reads=[res("pcst")], writes=[res("kdec")])
        P.op("act", lambda e: e.activation(out=c_act, in_=c_sb, func=AF.Silu), reads=[res("c_sb")], writes=[res("c_act")])

        wm = [A(A_HT + i * 8192, 8192).rearrange("p (c n) -> p c n", c=16) for i in range(2)]
        modrow = arena[0:1, A_V:A_V + 12288]
        w_mod_v = w_mod.rearrange("(p c) n -> p c n", c=16)
        P.dma("sp", lambda e: e.dma_start(out=modrow, in_=b_mod.rearrange("(o n) -> o n", o=1)), writes=[res("modrow")])
        for nb in range(24):
            wres = res(f"wm{nb % 2}")
            P.dma("sp" if nb % 2 == 0 else "act", lambda e, nb=nb: e.dma_start(out=wm[nb % 2], in_=w_mod_v[:, :, nb * 512:(nb + 1) * 512]),
                  writes=[wres])

            def mm(e, nb=nb):
                for c in range(16):
                    ins = e.matmul(psf[nb % 2][0:1, :], lhsT=c_act[:, c:c + 1], rhs=wm[nb % 2][:, c, :],
                                   start=(c == 0), stop=(c == 15))
                return ins
            P.op("pe", mm, reads=[wres, res("c_act")], writes=[bank[nb % 2]])
            P.op("dve", lambda e, nb=nb: e.tensor_tensor(
                out=modrow[:, nb * 512:(nb + 1) * 512], in0=psf[nb % 2][0:1, :],
                in1=modrow[:, nb * 512:(nb + 1) * 512], op=ALU.add),
                reads=[bank[nb % 2], res("modrow")], writes=[res("modrow")])
        P.dma("sp", lambda e: e.dma_start(out=mod_d.rearrange("(o n) -> o n", o=1), in_=modrow),
              reads=[res("modrow")], writes=[res("mod_d")])
        P.barrier()

        def bload(qn, dst, src_row, rname):
            return P.dma(qn, lambda e: e.dma_start(out=dst, in_=src_row.partition_broadcast(128)),
                         reads=[res("mod_d")], writes=[res(rname)])

        tmpb = A(A_SCR, 2048)
        bload("sp", brd[0], norm1_g, "brd0")
        bload("sp", tmpb, mod_d[D:2 * D], "tmpb")
        bload("sp", brd[1], mod_d[0:D], "brd1")
        P.op("dve", lambda e: e.scalar_tensor_tensor(out=brd[0], in0=tmpb, scalar=1.0, in1=brd[0],
                                                     op0=ALU.add, op1=ALU.mult),
             reads=[res("tmpb"), res("brd0")], writes=[res("brd0")])
        P.barrier()

        xs = [A(A_SCR + i * 2048, 2048) for i in range(2)]
        tmpf = A(A_SCR + 4096, 2048)
        hbf = [Ab(A_SCR + 6144 + i * 1024, 1024) for i in range(2)]
        t12 = [A(A_SCR + 8192 + i * 512, 512) for i in range(4)]
        ssq = small[:, 0:8]
        rstd = small[:, 8:16]

        def trig_tables(qi):
            ang = xs[0][:, 0:1024]
            kf = xs[0][:, 1024:2048]
            ki = kf.bitcast(I32)
            rr = xs[1][:, 0:1024]
            ab = xs[1][:, 1024:2048]
            rs_ = [res("xs0"), res("xs1")]
            P.op("dve", lambda e: e.tensor_scalar(out=ang, in0=iota_t, scalar1=pc_sb[:, PC_BASE + qi:PC_BASE + qi + 1],
                                                  scalar2=freq, op0=ALU.add, op1=ALU.mult),
                 reads=[res("cst"), res("pcst"), res("freq")], writes=[rs_[0]])
            P.op("dve", lambda e: e.tensor_scalar(out=rr.bitcast(I32), in0=ang, scalar1=INV_2PI, scalar2=None, op0=ALU.mult),
                 reads=[rs_[0]], writes=[rs_[1]])
            P.op("dve", lambda e: e.tensor_copy(out=kf, in_=rr.bitcast(I32)), reads=[rs_[1]], writes=[rs_[0]])
            P.op("dve", lambda e: e.scalar_tensor_tensor(out=rr, in0=kf, scalar=-TWO_PI_HI, in1=ang, op0=ALU.mult, op1=ALU.add),
                 reads=[rs_[0]], writes=[rs_[1]])
            P.op("dve", lambda e: e.scalar_tensor_tensor(out=rr, in0=kf, scalar=-TWO_PI_LO, in1=rr, op0=ALU.mult, op1=ALU.add),
                 reads=[rs_[0], rs_[1]], writes=[rs_[1]])
            P.op("dve", lambda e: e.tensor_scalar(out=rr, in0=rr, scalar1=PI_LO, scalar2=-PI_LO, op0=ALU.min, op1=ALU.max),
                 reads=[rs_[1]], writes=[rs_[1]])
            P.op("dve", lambda e: e.scalar_tensor_tensor(out=ab, in0=rr, scalar=-1.0, in1=rr, op0=ALU.mult, op1=ALU.max),
                 reads=[rs_[1]], writes=[rs_[1]])
            P.op("act", lambda e: e.activation(out=sin_t, in_=rr, func=AF.Sin, scale=col(C_SIGN)),
                 reads=[rs_[1], res("cst")], writes=[res("trig")])
            P.op("act", lambda e: e.activation(out=cos_t, in_=ab, func=AF.Sin, scale=-1.0, bias=col(C_HALFPI)),
                 reads=[rs_[1], res("cst")], writes=[res("trig")])

        hTB_lo = Ab(A_SBF, 4096).rearrange("p (c t) -> p c t", c=8)
        hTB_hi = Ab(A_YTR, 4096).rearrange("p (c t) -> p c t", c=8)

        class HBuf:
            def __init__(self, which):
                self.which = which
                self.r = res("hT" if which == "A" else "hTB")

            def chunk(self, c, tok):
                if self.which == "A":
                    return hT[:, c, tok]
                return (hTB_lo if c < 8 else hTB_hi)[:, c % 8, tok]

            def lo(self, tok):
                return hT[:, 0:8, tok] if self.which == "A" else hTB_lo[:, :, tok]

            def hi(self, tok):
                return hT[:, 8:16, tok] if self.which == "A" else hTB_hi[:, :, tok]

        HA, HB = HBuf("A"), HBuf("B")

        def p1_tile(qi, i, hb):
            x_src = x_q[qi].rearrange("(i p) d -> i p d", p=128)
            if True:
                xr = res(f"xs{i % 2}")
                hr = res(f"hbf{i % 2}")
                P.dma("sp", lambda e, i=i: e.dma_start(out=xs[i % 2], in_=x_src[i]), writes=[xr])
                P.op("act", lambda e, i=i: e.activation(out=hbf[i % 2], in_=xs[i % 2], func=AF.Square,
                                                       accum_out=ssq[:, i:i + 1]),
                     reads=[xr], writes=[hr, res(f"ssq{i}")])
                P.op("act", lambda e, i=i: e.activation(out=rstd[:, i:i + 1], in_=ssq[:, i:i + 1], func=AF.Sqrt,
                                                       scale=1.0 / D, bias=col(C_EPS)),
                     reads=[res(f"ssq{i}"), res("cst")], writes=[res(f"rstd{i}")])
                P.op("dve", lambda e, i=i: e.reciprocal(out=rstd[:, i:i + 1], in_=rstd[:, i:i + 1]),
                     reads=[res(f"rstd{i}")], writes=[res(f"rstd{i}")])
                P.op("dve", lambda e, i=i: e.scalar_tensor_tensor(out=tmpf, in0=xs[i % 2], scalar=rstd[:, i:i + 1],
                                                                   in1=brd[0], op0=ALU.mult, op1=ALU.mult),
                     reads=[xr, res(f"rstd{i}"), res("brd0")], writes=[res("tmpf")])
                P.op("pool", lambda e, i=i: e.tensor_tensor(out=hbf[i % 2], in0=tmpf, in1=brd[1], op=ALU.add),
                     reads=[res("tmpf"), res("brd1")], writes=[hr])
                pb = 2 * (i % 2)

                def tr(e, i=i, pb=pb):
                    for c in range(16):
                        ins = e.transpose(out=psb(pb + c // 8)[:, (c % 8) * 128:(c % 8 + 1) * 128],
                                          in_=hbf[i % 2][:, c * 128:(c + 1) * 128], identity=ident_bf)
                    return ins
                P.op("pe", tr, reads=[hr, res("ident_bf")], writes=[bank[pb], bank[pb + 1]])
                P.op("act", lambda e, i=i, pb=pb: e.activation(
                    out=hb.lo(slice(i * 128, (i + 1) * 128)), in_=psb(pb).rearrange("p (c t) -> p c t", c=8), func=AF.Copy),
                    reads=[bank[pb]], writes=[hb.r])
                P.op("dve", lambda e, i=i, pb=pb: e.tensor_copy(
                    out=hb.hi(slice(i * 128, (i + 1) * 128)), in_=psb(pb + 1).rearrange("p (c t) -> p c t", c=8)),
                    reads=[bank[pb + 1]], writes=[hb.r])

        def load_w(slot, cols0, ncols, swap=False):
            wr = res(f"wb{slot}")
            if not swap:
                dst = wb[slot][:, 0:16 * ncols].rearrange("p (c n) -> p c n", c=16)
                P.dma("pool", lambda e: e.dma_start(out=dst, in_=w_in_v[:, :, cols0:cols0 + ncols]), writes=[wr])
            else:
                dstv = wb[slot][:, 0:16 * 384].rearrange("p (c h n) -> p c h n", c=16, h=2)
                srcv = w_in_v[:, :, cols0:cols0 + 256].rearrange("p c (h d) -> p c h d", h=2)
                for hh in range(2):
                    P.dma("pool", lambda e, hh=hh: e.dma_start(out=dstv[:, :, hh, 64:192], in_=srcv[:, :, hh, :]), writes=[wr])
                    P.dma("pool", lambda e, hh=hh: e.dma_start(out=dstv[:, :, hh, 0:64], in_=srcv[:, :, hh, 64:128]), writes=[wr])
            return wr

        def rope_block(slot, h0, is_q, qdst=None, qtdst=None, decq=None, hb=None):
            hb = hb or HA
            wr = res(f"wb{slot}")
            wv = wb[slot][:, 0:16 * 384].rearrange("p (c h n) -> p c h n", c=16, h=2)
            for hh in range(2):
                for th in range(2):
                    ba, bb = 4 + 2 * ((hh * 2 + th) % 2), 5 + 2 * ((hh * 2 + th) % 2)
                    ta, tb = t12[2 * ((hh * 2 + th) % 2)], t12[2 * ((hh * 2 + th) % 2) + 1]
                    tra, trb = res(f"t12_{2 * ((hh * 2 + th) % 2)}"), res(f"t12_{2 * ((hh * 2 + th) % 2) + 1}")
                    tok = slice(th * 512, (th + 1) * 512)

                    def mma(e, hh=hh, tok=tok, ba=ba):
                        for c in range(16):
                            ins = e.matmul(psf[ba][:, :], lhsT=wv[:, c, hh, 64:192], rhs=hb.chunk(c, tok),
                                           start=(c == 0), stop=(c == 15))
                        return ins

                    def mmb(e, hh=hh, tok=tok, bb=bb):
                        for c in range(16):
                            ins = e.matmul(psf[bb][:, :], lhsT=wv[:, c, hh, 0:128], rhs=hb.chunk(c, tok),
                                           start=(c == 0), stop=(c == 15))
                        return ins
                    P.op("pe", mma, reads=[wr, hb.r], writes=[bank[ba]])
                    P.op("pe", mmb, reads=[wr, hb.r], writes=[bank[bb]])
                    P.op("dve", lambda e, ta=ta, ba=ba, tok=tok: e.tensor_tensor(out=ta, in0=psf[ba][:, :], in1=cos_t[:, tok], op=ALU.mult),
                         reads=[bank[ba], res("trig")], writes=[tra])
                    P.op("dve", lambda e, tb=tb, bb=bb, tok=tok: e.tensor_tensor(out=tb, in0=psf[bb][:, :], in1=sin_t[:, tok], op=ALU.mult),
                         reads=[bank[bb], res("trig")], writes=[trb])
                    if not is_q:
                        P.op("pool", lambda e, ta=ta, tb=tb, hh=hh, tok=tok: e.tensor_tensor(
                            out=kT[:, h0 + hh, tok], in0=ta, in1=tb, op=ALU.add),
                            reads=[tra, trb], writes=[res("kT")])
                    else:
                        P.op("pool", lambda e, ta=ta, tb=tb: e.tensor_tensor(out=ta, in0=ta, in1=tb, op=ALU.add),
                             reads=[tra, trb], writes=[tra])
                        P.op("act", lambda e, ta=ta, hh=hh, tok=tok: e.activation(out=qdst[:, hh, tok], in_=ta, func=AF.Copy, scale=QSCALE),
                             reads=[tra], writes=[res("qT")])
                        P.op("dve", lambda e, ta=ta, hh=hh, tok=tok: e.tensor_tensor(out=qtdst[:, hh, tok], in0=ta, in1=decq[hh][:, tok], op=ALU.mult),
                             reads=[tra, res(f"decq{hh}")], writes=[res("qtT")])

        def v_block(slot, vb, hb=None):
            hb = hb or HA
            wr = res(f"wb{slot}")
            wv = wb[slot][:, 0:16 * 256].rearrange("p (c n) -> p c n", c=16)
            for i in range(NT):
                b = 4 + (i % 4)

                def mm(e, i=i, b=b):
                    for c in range(16):
                        ins = e.matmul(psf[b][:, 0:256], lhsT=hb.chunk(c, slice(i * 128, (i + 1) * 128)), rhs=wv[:, c, :],
                                       start=(c == 0), stop=(c == 15))
                    return ins
                P.op("pe", mm, reads=[wr, hb.r], writes=[bank[b]])
                P.op("act", lambda e, i=i, b=b: e.activation(out=v_sb[:, i, vb * 256:(vb + 1) * 256], in_=psf[b][:, 0:256], func=AF.Copy),
                     reads=[bank[b]], writes=[res("v")])

        kdec_sb = Ab(A_SCR + 4096, 512).rearrange("p (h d) -> p h d", h=NH)

        def state_phase(qi, main):
            for i in range(NT):
                def tr(e, i=i):
                    for h in range(NH):
                        ins = e.transpose(out=psb(0)[:, h * 128:(h + 1) * 128], in_=kT[:, h, i * 128:(i + 1) * 128],
                                          identity=ident_bf)
                    return ins
                P.op("pe", tr, reads=[res("kT"), res("ident_bf")], writes=[bank[0]])
                P.op("dve", lambda e, i=i: e.tensor_tensor(
                    out=kdec_sb, in0=psb(0).rearrange("p (h d) -> p h d", h=NH),
                    in1=kdec_tab[:, qi * 8 + i, :].unsqueeze(2).broadcast_to([128, NH, 128]), op=ALU.mult),
                    reads=[bank[0], res("kdec")], writes=[res("tmpf")])

                def inc(e, i=i):
                    for h in range(NH):
                        ins = e.matmul(psf[2 + h // 4][:, (h % 4) * 128:(h % 4 + 1) * 128], lhsT=kdec_sb[:, h, :],
                                       rhs=v_sb[:, i, h * 128:(h + 1) * 128], start=True, stop=True)
                    return ins
                P.op("pe", inc, reads=[res("tmpf"), res("v")], writes=[bank[2], bank[3]])
                if main:
                    P.op("act", lambda e, i=i: e.activation(out=S_bf[:, i, :, :], in_=S_f32, func=AF.Copy),
                         reads=[res("S")], writes=[res("S_bf")])
                for hb in range(2):
                    P.op("dve", lambda e, hb=hb: e.tensor_tensor(
                        out=S_f32[:, hb * 4:(hb + 1) * 4, :], in0=S_f32[:, hb * 4:(hb + 1) * 4, :],
                        in1=psf[2 + hb][:, :].rearrange("p (h e) -> p h e", h=4), op=ALU.add),
                        reads=[bank[2 + hb], res("S")], writes=[res("S")])

        P.op("pool", lambda e: e.memset(S_f32, 0.0), writes=[res("S")])

        hbufs = [HB, HA, HB, HA]
        trig_tables(0)
        for i in range(NT):
            p1_tile(0, i, hbufs[0])
        for qi in range(4):
            main = qi == 3
            hb = hbufs[qi]
            if qi == 2:
                P.op("dve", lambda e: e.tensor_copy(out=hT_halo[:, 0:8, :], in_=hTB_lo[:, :, 1022:1024]),
                     reads=[HB.r], writes=[res("hT_halo")])
                P.op("dve", lambda e: e.tensor_copy(out=hT_halo[:, 8:16, :], in_=hTB_hi[:, :, 1022:1024]),
                     reads=[HB.r], writes=[res("hT_halo")])
            blocks = [("k", g) for g in range(4)] + [("v", g) for g in range(4)]
            load_w(0, D_RET + 0, 256, swap=True)
            for bi, (kind, g) in enumerate(blocks):
                slot = bi % 2
                if bi + 1 < len(blocks):
                    nk, ng = blocks[bi + 1]
                    if nk == "k":
                        load_w((bi + 1) % 2, D_RET + ng * 256, 256, swap=True)
                    else:
                        load_w((bi + 1) % 2, 2 * D_RET + ng * 256, 256)
                if kind == "k":
                    rope_block(slot, 2 * g, False, hb=hb)
                else:
                    v_block(slot, g, hb=hb)
                if qi < 3:
                    if bi == 4:
                        trig_tables(qi + 1)
                    p1_tile(qi + 1, bi, hbufs[qi + 1])
            state_phase(qi, main)
        P.barrier()

        qT = Ab(A_SCR + 0, 1024).rearrange("p (h t) -> p h t", h=2)
        qtT = Ab(A_SCR + 1024, 1024).rearrange("p (h t) -> p h t", h=2)
        sg = Ab(A_SCR + 2048, 1024).rearrange("p (i n) -> p i n", i=NT)
        decq = [A(A_SCR + 3072 + i * 1024, 1024) for i in range(2)]
        Pm = [Ab(A_SCR + 5120 + i * 128, 128).rearrange("p (h k) -> p h k", h=2) for i in range(2)]
        onb = [A(A_SCR + 5376 + i * 256, 256).rearrange("p (h k) -> p h k", h=2) for i in range(2)]
        yrb = [Ab(A_SCR + 5888 + i * 128, 128).rearrange("p (h k) -> p h k", h=2) for i in range(2)]
        bst = A(A_SCR + 6144, 32).rearrange("p (a h s) -> p a h s", a=2, h=2)
        bmv = A(A_SCR + 6176, 16).rearrange("p (a h s) -> p a h s", a=2, h=2)
        brs = A(A_SCR + 6192, 4).rearrange("p (a h) -> p a h", a=2)

        for g in range(4):
            for hh in range(2):
                P.op("act", lambda e, hh=hh, g=g: e.activation(out=decq[hh], in_=iota_t, func=AF.Exp,
                                                           scale=LOG_GAMMA[2 * g + hh], bias=col(C_LNQ)),
                     reads=[res("cst")], writes=[res(f"decq{hh}")])
            if g == 0:
                load_w(0, 0, 256, swap=True)
                load_w(1, 3 * D_RET, 256)
            rope_block(0, 2 * g, True, qdst=qT, qtdst=qtT, decq=decq)
            if g + 1 < 4:
                load_w(0, 2 * (g + 1) * 128, 256, swap=True)
            wgv = wb[1][:, 0:16 * 256].rearrange("p (c n) -> p c n", c=16)
            for i in range(NT):
                b = i % 2

                def mmg(e, i=i, b=b):
                    for c in range(16):
                        ins = e.matmul(psf[b][:, 0:256], lhsT=hT[:, c, i * 128:(i + 1) * 128], rhs=wgv[:, c, :],
                                       start=(c == 0), stop=(c == 15))
                    return ins
                P.op("pe", mmg, reads=[res("wb1"), res("hT")], writes=[bank[b]])
                P.op("act", lambda e, i=i, b=b: e.activation(out=sg[:, i, :], in_=psf[b][:, 0:256], func=AF.Silu),
                     reads=[bank[b]], writes=[res("sg")])
            if g + 1 < 4:
                load_w(1, 3 * D_RET + (g + 1) * 256, 256)
            for i in range(NT):
                a = i % 2
                tok = slice(i * 128, (i + 1) * 128)
                rPm, ron, ryr, rst = res(f"Pm{a}"), res(f"on{a}"), res(f"yr{a}"), res(f"bst{a}")

                def sc(e, g=g, tok=tok, a=a):
                    for hh in range(2):
                        ins = e.matmul(psf[2 + a][:, hh * 128:(hh + 1) * 128], lhsT=kT[:, 2 * g + hh, tok], rhs=qT[:, hh, tok],
                                       start=True, stop=True)
                    return ins
                P.op("pe", sc, reads=[res("kT"), res("qT")], writes=[bank[2 + a]])
                P.op("dve", lambda e, g=g, a=a: e.tensor_tensor(
                    out=Pm[a], in0=psf[2 + a][:, 0:256].rearrange("p (h k) -> p h k", h=2),
                    in1=maskT[:, 2 * g:2 * g + 2, :], op=ALU.mult),
                    reads=[bank[2 + a], res("maskT")], writes=[rPm])

                def om(e, g=g, i=i, tok=tok, a=a):
                    for hh in range(2):
                        h = 2 * g + hh
                        e.matmul(psf[4 + a][:, hh * 128:(hh + 1) * 128], lhsT=Pm[a][:, hh, :],
                                 rhs=v_sb[:, i, h * 128:(h + 1) * 128], start=True, stop=False)
                        ins = e.matmul(psf[4 + a][:, hh * 128:(hh + 1) * 128], lhsT=qtT[:, hh, tok],
                                       rhs=S_bf[:, i, h, :], start=False, stop=True)
                    return ins
                P.op("pe", om, reads=[rPm, res("v"), res("qtT"), res("S_bf")], writes=[bank[4 + a]])
                for hh in range(2):
                    P.op("dve", lambda e, hh=hh, a=a: e.bn_stats(out=bst[:, a, hh, 0:6], in_=psf[4 + a][:, hh * 128:(hh + 1) * 128]),
                         reads=[bank[4 + a]], writes=[rst])
                for hh in range(2):
                    P.op("dve", lambda e, hh=hh, a=a: e.bn_aggr(out=bmv[:, a, hh, 0:2], in_=bst[:, a, hh, 0:6]),
                         reads=[rst], writes=[rst])
                P.op("act", lambda e, a=a: e.activation(out=brs[:, a, :], in_=bmv[:, a, :, 1], func=AF.Sqrt,
                                                        bias=col(C_EPS)),
                     reads=[rst, res("cst")], writes=[res(f"brs{a}")])
                P.op("dve", lambda e, a=a: e.reciprocal(out=brs[:, a, :], in_=brs[:, a, :]),
                     reads=[res(f"brs{a}")], writes=[res(f"brs{a}")])
                for hh in range(2):
                    P.op("dve", lambda e, hh=hh, a=a: e.tensor_scalar(
                        out=onb[a][:, hh, :], in0=psf[4 + a][:, hh * 128:(hh + 1) * 128],
                        scalar1=bmv[:, a, hh, 0:1], scalar2=brs[:, a, hh:hh + 1], op0=ALU.subtract, op1=ALU.mult),
                        reads=[bank[4 + a], rst, res(f"brs{a}")], writes=[ron])
                P.op("pool", lambda e, i=i, a=a: e.tensor_tensor(
                    out=yrb[a], in0=onb[a], in1=sg[:, i, :].rearrange("p (h k) -> p h k", h=2), op=ALU.mult),
                    reads=[ron, res("sg")], writes=[ryr])

                def trY(e, a=a):
                    for hh in range(2):
                        ins = e.transpose(out=psb(6 + a)[:, hh * 128:(hh + 1) * 128], in_=yrb[a][:, hh, :], identity=ident_bf)
                    return ins
                P.op("pe", trY, reads=[ryr, res("ident_bf")], writes=[bank[6 + a]])
                P.op("act", lambda e, g=g, tok=tok, a=a: e.activation(
                    out=yTr[:, 2 * g:2 * g + 2, tok], in_=psb(6 + a)[:, 0:256].rearrange("p (h k) -> p h k", h=2), func=AF.Copy),
                    reads=[bank[6 + a]], writes=[res("yTr")])
        P.barrier()

        uT = A(A_SCR + 0, 2056)[:, 0:2052].rearrange("p (c t) -> p c t", c=2)
        acc = A(A_SCR + 2056, 2048).rearrange("p (c t) -> p c t", c=2)
        BASE_B, BASE_C, BASE_U = 4 * D_RET, 4 * D_RET + D_CONV, 4 * D_RET + 2 * D_CONV

        def conv_mm(slot, cc, with_halo):
            wv = wb[slot][:, 0:16 * 256].rearrange("p (c n) -> p c n", c=16)
            for th in range(2):
                b = cc * 2 + th

                def mm(e, b=b, th=th):
                    for c in range(16):
                        ins = e.matmul(psf[b][:, :], lhsT=wv[:, c, cc * 128:(cc + 1) * 128], rhs=hT[:, c, th * 512:(th + 1) * 512],
                                       start=(c == 0), stop=(c == 15))
                    return ins
                P.op("pe", mm, reads=[res(f"wb{slot}"), res("hT")], writes=[bank[b]])
            if with_halo:
                def mmh(e):
                    for c in range(16):
                        ins = e.matmul(psf[4 + cc][:, 0:2], lhsT=wv[:, c, cc * 128:(cc + 1) * 128], rhs=hT_halo[:, c, :],
                                       start=(c == 0), stop=(c == 15))
                    return ins
                P.op("pe", mmh, reads=[res(f"wb{slot}"), res("hT_halo")], writes=[bank[4 + cc]])

        cjobs = []
        for cg in range(4):
            cjobs += [("u", cg, BASE_U + cg * 256), ("c", cg, BASE_C + cg * 256), ("b", cg, BASE_B + cg * 256)]
        load_w(0, cjobs[0][2], 256)
        for ji, (kind, cg, cols0) in enumerate(cjobs):
            sl = ji % 2
            if ji + 1 < len(cjobs):
                load_w((ji + 1) % 2, cjobs[ji + 1][2], 256)
            if kind == "u":
                for cc in range(2):
                    conv_mm(sl, cc, True)
                    for th in range(2):
                        P.op("act", lambda e, cc=cc, th=th: e.activation(
                            out=uT[:, cc, 2 + th * 512:2 + (th + 1) * 512], in_=psf[cc * 2 + th][:, :], func=AF.Copy),
                            reads=[bank[cc * 2 + th]], writes=[res("uT")])
                    P.op("act", lambda e, cc=cc: e.activation(out=uT[:, cc, 0:2], in_=psf[4 + cc][:, 0:2], func=AF.Copy),
                         reads=[bank[4 + cc]], writes=[res("uT")])
            elif kind == "c":
                for cc in range(2):
                    conv_mm(sl, cc, True)
                    for th in range(2):
                        P.op("dve", lambda e, cc=cc, th=th: e.tensor_tensor(
                            out=uT[:, cc, 2 + th * 512:2 + (th + 1) * 512], in0=psf[cc * 2 + th][:, :],
                            in1=uT[:, cc, 2 + th * 512:2 + (th + 1) * 512], op=ALU.mult),
                            reads=[bank[cc * 2 + th], res("uT")], writes=[res("uT")])
                    P.op("dve", lambda e, cc=cc: e.scalar_tensor_tensor(
                        out=uT[:, cc, 0:2], in0=psf[4 + cc][:, 0:2], scalar=pc_sb[:, PC_FLAG:PC_FLAG + 1], in1=uT[:, cc, 0:2],
                        op0=ALU.mult, op1=ALU.mult),
                        reads=[bank[4 + cc], res("uT"), res("pcst")], writes=[res("uT")])
                for cc in range(2):
                    ch = cg * 2 + cc
                    P.op("pool", lambda e, cc=cc, ch=ch: e.tensor_scalar(
                        out=acc[:, cc, :], in0=uT[:, cc, 2:1026], scalar1=cw_sb[:, 2, ch:ch + 1], scalar2=None, op0=ALU.mult),
                        reads=[res("uT"), res("cw")], writes=[res("acc")])
                    P.op("dve", lambda e, cc=cc, ch=ch: e.scalar_tensor_tensor(
                        out=acc[:, cc, :], in0=uT[:, cc, 1:1025], scalar=cw_sb[:, 1, ch:ch + 1], in1=acc[:, cc, :],
                        op0=ALU.mult, op1=ALU.add),
                        reads=[res("uT"), res("cw"), res("acc")], writes=[res("acc")])
                    P.op("dve", lambda e, cc=cc, ch=ch: e.scalar_tensor_tensor(
                        out=acc[:, cc, :], in0=uT[:, cc, 0:1024], scalar=cw_sb[:, 0, ch:ch + 1], in1=acc[:, cc, :],
                        op0=ALU.mult, op1=ALU.add),
                        reads=[res("uT"), res("cw"), res("acc")], writes=[res("acc")])
            else:
                for cc in range(2):
                    conv_mm(sl, cc, False)
                    for th in range(2):
                        P.op("dve", lambda e, cc=cc, th=th, cg=cg: e.tensor_tensor(
                            out=yTc[:, cg * 2 + cc, th * 512:(th + 1) * 512], in0=psf[cc * 2 + th][:, :],
                            in1=acc[:, cc, th * 512:(th + 1) * 512], op=ALU.mult),
                            reads=[bank[cc * 2 + th], res("acc")], writes=[res("yTc")])
        P.barrier()

        x1 = A(A_V, 16384).rearrange("p (i d) -> p i d", i=NT)
        wo = [Ab(A_HT + i * 4096, 4096).rearrange("p (c n) -> p c n", c=16) for i in range(2)]
        otmp = [A(A_SCR + 8192 + i * 512, 512) for i in range(2)]
        P.dma("sp", lambda e: e.dma_start(out=x1, in_=x_q[3].rearrange("(i p) d -> p i d", p=128)), writes=[res("x1")])
        bload("act", brd[0], mod_d[2 * D:3 * D], "brd0")

        def load_wo(nb):
            P.dma("pool", lambda e: e.dma_start(out=wo[nb % 2], in_=w_out_v[:, :, nb * 512:(nb + 1) * 512]),
                  writes=[res(f"wo{nb % 2}")])
        load_wo(0)
        for nb in range(4):
            if nb + 1 < 4:
                load_wo(nb + 1)
            for i in range(NT):
                b = i % 4

                def mm(e, nb=nb, i=i, b=b):
                    for c in range(16):
                        src = yTr[:, c, i * 128:(i + 1) * 128] if c < 8 else yTc[:, c - 8, i * 128:(i + 1) * 128]
                        ins = e.matmul(psf[b][:, :], lhsT=src, rhs=wo[nb % 2][:, c, :], start=(c == 0), stop=(c == 15))
                    return ins
                P.op("pe", mm, reads=[res(f"wo{nb % 2}"), res("yTr"), res("yTc")], writes=[bank[b]])
                P.op("dve", lambda e, nb=nb, i=i, b=b: e.tensor_tensor(
                    out=otmp[i % 2], in0=psf[b][:, :], in1=brd[0][:, nb * 512:(nb + 1) * 512], op=ALU.mult),
                    reads=[bank[b], res("brd0")], writes=[res(f"otmp{i % 2}")])
                P.op("pool", lambda e, nb=nb, i=i: e.tensor_tensor(
                    out=x1[:, i, nb * 512:(nb + 1) * 512], in0=x1[:, i, nb * 512:(nb + 1) * 512], in1=otmp[i % 2], op=ALU.add),
                    reads=[res(f"otmp{i % 2}"), res("x1")], writes=[res("x1")])
        P.barrier()

        if stage == "x1":
            P.dma("sp", lambda e: e.dma_start(out=out.rearrange("(i p) d -> p i d", p=128), in_=x1), reads=[res("x1")])
            P.barrier()
        if stage != "x1":
            tmp2 = A(A_HT, 2048)
            hb2 = [Ab(A_HT + 2048 + i * 1024, 1024) for i in range(2)]
            h2Tf = A(A_HT + 4096, 2048).rearrange("p (c t) -> p c t", c=16)
            wr_sb = A(A_HT + 6144, 512).rearrange("p (c e) -> p c e", c=16)
            brow = arena[0:1, A_HT + 6656:A_HT + 6688]
            so = A_HT + 6688
            lg = A(so, 32); so += 32
            rk = A(so, 32); so += 32
            oh = A(so, 32); so += 32
            ecap = A(so, 32); so += 32
            mkf = A(so, 32); so += 32
            mx8 = A(so, 8); so += 8
            mi8 = A(so, 8).bitcast(U32); so += 8
            idf8 = A(so, 8); so += 8
            ex4 = A(so, 4); so += 4
            destf = A(so, 4); so += 4
            lim4 = A(so, 4); so += 4
            val4 = A(so, 4); so += 4
            nmx = A(so, 1); so += 1
            ssum = A(so, 1); so += 1
            so += 2
            tri_bf = Ab(so, 64); so += 64
            ones_bf = Ab(so, 64); so += 64
            mk_bf = Ab(so, 128).rearrange("p (i e) -> p i e", i=NT); so += 128
            assert so <= A_HT + 8192
            w4_all = A(A_YTR + 4096, 32).rearrange("p (i k) -> p i k", i=NT)
            dest_all = A(A_YTR + 4128, 32).bitcast(I32).rearrange("p (i k) -> p i k", i=NT)
            ssq2 = A(A_YTR + 4352, 8)
            rstd2 = A(A_YTR + 4360, 8)
            ones_row = cst_sb[0:1, C_ONES:C_ONES + 128]
            iota_e = cst_sb[:, C_IOTA_E:C_IOTA_E + 32]

            tmpc = A(A_TRIG, 2048)
            bload("sp", brd[0], norm2_g, "brd0")
            bload("sp", tmpc, mod_d[4 * D:5 * D], "tmpc")
            bload("act", brd[1], mod_d[3 * D:4 * D], "brd1")
            P.op("dve", lambda e: e.scalar_tensor_tensor(out=brd[0], in0=tmpc, scalar=1.0, in1=brd[0],
                                                         op0=ALU.add, op1=ALU.mult),
                 reads=[res("tmpc"), res("brd0")], writes=[res("brd0")])
            P.dma("sp", lambda e: e.dma_start(out=wr_sb, in_=w_router.rearrange("(c p) e -> p c e", p=128)),
                  writes=[res("wr_sb")])
            P.dma("sp", lambda e: e.dma_start(out=brow, in_=b_router.rearrange("(o n) -> o n", o=1)), writes=[res("brow")])
            P.op("dve", lambda e: e.tensor_copy(out=tri_bf, in_=cst_sb[:, C_TRI:C_TRI + 128]), reads=[res("cst")], writes=[res("tri")])
            P.op("dve", lambda e: e.tensor_copy(out=ones_bf, in_=cst_sb[:, C_ONES:C_ONES + 128]), reads=[res("cst")], writes=[res("tri")])
            P.op("dve", lambda e: e.tensor_scalar(out=ecap, in0=iota_e, scalar1=float(CAP), scalar2=None, op0=ALU.mult),
                 reads=[res("cst")], writes=[res("ecap")])

            for i in range(NT):
                hb = hb2[i % 2]
                rhb = res(f"hb2_{i % 2}")
                P.op("act", lambda e, i=i, hb=hb: e.activation(out=hb, in_=x1[:, i, :], func=AF.Square, accum_out=ssq2[:, i:i + 1]),
                     reads=[res("x1")], writes=[rhb, res("rs2")])
                P.op("act", lambda e, i=i: e.activation(out=rstd2[:, i:i + 1], in_=ssq2[:, i:i + 1], func=AF.Sqrt,
                                                       scale=1.0 / D, bias=col(C_EPS)),
                     reads=[res("rs2"), res("cst")], writes=[res("rs2")])
                P.op("dve", lambda e, i=i: e.reciprocal(out=rstd2[:, i:i + 1], in_=rstd2[:, i:i + 1]),
                     reads=[res("rs2")], writes=[res("rs2")])
                P.op("dve", lambda e, i=i: e.scalar_tensor_tensor(out=tmp2, in0=x1[:, i, :], scalar=rstd2[:, i:i + 1], in1=brd[0],
                                                                   op0=ALU.mult, op1=ALU.mult),
                     reads=[res("x1"), res("rs2"), res("brd0")], writes=[res("tmp2")])
                P.op("dve", lambda e: e.tensor_tensor(out=tmp2, in0=tmp2, in1=brd[1], op=ALU.add),
                     reads=[res("tmp2"), res("brd1")], writes=[res("tmp2")])
                P.op("act", lambda e, hb=hb: e.activation(out=hb, in_=tmp2, func=AF.Copy), reads=[res("tmp2")], writes=[rhb])

                def trf(e):
                    for c in range(16):
                        ins = e.transpose(out=psf[2 + c // 4][:, (c % 4) * 128:(c % 4 + 1) * 128],
                                          in_=tmp2[:, c * 128:(c + 1) * 128], identity=ident_f)
                    return ins
                P.op("pe", trf, reads=[res("tmp2"), res("cst")], writes=[bank[2], bank[3], bank[4], bank[5]])
                for k in range(4):
                    if k % 2 == 0:
                        P.op("act", lambda e, k=k: e.activation(out=h2Tf[:, 4 * k:4 * k + 4, :],
                                                                in_=psf[2 + k][:, :].rearrange("p (c t) -> p c t", c=4), func=AF.Copy),
                             reads=[bank[2 + k]], writes=[res("h2Tf")])
                    else:
                        P.op("dve", lambda e, k=k: e.tensor_copy(out=h2Tf[:, 4 * k:4 * k + 4, :],
                                                                 in_=psf[2 + k][:, :].rearrange("p (c t) -> p c t", c=4)),
                             reads=[bank[2 + k]], writes=[res("h2Tf")])

                def lgm(e):
                    for c in range(16):
                        e.matmul(psf[6][:, 0:32], lhsT=h2Tf[:, c, :], rhs=wr_sb[:, c, :], start=(c == 0), stop=False)
                    return e.matmul(psf[6][:, 0:32], lhsT=ones_row, rhs=brow, start=False, stop=True)
                P.op("pe", lgm, reads=[res("h2Tf"), res("wr_sb"), res("brow"), res("cst")], writes=[bank[6]])
                rt = res("rt")
                P.op("dve", lambda e: e.tensor_copy(out=lg, in_=psf[6][:, 0:32]), reads=[bank[6]], writes=[rt])
                P.op("dve", lambda e: e.max(out=mx8, in_=lg), reads=[rt], writes=[rt])
                P.op("dve", lambda e: e.max_index(out=mi8, in_max=mx8, in_values=lg), reads=[rt], writes=[rt])
                P.op("dve", lambda e: e.tensor_copy(out=idf8, in_=mi8), reads=[rt], writes=[rt])
                P.op("dve", lambda e: e.tensor_scalar(out=mkf, in0=lg, scalar1=mx8[:, 3:4], scalar2=None, op0=ALU.is_ge),
                     reads=[rt], writes=[rt])
                P.op("dve", lambda e, i=i: e.tensor_copy(out=mk_bf[:, i, :], in_=mkf), reads=[rt], writes=[res("mk_bf")])
                P.op("dve", lambda e: e.tensor_scalar(out=nmx, in0=mx8[:, 0:1], scalar1=-1.0, scalar2=None, op0=ALU.mult),
                     reads=[rt], writes=[rt])
                P.op("act", lambda e: e.activation(out=ex4, in_=mx8[:, 0:4], func=AF.Exp, bias=nmx), reads=[rt], writes=[rt])
                P.op("dve", lambda e: e.reduce_sum(out=ssum, in_=ex4, axis=mybir.AxisListType.X), reads=[rt], writes=[rt])
                P.op("dve", lambda e: e.reciprocal(out=ssum, in_=ssum), reads=[rt], writes=[rt])
                P.op("dve", lambda e, i=i: e.tensor_scalar(out=w4_all[:, i, :], in0=ex4, scalar1=ssum, scalar2=None, op0=ALU.mult),
                     reads=[rt], writes=[res("w4")])

                def rkm(e, i=i):
                    for ip in range(i):
                        e.matmul(psf[7][:, 0:32], lhsT=ones_bf, rhs=mk_bf[:, ip, :], start=(ip == 0), stop=False)
                    return e.matmul(psf[7][:, 0:32], lhsT=tri_bf, rhs=mk_bf[:, i, :], start=(i == 0), stop=True)
                P.op("pe", rkm, reads=[res("mk_bf"), res("tri")], writes=[bank[7]])
                P.op("dve", lambda e: e.tensor_tensor(out=rk, in0=psf[7][:, 0:32], in1=ecap, op=ALU.add),
                     reads=[bank[7], res("ecap")], writes=[rt])
                for k in range(TOPK):
                    P.op("dve", lambda e, k=k: e.tensor_scalar(out=oh, in0=iota_e, scalar1=idf8[:, k:k + 1], scalar2=None, op0=ALU.is_equal),
                         reads=[rt, res("cst")], writes=[rt])
                    P.op("dve", lambda e: e.tensor_tensor(out=oh, in0=oh, in1=rk, op=ALU.mult), reads=[rt], writes=[rt])
                    P.op("dve", lambda e, k=k: e.reduce_sum(out=destf[:, k:k + 1], in_=oh, axis=mybir.AxisListType.X),
                         reads=[rt], writes=[rt])
                P.op("dve", lambda e: e.tensor_scalar(out=lim4, in0=idf8[:, 0:4], scalar1=1.0, scalar2=float(CAP), op0=ALU.add, op1=ALU.mult),
                     reads=[rt], writes=[rt])
                P.op("dve", lambda e: e.tensor_tensor(out=val4, in0=destf, in1=lim4, op=ALU.is_lt), reads=[rt], writes=[rt])
                P.op("dve", lambda e: e.tensor_scalar(out=destf, in0=destf, scalar1=-float(NE * CAP), scalar2=None, op0=ALU.add),
                     reads=[rt], writes=[rt])
                P.op("dve", lambda e: e.tensor_tensor(out=destf, in0=destf, in1=val4, op=ALU.mult), reads=[rt], writes=[rt])
                P.op("dve", lambda e: e.tensor_scalar(out=destf, in0=destf, scalar1=float(NE * CAP), scalar2=None, op0=ALU.add),
                     reads=[rt], writes=[rt])
                P.op("dve", lambda e, i=i: e.tensor_tensor(out=w4_all[:, i, :], in0=w4_all[:, i, :], in1=val4, op=ALU.mult),
                     reads=[rt, res("w4")], writes=[res("w4")])
                P.op("dve", lambda e, i=i: e.tensor_copy(out=dest_all[:, i, :], in_=destf), reads=[rt], writes=[res("dest")])
                for k in range(TOPK):
                    P.dma("pool", lambda e, i=i, k=k, hb=hb: e.indirect_dma_start(
                        out=xe_d, out_offset=bass.IndirectOffsetOnAxis(ap=dest_all[:, i, k:k + 1], axis=0),
                        in_=hb, in_offset=None),
                        reads=[rhb, res("dest")], writes=[res("xe_d")])
            P.barrier()

            bgu_all = A(A_SF32, 1024).rearrange("p (c e) -> p c e", c=32)
            stage_b = arena[0:32, A_YTR:A_YTR + 4096]
            P.dma("sp", lambda e: e.dma_start(out=stage_b, in_=b_gate_up), writes=[res("stage_b")])
            for half in range(2):
                def trbias(e, half=half):
                    for cc in range(16):
                        ch = half * 16 + cc
                        ins = e.transpose(out=psf[half][:, cc * 32:(cc + 1) * 32], in_=stage_b[:, ch * 128:(ch + 1) * 128],
                                          identity=ident_f[0:32, 0:32])
                    return ins
                P.op("pe", trbias, reads=[res("stage_b"), res("cst")], writes=[bank[half]])
                P.op("dve", lambda e, half=half: e.tensor_copy(out=bgu_all[:, half * 16:(half + 1) * 16, :],
                                                               in_=psf[half][:, :].rearrange("p (c e) -> p c e", c=16)),
                     reads=[bank[half]], writes=[res("bgu")])
            P.barrier()

            NM = CAP // 128
            P.dma("sp", lambda e: e.dma_start(out=x1_d, in_=x1), reads=[res("x1")], writes=[res("x1_d")])
            P.barrier()
            xeT = [Ab(A_HT + i * 4096, 4096).rearrange("p (c s) -> p c s", c=16) for i in range(2)]
            actT = Ab(A_WB, 4096).rearrange("p (c s) -> p c s", c=16)
            xe_sb = [Ab(A_WB + 4096 + i * 1024, 1024) for i in range(4)]
            bd_bs = [A(A_WB + 8192, 2048), A(A_TRIG, 2048)]
            yst = [A(A_YTR + i * 2048, 2048).rearrange("p (m n) -> p m n", m=NM) for i in range(2)]
            g1 = A(A_SCR + 8192, 512)
            u1 = A(A_SCR + 8704, 512)
            sgm = A(A_SCR + 9216, 512)
            wgu = [Ab(A_V + i * 4096, 4096).rearrange("p (c n) -> p c n", c=16) for i in range(3)]
            wdn = [Ab(A_V + 12288, 4096).rearrange("p (c n) -> p c n", c=16), Ab(A_BRD, 4096).rearrange("p (c n) -> p c n", c=16)]
            NGS, NDS = len(wgu), len(wdn)

            def load_gu(e_, cq, slot):
                wv = w_gate_up[e_].rearrange("(c p) n -> p c n", p=128)
                P.dma("pool", lambda e: e.dma_start(out=wgu[slot][:, :, 0:256], in_=wv[:, :, cq * 256:(cq + 1) * 256]),
                      writes=[res(f"wgu{slot}")])
                P.dma("pool", lambda e: e.dma_start(out=wgu[slot][:, :, 256:512], in_=wv[:, :, DFF + cq * 256:DFF + (cq + 1) * 256]),
                      writes=[res(f"wgu{slot}")])

            def load_dn(e_, nq, slot):
                wv = w_down[e_].rearrange("(c p) n -> p c n", p=128)
                P.dma("pool", lambda e: e.dma_start(out=wdn[slot], in_=wv[:, :, nq * 512:(nq + 1) * 512]),
                      writes=[res(f"wdn{slot}")])

            xl = 0

            def load_xe(e_, m):
                nonlocal xl
                b_ = xl % 4
                xl += 1
                P.dma("sp", lambda e: e.dma_start(out=xe_sb[b_], in_=xe_d[e_ * CAP + m * 128:e_ * CAP + (m + 1) * 128, :]),
                      reads=[res("xe_d")], writes=[res(f"xe_sb{b_}")])
                return b_

            gu_jobs = [(e_, cq) for e_ in range(NEW) for cq in range(8)]
            dn_jobs = [(e_, nq) for e_ in range(NEW) for nq in range(4)]
            gl = 0
            dl = 0

            def pump_gu(upto):
                nonlocal gl
                while gl < len(gu_jobs) and gl < upto:
                    load_gu(gu_jobs[gl][0], gu_jobs[gl][1], gl % NGS)
                    gl += 1

            def pump_dn(upto):
                nonlocal dl
                while dl < len(dn_jobs) and dl < upto:
                    load_dn(dn_jobs[dl][0], dn_jobs[dl][1], dl % NDS)
                    dl += 1

            pump_gu(2)
            gi = 0
            di = 0
            tj = 0
            def transposes(e_):
                s_ = e_ % 2
                nonlocal tj
                for m in range(NM):
                    b_ = load_xe(e_, m)
                    pa = 2 * (tj % 2)
                    tj += 1

                    def trx(e, b_=b_, pa=pa):
                        for c in range(16):
                            ins = e.transpose(out=psb(pa + c // 8)[:, (c % 8) * 128:(c % 8 + 1) * 128],
                                              in_=xe_sb[b_][:, c * 128:(c + 1) * 128], identity=ident_bf)
                        return ins
                    P.op("pe", trx, reads=[res(f"xe_sb{b_}"), res("ident_bf")], writes=[bank[pa], bank[pa + 1]])
                    P.op("act", lambda e, m=m, s_=s_, pa=pa: e.activation(
                        out=xeT[s_][:, 0:8, m * 128:(m + 1) * 128], in_=psb(pa).rearrange("p (c t) -> p c t", c=8), func=AF.Copy),
                        reads=[bank[pa]], writes=[res(f"xeT{s_}")])
                    P.op("dve", lambda e, m=m, s_=s_, pa=pa: e.tensor_copy(
                        out=xeT[s_][:, 8:16, m * 128:(m + 1) * 128], in_=psb(pa + 1).rearrange("p (c t) -> p c t", c=8)),
                        reads=[bank[pa + 1]], writes=[res(f"xeT{s_}")])

            transposes(0)
            for e_ in range(NEW):
                s_ = e_ % 2
                bd_b = bd_bs[e_ % 2]
                rbd = res(f"bd_b{e_ % 2}")
                P.dma("sp", lambda e, e_=e_, bd_b=bd_b: e.dma_start(out=bd_b, in_=b_down[e_].partition_broadcast(128)), writes=[rbd])
                for cp in range(16):
                    slot = gi % NGS
                    hf = cp % 2
                    if hf == 0:
                        pump_gu(gi + NGS)
                    if cp in (0, 8):
                        pump_dn(di + NDS if cp == 8 else di + 1)
                    bg, bu = 4 + 2 * (cp % 2), 5 + 2 * (cp % 2)

                    def mg(e, slot=slot, s_=s_, bg=bg, hf=hf):
                        for c in range(16):
                            ins = e.matmul(psf[bg][:, 0:CAP], lhsT=wgu[slot][:, c, hf * 128:(hf + 1) * 128], rhs=xeT[s_][:, c, :],
                                           start=(c == 0), stop=(c == 15))
                        return ins

                    def mu(e, slot=slot, s_=s_, bu=bu, hf=hf):
                        for c in range(16):
                            ins = e.matmul(psf[bu][:, 0:CAP], lhsT=wgu[slot][:, c, 256 + hf * 128:256 + (hf + 1) * 128], rhs=xeT[s_][:, c, :],
                                           start=(c == 0), stop=(c == 15))
                        return ins
                    P.op("pe", mg, reads=[res(f"wgu{slot}"), res(f"xeT{s_}")], writes=[bank[bg]])
                    P.op("pe", mu, reads=[res(f"wgu{slot}"), res(f"xeT{s_}")], writes=[bank[bu]])
                    P.op("dve", lambda e, bg=bg, cp=cp, e_=e_: e.tensor_scalar(
                        out=g1, in0=psf[bg][:, 0:CAP], scalar1=bgu_all[:, cp, e_:e_ + 1], scalar2=7.0, op0=ALU.add, op1=ALU.min),
                        reads=[bank[bg], res("bgu")], writes=[res("g1")])
                    P.op("act", lambda e: e.activation(out=sgm, in_=g1, func=AF.Sigmoid, scale=1.702),
                         reads=[res("g1")], writes=[res("sgm")])
                    P.op("act", lambda e, bu=bu, cp=cp, e_=e_: e.activation(
                        out=u1, in_=psf[bu][:, 0:CAP], func=AF.Identity, bias=bgu_all[:, 16 + cp, e_:e_ + 1]),
                        reads=[bank[bu], res("bgu")], writes=[res("u1")])
                    P.op("dve", lambda e: e.tensor_scalar(out=u1, in0=u1, scalar1=-7.0, scalar2=7.0, op0=ALU.max, op1=ALU.min),
                         reads=[res("u1")], writes=[res("u1")])
                    P.op("dve", lambda e: e.tensor_tensor(out=g1, in0=g1, in1=sgm, op=ALU.mult),
                         reads=[res("g1"), res("sgm")], writes=[res("g1")])
                    P.op("dve", lambda e, cp=cp: e.scalar_tensor_tensor(
                        out=actT[:, cp, :], in0=u1, scalar=1.0, in1=g1, op0=ALU.add, op1=ALU.mult),
                        reads=[res("u1"), res("g1")], writes=[res("actT")])
                    if hf == 1:
                        gi += 1
                if e_ + 1 < NEW:
                    transposes(e_ + 1)
                for nq in range(4):
                    slot = di % NDS
                    pump_dn(di + NDS)
                    cols = slice(nq * 512, (nq + 1) * 512)
                    ys = yst[nq % 2]
                    rys = res(f"yst{nq % 2}")
                    for m in range(NM):
                        b0 = (nq * NM + m) % 4

                        def md(e, slot=slot, m=m, b0=b0):
                            for c in range(16):
                                ins = e.matmul(psf[b0][:, :], lhsT=actT[:, c, m * 128:(m + 1) * 128], rhs=wdn[slot][:, c, :],
                                               start=(c == 0), stop=(c == 15))
                            return ins
                        P.op("pe", md, reads=[res(f"wdn{slot}"), res("actT")], writes=[bank[b0]])
                        P.op("dve", lambda e, b0=b0, cols=cols, m=m, ys=ys, bd_b=bd_b: e.tensor_tensor(
                            out=ys[:, m, :], in0=psf[b0][:, :], in1=bd_b[:, cols], op=ALU.add),
                            reads=[bank[b0], rbd], writes=[rys])
                    P.dma("sp", lambda e, e_=e_, cols=cols, ys=ys: e.dma_start(
                        out=ye_d[e_ * CAP:(e_ + 1) * CAP, cols].rearrange("(m p) n -> p m n", p=128), in_=ys),
                        reads=[rys], writes=[res(f"ye_d_{e_}_{nq}")])
                    di += 1
            P.barrier()

            P.dma("sp", lambda e: e.dma_start(out=x1, in_=x1_d), reads=[res("x1_d")], writes=[res("x1")])
            bload("sp", brd[0], mod_d[5 * D:6 * D], "brd0")
            yg = [A(A_HT + i * 2048, 2048) for i in range(2)]
            ctm = A(A_HT + 4096, 2048)
            P.op("pool", lambda e: e.memset(ctm, 0.0), writes=[res("ctm")])
            P.dma("sp", lambda e: e.dma_start(out=ye_d[NE * CAP:NE * CAP + 128, :], in_=ctm), reads=[res("ctm")], writes=[res("ye_d")])
            for i in range(NT):
                for k in range(TOPK):
                    j = i * TOPK + k
                    P.dma("pool", lambda e, i=i, k=k, j=j: e.indirect_dma_start(
                        out=yg[j % 2], out_offset=None, in_=ye_d,
                        in_offset=bass.IndirectOffsetOnAxis(ap=dest_all[:, i, k:k + 1], axis=0)),
                        reads=[res("ye_d"), res("dest")], writes=[res(f"yg{j % 2}")])
                    P.op("dve", lambda e, j=j: e.tensor_tensor(out=ctm, in0=yg[j % 2], in1=brd[0], op=ALU.mult),
                         reads=[res(f"yg{j % 2}"), res("brd0")], writes=[res("ctm")])
                    P.op("dve", lambda e, i=i, k=k: e.scalar_tensor_tensor(
                        out=x1[:, i, :], in0=ctm, scalar=w4_all[:, i, k:k + 1], in1=x1[:, i, :], op0=ALU.mult, op1=ALU.add),
                        reads=[res("ctm"), res("w4"), res("x1")], writes=[res("x1")])
            P.barrier()

            fg_b = A(A_TRIG, 2048)
            ot = [A(A_WB + i * 2048, 2048) for i in range(2)]
            junk = Ab(A_WB + 4096, 1024)
            bload("sp", fg_b, final_g, "fg_b")
            out_v = out.rearrange("(i p) d -> i p d", p=128)
            for i in range(NT):
                P.op("act", lambda e, i=i: e.activation(out=junk, in_=x1[:, i, :], func=AF.Square, accum_out=ssq2[:, i:i + 1]),
                     reads=[res("x1")], writes=[res("junk"), res("rs3")])
                P.op("act", lambda e, i=i: e.activation(out=rstd2[:, i:i + 1], in_=ssq2[:, i:i + 1], func=AF.Sqrt,
                                                       scale=1.0 / D, bias=col(C_EPS)),
                     reads=[res("rs3"), res("cst")], writes=[res("rs3")])
                P.op("dve", lambda e, i=i: e.reciprocal(out=rstd2[:, i:i + 1], in_=rstd2[:, i:i + 1]),
                     reads=[res("rs3")], writes=[res("rs3")])
                P.op("dve", lambda e, i=i: e.scalar_tensor_tensor(out=ot[i % 2], in0=x1[:, i, :], scalar=rstd2[:, i:i + 1], in1=fg_b,
                                                                   op0=ALU.mult, op1=ALU.mult),
                     reads=[res("x1"), res("rs3"), res("fg_b")], writes=[res(f"ot{i % 2}")])
                P.dma("sp", lambda e, i=i: e.dma_start(out=out_v[i], in_=ot[i % 2]), reads=[res(f"ot{i % 2}")])
            P.barrier()


        with nc.Block() as block:
            @block.sync
            def _(e):
                P.emit("sp", e)

            @block.scalar
            def _(e):
                P.emit("act", e)

            @block.vector
            def _(e):
                P.emit("dve", e)

            @block.gpsimd
            def _(e):
                P.emit("pool", e)

            @block.tensor
            def _(e):
                P.emit("pe", e)
    return nc


def _consts():
    c = np.zeros((128, C_N), np.float32)
    p = np.arange(128)
    c[:, C_IOTA_T:C_IOTA_T + 1024] = np.arange(1024)[None, :]
    c[:, C_IDENT:C_IDENT + 128] = np.eye(128)
    kt = p[:, None]
    qt = p[None, :]
    c[:, C_DIST:C_DIST + 128] = np.abs(kt - qt)
    c[:, C_ALLOW:C_ALLOW + 128] = ((kt // 64) <= (qt // 64))
    c[:, C_TRI:C_TRI + 128] = (kt < qt)
    c[:, C_ONES:C_ONES + 128] = 1.0
    c[:, C_IOTA_E:C_IOTA_E + 32] = np.arange(32)[None, :]
    c[:, C_IMOD] = p % 64
    c[:, C_SIGN] = np.where(p < 64, -1.0, 1.0)
    c[:, C_HALFPI] = np.pi / 2
    c[:, C_EPS] = EPS
    c[:, C_LNQ] = np.log(QSCALE)
    c[:, C_ONE] = 1.0
    return c


def _percore(j):
    t = np.zeros((128, PC_N), np.float32)
    p = np.arange(128)
    for qi in range(4):
        if qi < 3:
            s = 3 - qi
            valid = s <= j
            t[:, PC_BASE + qi] = (j - s) * 1024 if valid else 0
            for i in range(8):
                t[:, PC_E + qi * 8 + i] = (1024 * s - (128 * i + p)) if valid else 1.0e6
        else:
            t[:, PC_BASE + qi] = j * 1024
            for i in range(8):
                t[:, PC_E + qi * 8 + i] = -(128 * i + p)
    t[:, PC_FLAG] = 1.0 if j > 0 else 0.0
    return t


_NC_CACHE = {}


def make_in_maps(x, c, norm1_g, w_mod, b_mod, w_in, conv_w, w_out, norm2_g, w_router, b_router,
                 w_gate_up, b_gate_up, w_down, b_down, final_g):
    f = lambda a: np.ascontiguousarray(np.asarray(a, dtype=np.float32))
    x = f(x); c = f(c)
    shared = dict(
        cst=_consts(), norm1_g=f(norm1_g[0]), w_mod=f(w_mod[0]), b_mod=f(b_mod[0]), w_in=f(w_in[0]),
        conv_w=f(conv_w[0]), w_out=f(w_out[0]), norm2_g=f(norm2_g[0]), w_router=f(w_router[0]),
        b_router=f(b_router[0]), w_gate_up=f(w_gate_up[0]), b_gate_up=f(b_gate_up[0]),
        w_down=f(w_down[0]), b_down=f(b_down[0]), final_g=f(final_g))
    in_maps = []
    for core in range(8):
        b, j = core // 4, core % 4
        xq = np.zeros((4, T, D), np.float32)
        for qi in range(3):
            s = 3 - qi
            if s <= j:
                xq[qi] = x[b, (j - s) * T:(j - s + 1) * T]
        xq[3] = x[b, j * T:(j + 1) * T]
        m = dict(shared)
        m["x_q"] = xq
        m["c_pc"] = np.ascontiguousarray(c[b].reshape(128, 16))
        m["pcst"] = _percore(j)
        in_maps.append(m)
    return in_maps


def kernel(**inputs):
    if "nc" not in _NC_CACHE:
        _NC_CACHE["nc"] = build_nc()
    nc = _NC_CACHE["nc"]
    in_maps = make_in_maps(**inputs)
    res = run_bass_kernel_spmd(nc, in_maps, core_ids=list(range(8)))
    outs = [np.asarray(r["out"], dtype=np.float32) for r in res.results]
    full = np.stack(outs, 0).reshape(2, 4, T, D).reshape(2, 4 * T, D)
    return full
```
